# Optimizing a Trainium2 kernel written in Bass

```python
import math
import jax, jax.numpy as jnp
from jax import lax
import numpy as np

D_MODEL = 1024
BATCH = 4
SEQ = 8192
DEPTH = 2

MLSTM_INNER = 2 * D_MODEL
MLSTM_HEADS = 4
MLSTM_HEAD_DIM = MLSTM_INNER // MLSTM_HEADS
MLSTM_CONV = 4
QKV_BLOCK = 4
MLSTM_CHUNK = 128
S5_GROUP = 16
S5_GROUPS = D_MODEL // S5_GROUP
S5_STATE = 64
S5_CHUNK = 128
DT_MIN = 0.001
DT_MAX = 0.1
D_FF = 2816
N_EXPERTS = 8
TOP_K = 2
N_EVEN = (DEPTH + 1) // 2
N_ODD = DEPTH // 2
DEEPNORM_ALPHA = (2 * DEPTH) ** 0.25
DEEPNORM_BETA = (8 * DEPTH) ** -0.25
LN_EPS = 1e-5

kernel_name = "hybrid_mlstm_s5_moe_deepnorm_adaln"


def layer_norm(x, g, b):
    xf = x.astype(jnp.float32)
    mu = jnp.mean(xf, -1, keepdims=True)
    var = jnp.mean(jnp.square(xf - mu), -1, keepdims=True)
    return ((xf - mu) * lax.rsqrt(var + LN_EPS) * g.astype(jnp.float32) + b.astype(jnp.float32)).astype(x.dtype)


def causal_depthwise_conv(x, w, b):
    K, C = w.shape
    y = lax.conv_general_dilated(x, w[:, None, :].astype(x.dtype), window_strides=(1,), padding=[(K - 1, 0)],
                                 dimension_numbers=('NWC', 'WIO', 'NWC'), feature_group_count=C)
    return y + b


def block_diag_proj(x, w):
    nb, blk, _ = w.shape
    xb = x.reshape(x.shape[:-1] + (nb, blk))
    return jnp.einsum('bsni,nio->bsno', xb, w).reshape(x.shape)


def swiglu(x, w13, w2):
    gate, up = jnp.split(x @ w13, 2, axis=-1)
    return (jax.nn.silu(gate) * up) @ w2


def mlstm_chunkwise(q, k, v, log_i, log_f):
    Bsz, NH, S, DH = q.shape
    L = MLSTM_CHUNK
    NC = S // L

    def chunks(t):
        return jnp.moveaxis(t.reshape((Bsz, NH, NC, L) + t.shape[3:]), 2, 0)

    causal = jnp.tril(jnp.ones((L, L), dtype=bool))

    def step(carry, inp):
        C, n, m = carry
        qc, kc, vc, ic, fc = inp
        b = jnp.cumsum(fc, axis=-1)
        d_log = jnp.where(causal, b[..., :, None] - b[..., None, :] + ic[..., None, :], -jnp.inf)
        inter = b + m[..., None]
        m_t = jnp.maximum(inter, jnp.max(d_log, axis=-1))
        scores = jnp.einsum('bhtd,bhsd->bhts', qc, kc) * jnp.exp(d_log - m_t[..., None])
        dec = jnp.exp(inter - m_t)
        num = jnp.einsum('bhts,bhse->bhte', scores, vc) + dec[..., None] * jnp.einsum('bhtd,bhde->bhte', qc, C)
        den = jnp.sum(scores, -1) + dec * jnp.einsum('bhtd,bhd->bht', qc, n)
        h = num / jnp.maximum(jnp.abs(den), jnp.exp(-m_t))[..., None]
        b_last = b[..., -1]
        g = b_last[..., None] - b + ic
        m_new = jnp.maximum(b_last + m, jnp.max(g, axis=-1))
        wk = jnp.exp(g - m_new[..., None])[..., None] * kc
        dC = jnp.exp(b_last + m - m_new)
        C_new = dC[..., None, None] * C + jnp.einsum('bhsd,bhse->bhde', wk, vc)
        n_new = dC[..., None] * n + jnp.sum(wk, axis=2)
        return (C_new, n_new, m_new), h

    init = (jnp.zeros((Bsz, NH, DH, DH), jnp.float32), jnp.zeros((Bsz, NH, DH), jnp.float32),
            jnp.zeros((Bsz, NH), jnp.float32))
    _, h = lax.scan(step, init, (chunks(q), chunks(k), chunks(v), chunks(log_i), chunks(log_f)))
    return jnp.moveaxis(h, 0, 2).reshape(Bsz, NH, S, DH)


def mlstm_mixer(u, w_in, conv_w, conv_b, wq, wk, wv, w_gates, b_gates, norm_w, skip, w_out):
    Bsz, S, _ = u.shape
    xm, z = jnp.split(u @ w_in, 2, axis=-1)
    xc = jax.nn.silu(causal_depthwise_conv(xm, conv_w, conv_b))
    q = block_diag_proj(xc, wq)
    k = block_diag_proj(xc, wk)
    v = block_diag_proj(xm, wv)
    wg_q, wg_k, wg_v = jnp.split(w_gates, 3, axis=0)
    gates = (q @ wg_q + k @ wg_k + v @ wg_v + b_gates).astype(jnp.float32)
    i_pre, f_pre = jnp.split(gates, 2, axis=-1)
    log_f = jax.nn.log_sigmoid(f_pre)

    def heads(t):
        return t.reshape(Bsz, S, MLSTM_HEADS, MLSTM_HEAD_DIM).transpose(0, 2, 1, 3).astype(jnp.float32)

    h = mlstm_chunkwise(heads(q) * (MLSTM_HEAD_DIM ** -0.5), heads(k), heads(v),
                        i_pre.transpose(0, 2, 1), log_f.transpose(0, 2, 1))
    mu = jnp.mean(h, -1, keepdims=True)
    var = jnp.mean(jnp.square(h - mu), -1, keepdims=True)
    h = ((h - mu) * lax.rsqrt(var + LN_EPS)).transpose(0, 2, 1, 3).reshape(Bsz, S, MLSTM_INNER).astype(u.dtype)
    h = (h * norm_w + skip * xc) * jax.nn.silu(z)
    return h @ w_out


def _complex_affine_combine(e1, e2):
    a1r, a1i, b1r, b1i = e1
    a2r, a2i, b2r, b2i = e2
    return (a2r * a1r - a2i * a1i, a2r * a1i + a2i * a1r,
            a2r * b1r - a2i * b1i + b2r, a2r * b1i + a2i * b1r + b2i)


def s5_mixer(u, lam_re, lam_im, b_re, b_im, c_re, c_im, d_skip, log_dt, w_glu, b_glu):
    Bsz, S, _ = u.shape
    G, P, H = b_re.shape
    L = S5_CHUNK
    NC = S // L
    f32 = lambda t: t.astype(jnp.float32)
    lr, li = f32(lam_re), f32(lam_im)
    dt = jnp.exp(f32(log_dt))[:, None]
    mag = jnp.exp(lr * dt)
    ar, ai = mag * jnp.cos(li * dt), mag * jnp.sin(li * dt)
    den = lr * lr + li * li
    kr = ((ar - 1.0) * lr + ai * li) / den
    ki = (ai * lr - (ar - 1.0) * li) / den
    br, bi = f32(b_re), f32(b_im)
    bbar_re = kr[..., None] * br - ki[..., None] * bi
    bbar_im = kr[..., None] * bi + ki[..., None] * br
    cr, ci = f32(c_re), f32(c_im)
    steps = jnp.arange(1, L + 1, dtype=jnp.float32)[:, None, None]
    pmag = jnp.exp(steps * lr * dt)
    pr = (pmag * jnp.cos(steps * li * dt))[:, None]
    pi_ = (pmag * jnp.sin(steps * li * dt))[:, None]
    uf = f32(u).reshape(Bsz, NC, L, G, H).transpose(1, 2, 0, 3, 4)

    def step(carry, uc):
        xr0, xi0 = carry
        bur = jnp.einsum('lbgh,gph->lbgp', uc, bbar_re)
        bui = jnp.einsum('lbgh,gph->lbgp', uc, bbar_im)
        a_r = jnp.broadcast_to(ar, bur.shape)
        a_i = jnp.broadcast_to(ai, bur.shape)
        _, _, sr, si = lax.associative_scan(_complex_affine_combine, (a_r, a_i, bur, bui), axis=0)
        xr = pr * xr0 - pi_ * xi0 + sr
        xi = pr * xi0 + pi_ * xr0 + si
        y = jnp.einsum('ghp,lbgp->lbgh', cr, xr) - jnp.einsum('ghp,lbgp->lbgh', ci, xi)
        return (xr[-1], xi[-1]), y

    init = (jnp.zeros((Bsz, G, P), jnp.float32), jnp.zeros((Bsz, G, P), jnp.float32))
    _, y = lax.scan(step, init, uf)
    y = y.transpose(2, 0, 1, 3, 4).reshape(Bsz, S, G * H) + f32(d_skip) * f32(u)
    g = jax.nn.gelu(y).astype(u.dtype)
    val, gate = jnp.split(g @ w_glu + b_glu, 2, axis=-1)
    return val * jax.nn.sigmoid(gate)


def moe_ffn(u, w_router, w13, w2):
    Bsz, S, D = u.shape
    t = u.reshape(-1, D)
    logits = (t @ w_router).astype(jnp.float32)
    top_val, top_idx = lax.top_k(logits, TOP_K)
    top_w = jax.nn.softmax(top_val, axis=-1)
    out = jnp.zeros_like(t)
    for e in range(N_EXPERTS):
        gate_e = jnp.sum(jnp.where(top_idx == e, top_w, 0.0), axis=-1).astype(t.dtype)
        out = out + gate_e[:, None] * swiglu(t, w13[e], w2[e])
    return out.reshape(Bsz, S, D)


def setup_inputs(seed: int = 0) -> dict:
    key = jax.random.key(seed)
    ks = iter(jax.random.split(key, 40))
    f32 = jnp.float32

    def nrm(shape, std):
        return std * jax.random.normal(next(ks), shape, f32)

    D, Di, NH, G, P, H, F, E = D_MODEL, MLSTM_INNER, MLSTM_HEADS, S5_GROUPS, S5_STATE, S5_GROUP, D_FF, N_EXPERTS
    beta = DEEPNORM_BETA
    x = nrm((BATCH, SEQ, D), 1.0)
    c = nrm((BATCH, D), 1.0)
    ada_w = nrm((DEPTH, D, 6 * D), 0.2 * D ** -0.5)
    ada_b = nrm((DEPTH, 6 * D), 0.01)
    ln_mix_g = 1.0 + nrm((DEPTH, D), 0.01)
    ln_mix_b = nrm((DEPTH, D), 0.01)
    ln_ffn_g = 1.0 + nrm((DEPTH, D), 0.01)
    ln_ffn_b = nrm((DEPTH, D), 0.01)
    mlstm_w_in = nrm((N_EVEN, D, 2 * Di), D ** -0.5)
    mlstm_conv_w = nrm((N_EVEN, MLSTM_CONV, Di), MLSTM_CONV ** -0.5)
    mlstm_conv_b = nrm((N_EVEN, Di), 0.01)
    mlstm_wq = nrm((N_EVEN, Di // QKV_BLOCK, QKV_BLOCK, QKV_BLOCK), QKV_BLOCK ** -0.5)
    mlstm_wk = nrm((N_EVEN, Di // QKV_BLOCK, QKV_BLOCK, QKV_BLOCK), QKV_BLOCK ** -0.5)
    mlstm_wv = nrm((N_EVEN, Di // QKV_BLOCK, QKV_BLOCK, QKV_BLOCK), QKV_BLOCK ** -0.5)
    mlstm_w_gates = nrm((N_EVEN, 3 * Di, 2 * NH), (3 * Di) ** -0.5)
    mlstm_b_gates = jnp.concatenate([nrm((N_EVEN, NH), 0.1),
                                     jnp.linspace(3.0, 6.0, NH, dtype=f32)[None] + nrm((N_EVEN, NH), 0.01)], axis=-1)
    mlstm_norm_w = 1.0 + nrm((N_EVEN, Di), 0.01)
    mlstm_skip = 1.0 + nrm((N_EVEN, Di), 0.01)
    mlstm_w_out = nrm((N_EVEN, Di, D), beta * Di ** -0.5)
    ffn_w13 = nrm((N_EVEN, D, 2 * F), D ** -0.5)
    ffn_w2 = nrm((N_EVEN, F, D), beta * F ** -0.5)
    s5_lambda_re = -0.5 + nrm((N_ODD, G, P), 0.01)
    s5_lambda_im = jnp.tile((math.pi * jnp.arange(P, dtype=f32))[None, None, :], (N_ODD, G, 1))
    s5_b_re = nrm((N_ODD, G, P, H), (2 * H) ** -0.5)
    s5_b_im = nrm((N_ODD, G, P, H), (2 * H) ** -0.5)
    s5_c_re = nrm((N_ODD, G, H, P), (2 * P) ** -0.5)
    s5_c_im = nrm((N_ODD, G, H, P), (2 * P) ** -0.5)
    s5_d = nrm((N_ODD, D), 1.0)
    s5_log_dt = jax.random.uniform(next(ks), (N_ODD, G), f32, math.log(DT_MIN), math.log(DT_MAX))
    s5_w_glu = jnp.concatenate([nrm((N_ODD, D, D), beta * D ** -0.5), nrm((N_ODD, D, D), D ** -0.5)], axis=-1)
    s5_b_glu = nrm((N_ODD, 2 * D), 0.01)
    moe_router = nrm((N_ODD, D, E), D ** -0.5)
    moe_w13 = nrm((N_ODD, E, D, 2 * F), D ** -0.5)
    moe_w2 = nrm((N_ODD, E, F, D), beta * F ** -0.5)
    return {"x": x, "c": c, "ada_w": ada_w, "ada_b": ada_b, "ln_mix_g": ln_mix_g, "ln_mix_b": ln_mix_b,
            "ln_ffn_g": ln_ffn_g, "ln_ffn_b": ln_ffn_b,
            "mlstm_w_in": mlstm_w_in, "mlstm_conv_w": mlstm_conv_w, "mlstm_conv_b": mlstm_conv_b,
            "mlstm_wq": mlstm_wq, "mlstm_wk": mlstm_wk, "mlstm_wv": mlstm_wv, "mlstm_w_gates": mlstm_w_gates,
            "mlstm_b_gates": mlstm_b_gates, "mlstm_norm_w": mlstm_norm_w, "mlstm_skip": mlstm_skip,
            "mlstm_w_out": mlstm_w_out, "ffn_w13": ffn_w13, "ffn_w2": ffn_w2,
            "s5_lambda_re": s5_lambda_re, "s5_lambda_im": s5_lambda_im, "s5_b_re": s5_b_re, "s5_b_im": s5_b_im,
            "s5_c_re": s5_c_re, "s5_c_im": s5_c_im, "s5_d": s5_d, "s5_log_dt": s5_log_dt,
            "s5_w_glu": s5_w_glu, "s5_b_glu": s5_b_glu,
            "moe_router": moe_router, "moe_w13": moe_w13, "moe_w2": moe_w2}


def reference(x, c, ada_w, ada_b, ln_mix_g, ln_mix_b, ln_ffn_g, ln_ffn_b,
              mlstm_w_in, mlstm_conv_w, mlstm_conv_b, mlstm_wq, mlstm_wk, mlstm_wv, mlstm_w_gates,
              mlstm_b_gates, mlstm_norm_w, mlstm_skip, mlstm_w_out, ffn_w13, ffn_w2,
              s5_lambda_re, s5_lambda_im, s5_b_re, s5_b_im, s5_c_re, s5_c_im, s5_d, s5_log_dt,
              s5_w_glu, s5_b_glu, moe_router, moe_w13, moe_w2):
    cond = jax.nn.silu(c)
    for i in range(DEPTH):
        j = i // 2
        mod = cond @ ada_w[i] + ada_b[i]
        sh_m, sc_m, g_m, sh_f, sc_f, g_f = [t[:, None, :] for t in jnp.split(mod, 6, axis=-1)]
        u = x * (1.0 + sc_m) + sh_m
        if i % 2 == 0:
            y = mlstm_mixer(u, mlstm_w_in[j], mlstm_conv_w[j], mlstm_conv_b[j], mlstm_wq[j], mlstm_wk[j],
                            mlstm_wv[j], mlstm_w_gates[j], mlstm_b_gates[j], mlstm_norm_w[j], mlstm_skip[j],
                            mlstm_w_out[j])
        else:
            y = s5_mixer(u, s5_lambda_re[j], s5_lambda_im[j], s5_b_re[j], s5_b_im[j], s5_c_re[j], s5_c_im[j],
                         s5_d[j], s5_log_dt[j], s5_w_glu[j], s5_b_glu[j])
        x = layer_norm(DEEPNORM_ALPHA * x + (1.0 + g_m) * y, ln_mix_g[i], ln_mix_b[i])
        u = x * (1.0 + sc_f) + sh_f
        if i % 2 == 0:
            y = swiglu(u, ffn_w13[j], ffn_w2[j])
        else:
            y = moe_ffn(u, moe_router[j], moe_w13[j], moe_w2[j])
        x = layer_norm(DEEPNORM_ALPHA * x + (1.0 + g_f) * y, ln_ffn_g[i], ln_ffn_b[i])
    return x
```

```python
import math
import numpy as np
from contextlib import ExitStack
import concourse.bass as bass
import concourse.mybir as mybir
from concourse.bass_utils import run_bass_kernel_spmd

F32 = mybir.dt.float32
BF16 = mybir.dt.bfloat16
AF = mybir.ActivationFunctionType
ALU = mybir.AluOpType
AX = mybir.AxisListType

NPOOL = 12
NOSYNC_SAME = ("pe",)


class Buf:
    __slots__ = ("ap", "last_w", "reads")

    def __init__(self, ap):
        self.ap = ap
        self.last_w = None
        self.reads = {}

    def __getitem__(self, idx):
        return self.ap[idx]


class Ctx:
    def __init__(self, nc):
        self.nc = nc
        self.engs = {"pe": nc.tensor, "act": nc.scalar, "dve": nc.vector,
                     "pool": nc.gpsimd, "sp": nc.sync}
        self.sem = {}
        self.cnt = {}
        for n in ("pe", "act", "dve", "pool"):
            self.sem[n] = nc.alloc_semaphore("s_" + n)
            self.cnt[n] = 0
        self.dma_pool = {}
        self.dma_i = {}
        self.known = {n: {} for n in ("pe", "act", "dve", "pool", "sp")}
        self.out_tokens = []
        self.n_ins = 0
        self.stack = None
        self.pref = ""
        self.dma_last = {}
        self.cc_toks = []

    def begin_stage(self, pref=""):
        self.stack = ExitStack()
        self.pref = pref

    def barrier(self):
        toks = [(n, self.sem[n], self.cnt[n]) for n in ("pe", "act", "dve", "pool") if self.cnt[n] > 0]
        for qn, d in self.dma_last.items():
            for i, v in d.items():
                toks.append(("dma_" + qn, self.dma_pool[qn][i], v))
        toks.extend(self.cc_toks)
        for en in ("pe", "act", "dve", "pool", "sp"):
            for tok in toks:
                self._wait(en, tok)

    def end_stage(self):
        self.barrier()
        self.stack.close()
        self.stack = None

    def collective(self, kind, srcs, dsts, groups):
        self.barrier()
        for src_ap, dst_ap in zip(srcs, dsts):
            sem = self.nc.alloc_semaphore("cc_%d" % len(self.cc_toks))
            ins = self.nc.gpsimd.collective_compute(kind, ALU.bypass, replica_groups=groups, ins=[src_ap.opt()], outs=[dst_ap.opt()])
            ins.then_inc(sem)
            self.cc_toks.append(("cc", sem, 1))
        self.barrier()

    def sb(self, name, shape, dtype=F32):
        if self.stack is not None:
            t = self.stack.enter_context(self.nc.sbuf_tensor(self.pref + name, list(shape), dtype))
        else:
            t = self.nc.alloc_sbuf_tensor(name, list(shape), dtype)
        return Buf(t.ap() if hasattr(t, "ap") else t)

    def ps(self, name, shape, dtype=F32):
        if self.stack is not None:
            t = self.stack.enter_context(self.nc.psum_tensor(self.pref + name, list(shape), dtype))
        else:
            t = self.nc.alloc_psum_tensor(name, list(shape), dtype)
        return Buf(t.ap() if hasattr(t, "ap") else t)

    def _wait(self, en, tok):
        src, sem, val = tok
        k = self.known[en]
        if k.get(sem.num, 0) >= val:
            return
        self.engs[en].wait_ge(sem, val)
        k[sem.num] = val

    def op(self, en, fn, reads=(), writes=(), dma=False, q=None):
        deps = []
        for b in reads:
            if b.last_w is not None:
                deps.append(b.last_w)
        for b in writes:
            if b.last_w is not None:
                deps.append(b.last_w)
            deps.extend(b.reads.values())
        for tok in deps:
            src = tok[0]
            if src == en and en in NOSYNC_SAME and not dma:
                continue
            self._wait(en, tok)
        e = self.engs[en]
        if dma:
            qn = en
            if qn not in self.dma_pool:
                self.dma_pool[qn] = [self.nc.alloc_semaphore(f"d_{qn}_{i}") for i in range(NPOOL)]
                self.dma_i[qn] = 0
            i = self.dma_i[qn]
            self.dma_i[qn] = i + 1
            sem = self.dma_pool[qn][i % NPOOL]
            rnd = i // NPOOL
            if rnd > 0:
                self._wait(en, ("dma_" + qn, sem, 16 * rnd))
            ins = fn(e)
            ins.then_inc(sem, 16)
            tok = ("dma_" + qn, sem, 16 * (rnd + 1))
            self.dma_last.setdefault(qn, {})[i % NPOOL] = 16 * (rnd + 1)
        else:
            ins = fn(e)
            self.cnt[en] += 1
            ins.then_inc(self.sem[en], 1)
            tok = (en, self.sem[en], self.cnt[en])
        self.n_ins += 1
        for b in reads:
            b.reads[tok[1].num] = tok
        for b in writes:
            b.last_w = tok
            b.reads = {}
        return tok

    def finish(self, toks):
        for tok in toks:
            self._wait("sp", tok)


class Fz:
    def __init__(self, nc, k):
        self.nc = nc; self.k = k; self.ext = {}; self.pref = ""


def mkD(nc, fz, pref):
    def D(n, s, dt=F32, kind="ExternalInput"):
        if fz is not None and n in fz.ext:
            return fz.ext[n]
        return nc.dram_tensor(pref + n, list(s), dt, kind=kind).ap()
    return D


class ChunkedAP:
    def __init__(self, aps, W):
        self.aps = aps; self.W = W

    def __getitem__(self, idx):
        rs, cs = idx
        j = cs.start // self.W
        assert (cs.stop - 1) // self.W == j
        return self.aps[j][rs, cs.start - j * self.W: cs.stop - j * self.W]

I32 = mybir.dt.int32
def sincos(k, a, N, outs, outc, pref="sc"):
    op = k.op
    t = k.sb(pref + "_t", [128, N]); ti = k.sb(pref + "_i", [128, N], I32); m = k.sb(pref + "_m", [128, N])
    for dst, sh in ((outs, 0.0), (outc, 0.5 * math.pi)):
        op("dve", lambda e: e.tensor_scalar(out=t[:], in0=a[:], scalar1=sh, scalar2=1.0 / (2 * math.pi), op0=ALU.add, op1=ALU.mult), reads=[a], writes=[t])
        op("dve", lambda e: e.tensor_copy(out=ti[:], in_=t[:]), reads=[t], writes=[ti])
        op("dve", lambda e: e.tensor_copy(out=m[:], in_=ti[:]), reads=[ti], writes=[m])
        op("dve", lambda e: e.tensor_scalar(out=t[:], in0=a[:], scalar1=sh, scalar2=None, op0=ALU.add), reads=[a], writes=[t])
        op("dve", lambda e: e.scalar_tensor_tensor(out=t[:], in0=m[:], scalar=-2 * math.pi, in1=t[:], op0=ALU.mult, op1=ALU.add), reads=[m, t], writes=[t])
        op("dve", lambda e: e.tensor_scalar(out=m[:], in0=t[:], scalar1=math.pi, scalar2=-2 * math.pi, op0=ALU.is_gt, op1=ALU.mult), reads=[t], writes=[m])
        op("dve", lambda e: e.tensor_tensor(out=t[:], in0=t[:], in1=m[:], op=ALU.add), reads=[t, m], writes=[t])
        op("dve", lambda e: e.tensor_scalar(out=m[:], in0=t[:], scalar1=-math.pi, scalar2=2 * math.pi, op0=ALU.is_lt, op1=ALU.mult), reads=[t], writes=[m])
        op("dve", lambda e: e.tensor_tensor(out=t[:], in0=t[:], in1=m[:], op=ALU.add), reads=[t, m], writes=[t])
        op("dve", lambda e: e.tensor_scalar(out=t[:], in0=t[:], scalar1=math.pi, scalar2=-math.pi, op0=ALU.min, op1=ALU.max), reads=[t], writes=[t])
        op("act", lambda e: e.activation(out=dst[:], in_=t[:], func=AF.Sin), reads=[t], writes=[dst])


DH = 512
TB = 256
NT = TB // 128


def build_m0(S, stage=99, HS=(0, 1), TGT=(0, 0), fz=None):
    if fz is None:
        nc = bass.Bass("TRN2", target_bir_lowering=False); k = Ctx(nc); pref = ""
    else:
        nc, k, pref = fz.nc, fz.k, fz.pref
    D = mkD(nc, fz, pref)
    k.begin_stage(pref)
    x = D("x", [S, 1024])
    condT_d = D("condT", [128, 8])
    adaw = D("adaw", [1024, 2048])
    adabT = D("adabT", [128, 16])
    w_in = D("w_in", [1024, 3072])
    convw = D("convw", [128, 16, 4])
    convb = D("convb", [128, 16])
    wqc = D("wqc", [128, 16, 4]); wkc = D("wkc", [128, 16, 4]); wvc = D("wvc", [128, 16, 4])
    wqt = D("wqt", [128, 16, 4]); wkt = D("wkt", [128, 16, 4]); wvt = D("wvt", [128, 16, 4])
    wgq = D("wgq", [128, 16, 4]); wgk = D("wgk", [128, 16, 4]); wgv = D("wgv", [128, 16, 4])
    bg = D("bg", [128, 4])
    nwT = D("nwT", [128, 8]); skT = D("skT", [128, 8])
    out = D("hgT", [1024, S], BF16, kind="ExternalOutput")

    op = k.op
    NB = S // TB

    identf = k.sb("identf", [128, 128]); ident = k.sb("ident", [128, 128], BF16)
    ntri = k.sb("ntri", [128, 128]); negm = k.sb("negm", [128, 128])
    bmask = k.sb("bmask", [128, 32, 4])
    onesb = k.sb("onesb", [128, 2], BF16); onesf = k.sb("onesf", [128, 128])
    op("pool", lambda e: e.memset(identf[:], 0.0), writes=[identf])
    op("pool", lambda e: e.affine_select(out=identf[:], in_=identf[:], pattern=[[-1, 128]], compare_op=ALU.not_equal,
                                         fill=1.0, base=0, channel_multiplier=1), reads=[identf], writes=[identf])
    op("dve", lambda e: e.tensor_copy(out=ident[:], in_=identf[:]), reads=[identf], writes=[ident])
    op("pool", lambda e: e.memset(ntri[:], -1.0), writes=[ntri])
    op("pool", lambda e: e.affine_select(out=ntri[:], in_=ntri[:], pattern=[[1, 128]], compare_op=ALU.is_ge,
                                         fill=0.0, base=0, channel_multiplier=-1), reads=[ntri], writes=[ntri])
    op("pool", lambda e: e.memset(negm[:], 0.0), writes=[negm])
    op("pool", lambda e: e.affine_select(out=negm[:], in_=negm[:], pattern=[[1, 128]], compare_op=ALU.is_ge,
                                         fill=-30000.0, base=0, channel_multiplier=-1), reads=[negm], writes=[negm])
    op("pool", lambda e: e.memset(bmask[:], 1.0), writes=[bmask])
    op("pool", lambda e: e.affine_select(out=bmask[:], in_=bmask[:], pattern=[[-4, 32], [0, 4]], compare_op=ALU.is_ge,
                                         fill=0.0, base=0, channel_multiplier=1), reads=[bmask], writes=[bmask])
    op("pool", lambda e: e.affine_select(out=bmask[:], in_=bmask[:], pattern=[[4, 32], [0, 4]], compare_op=ALU.is_ge,
                                         fill=0.0, base=3, channel_multiplier=-1), reads=[bmask], writes=[bmask])
    op("pool", lambda e: e.memset(onesb[:], 1.0), writes=[onesb])
    op("pool", lambda e: e.memset(onesf[:], 1.0), writes=[onesf])

    def load(name, src, shape, dt=F32, q="sp"):
        b = k.sb(name, shape, dt)
        op(q, lambda e: e.dma_start(out=b[:], in_=src), writes=[b], dma=True)
        return b
    condT = load("condT_s", condT_d, [128, 8]); adab = load("adab_s", adabT, [128, 16])
    cw = load("cw", convw, [128, 16, 4]); cb = load("cb", convb, [128, 16])
    wc = [load("wqc_s", wqc, [128, 16, 4]), load("wkc_s", wkc, [128, 16, 4]), load("wvc_s", wvc, [128, 16, 4])]
    wt = [load("wqt_s", wqt, [128, 16, 4]), load("wkt_s", wkt, [128, 16, 4]), load("wvt_s", wvt, [128, 16, 4])]
    wg = [load("wgq_s", wgq, [128, 16, 4]), load("wgk_s", wgk, [128, 16, 4]), load("wgv_s", wgv, [128, 16, 4])]
    bgs = load("bgs", bg, [128, 4]); nw = load("nw", nwT, [128, 8]); sk = load("sk", skT, [128, 8])

    pA = k.ps("pA", [128, 512]); pB = k.ps("pB", [128, 512])
    import os
    if os.environ.get("M0V", "0") == "1":
        hbA = k.ps("hbA", [128, 512]); hbB = k.ps("hbB", [128, 512])
        pbh = [hbA, hbA]; pSh = [hbB, hbB]
    else:
        hb = [k.ps("hb0", [128, 512]), k.ps("hb1", [128, 512])]
        pbh = [hb[0], hb[1]]
        pSh = [hb[0], hb[1]]
    pnh = [k.ps("pn0", [128, 512]), k.ps("pn1", [128, 512])]
    pS = pnh[0]
    pt = k.ps("pt", [128, 1024], BF16)
    ptF = k.ps("ptF", [128, 1024], BF16)
    ptFh = [ptF, ptF]
    pacc = [pA, pB]
    pacc_i = [0]

    def nextp():
        pacc_i[0] ^= 1
        return pacc[pacc_i[0]]

    cond = k.sb("cond", [128, 8])
    op("act", lambda e: e.activation(out=cond[:], in_=condT[:], func=AF.Silu), reads=[condT], writes=[cond])
    wst = [k.sb("wst0", [128, 8, 256]), k.sb("wst1", [128, 8, 256])]
    for j in range(16):
        st = wst[j % 2]
        op("sp", lambda e: e.dma_start(out=st[:, :, 0:128], in_=adaw[:, j * 128:(j + 1) * 128].rearrange("(c p) f -> p c f", p=128)),
           writes=[st], dma=True)
        for kc in range(8):
            op("pe", lambda e: e.matmul(pS[:, j:j + 1], lhsT=st[:, kc, 0:128], rhs=cond[:, kc:kc + 1], start=(kc == 0), stop=(kc == 7)),
               reads=[st, cond], writes=[pS])
    modT = k.sb("modT", [128, 16])
    op("dve", lambda e: e.tensor_tensor(out=modT[:], in0=pS[:, 0:16], in1=adab[:], op=ALU.add), reads=[pS, adab], writes=[modT])
    op("dve", lambda e: e.tensor_scalar_add(out=modT[:, 8:16], in0=modT[:, 8:16], scalar1=1.0), reads=[modT], writes=[modT])

    winb = k.sb("winb", [128, 8, 3072], BF16)
    for j in range(12):
        st = wst[j % 2]
        op("sp" if j % 2 == 0 else "act", lambda e: e.dma_start(out=st[:], in_=w_in[:, j * 256:(j + 1) * 256].rearrange("(c p) f -> p c f", p=128)),
           writes=[st], dma=True)
        op("pool", lambda e: e.tensor_copy(out=winb[:, :, j * 256:(j + 1) * 256], in_=st[:]), reads=[st], writes=[winb])

    BD = [[k.sb(f"bd{w}_{j}", [128, 128], BF16) for j in range(8)] for w in range(3)]
    for w in range(3):
        for j in range(8):
            op("dve", lambda e: e.tensor_tensor(out=BD[w][j][:].rearrange("p (n o) -> p n o", o=4),
                                                in0=wc[w][:, j:j + 1, :].to_broadcast([128, 32, 4]), in1=bmask[:], op=ALU.mult),
               reads=[wc[w], bmask], writes=[BD[w][j]])
    bdt = [k.sb("bdt0", [128, 128]), k.sb("bdt1", [128, 128])]
    wcg = k.sb("wcg", [128, 16, 4], BF16); wmg = k.sb("wmg", [128, 16, 4], BF16)
    ii = 0
    for j in range(16):
        for w in range(3):
            t = bdt[ii % 2]; ii += 1
            op("dve", lambda e: e.tensor_tensor(out=t[:].rearrange("p (n i) -> p n i", i=4),
                                                in0=wt[w][:, j:j + 1, :].to_broadcast([128, 32, 4]), in1=bmask[:], op=ALU.mult),
               reads=[wt[w], bmask], writes=[t])
            dst = pS[:, 64 + j * 4: 68 + j * 4] if w < 2 else pS[:, 192 + j * 4: 196 + j * 4]
            op("pe", lambda e: e.matmul(dst, lhsT=t[:], rhs=wg[w][:, j, :], start=(w != 1), stop=(w != 0)),
               reads=[t, wg[w]], writes=[pS])
    op("dve", lambda e: e.tensor_copy(out=wcg[:].rearrange("p a b -> p (a b)"), in_=pS[:, 64:128]), reads=[pS], writes=[wcg])
    op("dve", lambda e: e.tensor_copy(out=wmg[:].rearrange("p a b -> p (a b)"), in_=pS[:, 192:256]), reads=[pS], writes=[wmg])

    xs_ = [k.sb("x0", [128, 1024]), k.sb("x1", [128, 1024])]
    xb_ = [k.sb("xb0", [128, 1024], BF16), k.sb("xb1", [128, 1024], BF16)]
    uT = [k.sb("uT0", [128, 8, TB], BF16), k.sb("uT1", [128, 8, TB], BF16)]
    xmt = [k.sb(f"xmt{i}", [128, TB + 3]) for i in range(3)]
    hist = k.sb("hist", [128, 16, 3])
    acc = [k.sb("acc0", [128, TB]), k.sb("acc1", [128, TB])]
    xmb = k.sb("xmb", [128, 8, TB], BF16); xcT = k.sb("xcT", [128, 8, TB], BF16)
    xo = [k.sb(f"xo{i}", [128, 2, TB], BF16) for i in range(2)]
    sz = k.sb("sz", [128, 8, TB], BF16)
    qT = k.sb("qT", [128, 8, TB], BF16); kT = k.sb("kT", [128, 8, TB], BF16)
    ktm = k.sb("ktm", [128, NT, 1024], BF16); vtm = k.sb("vtm", [128, NT, 1024], BF16)
    hg = [k.sb("hg0", [128, 8, TB], BF16), k.sb("hg1", [128, 8, TB], BF16)]
    gT = k.sb("gT", [4, TB]); gts = k.sb("gts", [128, NT, 4]); sp_ = k.sb("sp_", [128, NT, 2]); ex_ = k.sb("ex_", [128, NT, 2])
    Cst = [k.sb(f"C{h}", [128, 4, 512]) for h in range(2)]
    Cb = [k.sb(f"Cb{h}", [128, 4, 512], BF16) for h in range(2)]
    nst = [k.sb(f"n{h}", [128, 4]) for h in range(2)]
    nb_ = [k.sb(f"nb{h}", [128, 4], BF16) for h in range(2)]
    for h in range(2):
        op("pool", lambda e: e.memset(Cst[h][:], 0.0), writes=[Cst[h]])
        op("pool", lambda e: e.memset(Cb[h][:], 0.0), writes=[Cb[h]])
        op("pool", lambda e: e.memset(nst[h][:], 0.0), writes=[nst[h]])
        op("pool", lambda e: e.memset(nb_[h][:], 0.0), writes=[nb_[h]])
    op("pool", lambda e: e.memset(hist[:], 0.0), writes=[hist])
    sprep = [k.sb(f"sprep{h}", [128, 128]) for h in range(2)]
    bias_s = [k.sb(f"bias{h}", [128, 1]) for h in range(2)]
    DT = [k.sb(f"DT{h}", [128, 128]) for h in range(2)]
    EB = [k.sb(f"EB{h}", [128, 128]) for h in range(2)]
    bL = [k.sb(f"bL{h}", [128, 1]) for h in range(2)]
    ws = [k.sb(f"ws{h}", [128, 1]) for h in range(2)]
    PT = [k.sb(f"PT{h}", [128, 128], BF16) for h in range(2)]
    qp = [k.sb(f"qp{h}", [128, 4, 128], BF16) for h in range(2)]
    wk_ = [k.sb(f"wk{h}", [128, 512], BF16) for h in range(2)]
    rden = [k.sb(f"rden{h}", [128, 1]) for h in range(2)]
    hs = [k.sb(f"hs{h}", [128, 512]) for h in range(2)]
    hn = [k.sb(f"hn{h}", [128, 512], BF16) for h in range(2)]
    st6 = [k.sb(f"st6{h}", [128, 6]) for h in range(2)]
    mv = [k.sb(f"mv{h}", [128, 2]) for h in range(2)]
    rstd = [k.sb(f"rstd{h}", [128, 1]) for h in range(2)]
    tmp2 = [k.sb(f"tmp2{h}", [128, 128]) for h in range(2)]
    tmp3 = [k.sb(f"tmp3{h}", [128, 128]) for h in range(2)]
    eps_t = k.sb("eps_t", [128, 1])
    op("pool", lambda e: e.memset(eps_t[:], 1e-5), writes=[eps_t])
    mhalf = k.sb("mhalf", [128, 1])
    op("pool", lambda e: e.memset(mhalf[:], -0.5), writes=[mhalf])
    one_t = k.sb("one_t", [128, 1])
    op("pool", lambda e: e.memset(one_t[:], 1.0), writes=[one_t])

    def bail():
        tk = op("sp", lambda e: e.dma_start(out=out[:, 0:TB].rearrange("(c p) t -> p c t", p=128), in_=hg[0][:]), reads=[hg[0]], dma=True)
        k.finish([tk])
        print("bail instrs", k.n_ins)
        return nc
    out_toks = []
    for blk in range(NB):
        t0 = blk * TB
        u = uT[blk % 2]
        for t4 in range(NT):
            xs = xs_[t4 % 2]; xb = xb_[t4 % 2]
            op("sp", lambda e: e.dma_start(out=xs[:], in_=x[t0 + t4 * 128: t0 + (t4 + 1) * 128, :]), writes=[xs], dma=True)
            op("pool", lambda e: e.tensor_copy(out=xb[:], in_=xs[:]), reads=[xs], writes=[xb])
            for kc in range(8):
                op("pe", lambda e: e.transpose(out=pt[:, kc * 128:(kc + 1) * 128], in_=xb[:, kc * 128:(kc + 1) * 128], identity=ident[:]),
                   reads=[xb, ident], writes=[pt])
            for kc in range(8):
                op("dve", lambda e: e.tensor_scalar(out=u[:, kc, t4 * 128:(t4 + 1) * 128], in0=pt[:, kc * 128:(kc + 1) * 128],
                                                    scalar1=modT[:, 8 + kc:9 + kc], scalar2=modT[:, kc:kc + 1], op0=ALU.mult, op1=ALU.add),
                   reads=[pt, modT], writes=[u])
        for oc in range(16):
            p = nextp()
            for kc in range(8):
                op("pe", lambda e: e.matmul(p[:, 0:TB], lhsT=winb[:, kc, oc * 128:(oc + 1) * 128], rhs=u[:, kc, :], start=(kc == 0), stop=(kc == 7)),
                   reads=[winb, u], writes=[p])
            xm = xmt[oc % 3]
            op("pool", lambda e: e.tensor_copy(out=xm[:, 0:3], in_=hist[:, oc, :]), reads=[hist], writes=[xm])
            op("act", lambda e: e.copy(out=xm[:, 3:TB + 3], in_=p[:, 0:TB]), reads=[p], writes=[xm])
            op("pool", lambda e: e.tensor_copy(out=hist[:, oc, :], in_=xm[:, TB:TB + 3]), reads=[xm], writes=[hist])
            xmdst = xmb[:, oc, :] if oc < 8 else xo[oc % 2][:, 1, :]
            xmdb = xmb if oc < 8 else xo[oc % 2]
            op("pool", lambda e: e.tensor_copy(out=xmdst, in_=xm[:, 3:TB + 3]), reads=[xm], writes=[xmdb])
            a = acc[oc % 2]
            op("dve", lambda e: e.tensor_scalar(out=a[:], in0=xm[:, 3:TB + 3], scalar1=cw[:, oc, 3:4], scalar2=None, op0=ALU.mult),
               reads=[xm, cw], writes=[a])
            for jj in (2, 1, 0):
                op("dve", lambda e: e.scalar_tensor_tensor(out=a[:], in0=xm[:, jj:TB + jj], scalar=cw[:, oc, jj:jj + 1], in1=a[:],
                                                           op0=ALU.mult, op1=ALU.add), reads=[xm, cw, a], writes=[a])
            xcdst = xcT[:, oc, :] if oc < 8 else xo[oc % 2][:, 0, :]
            xcdb = xcT if oc < 8 else xo[oc % 2]
            op("act", lambda e: e.activation(out=xcdst, in_=a[:], func=AF.Silu, bias=cb[:, oc:oc + 1]), reads=[a, cb], writes=[xcdb])
            xmsrc = xmdst
            op("pe", lambda e: e.matmul(pS[0:4, 0:TB], lhsT=wcg[:, oc, :], rhs=xcdst, start=(oc == 0), stop=False), reads=[wcg, xcdb], writes=[pS])
            op("pe", lambda e: e.matmul(pS[0:4, 0:TB], lhsT=wmg[:, oc, :], rhs=xmsrc, start=False, stop=(oc == 15)), reads=[wmg, xmdb], writes=[pS])
        op("act", lambda e: e.copy(out=gT[:], in_=pS[0:4, 0:TB]), reads=[pS], writes=[gT])
        for oc in range(8):
            p = nextp()
            for kc in range(8):
                op("pe", lambda e: e.matmul(p[:, 0:TB], lhsT=winb[:, kc, 2048 + oc * 128: 2048 + (oc + 1) * 128], rhs=u[:, kc, :], start=(kc == 0), stop=(kc == 7)),
                   reads=[winb, u], writes=[p])
            op("act", lambda e: e.activation(out=sz[:, oc, :], in_=p[:, 0:TB], func=AF.Silu), reads=[p], writes=[sz])
        for j in range(8):
            p = nextp()
            op("pe", lambda e: e.matmul(p[:, 0:TB], lhsT=BD[0][j][:], rhs=xcT[:, j, :], start=True, stop=True), reads=[BD[0][j], xcT], writes=[p])
            op("act", lambda e: e.mul(out=qT[:, j, :], in_=p[:, 0:TB], mul=DH ** -0.5), reads=[p], writes=[qT])
            p = nextp()
            op("pe", lambda e: e.matmul(p[:, 0:TB], lhsT=BD[1][j][:], rhs=xcT[:, j, :], start=True, stop=True), reads=[BD[1][j], xcT], writes=[p])
            op("dve", lambda e: e.tensor_copy(out=kT[:, j, :], in_=p[:, 0:TB]), reads=[p], writes=[kT])
        for t4 in range(NT):
            for half in range(2):
                p = nextp()
                for jj in range(4):
                    j = half * 4 + jj
                    op("pe", lambda e: e.matmul(p[:, jj * 128:(jj + 1) * 128], lhsT=xcT[:, j, t4 * 128:(t4 + 1) * 128], rhs=BD[1][j][:], start=True, stop=True),
                       reads=[xcT, BD[1][j]], writes=[p])
                op("act", lambda e: e.copy(out=ktm[:, t4, half * 512:(half + 1) * 512], in_=p[:, :]), reads=[p], writes=[ktm])
                p = nextp()
                for jj in range(4):
                    j = half * 4 + jj
                    op("pe", lambda e: e.matmul(p[:, jj * 128:(jj + 1) * 128], lhsT=xmb[:, j, t4 * 128:(t4 + 1) * 128], rhs=BD[2][j][:], start=True, stop=True),
                       reads=[xmb, BD[2][j]], writes=[p])
                op("dve", lambda e: e.tensor_copy(out=vtm[:, t4, half * 512:(half + 1) * 512], in_=p[:, :]), reads=[p], writes=[vtm])
        for t4 in range(NT):
            op("pe", lambda e: e.matmul(pS[:, 256 + t4 * 4: 260 + t4 * 4], lhsT=gT[:, t4 * 128:(t4 + 1) * 128], rhs=identf[0:4, 0:4], start=True, stop=True),
               reads=[gT, identf], writes=[pS])
        op("dve", lambda e: e.tensor_tensor(out=gts[:], in0=pS[:, 256:256 + 4 * NT].rearrange("p (a b) -> p a b", b=4),
                                            in1=bgs[:, None, :].to_broadcast([128, NT, 4]), op=ALU.add), reads=[pS, bgs], writes=[gts])
        op("act", lambda e: e.activation(out=ex_[:], in_=gts[:, :, 2:4], func=AF.Exp, scale=-1.0), reads=[gts], writes=[ex_])
        op("act", lambda e: e.activation(out=sp_[:], in_=ex_[:], func=AF.Ln, bias=one_t[:]), reads=[ex_, one_t], writes=[sp_])
        hgb = hg[blk % 2]

        def chain(t4, h, hgb=hgb):
            tsl = slice(t4 * 128, (t4 + 1) * 128)
            pb = pbh[h]; pS = pSh[h]; pn = pnh[h]; pt = ptFh[h]; pc = [pA, pB]
            if True:
                hc = slice(h * 512, (h + 1) * 512)
                op("dve", lambda e: e.tensor_scalar(out=sprep[h][:], in0=onesf[:], scalar1=sp_[:, t4, h:h + 1], scalar2=None, op0=ALU.mult),
                   reads=[onesf, sp_], writes=[sprep[h]])
                op("pe", lambda e: e.matmul(pb[:, 0:128], lhsT=sprep[h][:], rhs=ntri[:], start=True, stop=False), reads=[sprep[h], ntri], writes=[pb])
                op("pe", lambda e: e.matmul(pb[:, 0:128], lhsT=identf[:], rhs=negm[:], start=False, stop=True), reads=[identf, negm], writes=[pb])
                op("pe", lambda e: e.matmul(pb[:, 128:256], lhsT=sprep[h][:], rhs=ntri[:], start=True, stop=True), reads=[sprep[h], ntri], writes=[pb])
                op("pe", lambda e: e.matmul(pb[:, 256:258], lhsT=ntri[:], rhs=sp_[:, t4, :], start=True, stop=True), reads=[ntri, sp_], writes=[pb])
                yield
                op("dve", lambda e: e.tensor_tensor(out=bias_s[h][:], in0=gts[:, t4, h:h + 1], in1=pb[:, 256 + h:257 + h], op=ALU.subtract),
                   reads=[gts, pb], writes=[bias_s[h]])
                op("dve", lambda e: e.tensor_copy(out=bL[h][:], in_=pb[:, 255:256]), reads=[pb], writes=[bL[h]])
                yield
                op("act", lambda e: e.activation(out=DT[h][:], in_=pb[:, 0:128], func=AF.Exp, bias=bias_s[h][:]), reads=[pb, bias_s[h], bL[h]], writes=[DT[h]])
                op("act", lambda e: e.activation(out=EB[h][:], in_=pb[:, 128:256], func=AF.Exp), reads=[pb, bL[h]], writes=[EB[h]])
                op("act", lambda e: e.activation(out=ws[h][:], in_=bias_s[h][:], func=AF.Exp, bias=bL[h][:]), reads=[bias_s[h], bL[h]], writes=[ws[h]])
                yield
                for dc in range(4):
                    op("pe", lambda e: e.matmul(pS[:, 260:388], lhsT=kT[:, 4 * h + dc, tsl], rhs=qT[:, 4 * h + dc, tsl], start=(dc == 0), stop=(dc == 3)),
                       reads=[kT, qT], writes=[pS])
                yield
                op("dve", lambda e: e.tensor_tensor(out=PT[h][:], in0=pS[:, 260:388], in1=DT[h][:], op=ALU.mult), reads=[pS, DT[h]], writes=[PT[h]])
                yield
                for dc in range(4):
                    op("pool", lambda e: e.tensor_tensor(out=qp[h][:, dc, :], in0=qT[:, 4 * h + dc, tsl], in1=EB[h][:], op=ALU.mult),
                       reads=[qT, EB[h]], writes=[qp[h]])
                op("pool", lambda e: e.tensor_scalar(out=wk_[h][:], in0=ktm[:, t4, hc], scalar1=ws[h][:], scalar2=None, op0=ALU.mult),
                   reads=[ktm, ws[h]], writes=[wk_[h]])
                yield
                op("pe", lambda e: e.matmul(pn[:, :], lhsT=PT[h][:], rhs=vtm[:, t4, hc], start=True, stop=False), reads=[PT[h], vtm], writes=[pn])
                for dc in range(4):
                    op("pe", lambda e: e.matmul(pn[:, :], lhsT=qp[h][:, dc, :], rhs=Cb[h][:, dc, :], start=False, stop=(dc == 3)),
                       reads=[qp[h], Cb[h]], writes=[pn])
                op("pe", lambda e: e.matmul(pS[:, 388:389], lhsT=PT[h][:], rhs=onesb[:, 0:1], start=True, stop=False), reads=[PT[h], onesb], writes=[pS])
                for dc in range(4):
                    op("pe", lambda e: e.matmul(pS[:, 388:389], lhsT=qp[h][:, dc, :], rhs=nb_[h][:, dc:dc + 1], start=False, stop=(dc == 3)),
                       reads=[qp[h], nb_[h]], writes=[pS])
                yield
                for dc in range(4):
                    pcc = pc[dc % 2]
                    op("pe", lambda e: e.matmul(pcc[:, :], lhsT=wk_[h][:, dc * 128:(dc + 1) * 128], rhs=vtm[:, t4, hc], start=True, stop=True),
                       reads=[wk_[h], vtm], writes=[pcc])
                    op("dve", lambda e: e.scalar_tensor_tensor(out=Cst[h][:, dc, :], in0=Cst[h][:, dc, :], scalar=EB[h][:, 127:128], in1=pcc[:, :],
                                                               op0=ALU.mult, op1=ALU.add), reads=[Cst[h], EB[h], pcc], writes=[Cst[h]])
                    yield
                yield
                op("pool", lambda e: e.tensor_copy(out=Cb[h][:], in_=Cst[h][:]), reads=[Cst[h]], writes=[Cb[h]])
                for dc in range(4):
                    op("pe", lambda e: e.matmul(pS[:, 392 + dc:393 + dc], lhsT=wk_[h][:, dc * 128:(dc + 1) * 128], rhs=onesb[:, 0:1], start=True, stop=True),
                       reads=[wk_[h], onesb], writes=[pS])
                yield
                op("dve", lambda e: e.tensor_scalar(out=rden[h][:], in0=pS[:, 388:389], scalar1=-1.0, scalar2=None, op0=ALU.mult),
                   reads=[pS], writes=[rden[h]])
                op("dve", lambda e: e.tensor_tensor(out=rden[h][:], in0=rden[h][:], in1=pS[:, 388:389], op=ALU.max),
                   reads=[pS, rden[h]], writes=[rden[h]])
                op("dve", lambda e: e.tensor_scalar(out=rden[h][:], in0=rden[h][:], scalar1=1.0, scalar2=None, op0=ALU.max),
                   reads=[rden[h]], writes=[rden[h]])
                op("dve", lambda e: e.scalar_tensor_tensor(out=nst[h][:], in0=nst[h][:], scalar=EB[h][:, 127:128], in1=pS[:, 392:396],
                                                           op0=ALU.mult, op1=ALU.add), reads=[nst[h], EB[h], pS], writes=[nst[h]])
                op("dve", lambda e: e.tensor_copy(out=nb_[h][:], in_=nst[h][:]), reads=[nst[h]], writes=[nb_[h]])
                op("dve", lambda e: e.reciprocal(out=rden[h][:], in_=rden[h][:]), reads=[rden[h]], writes=[rden[h]])
                op("dve", lambda e: e.tensor_scalar(out=hs[h][:], in0=pn[:, :], scalar1=rden[h][:], scalar2=None, op0=ALU.mult), reads=[pn, rden[h]], writes=[hs[h]])
                yield
                op("dve", lambda e: e.bn_stats(out=st6[h][:], in_=hs[h][:]), reads=[hs[h]], writes=[st6[h]])
                op("dve", lambda e: e.bn_aggr(out=mv[h][:], in_=st6[h][:]), reads=[st6[h]], writes=[mv[h]])
                yield
                op("pool", lambda e: e.tensor_scalar(out=rstd[h][:], in0=mv[h][:, 1:2], scalar1=1e-5, scalar2=None, op0=ALU.add), reads=[mv[h]], writes=[rstd[h]])
                op("pool", lambda e: e.tensor_tensor(out=rstd[h][:], in0=rstd[h][:], in1=mhalf[:], op=ALU.pow), reads=[rstd[h], mhalf], writes=[rstd[h]])
                yield
                op("dve", lambda e: e.tensor_scalar(out=hn[h][:], in0=hs[h][:], scalar1=mv[h][:, 0:1], scalar2=rstd[h][:], op0=ALU.subtract, op1=ALU.mult),
                   reads=[hs[h], mv[h], rstd[h]], writes=[hn[h]])
                yield
                for dc in range(4):
                    op("pe", lambda e: e.transpose(out=pt[:, h * 512 + dc * 128:h * 512 + (dc + 1) * 128], in_=hn[h][:, dc * 128:(dc + 1) * 128], identity=ident[:]),
                       reads=[hn[h], ident], writes=[pt])
                yield
                for dc in range(4):
                    j = 4 * h + dc
                    op("pool", lambda e: e.tensor_scalar(out=tmp2[h][:], in0=xcT[:, j, tsl], scalar1=sk[:, j:j + 1], scalar2=None, op0=ALU.mult),
                       reads=[xcT, sk], writes=[tmp2[h]])
                    op("dve", lambda e: e.scalar_tensor_tensor(out=tmp3[h][:], in0=pt[:, h * 512 + dc * 128:h * 512 + (dc + 1) * 128], scalar=nw[:, j:j + 1], in1=tmp2[h][:],
                                                               op0=ALU.mult, op1=ALU.add), reads=[pt, nw, tmp2[h]], writes=[tmp3[h]])
                    op("pool", lambda e: e.tensor_tensor(out=hgb[:, j, tsl], in0=tmp3[h][:], in1=sz[:, j, tsl], op=ALU.mult),
                       reads=[tmp3[h], sz], writes=[hgb])
                    yield
        for t4 in range(NT):
            gens = [chain(t4, h) for h in HS]
            while gens:
                for g_ in list(gens):
                    try:
                        next(g_)
                    except StopIteration:
                        gens.remove(g_)
        tk = op("pool", lambda e: e.dma_start(out=out[:, t0:t0 + TB].rearrange("(c p) t -> p c t", p=128), in_=hgb[:]), reads=[hgb], dma=True)
        out_toks.append(tk)
    if fz is None:
        k.finish(out_toks)
    else:
        k.end_stage()
    print("m0 instructions:", k.n_ins)
    return nc


def m0_inputs(inp, b, hp, S):
    f = np.float32
    own = np.arange(hp * 1024, (hp + 1) * 1024); oth = np.arange((1 - hp) * 1024, (2 - hp) * 1024)
    ch = np.concatenate([own, oth])
    w_in = inp["mlstm_w_in"][0]
    d = {}
    d["x"] = np.ascontiguousarray(inp["x"][b, :S])
    d["condT"] = np.ascontiguousarray(inp["c"][b].reshape(8, 128).T)
    d["adaw"] = np.ascontiguousarray(inp["ada_w"][0][:, 0:2048])
    d["adabT"] = np.ascontiguousarray(inp["ada_b"][0][0:2048].reshape(16, 128).T)
    d["w_in"] = np.ascontiguousarray(np.concatenate([w_in[:, ch], w_in[:, 2048 + own]], axis=1))
    d["convw"] = np.ascontiguousarray(inp["mlstm_conv_w"][0].T[ch].reshape(16, 128, 4).transpose(1, 0, 2))
    d["convb"] = np.ascontiguousarray(inp["mlstm_conv_b"][0][ch].reshape(16, 128).T)
    blk = ch.reshape(-1, 4)[:, 0] // 4
    for nm, key in (("q", "mlstm_wq"), ("k", "mlstm_wk"), ("v", "mlstm_wv")):
        w = inp[key][0][blk]
        d["w%sc" % nm] = np.ascontiguousarray(w.reshape(16, 128, 4).transpose(1, 0, 2))
        d["w%st" % nm] = np.ascontiguousarray(w.transpose(0, 2, 1).reshape(16, 128, 4).transpose(1, 0, 2))
    gcols = [2 * hp, 2 * hp + 1, 4 + 2 * hp, 5 + 2 * hp]
    wgates = inp["mlstm_w_gates"][0]
    for i, nm in enumerate("qkv"):
        wg = wgates[i * 2048:(i + 1) * 2048][ch][:, gcols]
        d["wg" + nm] = np.ascontiguousarray(wg.reshape(16, 128, 4).transpose(1, 0, 2))
    d["bg"] = np.ascontiguousarray(np.tile(inp["mlstm_b_gates"][0][gcols][None, :], (128, 1)))
    d["nwT"] = np.ascontiguousarray(inp["mlstm_norm_w"][0][own].reshape(8, 128).T)
    d["skT"] = np.ascontiguousarray(inp["mlstm_skip"][0][own].reshape(8, 128).T)
    return {k_: v.astype(f) for k_, v in d.items()}


ALPHA = 4 ** 0.25
EPS = 1e-5


def consts(k):
    op = k.op
    c = {}
    c["identf"] = k.sb("identf", [128, 128])
    c["onesf"] = k.sb("onesf", [128, 128])
    c["mhalf"] = k.sb("mhalf", [128, 1])
    op("pool", lambda e: e.memset(c["identf"][:], 0.0), writes=[c["identf"]])
    op("pool", lambda e: e.affine_select(out=c["identf"][:], in_=c["identf"][:], pattern=[[-1, 128]], compare_op=ALU.not_equal,
                                         fill=1.0, base=0, channel_multiplier=1), reads=[c["identf"]], writes=[c["identf"]])
    op("pool", lambda e: e.memset(c["onesf"][:], 1.0), writes=[c["onesf"]])
    op("pool", lambda e: e.memset(c["mhalf"][:], -0.5), writes=[c["mhalf"]])
    return c


def adaln(k, c, condT_d, adaw_f, adab_f, nf, adaw_b, adab_b, nb, pbank, stg):
    op = k.op
    condT = k.sb("condT_s", [128, 8]); cond = k.sb("cond", [128, 8])
    op("sp", lambda e: e.dma_start(out=condT[:], in_=condT_d), writes=[condT], dma=True)
    op("act", lambda e: e.activation(out=cond[:], in_=condT[:], func=AF.Silu), reads=[condT], writes=[cond])
    modT = None
    if nf:
        modT = k.sb("modT", [128, nf * 8]); adab = k.sb("adabf", [128, nf * 8])
        op("sp", lambda e: e.dma_start(out=adab[:], in_=adab_f), writes=[adab], dma=True)
        for j in range(nf * 8):
            st = stg[j % 2]
            op("sp", lambda e: e.dma_start(out=st[:, :, 0:128], in_=adaw_f[:, j * 128:(j + 1) * 128].rearrange("(c p) f -> p c f", p=128)), writes=[st], dma=True)
            for kc in range(8):
                op("pe", lambda e: e.matmul(pbank[:, j:j + 1], lhsT=st[:, kc, 0:128], rhs=cond[:, kc:kc + 1], start=(kc == 0), stop=(kc == 7)),
                   reads=[st, cond], writes=[pbank])
        op("dve", lambda e: e.tensor_tensor(out=modT[:], in0=pbank[:, 0:nf * 8], in1=adab[:], op=ALU.add), reads=[pbank, adab], writes=[modT])
    bts = []
    if nb:
        crep = k.sb("crep", [128, 8, 128])
        for kc in range(8):
            op("dve", lambda e: e.tensor_scalar(out=crep[:, kc, :], in0=c["onesf"][:], scalar1=cond[:, kc:kc + 1], scalar2=None, op0=ALU.mult),
               reads=[c["onesf"], cond], writes=[crep])
        for v in range(nb):
            bt = k.sb(f"bt{v}", [128, 1024])
            op("sp", lambda e: e.dma_start(out=bt[:], in_=adab_b[:, v * 1024:(v + 1) * 1024]), writes=[bt], dma=True)
            for q in range(4):
                st = stg[q % 2]
                op("sp", lambda e: e.dma_start(out=st[:], in_=adaw_b[:, v * 1024 + q * 256: v * 1024 + (q + 1) * 256].rearrange("(c p) f -> p c f", p=128)),
                   writes=[st], dma=True)
                for kc in range(8):
                    op("pe", lambda e: e.matmul(pbank[:, 0:256], lhsT=crep[:, kc, :], rhs=st[:, kc, :], start=(kc == 0), stop=(kc == 7)),
                       reads=[crep, st], writes=[pbank])
                op("dve", lambda e: e.tensor_tensor(out=bt[:, q * 256:(q + 1) * 256], in0=bt[:, q * 256:(q + 1) * 256], in1=pbank[:, 0:256], op=ALU.add),
                   reads=[bt, pbank], writes=[bt])
            bts.append(bt)
    return modT, bts


def res_ln(k, c, ysrc, ybuf, xs, G, lng, lnb, tmp, st12, mv, rstd, xo):
    op = k.op
    op("dve", lambda e: e.tensor_tensor(out=tmp[:], in0=ysrc, in1=G[:], op=ALU.mult), reads=ybuf + [G], writes=[tmp])
    op("dve", lambda e: e.scalar_tensor_tensor(out=tmp[:], in0=xs[:], scalar=ALPHA, in1=tmp[:], op0=ALU.mult, op1=ALU.add), reads=[xs, tmp], writes=[tmp])
    for hh in range(2):
        op("dve", lambda e: e.bn_stats(out=st12[:, hh * 6:(hh + 1) * 6], in_=tmp[:, hh * 512:(hh + 1) * 512]), reads=[tmp], writes=[st12])
    op("dve", lambda e: e.bn_aggr(out=mv[:], in_=st12[:]), reads=[st12], writes=[mv])
    op("pool", lambda e: e.tensor_scalar(out=rstd[:], in0=mv[:, 1:2], scalar1=EPS, scalar2=None, op0=ALU.add), reads=[mv], writes=[rstd])
    op("pool", lambda e: e.tensor_tensor(out=rstd[:], in0=rstd[:], in1=c["mhalf"][:], op=ALU.pow), reads=[rstd, c["mhalf"]], writes=[rstd])
    op("dve", lambda e: e.tensor_scalar(out=tmp[:], in0=tmp[:], scalar1=mv[:, 0:1], scalar2=rstd[:], op0=ALU.subtract, op1=ALU.mult),
       reads=[tmp, mv, rstd], writes=[tmp])
    op("pool", lambda e: e.tensor_tensor(out=tmp[:], in0=tmp[:], in1=lng[:], op=ALU.mult), reads=[tmp, lng], writes=[tmp])
    op("pool", lambda e: e.tensor_tensor(out=xo[:], in0=tmp[:], in1=lnb[:], op=ALU.add), reads=[tmp, lnb], writes=[xo])


def mod_transpose(k, c, xo, modT, m0, pT, uTf, uTb):
    op = k.op
    for kc in range(8):
        op("pe", lambda e: e.transpose(out=pT[kc // 4][:, (kc % 4) * 128:(kc % 4 + 1) * 128], in_=xo[:, kc * 128:(kc + 1) * 128], identity=c["identf"][:]),
           reads=[xo, c["identf"]], writes=[pT[kc // 4]])
    for kc in range(8):
        dst = uTf if uTf is not None else uTb
        op("dve", lambda e: e.tensor_scalar(out=dst[:, kc, :], in0=pT[kc // 4][:, (kc % 4) * 128:(kc % 4 + 1) * 128],
                                            scalar1=modT[:, m0 + 8 + kc:m0 + 9 + kc], scalar2=modT[:, m0 + kc:m0 + kc + 1], op0=ALU.mult, op1=ALU.add),
           reads=[pT[kc // 4], modT], writes=[dst])
    if uTf is not None:
        op("pool", lambda e: e.tensor_copy(out=uTb[:], in_=uTf[:]), reads=[uTf], writes=[uTb])


def build_proj(TC, KC, glu, router, fz=None, blend=False):
    if fz is None:
        nc = bass.Bass("TRN2", target_bir_lowering=False); k = Ctx(nc); pref = ""
    else:
        nc, k, pref = fz.nc, fz.k, fz.pref
    D = mkD(nc, fz, pref)
    k.begin_stage(pref)
    NOUT = 2048 if glu else 1024
    inT = D("inT", [KC * 128, TC * (2 if blend else 1)], BF16)
    if blend:
        msk_d = D("msk", [128, 2])
    W = D("W", [KC * 128, NOUT])
    x = D("x", [TC, 1024])
    condT_d = D("condT", [128, 8])
    adaw_f = D("adaw_f", [1024, 2048]); adab_f = D("adab_f", [128, 16])
    adaw_b = D("adaw_b", [1024, 1024]); adab_b = D("adab_b", [128, 1024])
    lng_d = D("lng", [128, 1024]); lnb_d = D("lnb", [128, 1024])
    if glu:
        bglu_d = D("bglu", [128, 2048])
    if router:
        wr_d = D("wr", [128, 8, 8])
        gates_o = D("gates", [TC, 8], kind="ExternalOutput")
    xmid_o = D("xmid", [TC, 1024], kind="ExternalOutput")
    uT_o = D("uT", [1024, TC], BF16, kind="ExternalOutput")
    op = k.op
    c = consts(k)
    if blend:
        msk = k.sb("msk_s", [128, 2])
        op("sp", lambda e: e.dma_start(out=msk[:], in_=msk_d), writes=[msk], dma=True)
        itc = [[k.sb(f"itc{i}_{j}", [128, KC, 128], BF16) for j in range(2)] for i in range(2)]
    pY = [k.ps(f"pY{i}", [128, 512]) for i in range(4)]
    pT = [k.ps("pT0", [128, 512]), k.ps("pT1", [128, 512])]
    pM = k.ps("pM", [128, 512])
    stg = [k.sb("stg0", [128, 8, 256]), k.sb("stg1", [128, 8, 256])]
    modT, bts = adaln(k, c, condT_d, adaw_f, adab_f, 2, adaw_b, adab_b, 1, pM, stg)
    op("dve", lambda e: e.tensor_scalar_add(out=modT[:, 8:16], in0=modT[:, 8:16], scalar1=1.0), reads=[modT], writes=[modT])
    G = bts[0]
    op("dve", lambda e: e.tensor_scalar_add(out=G[:], in0=G[:], scalar1=1.0), reads=[G], writes=[G])
    lng = k.sb("lng_s", [128, 1024]); lnb = k.sb("lnb_s", [128, 1024])
    op("sp", lambda e: e.dma_start(out=lng[:], in_=lng_d), writes=[lng], dma=True)
    op("sp", lambda e: e.dma_start(out=lnb[:], in_=lnb_d), writes=[lnb], dma=True)
    if glu:
        bglu = k.sb("bglu_s", [128, 2048])
        op("sp", lambda e: e.dma_start(out=bglu[:], in_=bglu_d), writes=[bglu], dma=True)
    if router:
        wr = k.sb("wr_s", [128, 8, 8])
        op("sp", lambda e: e.dma_start(out=wr[:], in_=wr_d), writes=[wr], dma=True)
    Wb = k.sb("Wb", [128, KC, NOUT], BF16)
    ws2 = [k.sb("ws2_0", [128, NOUT]), k.sb("ws2_1", [128, NOUT])]
    for kc in range(KC):
        st = ws2[kc % 2]
        op("sp" if kc % 2 == 0 else "act", lambda e: e.dma_start(out=st[:], in_=W[kc * 128:(kc + 1) * 128, :]), writes=[st], dma=True)
        op("pool", lambda e: e.tensor_copy(out=Wb[:, kc, :], in_=st[:]), reads=[st], writes=[Wb])
    NTL = TC // 128
    it = [k.sb(f"it{i}", [128, KC, 128], BF16) for i in range(2)]
    xs_ = [k.sb(f"xs{i}", [128, 1024]) for i in range(2)]
    tmp = [k.sb(f"tmp{i}", [128, 1024]) for i in range(2)]
    xo = [k.sb(f"xo{i}", [128, 1024]) for i in range(2)]
    yv = [k.sb(f"yv{i}", [128, 1024]) for i in range(2)]
    sg = [k.sb(f"sg{i}", [128, 1024]) for i in range(2)]
    st12 = [k.sb(f"st12{i}", [128, 12]) for i in range(2)]
    mv = [k.sb(f"mv{i}", [128, 2]) for i in range(2)]
    rstd = [k.sb(f"rstd{i}", [128, 1]) for i in range(2)]
    uTf = [k.sb(f"uTf{i}", [128, 8, 128]) for i in range(2)]
    uTb = [k.sb(f"uTb{i}", [128, 8, 128], BF16) for i in range(2)]
    if router:
        m8 = [k.sb(f"m8{i}", [128, 8]) for i in range(2)]
        lg = [k.sb(f"lg{i}", [128, 8]) for i in range(2)]
        gw = [k.sb(f"gw{i}", [128, 2]) for i in range(2)]
        gt = [k.sb(f"gt{i}", [128, 8]) for i in range(2)]
        eq = [k.sb(f"eq{i}", [128, 8]) for i in range(2)]
    toks = []
    for t in range(NTL):
        i2 = t % 2
        tsl = slice(t * 128, (t + 1) * 128)
        if not blend:
            op("sp", lambda e: e.dma_start(out=it[i2][:], in_=inT[:, tsl].rearrange("(c p) t -> p c t", p=128)), writes=[it[i2]], dma=True)
        else:
            for j in range(2):
                cs_ = slice(j * TC + t * 128, j * TC + (t + 1) * 128)
                op("sp", lambda e: e.dma_start(out=itc[i2][j][:], in_=inT[:, cs_].rearrange("(c p) t -> p c t", p=128)), writes=[itc[i2][j]], dma=True)
            op("pool", lambda e: e.tensor_scalar(out=itc[i2][0][:], in0=itc[i2][0][:], scalar1=msk[:, 0:1], scalar2=None, op0=ALU.mult), reads=[itc[i2][0], msk], writes=[itc[i2][0]])
            op("dve", lambda e: e.scalar_tensor_tensor(out=it[i2][:], in0=itc[i2][1][:], scalar=msk[:, 1:2], in1=itc[i2][0][:], op0=ALU.mult, op1=ALU.add),
               reads=[itc[i2][0], itc[i2][1], msk], writes=[it[i2]])
        op("act", lambda e: e.dma_start(out=xs_[i2][:], in_=x[tsl, :]), writes=[xs_[i2]], dma=True)
        for nb in range(NOUT // 512):
            p = pY[nb]
            for kc in range(KC):
                op("pe", lambda e: e.matmul(p[:, :], lhsT=it[i2][:, kc, :], rhs=Wb[:, kc, nb * 512:(nb + 1) * 512], start=(kc == 0), stop=(kc == KC - 1)),
                   reads=[it[i2], Wb], writes=[p])
        if glu:
            for nb in range(2):
                op("dve", lambda e: e.tensor_tensor(out=sg[i2][:, nb * 512:(nb + 1) * 512], in0=pY[2 + nb][:, :], in1=bglu[:, 1024 + nb * 512:1024 + (nb + 1) * 512], op=ALU.add),
                   reads=[pY[2 + nb], bglu], writes=[sg[i2]])
                op("dve", lambda e: e.tensor_tensor(out=yv[i2][:, nb * 512:(nb + 1) * 512], in0=pY[nb][:, :], in1=bglu[:, nb * 512:(nb + 1) * 512], op=ALU.add),
                   reads=[pY[nb], bglu], writes=[yv[i2]])
            op("act", lambda e: e.activation(out=sg[i2][:], in_=sg[i2][:], func=AF.Sigmoid), reads=[sg[i2]], writes=[sg[i2]])
            op("pool", lambda e: e.tensor_tensor(out=yv[i2][:], in0=yv[i2][:], in1=sg[i2][:], op=ALU.mult), reads=[yv[i2], sg[i2]], writes=[yv[i2]])
        else:
            for nb in range(2):
                op("act", lambda e: e.copy(out=yv[i2][:, nb * 512:(nb + 1) * 512], in_=pY[nb][:, :]), reads=[pY[nb]], writes=[yv[i2]])
        res_ln(k, c, yv[i2][:], [yv[i2]], xs_[i2], G, lng, lnb, tmp[i2], st12[i2], mv[i2], rstd[i2], xo[i2])
        toks.append(op("pool", lambda e: e.dma_start(out=xmid_o[tsl, :], in_=xo[i2][:]), reads=[xo[i2]], dma=True))
        mod_transpose(k, c, xo[i2], modT, 0, pT, uTf[i2], uTb[i2])
        toks.append(op("pool", lambda e: e.dma_start(out=uT_o[:, tsl].rearrange("(c p) t -> p c t", p=128), in_=uTb[i2][:]), reads=[uTb[i2]], dma=True))
        if router:
            for kc in range(8):
                op("pe", lambda e: e.matmul(pM[:, 0:8], lhsT=uTf[i2][:, kc, :], rhs=wr[:, kc, :], start=(kc == 0), stop=(kc == 7)),
                   reads=[uTf[i2], wr], writes=[pM])
            op("dve", lambda e: e.tensor_copy(out=lg[i2][:], in_=pM[:, 0:8]), reads=[pM], writes=[lg[i2]])
            op("dve", lambda e: e.max(out=m8[i2][:], in_=lg[i2][:]), reads=[lg[i2]], writes=[m8[i2]])
            op("dve", lambda e: e.tensor_tensor(out=gw[i2][:, 0:1], in0=m8[i2][:, 1:2], in1=m8[i2][:, 0:1], op=ALU.subtract), reads=[m8[i2]], writes=[gw[i2]])
            op("act", lambda e: e.activation(out=gw[i2][:, 1:2], in_=gw[i2][:, 0:1], func=AF.Sigmoid), reads=[gw[i2]], writes=[gw[i2]])
            op("dve", lambda e: e.tensor_scalar(out=gw[i2][:, 0:1], in0=gw[i2][:, 1:2], scalar1=-1.0, scalar2=1.0, op0=ALU.mult, op1=ALU.add),
               reads=[gw[i2]], writes=[gw[i2]])
            op("dve", lambda e: e.tensor_scalar(out=gt[i2][:], in0=lg[i2][:], scalar1=m8[i2][:, 0:1], scalar2=gw[i2][:, 0:1], op0=ALU.is_equal, op1=ALU.mult),
               reads=[lg[i2], m8[i2], gw[i2]], writes=[gt[i2]])
            op("dve", lambda e: e.tensor_scalar(out=eq[i2][:], in0=lg[i2][:], scalar1=m8[i2][:, 1:2], scalar2=gw[i2][:, 1:2], op0=ALU.is_equal, op1=ALU.mult),
               reads=[lg[i2], m8[i2], gw[i2]], writes=[eq[i2]])
            op("dve", lambda e: e.tensor_tensor(out=gt[i2][:], in0=gt[i2][:], in1=eq[i2][:], op=ALU.add), reads=[gt[i2], eq[i2]], writes=[gt[i2]])
            toks.append(op("pool", lambda e: e.dma_start(out=gates_o[tsl, :], in_=gt[i2][:]), reads=[gt[i2]], dma=True))
    if fz is None:
        k.finish(toks)
    else:
        k.end_stage()
    print("proj instructions", k.n_ins)
    return nc


def build_ffn(TC, NE, T, emit_u, fz=None):
    if fz is None:
        nc = bass.Bass("TRN2", target_bir_lowering=False); k = Ctx(nc); pref = ""
    else:
        nc, k, pref = fz.nc, fz.k, fz.pref
    D = mkD(nc, fz, pref)
    k.begin_stage(pref)
    uT_d = D("uT", [1024, TC], BF16)
    xmid = D("xmid", [TC, 1024])
    w13 = D("w13", [NE, 1024, 5632]); w2 = D("w2", [NE, 2816, 1024])
    if NE > 1:
        gates_d = D("gates", [TC, NE])
    condT_d = D("condT", [128, 8])
    adaw_b = D("adaw_b", [1024, 1024]); adab_b = D("adab_b", [128, 1024])
    lng_d = D("lng", [128, 1024]); lnb_d = D("lnb", [128, 1024])
    if emit_u:
        adaw_f = D("adaw_f", [1024, 2048]); adab_f = D("adab_f", [128, 16])
        uT_o = D("uTn", [1024, TC], BF16, kind="ExternalOutput")
    xout = D("xout", [TC, 1024], kind="ExternalOutput")
    op = k.op
    c = consts(k)
    pH = [k.ps(f"pH{i}", [128, 512]) for i in range(4)]
    pY = [k.ps(f"pY{i}", [128, 512]) for i in range(2)]
    pT = [k.ps("pT0", [128, 512]), k.ps("pT1", [128, 512])]
    stg = [k.sb("stg0", [128, 8, 256]), k.sb("stg1", [128, 8, 256])]
    modT, bts = adaln(k, c, condT_d, adaw_f if emit_u else None, adab_f if emit_u else None, 2 if emit_u else 0, adaw_b, adab_b, 1, pT[0], stg)
    if emit_u:
        op("dve", lambda e: e.tensor_scalar_add(out=modT[:, 8:16], in0=modT[:, 8:16], scalar1=1.0), reads=[modT], writes=[modT])
    G = bts[0]
    op("dve", lambda e: e.tensor_scalar_add(out=G[:], in0=G[:], scalar1=1.0), reads=[G], writes=[G])
    lng = k.sb("lng_s", [128, 1024]); lnb = k.sb("lnb_s", [128, 1024])
    op("sp", lambda e: e.dma_start(out=lng[:], in_=lng_d), writes=[lng], dma=True)
    op("sp", lambda e: e.dma_start(out=lnb[:], in_=lnb_d), writes=[lnb], dma=True)
    NTB = T // 128
    NBLK = TC // T
    if NBLK > 1:
        scr13 = nc.dram_tensor(pref + "scr13", [NE * 22 * 128, 2048], BF16).ap()
        scr2 = nc.dram_tensor(pref + "scr2", [NE * 4 * 128, 11 * 512], BF16).ap()
        S13 = [[Buf(scr13[(ex * 22 + j) * 128:(ex * 22 + j + 1) * 128, :]) for j in range(22)] for ex in range(NE)]
        S2 = [[Buf(scr2[(ex * 4 + q4) * 128:(ex * 4 + q4 + 1) * 128, :]) for q4 in range(4)] for ex in range(NE)]
    uT = k.sb("uT_s", [128, 8, T], BF16)
    aT = k.sb("aT", [128, 22, T], BF16)
    yacc = [k.sb(f"yacc{i}", [128, 1024]) for i in range(NTB)]
    w13b = [k.sb(f"w13b{i}", [128, 8, 256], BF16) for i in range(2)]
    w2s = [k.sb(f"w2s{i}", [128, 512]) for i in range(2)]
    w2b = [k.sb(f"w2b{i}", [128, 11, 512], BF16) for i in range(2)]
    qi = [0]
    sgt = [k.sb(f"sgt{i}", [128, 512], BF16) for i in range(2)]
    if NE > 1:
        gts = k.sb("gts", [128, NTB, NE])
    xs_ = [k.sb(f"xs{i}", [128, 1024]) for i in range(2)]
    tmp = [k.sb(f"tmp{i}", [128, 1024]) for i in range(2)]
    xo = [k.sb(f"xo{i}", [128, 1024]) for i in range(2)]
    st12 = [k.sb(f"st12{i}", [128, 12]) for i in range(2)]
    mv = [k.sb(f"mv{i}", [128, 2]) for i in range(2)]
    rstd = [k.sb(f"rstd{i}", [128, 1]) for i in range(2)]
    uTb = [k.sb(f"uTb{i}", [128, 8, 128], BF16) for i in range(2)]
    toks = []
    ph_i = 0
    dq = 0
    for blk in range(TC // T):
        t0 = blk * T
        op("sp", lambda e: e.dma_start(out=uT[:], in_=uT_d[:, t0:t0 + T].rearrange("(c p) t -> p c t", p=128)), writes=[uT], dma=True)
        if NE > 1:
            op("sp", lambda e: e.dma_start(out=gts[:], in_=gates_d[t0:t0 + T, :].rearrange("(n p) g -> p n g", p=128)), writes=[gts], dma=True)
        for ex in range(NE):
            for j in range(22):
                st = stg[j % 2]; wb = w13b[j % 2]
                if blk == 0:
                    op("sp", lambda e: e.dma_start(out=st[:, :, 0:128], in_=w13[ex, :, j * 128:(j + 1) * 128].rearrange("(c p) f -> p c f", p=128)), writes=[st], dma=True)
                    op("act", lambda e: e.dma_start(out=st[:, :, 128:256], in_=w13[ex, :, 2816 + j * 128:2816 + (j + 1) * 128].rearrange("(c p) f -> p c f", p=128)), writes=[st], dma=True)
                    op("pool", lambda e: e.tensor_copy(out=wb[:], in_=st[:]), reads=[st], writes=[wb])
                    if NBLK > 1:
                        op("pool", lambda e: e.dma_start(out=S13[ex][j][:, :], in_=wb[:].rearrange("p a b -> p (a b)")), reads=[wb], writes=[S13[ex][j]], dma=True)
                else:
                    op("sp" if j % 2 == 0 else "act", lambda e: e.dma_start(out=wb[:].rearrange("p a b -> p (a b)"), in_=S13[ex][j][:, :]), reads=[S13[ex][j]], writes=[wb], dma=True)
                for tb in range(T // 512):
                    pg = pH[ph_i % 4]; pu = pH[(ph_i + 1) % 4]; ph_i += 2
                    for kc in range(8):
                        op("pe", lambda e: e.matmul(pg[:, :], lhsT=wb[:, kc, 0:128], rhs=uT[:, kc, tb * 512:(tb + 1) * 512], start=(kc == 0), stop=(kc == 7)),
                           reads=[wb, uT], writes=[pg])
                    for kc in range(8):
                        op("pe", lambda e: e.matmul(pu[:, :], lhsT=wb[:, kc, 128:256], rhs=uT[:, kc, tb * 512:(tb + 1) * 512], start=(kc == 0), stop=(kc == 7)),
                           reads=[wb, uT], writes=[pu])
                    s_ = sgt[tb % 2]
                    op("act", lambda e: e.activation(out=s_[:], in_=pg[:, :], func=AF.Silu), reads=[pg], writes=[s_])
                    op("dve", lambda e: e.tensor_tensor(out=aT[:, j, tb * 512:(tb + 1) * 512], in0=s_[:], in1=pu[:, :], op=ALU.mult), reads=[s_, pu], writes=[aT])
            for half in range(2):
                for kh in range(2):
                    wb2 = w2b[qi[0] % 2]; qi[0] += 1
                    q4 = half * 2 + kh
                    if blk == 0:
                        for jj in range(11):
                            j = kh * 11 + jj
                            st = w2s[jj % 2]
                            op("sp" if jj % 2 == 0 else "act", lambda e: e.dma_start(out=st[:], in_=w2[ex, j * 128:(j + 1) * 128, half * 512:(half + 1) * 512]), writes=[st], dma=True)
                            op("pool", lambda e: e.tensor_copy(out=wb2[:, jj, :], in_=st[:]), reads=[st], writes=[wb2])
                        if NBLK > 1:
                            op("pool", lambda e: e.dma_start(out=S2[ex][q4][:, :], in_=wb2[:].rearrange("p a b -> p (a b)")), reads=[wb2], writes=[S2[ex][q4]], dma=True)
                    else:
                        op("sp" if q4 % 2 == 0 else "act", lambda e: e.dma_start(out=wb2[:].rearrange("p a b -> p (a b)"), in_=S2[ex][q4][:, :]), reads=[S2[ex][q4]], writes=[wb2], dma=True)
                    for tl in range(NTB):
                        p = pY[tl % 2]
                        for jj in range(11):
                            j = kh * 11 + jj
                            op("pe", lambda e: e.matmul(p[:, :], lhsT=aT[:, j, tl * 128:(tl + 1) * 128], rhs=wb2[:, jj, :], start=(jj == 0), stop=(jj == 10)),
                               reads=[aT, wb2], writes=[p])
                        ydst = yacc[tl][:, half * 512:(half + 1) * 512]
                        first = (ex == 0 and kh == 0)
                        if NE == 1:
                            if first:
                                op("act", lambda e: e.copy(out=ydst, in_=p[:, :]), reads=[p], writes=[yacc[tl]])
                            else:
                                op("dve", lambda e: e.tensor_tensor(out=ydst, in0=ydst, in1=p[:, :], op=ALU.add), reads=[p, yacc[tl]], writes=[yacc[tl]])
                        elif first:
                            op("dve", lambda e: e.tensor_scalar(out=ydst, in0=p[:, :], scalar1=gts[:, tl, ex:ex + 1], scalar2=None, op0=ALU.mult),
                               reads=[p, gts], writes=[yacc[tl]])
                        else:
                            op("dve", lambda e: e.scalar_tensor_tensor(out=ydst, in0=p[:, :], scalar=gts[:, tl, ex:ex + 1], in1=ydst, op0=ALU.mult, op1=ALU.add),
                               reads=[p, gts, yacc[tl]], writes=[yacc[tl]])
        for tl in range(NTB):
            i2 = tl % 2
            tsl = slice(t0 + tl * 128, t0 + (tl + 1) * 128)
            op("act", lambda e: e.dma_start(out=xs_[i2][:], in_=xmid[tsl, :]), writes=[xs_[i2]], dma=True)
            res_ln(k, c, yacc[tl][:], [yacc[tl]], xs_[i2], G, lng, lnb, tmp[i2], st12[i2], mv[i2], rstd[i2], xo[i2])
            toks.append(op("pool", lambda e: e.dma_start(out=xout[tsl, :], in_=xo[i2][:]), reads=[xo[i2]], dma=True))
            if emit_u:
                mod_transpose(k, c, xo[i2], modT, 0, pT, None, uTb[i2])
                toks.append(op("pool", lambda e: e.dma_start(out=uT_o[:, tsl].rearrange("(c p) t -> p c t", p=128), in_=uTb[i2][:]), reads=[uTb[i2]], dma=True))
    if fz is None or fz.final:
        k.finish(toks)
    if fz is not None:
        k.end_stage()
    print("ffn instructions", k.n_ins)
    return nc


def bc(v, n=128):
    return np.ascontiguousarray(np.tile(np.asarray(v, np.float32)[None, :], (n, 1)))


def fT(v):
    v = np.asarray(v, np.float32)
    return np.ascontiguousarray(v.reshape(-1, 128).T)


I32 = mybir.dt.int32


def build_s5(S, fz=None, blend=False):
    if fz is None:
        nc = bass.Bass("TRN2", target_bir_lowering=False); k = Ctx(nc); pref = ""
    else:
        nc, k, pref = fz.nc, fz.k, fz.pref
    D = mkD(nc, fz, pref)
    k.begin_stage(pref)
    TC = S // 2
    uT_d = D("uT", [2 * 1024, TC] if blend else [512, S], BF16)
    if blend:
        msk_d = D("msk", [128, 2])
    lamS = D("lamS", [128, 3, 16])
    lamB = D("lamB", [4, 128, 5, 64])
    cC = D("cC", [16, 128, 2, 16])
    dT = D("dT", [128, 4])
    out = D("gT", [512, S], BF16, kind="ExternalOutput")
    op = k.op
    NB = S // 512

    onesf = k.sb("onesf", [128, 128])
    op("pool", lambda e: e.memset(onesf[:], 1.0), writes=[onesf])
    ls = k.sb("ls", [128, 3, 16]); dsk = k.sb("dsk", [128, 4])
    op("sp", lambda e: e.dma_start(out=ls[:], in_=lamS), writes=[ls], dma=True)
    op("sp", lambda e: e.dma_start(out=dsk[:], in_=dT), writes=[dsk], dma=True)
    dtS = k.sb("dtS", [128, 16]); thS = k.sb("thS", [128, 16]); rS = k.sb("rS", [128, 16])
    op("act", lambda e: e.activation(out=dtS[:], in_=ls[:, 2, :], func=AF.Exp), reads=[ls], writes=[dtS])
    op("dve", lambda e: e.tensor_tensor(out=thS[:], in0=ls[:, 1, :], in1=dtS[:], op=ALU.mult), reads=[ls, dtS], writes=[thS])
    op("dve", lambda e: e.tensor_tensor(out=rS[:], in0=ls[:, 0, :], in1=dtS[:], op=ALU.mult), reads=[ls, dtS], writes=[rS])
    op("act", lambda e: e.activation(out=rS[:], in_=rS[:], func=AF.Exp), reads=[rS], writes=[rS])
    ioi = k.sb("ioi", [128, 128], I32); io = k.sb("io", [128, 128])
    op("pool", lambda e: e.iota(ioi[:], pattern=[[1, 128]], base=1, channel_multiplier=0), writes=[ioi])
    op("dve", lambda e: e.tensor_copy(out=io[:], in_=ioi[:]), reads=[ioi], writes=[io])
    ang = k.sb("ang", [128, 16 * 128]); st = k.sb("st", [128, 16 * 128]); ct = k.sb("ct", [128, 16 * 128])
    rtab = k.sb("rtab", [128, 16, 128])
    for q in range(16):
        op("dve", lambda e: e.tensor_scalar(out=ang[:, q * 128:(q + 1) * 128], in0=io[:], scalar1=thS[:, q:q + 1], scalar2=None, op0=ALU.mult),
           reads=[io, thS], writes=[ang])
        op("pool", lambda e: e.tensor_scalar(out=rtab[:, q, :], in0=onesf[:], scalar1=rS[:, q:q + 1], scalar2=None, op0=ALU.mult),
           reads=[onesf, rS], writes=[rtab])
    sincos(k, ang, 16 * 128, st, ct, "scS")
    rm = k.sb("rm", [128, 8])
    op("pool", lambda e: e.memset(rm[:], 1.0), writes=[rm])
    op("pool", lambda e: e.affine_select(out=rm[:], in_=rm[:], pattern=[[-16, 8]], compare_op=ALU.is_ge, fill=0.0, base=0, channel_multiplier=1),
       reads=[rm], writes=[rm])
    op("pool", lambda e: e.affine_select(out=rm[:], in_=rm[:], pattern=[[16, 8]], compare_op=ALU.is_ge, fill=0.0, base=15, channel_multiplier=-1),
       reads=[rm], writes=[rm])
    BtR = [k.sb(f"BtR{q}", [128, 2, 64], BF16) for q in range(16)]
    BtI = [k.sb(f"BtI{q}", [128, 2, 64], BF16) for q in range(16)]
    lb = k.sb("lb", [128, 5, 64])
    W = lambda n: k.sb(n, [128, 64])
    dtB, lrdt, lidt, mag, sB, cB, ar, ai, den, t1, t2, kr, ki, bbr, bbi = [W(n) for n in
        ("dtB", "lrdt", "lidt", "mag", "sB", "cB", "ar", "ai", "den", "t1", "t2", "kr", "ki", "bbr", "bbi")]
    TT = lambda o, a, b, o_: op("dve", lambda e: e.tensor_tensor(out=o[:], in0=a, in1=b, op=o_), reads=[lb, dtB, lrdt, lidt, mag, sB, cB, ar, ai, den, t1, t2, kr, ki], writes=[o])
    for cc in range(4):
        op("sp", lambda e: e.dma_start(out=lb[:], in_=lamB[cc]), writes=[lb], dma=True)
        op("act", lambda e: e.activation(out=dtB[:], in_=lb[:, 2, :], func=AF.Exp), reads=[lb], writes=[dtB])
        TT(lrdt, lb[:, 0, :], dtB[:], ALU.mult)
        TT(lidt, lb[:, 1, :], dtB[:], ALU.mult)
        op("act", lambda e: e.activation(out=mag[:], in_=lrdt[:], func=AF.Exp), reads=[lrdt], writes=[mag])
        sincos(k, lidt, 64, sB, cB, f"scB{cc}")
        TT(ar, mag[:], cB[:], ALU.mult)
        TT(ai, mag[:], sB[:], ALU.mult)
        op("dve", lambda e: e.tensor_scalar_add(out=ar[:], in0=ar[:], scalar1=-1.0), reads=[ar], writes=[ar])
        TT(den, lb[:, 0, :], lb[:, 0, :], ALU.mult)
        TT(t1, lb[:, 1, :], lb[:, 1, :], ALU.mult)
        TT(den, den[:], t1[:], ALU.add)
        op("dve", lambda e: e.reciprocal(out=den[:], in_=den[:]), reads=[den], writes=[den])
        TT(t1, ar[:], lb[:, 0, :], ALU.mult)
        TT(t2, ai[:], lb[:, 1, :], ALU.mult)
        TT(kr, t1[:], t2[:], ALU.add)
        TT(kr, kr[:], den[:], ALU.mult)
        TT(t1, ai[:], lb[:, 0, :], ALU.mult)
        TT(t2, ar[:], lb[:, 1, :], ALU.mult)
        TT(ki, t1[:], t2[:], ALU.subtract)
        TT(ki, ki[:], den[:], ALU.mult)
        TT(t1, kr[:], lb[:, 3, :], ALU.mult)
        TT(t2, ki[:], lb[:, 4, :], ALU.mult)
        op("dve", lambda e: e.tensor_tensor(out=bbr[:], in0=t1[:], in1=t2[:], op=ALU.subtract), reads=[t1, t2], writes=[bbr])
        TT(t1, kr[:], lb[:, 4, :], ALU.mult)
        TT(t2, ki[:], lb[:, 3, :], ALU.mult)
        op("dve", lambda e: e.tensor_tensor(out=bbi[:], in0=t1[:], in1=t2[:], op=ALU.add), reads=[t1, t2], writes=[bbi])
        for ql in range(4):
            q = cc * 4 + ql
            for g2 in range(2):
                op("dve", lambda e: e.tensor_scalar(out=BtR[q][:, g2, :], in0=bbr[:], scalar1=rm[:, 2 * ql + g2:2 * ql + g2 + 1], scalar2=None, op0=ALU.mult),
                   reads=[bbr, rm], writes=[BtR[q]])
                op("dve", lambda e: e.tensor_scalar(out=BtI[q][:, g2, :], in0=bbi[:], scalar1=rm[:, 2 * ql + g2:2 * ql + g2 + 1], scalar2=None, op0=ALU.mult),
                   reads=[bbi, rm], writes=[BtI[q]])
    cst = k.sb("cst", [128, 16, 2, 16])
    op("sp", lambda e: e.dma_start(out=cst[:], in_=cC.rearrange("q s r h -> s q r h")), writes=[cst], dma=True)
    CtR = [k.sb(f"CtR{q}", [128, 128], BF16) for q in range(16)]
    CtI = [k.sb(f"CtI{q}", [128, 128], BF16) for q in range(16)]
    for q in range(16):
        ql = q % 4
        op("pool", lambda e: e.memset(CtR[q][:], 0.0), writes=[CtR[q]])
        op("pool", lambda e: e.memset(CtI[q][:], 0.0), writes=[CtI[q]])
        for g2 in range(2):
            ps_ = slice(64 * g2, 64 * g2 + 64)
            cs_ = slice((2 * ql + g2) * 16, (2 * ql + g2) * 16 + 16)
            op("dve", lambda e: e.tensor_copy(out=CtR[q][ps_, cs_], in_=cst[ps_, q, 0, :]), reads=[cst], writes=[CtR[q]])
            op("dve", lambda e: e.tensor_scalar(out=CtI[q][ps_, cs_], in0=cst[ps_, q, 1, :], scalar1=-1.0, scalar2=None, op0=ALU.mult), reads=[cst], writes=[CtI[q]])
    uT = k.sb("uT_s", [128, 4, S], BF16)
    if not blend:
        for cc in range(4):
            op("sp" if cc % 2 == 0 else "act", lambda e: e.dma_start(out=uT[:, cc, :], in_=uT_d[cc * 128:(cc + 1) * 128, :]), writes=[uT], dma=True)
    else:
        msk = k.sb("msk_s", [128, 2])
        op("sp", lambda e: e.dma_start(out=msk[:], in_=msk_d), writes=[msk], dma=True)
        CW = TC // 4
        cand = [[k.sb(f"cand{i}_{j}", [128, CW], BF16) for j in range(2)] for i in range(2)]
        n_ = 0
        for i in range(2):
            for cc in range(4):
                for qq in range(4):
                    cd = cand[n_ % 2]; n_ += 1
                    for j in range(2):
                        r0 = i * 1024 + j * 512 + cc * 128
                        op("sp" if j == 0 else "act", lambda e: e.dma_start(out=cd[j][:], in_=uT_d[r0:r0 + 128, qq * CW:(qq + 1) * CW]), writes=[cd[j]], dma=True)
                    op("pool", lambda e: e.tensor_scalar(out=cd[0][:], in0=cd[0][:], scalar1=msk[:, 0:1], scalar2=None, op0=ALU.mult), reads=[cd[0], msk], writes=[cd[0]])
                    op("dve", lambda e: e.scalar_tensor_tensor(out=uT[:, cc, i * TC + qq * CW:i * TC + (qq + 1) * CW], in0=cd[1][:], scalar=msk[:, 1:2], in1=cd[0][:], op0=ALU.mult, op1=ALU.add),
                       reads=[cd[0], cd[1], msk], writes=[uT])
    pBr = [k.ps(f"pBr{i}", [128, 512]) for i in range(2)]
    pBi = [k.ps(f"pBi{i}", [128, 512]) for i in range(2)]
    pY = [k.ps(f"pY{i}", [128, 512]) for i in range(2)]
    bur = [k.sb(f"bur{i}", [128, 512]) for i in range(2)]; bui = [k.sb(f"bui{i}", [128, 512]) for i in range(2)]
    bpr = [k.sb(f"bpr{i}", [128, 512]) for i in range(2)]; bpi = [k.sb(f"bpi{i}", [128, 512]) for i in range(2)]
    vr = [k.sb(f"vr{i}", [128, 512]) for i in range(2)]; vi = [k.sb(f"vi{i}", [128, 512]) for i in range(2)]
    pt1 = k.sb("pt1", [128, 512]); pt2 = k.sb("pt2", [128, 512])
    dt1 = k.sb("dt1", [128, 512]); dt2 = k.sb("dt2", [128, 512])
    xr = [k.sb(f"xr{i}", [128, 512], BF16) for i in range(4)]; xi = [k.sb(f"xi{i}", [128, 512], BF16) for i in range(4)]
    car = [k.sb(f"car{q}", [128, 2]) for q in range(16)]
    cat = [k.sb(f"cat{i}", [128, 2]) for i in range(2)]
    for q in range(16):
        op("pool", lambda e: e.memset(car[q][:], 0.0), writes=[car[q]])
    yt = [k.sb(f"yt{i}", [128, 512]) for i in range(2)]; y2 = [k.sb(f"y2{i}", [128, 512]) for i in range(2)]
    go = [k.sb(f"go{i}", [128, 512], BF16) for i in range(2)]
    toks = []
    it = 0
    for blk in range(NB):
        tsl = slice(blk * 512, (blk + 1) * 512)
        for cc in range(4):
            for ql in range(4):
                q = cc * 4 + ql
                i2 = it % 2; it += 1
                c3 = lambda tab: tab[:, q * 128:(q + 1) * 128][:, None, :].to_broadcast([128, 4, 128]) if False else None
                op("pe", lambda e: e.matmul(pBr[i2][:, :], lhsT=BtR[q][:].rearrange("p a b -> p (a b)"), rhs=uT[:, cc, tsl], start=True, stop=True), reads=[BtR[q], uT], writes=[pBr[i2]])
                op("pe", lambda e: e.matmul(pBi[i2][:, :], lhsT=BtI[q][:].rearrange("p a b -> p (a b)"), rhs=uT[:, cc, tsl], start=True, stop=True), reads=[BtI[q], uT], writes=[pBi[i2]])
                op("act", lambda e: e.copy(out=bur[i2][:], in_=pBr[i2][:, :]), reads=[pBr[i2]], writes=[bur[i2]])
                op("act", lambda e: e.copy(out=bui[i2][:], in_=pBi[i2][:, :]), reads=[pBi[i2]], writes=[bui[i2]])
                ctq = ct[:, q * 128:(q + 1) * 128]; stq = st[:, q * 128:(q + 1) * 128]
                for c4 in range(4):
                    cs = slice(c4 * 128, (c4 + 1) * 128)
                    op("pool", lambda e: e.tensor_tensor(out=pt1[:, cs], in0=bur[i2][:, cs], in1=ctq, op=ALU.mult), reads=[bur[i2], ct], writes=[pt1])
                    op("pool", lambda e: e.tensor_tensor(out=pt2[:, cs], in0=bui[i2][:, cs], in1=stq, op=ALU.mult), reads=[bui[i2], st], writes=[pt2])
                op("pool", lambda e: e.tensor_tensor(out=bpr[i2][:], in0=pt1[:], in1=pt2[:], op=ALU.add), reads=[pt1, pt2], writes=[bpr[i2]])
                for c4 in range(4):
                    cs = slice(c4 * 128, (c4 + 1) * 128)
                    op("pool", lambda e: e.tensor_tensor(out=pt1[:, cs], in0=bui[i2][:, cs], in1=ctq, op=ALU.mult), reads=[bui[i2], ct], writes=[pt1])
                    op("pool", lambda e: e.tensor_tensor(out=pt2[:, cs], in0=bur[i2][:, cs], in1=stq, op=ALU.mult), reads=[bur[i2], st], writes=[pt2])
                op("pool", lambda e: e.tensor_tensor(out=bpi[i2][:], in0=pt1[:], in1=pt2[:], op=ALU.subtract), reads=[pt1, pt2], writes=[bpi[i2]])
                for c4 in range(4):
                    cs = slice(c4 * 128, (c4 + 1) * 128)
                    op("dve", lambda e: e.tensor_tensor_scan(out=vr[i2][:, cs], data0=rtab[:, q, :], data1=bpr[i2][:, cs], initial=car[q][:, 0:1], op0=ALU.mult, op1=ALU.add),
                       reads=[rtab, bpr[i2], car[q]], writes=[vr[i2]])
                    op("dve", lambda e: e.tensor_tensor_scan(out=vi[i2][:, cs], data0=rtab[:, q, :], data1=bpi[i2][:, cs], initial=car[q][:, 1:2], op0=ALU.mult, op1=ALU.add),
                       reads=[rtab, bpi[i2], car[q]], writes=[vi[i2]])
                    l = c4 * 128 + 127
                    c128 = ct[:, q * 128 + 127:q * 128 + 128]; s128 = st[:, q * 128 + 127:q * 128 + 128]
                    ca = cat[c4 % 2]
                    op("dve", lambda e: e.tensor_scalar(out=ca[:, 0:1], in0=vi[i2][:, l:l + 1], scalar1=s128, scalar2=None, op0=ALU.mult), reads=[vi[i2], st], writes=[ca])
                    op("dve", lambda e: e.tensor_scalar(out=ca[:, 1:2], in0=vi[i2][:, l:l + 1], scalar1=c128, scalar2=None, op0=ALU.mult), reads=[vi[i2], ct], writes=[ca])
                    op("dve", lambda e: e.scalar_tensor_tensor(out=car[q][:, 0:1], in0=vr[i2][:, l:l + 1], scalar=c128, in1=ca[:, 0:1], op0=ALU.mult, op1=ALU.subtract),
                       reads=[vr[i2], ct, ca], writes=[car[q]])
                    op("dve", lambda e: e.scalar_tensor_tensor(out=car[q][:, 1:2], in0=vr[i2][:, l:l + 1], scalar=s128, in1=ca[:, 1:2], op0=ALU.mult, op1=ALU.add),
                       reads=[vr[i2], st, ca], writes=[car[q]])
                for c4 in range(4):
                    cs = slice(c4 * 128, (c4 + 1) * 128)
                    op("dve", lambda e: e.tensor_tensor(out=dt1[:, cs], in0=vr[i2][:, cs], in1=ctq, op=ALU.mult), reads=[vr[i2], ct], writes=[dt1])
                    op("dve", lambda e: e.tensor_tensor(out=dt2[:, cs], in0=vi[i2][:, cs], in1=stq, op=ALU.mult), reads=[vi[i2], st], writes=[dt2])
                op("dve", lambda e: e.tensor_tensor(out=xr[ql][:], in0=dt1[:], in1=dt2[:], op=ALU.subtract), reads=[dt1, dt2], writes=[xr[ql]])
                for c4 in range(4):
                    cs = slice(c4 * 128, (c4 + 1) * 128)
                    op("dve", lambda e: e.tensor_tensor(out=dt1[:, cs], in0=vr[i2][:, cs], in1=stq, op=ALU.mult), reads=[vr[i2], st], writes=[dt1])
                    op("dve", lambda e: e.tensor_tensor(out=dt2[:, cs], in0=vi[i2][:, cs], in1=ctq, op=ALU.mult), reads=[vi[i2], ct], writes=[dt2])
                op("dve", lambda e: e.tensor_tensor(out=xi[ql][:], in0=dt1[:], in1=dt2[:], op=ALU.add), reads=[dt1, dt2], writes=[xi[ql]])
            j2 = (blk * 4 + cc) % 2
            p = pY[j2]
            for ql in range(4):
                q = cc * 4 + ql
                op("pe", lambda e: e.matmul(p[:, :], lhsT=CtR[q][:], rhs=xr[ql][:], start=(ql == 0), stop=False), reads=[CtR[q], xr[ql]], writes=[p])
                op("pe", lambda e: e.matmul(p[:, :], lhsT=CtI[q][:], rhs=xi[ql][:], start=False, stop=(ql == 3)), reads=[CtI[q], xi[ql]], writes=[p])
            y = yt[j2]; z = y2[j2]
            op("dve", lambda e: e.scalar_tensor_tensor(out=y[:], in0=uT[:, cc, tsl], scalar=dsk[:, cc:cc + 1], in1=p[:, :], op0=ALU.mult, op1=ALU.add),
               reads=[uT, dsk, p], writes=[y])
            op("pool", lambda e: e.tensor_tensor(out=z[:], in0=y[:], in1=y[:], op=ALU.mult), reads=[y], writes=[z])
            op("pool", lambda e: e.tensor_scalar(out=z[:], in0=z[:], scalar1=0.044715, scalar2=1.0, op0=ALU.mult, op1=ALU.add), reads=[z], writes=[z])
            op("pool", lambda e: e.tensor_tensor(out=z[:], in0=z[:], in1=y[:], op=ALU.mult), reads=[z, y], writes=[z])
            op("act", lambda e: e.activation(out=z[:], in_=z[:], func=AF.Sigmoid, scale=2.0 * math.sqrt(2.0 / math.pi)), reads=[z], writes=[z])
            op("pool", lambda e: e.tensor_tensor(out=go[j2][:], in0=z[:], in1=y[:], op=ALU.mult), reads=[z, y], writes=[go[j2]])
            toks.append(op("sp", lambda e: e.dma_start(out=out[cc * 128:(cc + 1) * 128, tsl], in_=go[j2][:]), reads=[go[j2]], dma=True))
    if fz is None:
        k.finish(toks)
    else:
        k.end_stage()
    print("s5 instructions", k.n_ins)
    return nc


def s5_inputs(inp, uT_full_b, half, S):
    f = np.float32
    g0 = half * 32
    lr = inp["s5_lambda_re"][0][g0:g0 + 32]; li = inp["s5_lambda_im"][0][g0:g0 + 32]; ld = inp["s5_log_dt"][0][g0:g0 + 32]
    ldx = np.repeat(ld[:, None], 64, axis=1)
    toS = lambda a: a.reshape(16, 2, 64).transpose(1, 2, 0).reshape(128, 16)
    lamS = np.stack([toS(lr), toS(li), toS(ldx)], axis=1)
    br = inp["s5_b_re"][0][g0:g0 + 32]; bi = inp["s5_b_im"][0][g0:g0 + 32]
    rep = lambda a: np.repeat(a[:, None, :], 16, axis=1)
    toB = lambda a: a.reshape(4, 8 * 16, 64)
    lamB = np.stack([toB(rep(lr)), toB(rep(li)), toB(rep(ldx)), toB(br.transpose(0, 2, 1)), toB(bi.transpose(0, 2, 1))], axis=2)
    cr = inp["s5_c_re"][0][g0:g0 + 32]; ci = inp["s5_c_im"][0][g0:g0 + 32]
    toC = lambda a: a.reshape(16, 2, 16, 64).transpose(0, 1, 3, 2).reshape(16, 128, 16)
    cC = np.stack([toC(cr), toC(ci)], axis=2)
    dT = inp["s5_d"][0][half * 512:(half + 1) * 512].reshape(4, 128).T
    return {"uT": np.ascontiguousarray(uT_full_b[half * 512:(half + 1) * 512, :S]),
            "lamS": np.ascontiguousarray(lamS.astype(f)), "lamB": np.ascontiguousarray(lamB.astype(f)),
            "cC": np.ascontiguousarray(cC.astype(f)), "dT": np.ascontiguousarray(dT.astype(f))}


import ml_dtypes as _mld
import os

_CACHE = {}


def build_fused(S, NB_):
    TC = S // 2
    nc = bass.Bass("TRN2", target_bir_lowering=False)
    k = Ctx(nc)
    fz = Fz(nc, k)
    fz.final = False
    I = lambda n, s, dt: nc.dram_tensor(n, list(s), dt).ap()

    def chunked(name, R_, Cn, W):
        W = min(W, Cn)
        src = [I(f"{name}_s{j}", [R_, W], BF16) for j in range(Cn // W)]
        dst = [I(f"{name}_a{j}", [2 * R_, W], BF16) for j in range(Cn // W)]
        return src, dst, ChunkedAP(src, W), ChunkedAP(dst, W)
    hg_s, hg_d, hg_src, hg_all = chunked("i_hg", 1024, S, 1024)
    u1_s, u1_d, u1_src, u1_all = chunked("i_u1", 1024, TC, 1024)
    g_s, g_d, g_src, g_all = chunked("i_g", 512, S, 2048)
    xmid1 = I("i_xmid1", [TC, 1024], F32); uT1 = I("i_uT1", [1024, TC], BF16)
    x1 = I("i_x1", [TC, 1024], F32)
    xmid2 = I("i_xmid2", [TC, 1024], F32); uT2 = I("i_uT2", [1024, TC], BF16); gates = I("i_gates", [TC, 8], F32)
    groups = [[2 * i, 2 * i + 1] for i in range(NB_)]
    fz.pref = "a_"; fz.ext = {"hgT": hg_src}
    build_m0(S, fz=fz)
    k.collective("AllGather", hg_s, hg_d, groups)
    fz.pref = "b_"; fz.ext = {"inT": hg_all, "xmid": xmid1, "uT": uT1}
    build_proj(TC, 16, False, False, fz=fz, blend=True)
    fz.pref = "c_"; fz.ext = {"uT": uT1, "xmid": xmid1, "xout": x1, "uTn": u1_src}
    build_ffn(TC, 1, min(512, TC), True, fz=fz)
    k.collective("AllGather", u1_s, u1_d, groups)
    fz.pref = "d_"; fz.ext = {"uT": u1_all, "gT": g_src}
    build_s5(S, fz=fz, blend=True)
    k.collective("AllGather", g_s, g_d, groups)
    fz.pref = "e_"; fz.ext = {"inT": g_all, "x": x1, "xmid": xmid2, "uT": uT2, "gates": gates}
    build_proj(TC, 8, True, True, fz=fz, blend=True)
    fz.pref = "f_"; fz.ext = {"uT": uT2, "xmid": xmid2, "gates": gates}; fz.final = True
    build_ffn(TC, 8, min(int(os.environ.get('T2', 1024)), TC), False, fz=fz)
    print("fused instructions", k.n_ins)
    return nc


def kernel_impl(inp, S, NB_=4):
    inp = {k_: np.asarray(v) for k_, v in inp.items()}
    NCO = 2 * NB_
    TC = S // 2
    f32 = np.float32
    cT = lambda b: np.ascontiguousarray(inp["c"][b].reshape(8, 128).T.astype(f32))
    aw = inp["ada_w"]; ab = inp["ada_b"]
    C = lambda a: np.ascontiguousarray(a)
    key = ("fused", S, NB_)
    if key not in _CACHE:
        _CACHE[key] = build_fused(S, NB_)
    nc = _CACHE[key]
    shared = {}
    shared["b_W"] = C(inp["mlstm_w_out"][0])
    shared["b_adaw_f"] = C(aw[0][:, 3072:5120]); shared["b_adab_f"] = fT(ab[0][3072:5120])
    shared["b_adaw_b"] = C(aw[0][:, 2048:3072]); shared["b_adab_b"] = bc(ab[0][2048:3072])
    shared["b_lng"] = bc(inp["ln_mix_g"][0]); shared["b_lnb"] = bc(inp["ln_mix_b"][0])
    shared["c_w13"] = C(inp["ffn_w13"]); shared["c_w2"] = C(inp["ffn_w2"])
    shared["c_adaw_b"] = C(aw[0][:, 5120:6144]); shared["c_adab_b"] = bc(ab[0][5120:6144])
    shared["c_lng"] = bc(inp["ln_ffn_g"][0]); shared["c_lnb"] = bc(inp["ln_ffn_b"][0])
    shared["c_adaw_f"] = C(aw[1][:, 0:2048]); shared["c_adab_f"] = fT(ab[1][0:2048])
    shared["e_W"] = C(inp["s5_w_glu"][0])
    shared["e_adaw_f"] = C(aw[1][:, 3072:5120]); shared["e_adab_f"] = fT(ab[1][3072:5120])
    shared["e_adaw_b"] = C(aw[1][:, 2048:3072]); shared["e_adab_b"] = bc(ab[1][2048:3072])
    shared["e_lng"] = bc(inp["ln_mix_g"][1]); shared["e_lnb"] = bc(inp["ln_mix_b"][1])
    shared["e_bglu"] = bc(inp["s5_b_glu"][0])
    shared["e_wr"] = C(inp["moe_router"][0].reshape(8, 128, 8).transpose(1, 0, 2).astype(f32))
    shared["f_w13"] = C(inp["moe_w13"][0]); shared["f_w2"] = C(inp["moe_w2"][0])
    shared["f_adaw_b"] = C(aw[1][:, 5120:6144]); shared["f_adab_b"] = bc(ab[1][5120:6144])
    shared["f_lng"] = bc(inp["ln_ffn_g"][1]); shared["f_lnb"] = bc(inp["ln_ffn_b"][1])
    dummy_u = np.zeros((1024, S), _mld.bfloat16)
    maps = []
    for c in range(NCO):
        b, h = c // 2, c % 2
        tk = slice(h * TC, (h + 1) * TC)
        m = dict(shared)
        for k_, v in m0_inputs(inp, b, h, S).items():
            m["a_" + k_] = v
        msk = np.zeros((128, 2), f32); msk[:, h] = 1.0
        m["b_msk"] = msk; m["d_msk"] = msk; m["e_msk"] = msk
        m["b_x"] = C(inp["x"][b, :S][tk])
        for p_ in "bcef":
            m[p_ + "_condT"] = cT(b)
        for k_, v in s5_inputs(inp, dummy_u, h, S).items():
            if k_ != "uT":
                m["d_" + k_] = v
        maps.append(m)
    res = run_bass_kernel_spmd(nc, maps, core_ids=list(range(NCO)))
    out = np.zeros((NB_, S, 1024), f32)
    for c in range(NCO):
        b, h = c // 2, c % 2
        out[b, h * TC:(h + 1) * TC] = np.asarray(res.results[c]["f_xout"])
    return out


def kernel(**inputs):
    return kernel_impl(inputs, 8192, 4)
```

```python
import math
import numpy as np
from contextlib import ExitStack
import concourse.bass as bass
import concourse.mybir as mybir
from concourse.bass_utils import run_bass_kernel_spmd

F32 = mybir.dt.float32
BF16 = mybir.dt.bfloat16
AF = mybir.ActivationFunctionType
ALU = mybir.AluOpType
AX = mybir.AxisListType

NPOOL = 12
NOSYNC_SAME = ("pe",)


class Buf:
    __slots__ = ("ap", "last_w", "reads")

    def __init__(self, ap):
        self.ap = ap
        self.last_w = None
        self.reads = {}

    def __getitem__(self, idx):
        return self.ap[idx]


class Ctx:
    def __init__(self, nc):
        self.nc = nc
        self.engs = {"pe": nc.tensor, "act": nc.scalar, "dve": nc.vector,
                     "pool": nc.gpsimd, "sp": nc.sync}
        self.sem = {}
        self.cnt = {}
        for n in ("pe", "act", "dve", "pool"):
            self.sem[n] = nc.alloc_semaphore("s_" + n)
            self.cnt[n] = 0
        self.dma_pool = {}
        self.dma_i = {}
        self.known = {n: {} for n in ("pe", "act", "dve", "pool", "sp")}
        self.out_tokens = []
        self.n_ins = 0
        self.stack = None
        self.pref = ""
        self.dma_last = {}
        self.cc_toks = []

    def begin_stage(self, pref=""):
        self.stack = ExitStack()
        self.pref = pref

    def barrier(self):
        toks = [(n, self.sem[n], self.cnt[n]) for n in ("pe", "act", "dve", "pool") if self.cnt[n] > 0]
        for qn, d in self.dma_last.items():
            for i, v in d.items():
                toks.append(("dma_" + qn, self.dma_pool[qn][i], v))
        toks.extend(self.cc_toks)
        for en in ("pe", "act", "dve", "pool", "sp"):
            for tok in toks:
                self._wait(en, tok)

    def end_stage(self):
        self.barrier()
        self.stack.close()
        self.stack = None

    def collective(self, kind, srcs, dsts, groups):
        self.barrier()
        for src_ap, dst_ap in zip(srcs, dsts):
            sem = self.nc.alloc_semaphore("cc_%d" % len(self.cc_toks))
            ins = self.nc.gpsimd.collective_compute(kind, ALU.bypass, replica_groups=groups, ins=[src_ap.opt()], outs=[dst_ap.opt()])
            ins.then_inc(sem)
            self.cc_toks.append(("cc", sem, 1))
        self.barrier()

    def sb(self, name, shape, dtype=F32):
        if self.stack is not None:
            t = self.stack.enter_context(self.nc.sbuf_tensor(self.pref + name, list(shape), dtype))
        else:
            t = self.nc.alloc_sbuf_tensor(name, list(shape), dtype)
        return Buf(t.ap() if hasattr(t, "ap") else t)

    def ps(self, name, shape, dtype=F32):
        if self.stack is not None:
            t = self.stack.enter_context(self.nc.psum_tensor(self.pref + name, list(shape), dtype))
        else:
            t = self.nc.alloc_psum_tensor(name, list(shape), dtype)
        return Buf(t.ap() if hasattr(t, "ap") else t)

    def _wait(self, en, tok):
        src, sem, val = tok
        k = self.known[en]
        if k.get(sem.num, 0) >= val:
            return
        self.engs[en].wait_ge(sem, val)
        k[sem.num] = val

    def op(self, en, fn, reads=(), writes=(), dma=False, q=None):
        deps = []
        for b in reads:
            if b.last_w is not None:
                deps.append(b.last_w)
        for b in writes:
            if b.last_w is not None:
                deps.append(b.last_w)
            deps.extend(b.reads.values())
        for tok in deps:
            src = tok[0]
            if src == en and en in NOSYNC_SAME and not dma:
                continue
            self._wait(en, tok)
        e = self.engs[en]
        if dma:
            qn = en
            if qn not in self.dma_pool:
                self.dma_pool[qn] = [self.nc.alloc_semaphore(f"d_{qn}_{i}") for i in range(NPOOL)]
                self.dma_i[qn] = 0
            i = self.dma_i[qn]
            self.dma_i[qn] = i + 1
            sem = self.dma_pool[qn][i % NPOOL]
            rnd = i // NPOOL
            if rnd > 0:
                self._wait(en, ("dma_" + qn, sem, 16 * rnd))
            ins = fn(e)
            ins.then_inc(sem, 16)
            tok = ("dma_" + qn, sem, 16 * (rnd + 1))
            self.dma_last.setdefault(qn, {})[i % NPOOL] = 16 * (rnd + 1)
        else:
            ins = fn(e)
            self.cnt[en] += 1
            ins.then_inc(self.sem[en], 1)
            tok = (en, self.sem[en], self.cnt[en])
        self.n_ins += 1
        for b in reads:
            b.reads[tok[1].num] = tok
        for b in writes:
            b.last_w = tok
            b.reads = {}
        return tok

    def finish(self, toks):
        for tok in toks:
            self._wait("sp", tok)


class Fz:
    def __init__(self, nc, k):
        self.nc = nc; self.k = k; self.ext = {}; self.pref = ""


def mkD(nc, fz, pref):
    def D(n, s, dt=F32, kind="ExternalInput"):
        if fz is not None and n in fz.ext:
            return fz.ext[n]
        return nc.dram_tensor(pref + n, list(s), dt, kind=kind).ap()
    return D


class ChunkedAP:
    def __init__(self, aps, W):
        self.aps = aps; self.W = W

    def __getitem__(self, idx):
        rs, cs = idx
        j = cs.start // self.W
        assert (cs.stop - 1) // self.W == j
        return self.aps[j][rs, cs.start - j * self.W: cs.stop - j * self.W]

I32 = mybir.dt.int32
def sincos(k, a, N, outs, outc, pref="sc"):
    op = k.op
    t = k.sb(pref + "_t", [128, N]); ti = k.sb(pref + "_i", [128, N], I32); m = k.sb(pref + "_m", [128, N])
    for dst, sh in ((outs, 0.0), (outc, 0.5 * math.pi)):
        op("dve", lambda e: e.tensor_scalar(out=t[:], in0=a[:], scalar1=sh, scalar2=1.0 / (2 * math.pi), op0=ALU.add, op1=ALU.mult), reads=[a], writes=[t])
        op("dve", lambda e: e.tensor_copy(out=ti[:], in_=t[:]), reads=[t], writes=[ti])
        op("dve", lambda e: e.tensor_copy(out=m[:], in_=ti[:]), reads=[ti], writes=[m])
        op("dve", lambda e: e.tensor_scalar(out=t[:], in0=a[:], scalar1=sh, scalar2=None, op0=ALU.add), reads=[a], writes=[t])
        op("dve", lambda e: e.scalar_tensor_tensor(out=t[:], in0=m[:], scalar=-2 * math.pi, in1=t[:], op0=ALU.mult, op1=ALU.add), reads=[m, t], writes=[t])
        op("dve", lambda e: e.tensor_scalar(out=m[:], in0=t[:], scalar1=math.pi, scalar2=-2 * math.pi, op0=ALU.is_gt, op1=ALU.mult), reads=[t], writes=[m])
        op("dve", lambda e: e.tensor_tensor(out=t[:], in0=t[:], in1=m[:], op=ALU.add), reads=[t, m], writes=[t])
        op("dve", lambda e: e.tensor_scalar(out=m[:], in0=t[:], scalar1=-math.pi, scalar2=2 * math.pi, op0=ALU.is_lt, op1=ALU.mult), reads=[t], writes=[m])
        op("dve", lambda e: e.tensor_tensor(out=t[:], in0=t[:], in1=m[:], op=ALU.add), reads=[t, m], writes=[t])
        op("dve", lambda e: e.tensor_scalar(out=t[:], in0=t[:], scalar1=math.pi, scalar2=-math.pi, op0=ALU.min, op1=ALU.max), reads=[t], writes=[t])
        op("act", lambda e: e.activation(out=dst[:], in_=t[:], func=AF.Sin), reads=[t], writes=[dst])


DH = 512
TB = 256
NT = TB // 128


def build_m0(S, stage=99, HS=(0, 1), TGT=(0, 0), fz=None):
    if fz is None:
        nc = bass.Bass("TRN2", target_bir_lowering=False); k = Ctx(nc); pref = ""
    else:
        nc, k, pref = fz.nc, fz.k, fz.pref
    D = mkD(nc, fz, pref)
    k.begin_stage(pref)
    x = D("x", [S, 1024])
    condT_d = D("condT", [128, 8])
    adaw = D("adaw", [1024, 2048])
    adabT = D("adabT", [128, 16])
    w_in = D("w_in", [1024, 3072])
    convw = D("convw", [128, 16, 4])
    convb = D("convb", [128, 16])
    wqc = D("wqc", [128, 16, 4]); wkc = D("wkc", [128, 16, 4]); wvc = D("wvc", [128, 16, 4])
    wqt = D("wqt", [128, 16, 4]); wkt = D("wkt", [128, 16, 4]); wvt = D("wvt", [128, 16, 4])
    wgq = D("wgq", [128, 16, 4]); wgk = D("wgk", [128, 16, 4]); wgv = D("wgv", [128, 16, 4])
    bg = D("bg", [128, 4])
    nwT = D("nwT", [128, 8]); skT = D("skT", [128, 8])
    out = D("hgT", [1024, S], BF16, kind="ExternalOutput")

    op = k.op
    NB = S // TB

    identf = k.sb("identf", [128, 128]); ident = k.sb("ident", [128, 128], BF16)
    ntri = k.sb("ntri", [128, 128]); negm = k.sb("negm", [128, 128])
    bmask = k.sb("bmask", [128, 32, 4])
    onesb = k.sb("onesb", [128, 2], BF16); onesf = k.sb("onesf", [128, 128])
    op("pool", lambda e: e.memset(identf[:], 0.0), writes=[identf])
    op("pool", lambda e: e.affine_select(out=identf[:], in_=identf[:], pattern=[[-1, 128]], compare_op=ALU.not_equal,
                                         fill=1.0, base=0, channel_multiplier=1), reads=[identf], writes=[identf])
    op("dve", lambda e: e.tensor_copy(out=ident[:], in_=identf[:]), reads=[identf], writes=[ident])
    op("pool", lambda e: e.memset(ntri[:], -1.0), writes=[ntri])
    op("pool", lambda e: e.affine_select(out=ntri[:], in_=ntri[:], pattern=[[1, 128]], compare_op=ALU.is_ge,
                                         fill=0.0, base=0, channel_multiplier=-1), reads=[ntri], writes=[ntri])
    op("pool", lambda e: e.memset(negm[:], 0.0), writes=[negm])
    op("pool", lambda e: e.affine_select(out=negm[:], in_=negm[:], pattern=[[1, 128]], compare_op=ALU.is_ge,
                                         fill=-30000.0, base=0, channel_multiplier=-1), reads=[negm], writes=[negm])
    op("pool", lambda e: e.memset(bmask[:], 1.0), writes=[bmask])
    op("pool", lambda e: e.affine_select(out=bmask[:], in_=bmask[:], pattern=[[-4, 32], [0, 4]], compare_op=ALU.is_ge,
                                         fill=0.0, base=0, channel_multiplier=1), reads=[bmask], writes=[bmask])
    op("pool", lambda e: e.affine_select(out=bmask[:], in_=bmask[:], pattern=[[4, 32], [0, 4]], compare_op=ALU.is_ge,
                                         fill=0.0, base=3, channel_multiplier=-1), reads=[bmask], writes=[bmask])
    op("pool", lambda e: e.memset(onesb[:], 1.0), writes=[onesb])
    op("pool", lambda e: e.memset(onesf[:], 1.0), writes=[onesf])

    def load(name, src, shape, dt=F32, q="sp"):
        b = k.sb(name, shape, dt)
        op(q, lambda e: e.dma_start(out=b[:], in_=src), writes=[b], dma=True)
        return b
    condT = load("condT_s", condT_d, [128, 8]); adab = load("adab_s", adabT, [128, 16])
    cw = load("cw", convw, [128, 16, 4]); cb = load("cb", convb, [128, 16])
    wc = [load("wqc_s", wqc, [128, 16, 4]), load("wkc_s", wkc, [128, 16, 4]), load("wvc_s", wvc, [128, 16, 4])]
    wt = [load("wqt_s", wqt, [128, 16, 4]), load("wkt_s", wkt, [128, 16, 4]), load("wvt_s", wvt, [128, 16, 4])]
    wg = [load("wgq_s", wgq, [128, 16, 4]), load("wgk_s", wgk, [128, 16, 4]), load("wgv_s", wgv, [128, 16, 4])]
    bgs = load("bgs", bg, [128, 4]); nw = load("nw", nwT, [128, 8]); sk = load("sk", skT, [128, 8])

    pA = k.ps("pA", [128, 512]); pB = k.ps("pB", [128, 512])
    import os
    if os.environ.get("M0V", "0") == "1":
        hbA = k.ps("hbA", [128, 512]); hbB = k.ps("hbB", [128, 512])
        pbh = [hbA, hbA]; pSh = [hbB, hbB]
    else:
        hb = [k.ps("hb0", [128, 512]), k.ps("hb1", [128, 512])]
        pbh = [hb[0], hb[1]]
        pSh = [hb[0], hb[1]]
    pnh = [k.ps("pn0", [128, 512]), k.ps("pn1", [128, 512])]
    pS = pnh[0]
    pt = k.ps("pt", [128, 1024], BF16)
    ptF = k.ps("ptF", [128, 1024], BF16)
    ptFh = [ptF, ptF]
    pacc = [pA, pB]
    pacc_i = [0]

    def nextp():
        pacc_i[0] ^= 1
        return pacc[pacc_i[0]]

    cond = k.sb("cond", [128, 8])
    op("act", lambda e: e.activation(out=cond[:], in_=condT[:], func=AF.Silu), reads=[condT], writes=[cond])
    wst = [k.sb("wst0", [128, 8, 256]), k.sb("wst1", [128, 8, 256])]
    for j in range(16):
        st = wst[j % 2]
        op("sp", lambda e: e.dma_start(out=st[:, :, 0:128], in_=adaw[:, j * 128:(j + 1) * 128].rearrange("(c p) f -> p c f", p=128)),
           writes=[st], dma=True)
        for kc in range(8):
            op("pe", lambda e: e.matmul(pS[:, j:j + 1], lhsT=st[:, kc, 0:128], rhs=cond[:, kc:kc + 1], start=(kc == 0), stop=(kc == 7)),
               reads=[st, cond], writes=[pS])
    modT = k.sb("modT", [128, 16])
    op("dve", lambda e: e.tensor_tensor(out=modT[:], in0=pS[:, 0:16], in1=adab[:], op=ALU.add), reads=[pS, adab], writes=[modT])
    op("dve", lambda e: e.tensor_scalar_add(out=modT[:, 8:16], in0=modT[:, 8:16], scalar1=1.0), reads=[modT], writes=[modT])

    winb = k.sb("winb", [128, 8, 3072], BF16)
    for j in range(12):
        st = wst[j % 2]
        op("sp" if j % 2 == 0 else "act", lambda e: e.dma_start(out=st[:], in_=w_in[:, j * 256:(j + 1) * 256].rearrange("(c p) f -> p c f", p=128)),
           writes=[st], dma=True)
        op("pool", lambda e: e.tensor_copy(out=winb[:, :, j * 256:(j + 1) * 256], in_=st[:]), reads=[st], writes=[winb])

    BD = [[k.sb(f"bd{w}_{j}", [128, 128], BF16) for j in range(8)] for w in range(3)]
    for w in range(3):
        for j in range(8):
            op("dve", lambda e: e.tensor_tensor(out=BD[w][j][:].rearrange("p (n o) -> p n o", o=4),
                                                in0=wc[w][:, j:j + 1, :].to_broadcast([128, 32, 4]), in1=bmask[:], op=ALU.mult),
               reads=[wc[w], bmask], writes=[BD[w][j]])
    bdt = [k.sb("bdt0", [128, 128]), k.sb("bdt1", [128, 128])]
    wcg = k.sb("wcg", [128, 16, 4], BF16); wmg = k.sb("wmg", [128, 16, 4], BF16)
    ii = 0
    for j in range(16):
        for w in range(3):
            t = bdt[ii % 2]; ii += 1
            op("dve", lambda e: e.tensor_tensor(out=t[:].rearrange("p (n i) -> p n i", i=4),
                                                in0=wt[w][:, j:j + 1, :].to_broadcast([128, 32, 4]), in1=bmask[:], op=ALU.mult),
               reads=[wt[w], bmask], writes=[t])
            dst = pS[:, 64 + j * 4: 68 + j * 4] if w < 2 else pS[:, 192 + j * 4: 196 + j * 4]
            op("pe", lambda e: e.matmul(dst, lhsT=t[:], rhs=wg[w][:, j, :], start=(w != 1), stop=(w != 0)),
               reads=[t, wg[w]], writes=[pS])
    op("dve", lambda e: e.tensor_copy(out=wcg[:].rearrange("p a b -> p (a b)"), in_=pS[:, 64:128]), reads=[pS], writes=[wcg])
    op("dve", lambda e: e.tensor_copy(out=wmg[:].rearrange("p a b -> p (a b)"), in_=pS[:, 192:256]), reads=[pS], writes=[wmg])

    xs_ = [k.sb("x0", [128, 1024]), k.sb("x1", [128, 1024])]
    xb_ = [k.sb("xb0", [128, 1024], BF16), k.sb("xb1", [128, 1024], BF16)]
    uT = [k.sb("uT0", [128, 8, TB], BF16), k.sb("uT1", [128, 8, TB], BF16)]
    xmt = [k.sb(f"xmt{i}", [128, TB + 3]) for i in range(3)]
    hist = k.sb("hist", [128, 16, 3])
    acc = [k.sb("acc0", [128, TB]), k.sb("acc1", [128, TB])]
    xmb = k.sb("xmb", [128, 8, TB], BF16); xcT = k.sb("xcT", [128, 8, TB], BF16)
    xo = [k.sb(f"xo{i}", [128, 2, TB], BF16) for i in range(2)]
    sz = k.sb("sz", [128, 8, TB], BF16)
    qT = k.sb("qT", [128, 8, TB], BF16); kT = k.sb("kT", [128, 8, TB], BF16)
    ktm = k.sb("ktm", [128, NT, 1024], BF16); vtm = k.sb("vtm", [128, NT, 1024], BF16)
    hg = [k.sb("hg0", [128, 8, TB], BF16), k.sb("hg1", [128, 8, TB], BF16)]
    gT = k.sb("gT", [4, TB]); gts = k.sb("gts", [128, NT, 4]); sp_ = k.sb("sp_", [128, NT, 2]); ex_ = k.sb("ex_", [128, NT, 2])
    Cst = [k.sb(f"C{h}", [128, 4, 512]) for h in range(2)]
    Cb = [k.sb(f"Cb{h}", [128, 4, 512], BF16) for h in range(2)]
    nst = [k.sb(f"n{h}", [128, 4]) for h in range(2)]
    nb_ = [k.sb(f"nb{h}", [128, 4], BF16) for h in range(2)]
    for h in range(2):
        op("pool", lambda e: e.memset(Cst[h][:], 0.0), writes=[Cst[h]])
        op("pool", lambda e: e.memset(Cb[h][:], 0.0), writes=[Cb[h]])
        op("pool", lambda e: e.memset(nst[h][:], 0.0), writes=[nst[h]])
        op("pool", lambda e: e.memset(nb_[h][:], 0.0), writes=[nb_[h]])
    op("pool", lambda e: e.memset(hist[:], 0.0), writes=[hist])
    sprep = [k.sb(f"sprep{h}", [128, 128]) for h in range(2)]
    bias_s = [k.sb(f"bias{h}", [128, 1]) for h in range(2)]
    DT = [k.sb(f"DT{h}", [128, 128]) for h in range(2)]
    EB = [k.sb(f"EB{h}", [128, 128]) for h in range(2)]
    bL = [k.sb(f"bL{h}", [128, 1]) for h in range(2)]
    ws = [k.sb(f"ws{h}", [128, 1]) for h in range(2)]
    PT = [k.sb(f"PT{h}", [128, 128], BF16) for h in range(2)]
    qp = [k.sb(f"qp{h}", [128, 4, 128], BF16) for h in range(2)]
    wk_ = [k.sb(f"wk{h}", [128, 512], BF16) for h in range(2)]
    rden = [k.sb(f"rden{h}", [128, 1]) for h in range(2)]
    hs = [k.sb(f"hs{h}", [128, 512]) for h in range(2)]
    hn = [k.sb(f"hn{h}", [128, 512], BF16) for h in range(2)]
    st6 = [k.sb(f"st6{h}", [128, 6]) for h in range(2)]
    mv = [k.sb(f"mv{h}", [128, 2]) for h in range(2)]
    rstd = [k.sb(f"rstd{h}", [128, 1]) for h in range(2)]
    tmp2 = [k.sb(f"tmp2{h}", [128, 128]) for h in range(2)]
    tmp3 = [k.sb(f"tmp3{h}", [128, 128]) for h in range(2)]
    eps_t = k.sb("eps_t", [128, 1])
    op("pool", lambda e: e.memset(eps_t[:], 1e-5), writes=[eps_t])
    mhalf = k.sb("mhalf", [128, 1])
    op("pool", lambda e: e.memset(mhalf[:], -0.5), writes=[mhalf])
    one_t = k.sb("one_t", [128, 1])
    op("pool", lambda e: e.memset(one_t[:], 1.0), writes=[one_t])

    def bail():
        tk = op("sp", lambda e: e.dma_start(out=out[:, 0:TB].rearrange("(c p) t -> p c t", p=128), in_=hg[0][:]), reads=[hg[0]], dma=True)
        k.finish([tk])
        print("bail instrs", k.n_ins)
        return nc
    out_toks = []
    for blk in range(NB):
        t0 = blk * TB
        u = uT[blk % 2]
        for t4 in range(NT):
            xs = xs_[t4 % 2]; xb = xb_[t4 % 2]
            op("sp", lambda e: e.dma_start(out=xs[:], in_=x[t0 + t4 * 128: t0 + (t4 + 1) * 128, :]), writes=[xs], dma=True)
            op("act", lambda e: e.copy(out=xb[:], in_=xs[:]), reads=[xs], writes=[xb])
            for kc in range(8):
                op("pe", lambda e: e.transpose(out=pt[:, kc * 128:(kc + 1) * 128], in_=xb[:, kc * 128:(kc + 1) * 128], identity=ident[:]),
                   reads=[xb, ident], writes=[pt])
            for kc in range(8):
                op("dve", lambda e: e.tensor_scalar(out=u[:, kc, t4 * 128:(t4 + 1) * 128], in0=pt[:, kc * 128:(kc + 1) * 128],
                                                    scalar1=modT[:, 8 + kc:9 + kc], scalar2=modT[:, kc:kc + 1], op0=ALU.mult, op1=ALU.add),
                   reads=[pt, modT], writes=[u])
        for oc in range(16):
            p = nextp()
            for kc in range(8):
                op("pe", lambda e: e.matmul(p[:, 0:TB], lhsT=winb[:, kc, oc * 128:(oc + 1) * 128], rhs=u[:, kc, :], start=(kc == 0), stop=(kc == 7)),
                   reads=[winb, u], writes=[p])
            xm = xmt[oc % 3]
            op("dve", lambda e: e.tensor_copy(out=xm[:, 0:3], in_=hist[:, oc, :]), reads=[hist], writes=[xm])
            op("act", lambda e: e.copy(out=xm[:, 3:TB + 3], in_=p[:, 0:TB]), reads=[p], writes=[xm])
            op("dve", lambda e: e.tensor_copy(out=hist[:, oc, :], in_=xm[:, TB:TB + 3]), reads=[xm], writes=[hist])
            xmdst = xmb[:, oc, :] if oc < 8 else xo[oc % 2][:, 1, :]
            xmdb = xmb if oc < 8 else xo[oc % 2]
            op("act", lambda e: e.copy(out=xmdst, in_=xm[:, 3:TB + 3]), reads=[xm], writes=[xmdb])
            a = acc[oc % 2]
            op("dve", lambda e: e.tensor_scalar(out=a[:], in0=xm[:, 3:TB + 3], scalar1=cw[:, oc, 3:4], scalar2=None, op0=ALU.mult),
               reads=[xm, cw], writes=[a])
            for jj in (2, 1, 0):
                op("dve", lambda e: e.scalar_tensor_tensor(out=a[:], in0=xm[:, jj:TB + jj], scalar=cw[:, oc, jj:jj + 1], in1=a[:],
                                                           op0=ALU.mult, op1=ALU.add), reads=[xm, cw, a], writes=[a])
            xcdst = xcT[:, oc, :] if oc < 8 else xo[oc % 2][:, 0, :]
            xcdb = xcT if oc < 8 else xo[oc % 2]
            op("act", lambda e: e.activation(out=xcdst, in_=a[:], func=AF.Silu, bias=cb[:, oc:oc + 1]), reads=[a, cb], writes=[xcdb])
            xmsrc = xmdst
            op("pe", lambda e: e.matmul(pS[0:4, 0:TB], lhsT=wcg[:, oc, :], rhs=xcdst, start=(oc == 0), stop=False), reads=[wcg, xcdb], writes=[pS])
            op("pe", lambda e: e.matmul(pS[0:4, 0:TB], lhsT=wmg[:, oc, :], rhs=xmsrc, start=False, stop=(oc == 15)), reads=[wmg, xmdb], writes=[pS])
        op("act", lambda e: e.copy(out=gT[:], in_=pS[0:4, 0:TB]), reads=[pS], writes=[gT])
        for oc in range(8):
            p = nextp()
            for kc in range(8):
                op("pe", lambda e: e.matmul(p[:, 0:TB], lhsT=winb[:, kc, 2048 + oc * 128: 2048 + (oc + 1) * 128], rhs=u[:, kc, :], start=(kc == 0), stop=(kc == 7)),
                   reads=[winb, u], writes=[p])
            op("act", lambda e: e.activation(out=sz[:, oc, :], in_=p[:, 0:TB], func=AF.Silu), reads=[p], writes=[sz])
        for j in range(8):
            p = nextp()
            op("pe", lambda e: e.matmul(p[:, 0:TB], lhsT=BD[0][j][:], rhs=xcT[:, j, :], start=True, stop=True), reads=[BD[0][j], xcT], writes=[p])
            op("act", lambda e: e.mul(out=qT[:, j, :], in_=p[:, 0:TB], mul=DH ** -0.5), reads=[p], writes=[qT])
            p = nextp()
            op("pe", lambda e: e.matmul(p[:, 0:TB], lhsT=BD[1][j][:], rhs=xcT[:, j, :], start=True, stop=True), reads=[BD[1][j], xcT], writes=[p])
            op("dve", lambda e: e.tensor_copy(out=kT[:, j, :], in_=p[:, 0:TB]), reads=[p], writes=[kT])
        for t4 in range(NT):
            for half in range(2):
                p = nextp()
                for jj in range(4):
                    j = half * 4 + jj
                    op("pe", lambda e: e.matmul(p[:, jj * 128:(jj + 1) * 128], lhsT=xcT[:, j, t4 * 128:(t4 + 1) * 128], rhs=BD[1][j][:], start=True, stop=True),
                       reads=[xcT, BD[1][j]], writes=[p])
                op("act", lambda e: e.copy(out=ktm[:, t4, half * 512:(half + 1) * 512], in_=p[:, :]), reads=[p], writes=[ktm])
                p = nextp()
                for jj in range(4):
                    j = half * 4 + jj
                    op("pe", lambda e: e.matmul(p[:, jj * 128:(jj + 1) * 128], lhsT=xmb[:, j, t4 * 128:(t4 + 1) * 128], rhs=BD[2][j][:], start=True, stop=True),
                       reads=[xmb, BD[2][j]], writes=[p])
                op("dve", lambda e: e.tensor_copy(out=vtm[:, t4, half * 512:(half + 1) * 512], in_=p[:, :]), reads=[p], writes=[vtm])
        for t4 in range(NT):
            op("pe", lambda e: e.matmul(pS[:, 256 + t4 * 4: 260 + t4 * 4], lhsT=gT[:, t4 * 128:(t4 + 1) * 128], rhs=identf[0:4, 0:4], start=True, stop=True),
               reads=[gT, identf], writes=[pS])
        op("dve", lambda e: e.tensor_tensor(out=gts[:], in0=pS[:, 256:256 + 4 * NT].rearrange("p (a b) -> p a b", b=4),
                                            in1=bgs[:, None, :].to_broadcast([128, NT, 4]), op=ALU.add), reads=[pS, bgs], writes=[gts])
        op("act", lambda e: e.activation(out=ex_[:], in_=gts[:, :, 2:4], func=AF.Exp, scale=-1.0), reads=[gts], writes=[ex_])
        op("act", lambda e: e.activation(out=sp_[:], in_=ex_[:], func=AF.Ln, bias=one_t[:]), reads=[ex_, one_t], writes=[sp_])
        hgb = hg[blk % 2]

        def chain(t4, h, hgb=hgb):
            tsl = slice(t4 * 128, (t4 + 1) * 128)
            pb = pbh[h]; pS = pSh[h]; pn = pnh[h]; pt = ptFh[h]; pc = [pA, pB]
            if True:
                hc = slice(h * 512, (h + 1) * 512)
                op("dve", lambda e: e.tensor_scalar(out=sprep[h][:], in0=onesf[:], scalar1=sp_[:, t4, h:h + 1], scalar2=None, op0=ALU.mult),
                   reads=[onesf, sp_], writes=[sprep[h]])
                op("pe", lambda e: e.matmul(pb[:, 0:128], lhsT=sprep[h][:], rhs=ntri[:], start=True, stop=False), reads=[sprep[h], ntri], writes=[pb])
                op("pe", lambda e: e.matmul(pb[:, 0:128], lhsT=identf[:], rhs=negm[:], start=False, stop=True), reads=[identf, negm], writes=[pb])
                op("pe", lambda e: e.matmul(pb[:, 128:256], lhsT=sprep[h][:], rhs=ntri[:], start=True, stop=True), reads=[sprep[h], ntri], writes=[pb])
                op("pe", lambda e: e.matmul(pb[:, 256:258], lhsT=ntri[:], rhs=sp_[:, t4, :], start=True, stop=True), reads=[ntri, sp_], writes=[pb])
                yield
                op("dve", lambda e: e.tensor_tensor(out=bias_s[h][:], in0=gts[:, t4, h:h + 1], in1=pb[:, 256 + h:257 + h], op=ALU.subtract),
                   reads=[gts, pb], writes=[bias_s[h]])
                op("dve", lambda e: e.tensor_copy(out=bL[h][:], in_=pb[:, 255:256]), reads=[pb], writes=[bL[h]])
                yield
                op("act", lambda e: e.activation(out=DT[h][:], in_=pb[:, 0:128], func=AF.Exp, bias=bias_s[h][:]), reads=[pb, bias_s[h], bL[h]], writes=[DT[h]])
                op("act", lambda e: e.activation(out=EB[h][:], in_=pb[:, 128:256], func=AF.Exp), reads=[pb, bL[h]], writes=[EB[h]])
                op("act", lambda e: e.activation(out=ws[h][:], in_=bias_s[h][:], func=AF.Exp, bias=bL[h][:]), reads=[bias_s[h], bL[h]], writes=[ws[h]])
                yield
                for dc in range(4):
                    op("pe", lambda e: e.matmul(pS[:, 260:388], lhsT=kT[:, 4 * h + dc, tsl], rhs=qT[:, 4 * h + dc, tsl], start=(dc == 0), stop=(dc == 3)),
                       reads=[kT, qT], writes=[pS])
                yield
                op("dve", lambda e: e.tensor_tensor(out=PT[h][:], in0=pS[:, 260:388], in1=DT[h][:], op=ALU.mult), reads=[pS, DT[h]], writes=[PT[h]])
                yield
                for dc in range(4):
                    op("dve", lambda e: e.tensor_tensor(out=qp[h][:, dc, :], in0=qT[:, 4 * h + dc, tsl], in1=EB[h][:], op=ALU.mult),
                       reads=[qT, EB[h]], writes=[qp[h]])
                op("dve", lambda e: e.tensor_scalar(out=wk_[h][:], in0=ktm[:, t4, hc], scalar1=ws[h][:], scalar2=None, op0=ALU.mult),
                   reads=[ktm, ws[h]], writes=[wk_[h]])
                yield
                op("pe", lambda e: e.matmul(pn[:, :], lhsT=PT[h][:], rhs=vtm[:, t4, hc], start=True, stop=False), reads=[PT[h], vtm], writes=[pn])
                for dc in range(4):
                    op("pe", lambda e: e.matmul(pn[:, :], lhsT=qp[h][:, dc, :], rhs=Cb[h][:, dc, :], start=False, stop=(dc == 3)),
                       reads=[qp[h], Cb[h]], writes=[pn])
                op("pe", lambda e: e.matmul(pS[:, 388:389], lhsT=PT[h][:], rhs=onesb[:, 0:1], start=True, stop=False), reads=[PT[h], onesb], writes=[pS])
                for dc in range(4):
                    op("pe", lambda e: e.matmul(pS[:, 388:389], lhsT=qp[h][:, dc, :], rhs=nb_[h][:, dc:dc + 1], start=False, stop=(dc == 3)),
                       reads=[qp[h], nb_[h]], writes=[pS])
                yield
                for dc in range(4):
                    pcc = pc[dc % 2]
                    op("pe", lambda e: e.matmul(pcc[:, :], lhsT=wk_[h][:, dc * 128:(dc + 1) * 128], rhs=vtm[:, t4, hc], start=True, stop=True),
                       reads=[wk_[h], vtm], writes=[pcc])
                    op("dve", lambda e: e.scalar_tensor_tensor(out=Cst[h][:, dc, :], in0=Cst[h][:, dc, :], scalar=EB[h][:, 127:128], in1=pcc[:, :],
                                                               op0=ALU.mult, op1=ALU.add), reads=[Cst[h], EB[h], pcc], writes=[Cst[h]])
                    yield
                yield
                op("act", lambda e: e.copy(out=Cb[h][:], in_=Cst[h][:]), reads=[Cst[h]], writes=[Cb[h]])
                for dc in range(4):
                    op("pe", lambda e: e.matmul(pS[:, 392 + dc:393 + dc], lhsT=wk_[h][:, dc * 128:(dc + 1) * 128], rhs=onesb[:, 0:1], start=True, stop=True),
                       reads=[wk_[h], onesb], writes=[pS])
                yield
                op("dve", lambda e: e.tensor_scalar(out=rden[h][:], in0=pS[:, 388:389], scalar1=-1.0, scalar2=None, op0=ALU.mult),
                   reads=[pS], writes=[rden[h]])
                op("dve", lambda e: e.tensor_tensor(out=rden[h][:], in0=rden[h][:], in1=pS[:, 388:389], op=ALU.max),
                   reads=[pS, rden[h]], writes=[rden[h]])
                op("dve", lambda e: e.tensor_scalar(out=rden[h][:], in0=rden[h][:], scalar1=1.0, scalar2=None, op0=ALU.max),
                   reads=[rden[h]], writes=[rden[h]])
                op("dve", lambda e: e.scalar_tensor_tensor(out=nst[h][:], in0=nst[h][:], scalar=EB[h][:, 127:128], in1=pS[:, 392:396],
                                                           op0=ALU.mult, op1=ALU.add), reads=[nst[h], EB[h], pS], writes=[nst[h]])
                op("dve", lambda e: e.tensor_copy(out=nb_[h][:], in_=nst[h][:]), reads=[nst[h]], writes=[nb_[h]])
                op("dve", lambda e: e.reciprocal(out=rden[h][:], in_=rden[h][:]), reads=[rden[h]], writes=[rden[h]])
                op("dve", lambda e: e.tensor_scalar(out=hs[h][:], in0=pn[:, :], scalar1=rden[h][:], scalar2=None, op0=ALU.mult), reads=[pn, rden[h]], writes=[hs[h]])
                yield
                op("dve", lambda e: e.bn_stats(out=st6[h][:], in_=hs[h][:]), reads=[hs[h]], writes=[st6[h]])
                op("dve", lambda e: e.bn_aggr(out=mv[h][:], in_=st6[h][:]), reads=[st6[h]], writes=[mv[h]])
                yield
                op("pool", lambda e: e.tensor_scalar(out=rstd[h][:], in0=mv[h][:, 1:2], scalar1=1e-5, scalar2=None, op0=ALU.add), reads=[mv[h]], writes=[rstd[h]])
                op("pool", lambda e: e.tensor_tensor(out=rstd[h][:], in0=rstd[h][:], in1=mhalf[:], op=ALU.pow), reads=[rstd[h], mhalf], writes=[rstd[h]])
                yield
                op("dve", lambda e: e.tensor_scalar(out=hn[h][:], in0=hs[h][:], scalar1=mv[h][:, 0:1], scalar2=rstd[h][:], op0=ALU.subtract, op1=ALU.mult),
                   reads=[hs[h], mv[h], rstd[h]], writes=[hn[h]])
                yield
                for dc in range(4):
                    op("pe", lambda e: e.transpose(out=pt[:, h * 512 + dc * 128:h * 512 + (dc + 1) * 128], in_=hn[h][:, dc * 128:(dc + 1) * 128], identity=ident[:]),
                       reads=[hn[h], ident], writes=[pt])
                yield
                for dc in range(4):
                    j = 4 * h + dc
                    op("dve", lambda e: e.tensor_scalar(out=tmp2[h][:], in0=xcT[:, j, tsl], scalar1=sk[:, j:j + 1], scalar2=None, op0=ALU.mult),
                       reads=[xcT, sk], writes=[tmp2[h]])
                    op("dve", lambda e: e.scalar_tensor_tensor(out=tmp3[h][:], in0=pt[:, h * 512 + dc * 128:h * 512 + (dc + 1) * 128], scalar=nw[:, j:j + 1], in1=tmp2[h][:],
                                                               op0=ALU.mult, op1=ALU.add), reads=[pt, nw, tmp2[h]], writes=[tmp3[h]])
                    op("dve", lambda e: e.tensor_tensor(out=hgb[:, j, tsl], in0=tmp3[h][:], in1=sz[:, j, tsl], op=ALU.mult),
                       reads=[tmp3[h], sz], writes=[hgb])
                    yield
        for t4 in range(NT):
            gens = [chain(t4, h) for h in HS]
            while gens:
                for g_ in list(gens):
                    try:
                        next(g_)
                    except StopIteration:
                        gens.remove(g_)
        tk = op("pool", lambda e: e.dma_start(out=out[:, t0:t0 + TB].rearrange("(c p) t -> p c t", p=128), in_=hgb[:]), reads=[hgb], dma=True)
        out_toks.append(tk)
    if fz is None:
        k.finish(out_toks)
    else:
        k.end_stage()
    print("m0 instructions:", k.n_ins)
    return nc


def m0_inputs(inp, b, hp, S):
    f = np.float32
    own = np.arange(hp * 1024, (hp + 1) * 1024); oth = np.arange((1 - hp) * 1024, (2 - hp) * 1024)
    ch = np.concatenate([own, oth])
    w_in = inp["mlstm_w_in"][0]
    d = {}
    d["x"] = np.ascontiguousarray(inp["x"][b, :S])
    d["condT"] = np.ascontiguousarray(inp["c"][b].reshape(8, 128).T)
    d["adaw"] = np.ascontiguousarray(inp["ada_w"][0][:, 0:2048])
    d["adabT"] = np.ascontiguousarray(inp["ada_b"][0][0:2048].reshape(16, 128).T)
    d["w_in"] = np.ascontiguousarray(np.concatenate([w_in[:, ch], w_in[:, 2048 + own]], axis=1))
    d["convw"] = np.ascontiguousarray(inp["mlstm_conv_w"][0].T[ch].reshape(16, 128, 4).transpose(1, 0, 2))
    d["convb"] = np.ascontiguousarray(inp["mlstm_conv_b"][0][ch].reshape(16, 128).T)
    blk = ch.reshape(-1, 4)[:, 0] // 4
    for nm, key in (("q", "mlstm_wq"), ("k", "mlstm_wk"), ("v", "mlstm_wv")):
        w = inp[key][0][blk]
        d["w%sc" % nm] = np.ascontiguousarray(w.reshape(16, 128, 4).transpose(1, 0, 2))
        d["w%st" % nm] = np.ascontiguousarray(w.transpose(0, 2, 1).reshape(16, 128, 4).transpose(1, 0, 2))
    gcols = [2 * hp, 2 * hp + 1, 4 + 2 * hp, 5 + 2 * hp]
    wgates = inp["mlstm_w_gates"][0]
    for i, nm in enumerate("qkv"):
        wg = wgates[i * 2048:(i + 1) * 2048][ch][:, gcols]
        d["wg" + nm] = np.ascontiguousarray(wg.reshape(16, 128, 4).transpose(1, 0, 2))
    d["bg"] = np.ascontiguousarray(np.tile(inp["mlstm_b_gates"][0][gcols][None, :], (128, 1)))
    d["nwT"] = np.ascontiguousarray(inp["mlstm_norm_w"][0][own].reshape(8, 128).T)
    d["skT"] = np.ascontiguousarray(inp["mlstm_skip"][0][own].reshape(8, 128).T)
    return {k_: v.astype(f) for k_, v in d.items()}


ALPHA = 4 ** 0.25
EPS = 1e-5


def consts(k):
    op = k.op
    c = {}
    c["identf"] = k.sb("identf", [128, 128])
    c["onesf"] = k.sb("onesf", [128, 128])
    c["mhalf"] = k.sb("mhalf", [128, 1])
    op("pool", lambda e: e.memset(c["identf"][:], 0.0), writes=[c["identf"]])
    op("pool", lambda e: e.affine_select(out=c["identf"][:], in_=c["identf"][:], pattern=[[-1, 128]], compare_op=ALU.not_equal,
                                         fill=1.0, base=0, channel_multiplier=1), reads=[c["identf"]], writes=[c["identf"]])
    op("pool", lambda e: e.memset(c["onesf"][:], 1.0), writes=[c["onesf"]])
    op("pool", lambda e: e.memset(c["mhalf"][:], -0.5), writes=[c["mhalf"]])
    return c


def adaln(k, c, condT_d, adaw_f, adab_f, nf, adaw_b, adab_b, nb, pbank, stg):
    op = k.op
    condT = k.sb("condT_s", [128, 8]); cond = k.sb("cond", [128, 8])
    op("sp", lambda e: e.dma_start(out=condT[:], in_=condT_d), writes=[condT], dma=True)
    op("act", lambda e: e.activation(out=cond[:], in_=condT[:], func=AF.Silu), reads=[condT], writes=[cond])
    modT = None
    if nf:
        modT = k.sb("modT", [128, nf * 8]); adab = k.sb("adabf", [128, nf * 8])
        op("sp", lambda e: e.dma_start(out=adab[:], in_=adab_f), writes=[adab], dma=True)
        for j in range(nf * 8):
            st = stg[j % 2]
            op("sp", lambda e: e.dma_start(out=st[:, :, 0:128], in_=adaw_f[:, j * 128:(j + 1) * 128].rearrange("(c p) f -> p c f", p=128)), writes=[st], dma=True)
            for kc in range(8):
                op("pe", lambda e: e.matmul(pbank[:, j:j + 1], lhsT=st[:, kc, 0:128], rhs=cond[:, kc:kc + 1], start=(kc == 0), stop=(kc == 7)),
                   reads=[st, cond], writes=[pbank])
        op("dve", lambda e: e.tensor_tensor(out=modT[:], in0=pbank[:, 0:nf * 8], in1=adab[:], op=ALU.add), reads=[pbank, adab], writes=[modT])
    bts = []
    if nb:
        crep = k.sb("crep", [128, 8, 128])
        for kc in range(8):
            op("dve", lambda e: e.tensor_scalar(out=crep[:, kc, :], in0=c["onesf"][:], scalar1=cond[:, kc:kc + 1], scalar2=None, op0=ALU.mult),
               reads=[c["onesf"], cond], writes=[crep])
        for v in range(nb):
            bt = k.sb(f"bt{v}", [128, 1024])
            op("sp", lambda e: e.dma_start(out=bt[:], in_=adab_b[:, v * 1024:(v + 1) * 1024]), writes=[bt], dma=True)
            for q in range(4):
                st = stg[q % 2]
                op("sp", lambda e: e.dma_start(out=st[:], in_=adaw_b[:, v * 1024 + q * 256: v * 1024 + (q + 1) * 256].rearrange("(c p) f -> p c f", p=128)),
                   writes=[st], dma=True)
                for kc in range(8):
                    op("pe", lambda e: e.matmul(pbank[:, 0:256], lhsT=crep[:, kc, :], rhs=st[:, kc, :], start=(kc == 0), stop=(kc == 7)),
                       reads=[crep, st], writes=[pbank])
                op("dve", lambda e: e.tensor_tensor(out=bt[:, q * 256:(q + 1) * 256], in0=bt[:, q * 256:(q + 1) * 256], in1=pbank[:, 0:256], op=ALU.add),
                   reads=[bt, pbank], writes=[bt])
            bts.append(bt)
    return modT, bts


def res_ln(k, c, ysrc, ybuf, xs, G, lng, lnb, tmp, st12, mv, rstd, xo):
    op = k.op
    op("dve", lambda e: e.tensor_tensor(out=tmp[:], in0=ysrc, in1=G[:], op=ALU.mult), reads=ybuf + [G], writes=[tmp])
    op("dve", lambda e: e.scalar_tensor_tensor(out=tmp[:], in0=xs[:], scalar=ALPHA, in1=tmp[:], op0=ALU.mult, op1=ALU.add), reads=[xs, tmp], writes=[tmp])
    for hh in range(2):
        op("dve", lambda e: e.bn_stats(out=st12[:, hh * 6:(hh + 1) * 6], in_=tmp[:, hh * 512:(hh + 1) * 512]), reads=[tmp], writes=[st12])
    op("dve", lambda e: e.bn_aggr(out=mv[:], in_=st12[:]), reads=[st12], writes=[mv])
    op("pool", lambda e: e.tensor_scalar(out=rstd[:], in0=mv[:, 1:2], scalar1=EPS, scalar2=None, op0=ALU.add), reads=[mv], writes=[rstd])
    op("pool", lambda e: e.tensor_tensor(out=rstd[:], in0=rstd[:], in1=c["mhalf"][:], op=ALU.pow), reads=[rstd, c["mhalf"]], writes=[rstd])
    op("dve", lambda e: e.tensor_scalar(out=tmp[:], in0=tmp[:], scalar1=mv[:, 0:1], scalar2=rstd[:], op0=ALU.subtract, op1=ALU.mult),
       reads=[tmp, mv, rstd], writes=[tmp])
    op("dve", lambda e: e.tensor_tensor(out=tmp[:], in0=tmp[:], in1=lng[:], op=ALU.mult), reads=[tmp, lng], writes=[tmp])
    op("dve", lambda e: e.tensor_tensor(out=xo[:], in0=tmp[:], in1=lnb[:], op=ALU.add), reads=[tmp, lnb], writes=[xo])


def mod_transpose(k, c, xo, modT, m0, pT, uTf, uTb):
    op = k.op
    for kc in range(8):
        op("pe", lambda e: e.transpose(out=pT[kc // 4][:, (kc % 4) * 128:(kc % 4 + 1) * 128], in_=xo[:, kc * 128:(kc + 1) * 128], identity=c["identf"][:]),
           reads=[xo, c["identf"]], writes=[pT[kc // 4]])
    for kc in range(8):
        dst = uTf if uTf is not None else uTb
        op("dve", lambda e: e.tensor_scalar(out=dst[:, kc, :], in0=pT[kc // 4][:, (kc % 4) * 128:(kc % 4 + 1) * 128],
                                            scalar1=modT[:, m0 + 8 + kc:m0 + 9 + kc], scalar2=modT[:, m0 + kc:m0 + kc + 1], op0=ALU.mult, op1=ALU.add),
           reads=[pT[kc // 4], modT], writes=[dst])
    if uTf is not None:
        op("act", lambda e: e.copy(out=uTb[:], in_=uTf[:]), reads=[uTf], writes=[uTb])


def build_proj(TC, KC, glu, router, fz=None, blend=False):
    if fz is None:
        nc = bass.Bass("TRN2", target_bir_lowering=False); k = Ctx(nc); pref = ""
    else:
        nc, k, pref = fz.nc, fz.k, fz.pref
    D = mkD(nc, fz, pref)
    k.begin_stage(pref)
    NOUT = 2048 if glu else 1024
    inT = D("inT", [KC * 128, TC * (2 if blend else 1)], BF16)
    if blend:
        msk_d = D("msk", [128, 2])
    W = D("W", [KC * 128, NOUT])
    x = D("x", [TC, 1024])
    condT_d = D("condT", [128, 8])
    adaw_f = D("adaw_f", [1024, 2048]); adab_f = D("adab_f", [128, 16])
    adaw_b = D("adaw_b", [1024, 1024]); adab_b = D("adab_b", [128, 1024])
    lng_d = D("lng", [128, 1024]); lnb_d = D("lnb", [128, 1024])
    if glu:
        bglu_d = D("bglu", [128, 2048])
    if router:
        wr_d = D("wr", [128, 8, 8])
        gates_o = D("gates", [TC, 8], kind="ExternalOutput")
    xmid_o = D("xmid", [TC, 1024], kind="ExternalOutput")
    uT_o = D("uT", [1024, TC], BF16, kind="ExternalOutput")
    op = k.op
    c = consts(k)
    if blend:
        msk = k.sb("msk_s", [128, 2])
        op("sp", lambda e: e.dma_start(out=msk[:], in_=msk_d), writes=[msk], dma=True)
        itc = [[k.sb(f"itc{i}_{j}", [128, KC, 128], BF16) for j in range(2)] for i in range(2)]
    pY = [k.ps(f"pY{i}", [128, 512]) for i in range(4)]
    pT = [k.ps("pT0", [128, 512]), k.ps("pT1", [128, 512])]
    pM = k.ps("pM", [128, 512])
    stg = [k.sb("stg0", [128, 8, 256]), k.sb("stg1", [128, 8, 256])]
    modT, bts = adaln(k, c, condT_d, adaw_f, adab_f, 2, adaw_b, adab_b, 1, pM, stg)
    op("dve", lambda e: e.tensor_scalar_add(out=modT[:, 8:16], in0=modT[:, 8:16], scalar1=1.0), reads=[modT], writes=[modT])
    G = bts[0]
    op("dve", lambda e: e.tensor_scalar_add(out=G[:], in0=G[:], scalar1=1.0), reads=[G], writes=[G])
    lng = k.sb("lng_s", [128, 1024]); lnb = k.sb("lnb_s", [128, 1024])
    op("sp", lambda e: e.dma_start(out=lng[:], in_=lng_d), writes=[lng], dma=True)
    op("sp", lambda e: e.dma_start(out=lnb[:], in_=lnb_d), writes=[lnb], dma=True)
    if glu:
        bglu = k.sb("bglu_s", [128, 2048])
        op("sp", lambda e: e.dma_start(out=bglu[:], in_=bglu_d), writes=[bglu], dma=True)
    if router:
        wr = k.sb("wr_s", [128, 8, 8])
        op("sp", lambda e: e.dma_start(out=wr[:], in_=wr_d), writes=[wr], dma=True)
    Wb = k.sb("Wb", [128, KC, NOUT], BF16)
    ws2 = [k.sb("ws2_0", [128, NOUT]), k.sb("ws2_1", [128, NOUT])]
    for kc in range(KC):
        st = ws2[kc % 2]
        op("sp" if kc % 2 == 0 else "act", lambda e: e.dma_start(out=st[:], in_=W[kc * 128:(kc + 1) * 128, :]), writes=[st], dma=True)
        op("pool", lambda e: e.tensor_copy(out=Wb[:, kc, :], in_=st[:]), reads=[st], writes=[Wb])
    NTL = TC // 128
    it = [k.sb(f"it{i}", [128, KC, 128], BF16) for i in range(2)]
    xs_ = [k.sb(f"xs{i}", [128, 1024]) for i in range(2)]
    tmp = [k.sb(f"tmp{i}", [128, 1024]) for i in range(2)]
    xo = [k.sb(f"xo{i}", [128, 1024]) for i in range(2)]
    yv = [k.sb(f"yv{i}", [128, 1024]) for i in range(2)]
    sg = [k.sb(f"sg{i}", [128, 1024]) for i in range(2)]
    st12 = [k.sb(f"st12{i}", [128, 12]) for i in range(2)]
    mv = [k.sb(f"mv{i}", [128, 2]) for i in range(2)]
    rstd = [k.sb(f"rstd{i}", [128, 1]) for i in range(2)]
    uTf = [k.sb(f"uTf{i}", [128, 8, 128]) for i in range(2)]
    uTb = [k.sb(f"uTb{i}", [128, 8, 128], BF16) for i in range(2)]
    if router:
        m8 = [k.sb(f"m8{i}", [128, 8]) for i in range(2)]
        lg = [k.sb(f"lg{i}", [128, 8]) for i in range(2)]
        gw = [k.sb(f"gw{i}", [128, 2]) for i in range(2)]
        gt = [k.sb(f"gt{i}", [128, 8]) for i in range(2)]
        eq = [k.sb(f"eq{i}", [128, 8]) for i in range(2)]
    toks = []
    for t in range(NTL):
        i2 = t % 2
        tsl = slice(t * 128, (t + 1) * 128)
        if not blend:
            op("sp", lambda e: e.dma_start(out=it[i2][:], in_=inT[:, tsl].rearrange("(c p) t -> p c t", p=128)), writes=[it[i2]], dma=True)
        else:
            for j in range(2):
                cs_ = slice(j * TC + t * 128, j * TC + (t + 1) * 128)
                op("sp", lambda e: e.dma_start(out=itc[i2][j][:], in_=inT[:, cs_].rearrange("(c p) t -> p c t", p=128)), writes=[itc[i2][j]], dma=True)
            op("dve", lambda e: e.tensor_scalar(out=itc[i2][0][:], in0=itc[i2][0][:], scalar1=msk[:, 0:1], scalar2=None, op0=ALU.mult), reads=[itc[i2][0], msk], writes=[itc[i2][0]])
            op("dve", lambda e: e.scalar_tensor_tensor(out=it[i2][:], in0=itc[i2][1][:], scalar=msk[:, 1:2], in1=itc[i2][0][:], op0=ALU.mult, op1=ALU.add),
               reads=[itc[i2][0], itc[i2][1], msk], writes=[it[i2]])
        op("act", lambda e: e.dma_start(out=xs_[i2][:], in_=x[tsl, :]), writes=[xs_[i2]], dma=True)
        for nb in range(NOUT // 512):
            p = pY[nb]
            for kc in range(KC):
                op("pe", lambda e: e.matmul(p[:, :], lhsT=it[i2][:, kc, :], rhs=Wb[:, kc, nb * 512:(nb + 1) * 512], start=(kc == 0), stop=(kc == KC - 1)),
                   reads=[it[i2], Wb], writes=[p])
        if glu:
            for nb in range(2):
                op("dve", lambda e: e.tensor_tensor(out=sg[i2][:, nb * 512:(nb + 1) * 512], in0=pY[2 + nb][:, :], in1=bglu[:, 1024 + nb * 512:1024 + (nb + 1) * 512], op=ALU.add),
                   reads=[pY[2 + nb], bglu], writes=[sg[i2]])
                op("dve", lambda e: e.tensor_tensor(out=yv[i2][:, nb * 512:(nb + 1) * 512], in0=pY[nb][:, :], in1=bglu[:, nb * 512:(nb + 1) * 512], op=ALU.add),
                   reads=[pY[nb], bglu], writes=[yv[i2]])
            op("act", lambda e: e.activation(out=sg[i2][:], in_=sg[i2][:], func=AF.Sigmoid), reads=[sg[i2]], writes=[sg[i2]])
            op("dve", lambda e: e.tensor_tensor(out=yv[i2][:], in0=yv[i2][:], in1=sg[i2][:], op=ALU.mult), reads=[yv[i2], sg[i2]], writes=[yv[i2]])
        else:
            for nb in range(2):
                op("act", lambda e: e.copy(out=yv[i2][:, nb * 512:(nb + 1) * 512], in_=pY[nb][:, :]), reads=[pY[nb]], writes=[yv[i2]])
        res_ln(k, c, yv[i2][:], [yv[i2]], xs_[i2], G, lng, lnb, tmp[i2], st12[i2], mv[i2], rstd[i2], xo[i2])
        toks.append(op("pool", lambda e: e.dma_start(out=xmid_o[tsl, :], in_=xo[i2][:]), reads=[xo[i2]], dma=True))
        mod_transpose(k, c, xo[i2], modT, 0, pT, uTf[i2], uTb[i2])
        toks.append(op("pool", lambda e: e.dma_start(out=uT_o[:, tsl].rearrange("(c p) t -> p c t", p=128), in_=uTb[i2][:]), reads=[uTb[i2]], dma=True))
        if router:
            for kc in range(8):
                op("pe", lambda e: e.matmul(pM[:, 0:8], lhsT=uTf[i2][:, kc, :], rhs=wr[:, kc, :], start=(kc == 0), stop=(kc == 7)),
                   reads=[uTf[i2], wr], writes=[pM])
            op("dve", lambda e: e.tensor_copy(out=lg[i2][:], in_=pM[:, 0:8]), reads=[pM], writes=[lg[i2]])
            op("dve", lambda e: e.max(out=m8[i2][:], in_=lg[i2][:]), reads=[lg[i2]], writes=[m8[i2]])
            op("dve", lambda e: e.tensor_tensor(out=gw[i2][:, 0:1], in0=m8[i2][:, 1:2], in1=m8[i2][:, 0:1], op=ALU.subtract), reads=[m8[i2]], writes=[gw[i2]])
            op("act", lambda e: e.activation(out=gw[i2][:, 1:2], in_=gw[i2][:, 0:1], func=AF.Sigmoid), reads=[gw[i2]], writes=[gw[i2]])
            op("dve", lambda e: e.tensor_scalar(out=gw[i2][:, 0:1], in0=gw[i2][:, 1:2], scalar1=-1.0, scalar2=1.0, op0=ALU.mult, op1=ALU.add),
               reads=[gw[i2]], writes=[gw[i2]])
            op("dve", lambda e: e.tensor_scalar(out=gt[i2][:], in0=lg[i2][:], scalar1=m8[i2][:, 0:1], scalar2=gw[i2][:, 0:1], op0=ALU.is_equal, op1=ALU.mult),
               reads=[lg[i2], m8[i2], gw[i2]], writes=[gt[i2]])
            op("dve", lambda e: e.tensor_scalar(out=eq[i2][:], in0=lg[i2][:], scalar1=m8[i2][:, 1:2], scalar2=gw[i2][:, 1:2], op0=ALU.is_equal, op1=ALU.mult),
               reads=[lg[i2], m8[i2], gw[i2]], writes=[eq[i2]])
            op("dve", lambda e: e.tensor_tensor(out=gt[i2][:], in0=gt[i2][:], in1=eq[i2][:], op=ALU.add), reads=[gt[i2], eq[i2]], writes=[gt[i2]])
            toks.append(op("pool", lambda e: e.dma_start(out=gates_o[tsl, :], in_=gt[i2][:]), reads=[gt[i2]], dma=True))
    if fz is None:
        k.finish(toks)
    else:
        k.end_stage()
    print("proj instructions", k.n_ins)
    return nc


def build_ffn(TC, NE, T, emit_u, fz=None):
    if fz is None:
        nc = bass.Bass("TRN2", target_bir_lowering=False); k = Ctx(nc); pref = ""
    else:
        nc, k, pref = fz.nc, fz.k, fz.pref
    D = mkD(nc, fz, pref)
    k.begin_stage(pref)
    uT_d = D("uT", [1024, TC], BF16)
    xmid = D("xmid", [TC, 1024])
    w13 = D("w13", [NE, 1024, 5632]); w2 = D("w2", [NE, 2816, 1024])
    if NE > 1:
        gates_d = D("gates", [TC, NE])
    condT_d = D("condT", [128, 8])
    adaw_b = D("adaw_b", [1024, 1024]); adab_b = D("adab_b", [128, 1024])
    lng_d = D("lng", [128, 1024]); lnb_d = D("lnb", [128, 1024])
    if emit_u:
        adaw_f = D("adaw_f", [1024, 2048]); adab_f = D("adab_f", [128, 16])
        uT_o = D("uTn", [1024, TC], BF16, kind="ExternalOutput")
    xout = D("xout", [TC, 1024], kind="ExternalOutput")
    op = k.op
    c = consts(k)
    pH = [k.ps(f"pH{i}", [128, 512]) for i in range(4)]
    pY = [k.ps(f"pY{i}", [128, 512]) for i in range(2)]
    pT = [k.ps("pT0", [128, 512]), k.ps("pT1", [128, 512])]
    stg = [k.sb("stg0", [128, 8, 256]), k.sb("stg1", [128, 8, 256])]
    modT, bts = adaln(k, c, condT_d, adaw_f if emit_u else None, adab_f if emit_u else None, 2 if emit_u else 0, adaw_b, adab_b, 1, pT[0], stg)
    if emit_u:
        op("dve", lambda e: e.tensor_scalar_add(out=modT[:, 8:16], in0=modT[:, 8:16], scalar1=1.0), reads=[modT], writes=[modT])
    G = bts[0]
    op("dve", lambda e: e.tensor_scalar_add(out=G[:], in0=G[:], scalar1=1.0), reads=[G], writes=[G])
    lng = k.sb("lng_s", [128, 1024]); lnb = k.sb("lnb_s", [128, 1024])
    op("sp", lambda e: e.dma_start(out=lng[:], in_=lng_d), writes=[lng], dma=True)
    op("sp", lambda e: e.dma_start(out=lnb[:], in_=lnb_d), writes=[lnb], dma=True)
    NTB = T // 128
    NBLK = TC // T
    if NBLK > 1:
        scr13 = nc.dram_tensor(pref + "scr13", [NE * 22 * 128, 2048], BF16).ap()
        scr2 = nc.dram_tensor(pref + "scr2", [NE * 4 * 128, 11 * 512], BF16).ap()
        S13 = [[Buf(scr13[(ex * 22 + j) * 128:(ex * 22 + j + 1) * 128, :]) for j in range(22)] for ex in range(NE)]
        S2 = [[Buf(scr2[(ex * 4 + q4) * 128:(ex * 4 + q4 + 1) * 128, :]) for q4 in range(4)] for ex in range(NE)]
    uT = k.sb("uT_s", [128, 8, T], BF16)
    aT = k.sb("aT", [128, 22, T], BF16)
    yacc = [k.sb(f"yacc{i}", [128, 1024]) for i in range(NTB)]
    w13b = [k.sb(f"w13b{i}", [128, 8, 256], BF16) for i in range(2)]
    w2s = [k.sb(f"w2s{i}", [128, 512]) for i in range(2)]
    w2b = [k.sb(f"w2b{i}", [128, 11, 512], BF16) for i in range(2)]
    qi = [0]
    sgt = [k.sb(f"sgt{i}", [128, 512], BF16) for i in range(2)]
    if NE > 1:
        gts = k.sb("gts", [128, NTB, NE])
    xs_ = [k.sb(f"xs{i}", [128, 1024]) for i in range(2)]
    tmp = [k.sb(f"tmp{i}", [128, 1024]) for i in range(2)]
    xo = [k.sb(f"xo{i}", [128, 1024]) for i in range(2)]
    st12 = [k.sb(f"st12{i}", [128, 12]) for i in range(2)]
    mv = [k.sb(f"mv{i}", [128, 2]) for i in range(2)]
    rstd = [k.sb(f"rstd{i}", [128, 1]) for i in range(2)]
    uTb = [k.sb(f"uTb{i}", [128, 8, 128], BF16) for i in range(2)]
    toks = []
    ph_i = 0
    dq = 0
    for blk in range(TC // T):
        t0 = blk * T
        op("sp", lambda e: e.dma_start(out=uT[:], in_=uT_d[:, t0:t0 + T].rearrange("(c p) t -> p c t", p=128)), writes=[uT], dma=True)
        if NE > 1:
            op("sp", lambda e: e.dma_start(out=gts[:], in_=gates_d[t0:t0 + T, :].rearrange("(n p) g -> p n g", p=128)), writes=[gts], dma=True)
        for ex in range(NE):
            for j in range(22):
                st = stg[j % 2]; wb = w13b[j % 2]
                if blk == 0:
                    op("sp", lambda e: e.dma_start(out=st[:, :, 0:128], in_=w13[ex, :, j * 128:(j + 1) * 128].rearrange("(c p) f -> p c f", p=128)), writes=[st], dma=True)
                    op("act", lambda e: e.dma_start(out=st[:, :, 128:256], in_=w13[ex, :, 2816 + j * 128:2816 + (j + 1) * 128].rearrange("(c p) f -> p c f", p=128)), writes=[st], dma=True)
                    op("pool", lambda e: e.tensor_copy(out=wb[:], in_=st[:]), reads=[st], writes=[wb])
                    if NBLK > 1:
                        op("pool", lambda e: e.dma_start(out=S13[ex][j][:, :], in_=wb[:].rearrange("p a b -> p (a b)")), reads=[wb], writes=[S13[ex][j]], dma=True)
                else:
                    op("sp" if j % 2 == 0 else "act", lambda e: e.dma_start(out=wb[:].rearrange("p a b -> p (a b)"), in_=S13[ex][j][:, :]), reads=[S13[ex][j]], writes=[wb], dma=True)
                for tb in range(T // 512):
                    pg = pH[ph_i % 4]; pu = pH[(ph_i + 1) % 4]; ph_i += 2
                    for kc in range(8):
                        op("pe", lambda e: e.matmul(pg[:, :], lhsT=wb[:, kc, 0:128], rhs=uT[:, kc, tb * 512:(tb + 1) * 512], start=(kc == 0), stop=(kc == 7)),
                           reads=[wb, uT], writes=[pg])
                    for kc in range(8):
                        op("pe", lambda e: e.matmul(pu[:, :], lhsT=wb[:, kc, 128:256], rhs=uT[:, kc, tb * 512:(tb + 1) * 512], start=(kc == 0), stop=(kc == 7)),
                           reads=[wb, uT], writes=[pu])
                    s_ = sgt[tb % 2]
                    op("act", lambda e: e.activation(out=s_[:], in_=pg[:, :], func=AF.Silu), reads=[pg], writes=[s_])
                    op("dve", lambda e: e.tensor_tensor(out=aT[:, j, tb * 512:(tb + 1) * 512], in0=s_[:], in1=pu[:, :], op=ALU.mult), reads=[s_, pu], writes=[aT])
            for half in range(2):
                for kh in range(2):
                    wb2 = w2b[qi[0] % 2]; qi[0] += 1
                    q4 = half * 2 + kh
                    if blk == 0:
                        for jj in range(11):
                            j = kh * 11 + jj
                            st = w2s[jj % 2]
                            op("sp" if jj % 2 == 0 else "act", lambda e: e.dma_start(out=st[:], in_=w2[ex, j * 128:(j + 1) * 128, half * 512:(half + 1) * 512]), writes=[st], dma=True)
                            op("pool", lambda e: e.tensor_copy(out=wb2[:, jj, :], in_=st[:]), reads=[st], writes=[wb2])
                        if NBLK > 1:
                            op("pool", lambda e: e.dma_start(out=S2[ex][q4][:, :], in_=wb2[:].rearrange("p a b -> p (a b)")), reads=[wb2], writes=[S2[ex][q4]], dma=True)
                    else:
                        op("sp" if q4 % 2 == 0 else "act", lambda e: e.dma_start(out=wb2[:].rearrange("p a b -> p (a b)"), in_=S2[ex][q4][:, :]), reads=[S2[ex][q4]], writes=[wb2], dma=True)
                    for tl in range(NTB):
                        p = pY[tl % 2]
                        for jj in range(11):
                            j = kh * 11 + jj
                            op("pe", lambda e: e.matmul(p[:, :], lhsT=aT[:, j, tl * 128:(tl + 1) * 128], rhs=wb2[:, jj, :], start=(jj == 0), stop=(jj == 10)),
                               reads=[aT, wb2], writes=[p])
                        ydst = yacc[tl][:, half * 512:(half + 1) * 512]
                        first = (ex == 0 and kh == 0)
                        if NE == 1:
                            if first:
                                op("act", lambda e: e.copy(out=ydst, in_=p[:, :]), reads=[p], writes=[yacc[tl]])
                            else:
                                op("dve", lambda e: e.tensor_tensor(out=ydst, in0=ydst, in1=p[:, :], op=ALU.add), reads=[p, yacc[tl]], writes=[yacc[tl]])
                        elif first:
                            op("dve", lambda e: e.tensor_scalar(out=ydst, in0=p[:, :], scalar1=gts[:, tl, ex:ex + 1], scalar2=None, op0=ALU.mult),
                               reads=[p, gts], writes=[yacc[tl]])
                        else:
                            op("dve", lambda e: e.scalar_tensor_tensor(out=ydst, in0=p[:, :], scalar=gts[:, tl, ex:ex + 1], in1=ydst, op0=ALU.mult, op1=ALU.add),
                               reads=[p, gts, yacc[tl]], writes=[yacc[tl]])
        for tl in range(NTB):
            i2 = tl % 2
            tsl = slice(t0 + tl * 128, t0 + (tl + 1) * 128)
            op("act", lambda e: e.dma_start(out=xs_[i2][:], in_=xmid[tsl, :]), writes=[xs_[i2]], dma=True)
            res_ln(k, c, yacc[tl][:], [yacc[tl]], xs_[i2], G, lng, lnb, tmp[i2], st12[i2], mv[i2], rstd[i2], xo[i2])
            toks.append(op("pool", lambda e: e.dma_start(out=xout[tsl, :], in_=xo[i2][:]), reads=[xo[i2]], dma=True))
            if emit_u:
                mod_transpose(k, c, xo[i2], modT, 0, pT, None, uTb[i2])
                toks.append(op("pool", lambda e: e.dma_start(out=uT_o[:, tsl].rearrange("(c p) t -> p c t", p=128), in_=uTb[i2][:]), reads=[uTb[i2]], dma=True))
    if fz is None or fz.final:
        k.finish(toks)
    if fz is not None:
        k.end_stage()
    print("ffn instructions", k.n_ins)
    return nc


def bc(v, n=128):
    return np.ascontiguousarray(np.tile(np.asarray(v, np.float32)[None, :], (n, 1)))


def fT(v):
    v = np.asarray(v, np.float32)
    return np.ascontiguousarray(v.reshape(-1, 128).T)


I32 = mybir.dt.int32


def build_s5(S, fz=None, blend=False):
    if fz is None:
        nc = bass.Bass("TRN2", target_bir_lowering=False); k = Ctx(nc); pref = ""
    else:
        nc, k, pref = fz.nc, fz.k, fz.pref
    D = mkD(nc, fz, pref)
    k.begin_stage(pref)
    TC = S // 2
    uT_d = D("uT", [2 * 1024, TC] if blend else [512, S], BF16)
    if blend:
        msk_d = D("msk", [128, 2])
    lamS = D("lamS", [128, 3, 16])
    lamB = D("lamB", [4, 128, 5, 64])
    cC = D("cC", [16, 128, 2, 16])
    dT = D("dT", [128, 4])
    out = D("gT", [512, S], BF16, kind="ExternalOutput")
    op = k.op
    NB = S // 512

    onesf = k.sb("onesf", [128, 128])
    op("pool", lambda e: e.memset(onesf[:], 1.0), writes=[onesf])
    ls = k.sb("ls", [128, 3, 16]); dsk = k.sb("dsk", [128, 4])
    op("sp", lambda e: e.dma_start(out=ls[:], in_=lamS), writes=[ls], dma=True)
    op("sp", lambda e: e.dma_start(out=dsk[:], in_=dT), writes=[dsk], dma=True)
    dtS = k.sb("dtS", [128, 16]); thS = k.sb("thS", [128, 16]); rS = k.sb("rS", [128, 16])
    op("act", lambda e: e.activation(out=dtS[:], in_=ls[:, 2, :], func=AF.Exp), reads=[ls], writes=[dtS])
    op("dve", lambda e: e.tensor_tensor(out=thS[:], in0=ls[:, 1, :], in1=dtS[:], op=ALU.mult), reads=[ls, dtS], writes=[thS])
    op("dve", lambda e: e.tensor_tensor(out=rS[:], in0=ls[:, 0, :], in1=dtS[:], op=ALU.mult), reads=[ls, dtS], writes=[rS])
    op("act", lambda e: e.activation(out=rS[:], in_=rS[:], func=AF.Exp), reads=[rS], writes=[rS])
    ioi = k.sb("ioi", [128, 128], I32); io = k.sb("io", [128, 128])
    op("pool", lambda e: e.iota(ioi[:], pattern=[[1, 128]], base=1, channel_multiplier=0), writes=[ioi])
    op("dve", lambda e: e.tensor_copy(out=io[:], in_=ioi[:]), reads=[ioi], writes=[io])
    ang = k.sb("ang", [128, 16 * 128]); st = k.sb("st", [128, 16 * 128]); ct = k.sb("ct", [128, 16 * 128])
    rtab = k.sb("rtab", [128, 16, 128])
    for q in range(16):
        op("dve", lambda e: e.tensor_scalar(out=ang[:, q * 128:(q + 1) * 128], in0=io[:], scalar1=thS[:, q:q + 1], scalar2=None, op0=ALU.mult),
           reads=[io, thS], writes=[ang])
        op("pool", lambda e: e.tensor_scalar(out=rtab[:, q, :], in0=onesf[:], scalar1=rS[:, q:q + 1], scalar2=None, op0=ALU.mult),
           reads=[onesf, rS], writes=[rtab])
    sincos(k, ang, 16 * 128, st, ct, "scS")
    rm = k.sb("rm", [128, 8])
    op("pool", lambda e: e.memset(rm[:], 1.0), writes=[rm])
    op("pool", lambda e: e.affine_select(out=rm[:], in_=rm[:], pattern=[[-16, 8]], compare_op=ALU.is_ge, fill=0.0, base=0, channel_multiplier=1),
       reads=[rm], writes=[rm])
    op("pool", lambda e: e.affine_select(out=rm[:], in_=rm[:], pattern=[[16, 8]], compare_op=ALU.is_ge, fill=0.0, base=15, channel_multiplier=-1),
       reads=[rm], writes=[rm])
    BtR = [k.sb(f"BtR{q}", [128, 2, 64], BF16) for q in range(16)]
    BtI = [k.sb(f"BtI{q}", [128, 2, 64], BF16) for q in range(16)]
    lb = k.sb("lb", [128, 5, 64])
    W = lambda n: k.sb(n, [128, 64])
    dtB, lrdt, lidt, mag, sB, cB, ar, ai, den, t1, t2, kr, ki, bbr, bbi = [W(n) for n in
        ("dtB", "lrdt", "lidt", "mag", "sB", "cB", "ar", "ai", "den", "t1", "t2", "kr", "ki", "bbr", "bbi")]
    TT = lambda o, a, b, o_: op("dve", lambda e: e.tensor_tensor(out=o[:], in0=a, in1=b, op=o_), reads=[lb, dtB, lrdt, lidt, mag, sB, cB, ar, ai, den, t1, t2, kr, ki], writes=[o])
    for cc in range(4):
        op("sp", lambda e: e.dma_start(out=lb[:], in_=lamB[cc]), writes=[lb], dma=True)
        op("act", lambda e: e.activation(out=dtB[:], in_=lb[:, 2, :], func=AF.Exp), reads=[lb], writes=[dtB])
        TT(lrdt, lb[:, 0, :], dtB[:], ALU.mult)
        TT(lidt, lb[:, 1, :], dtB[:], ALU.mult)
        op("act", lambda e: e.activation(out=mag[:], in_=lrdt[:], func=AF.Exp), reads=[lrdt], writes=[mag])
        sincos(k, lidt, 64, sB, cB, f"scB{cc}")
        TT(ar, mag[:], cB[:], ALU.mult)
        TT(ai, mag[:], sB[:], ALU.mult)
        op("dve", lambda e: e.tensor_scalar_add(out=ar[:], in0=ar[:], scalar1=-1.0), reads=[ar], writes=[ar])
        TT(den, lb[:, 0, :], lb[:, 0, :], ALU.mult)
        TT(t1, lb[:, 1, :], lb[:, 1, :], ALU.mult)
        TT(den, den[:], t1[:], ALU.add)
        op("dve", lambda e: e.reciprocal(out=den[:], in_=den[:]), reads=[den], writes=[den])
        TT(t1, ar[:], lb[:, 0, :], ALU.mult)
        TT(t2, ai[:], lb[:, 1, :], ALU.mult)
        TT(kr, t1[:], t2[:], ALU.add)
        TT(kr, kr[:], den[:], ALU.mult)
        TT(t1, ai[:], lb[:, 0, :], ALU.mult)
        TT(t2, ar[:], lb[:, 1, :], ALU.mult)
        TT(ki, t1[:], t2[:], ALU.subtract)
        TT(ki, ki[:], den[:], ALU.mult)
        TT(t1, kr[:], lb[:, 3, :], ALU.mult)
        TT(t2, ki[:], lb[:, 4, :], ALU.mult)
        op("dve", lambda e: e.tensor_tensor(out=bbr[:], in0=t1[:], in1=t2[:], op=ALU.subtract), reads=[t1, t2], writes=[bbr])
        TT(t1, kr[:], lb[:, 4, :], ALU.mult)
        TT(t2, ki[:], lb[:, 3, :], ALU.mult)
        op("dve", lambda e: e.tensor_tensor(out=bbi[:], in0=t1[:], in1=t2[:], op=ALU.add), reads=[t1, t2], writes=[bbi])
        for ql in range(4):
            q = cc * 4 + ql
            for g2 in range(2):
                op("dve", lambda e: e.tensor_scalar(out=BtR[q][:, g2, :], in0=bbr[:], scalar1=rm[:, 2 * ql + g2:2 * ql + g2 + 1], scalar2=None, op0=ALU.mult),
                   reads=[bbr, rm], writes=[BtR[q]])
                op("dve", lambda e: e.tensor_scalar(out=BtI[q][:, g2, :], in0=bbi[:], scalar1=rm[:, 2 * ql + g2:2 * ql + g2 + 1], scalar2=None, op0=ALU.mult),
                   reads=[bbi, rm], writes=[BtI[q]])
    cst = k.sb("cst", [128, 16, 2, 16])
    op("sp", lambda e: e.dma_start(out=cst[:], in_=cC.rearrange("q s r h -> s q r h")), writes=[cst], dma=True)
    CtR = [k.sb(f"CtR{q}", [128, 128], BF16) for q in range(16)]
    CtI = [k.sb(f"CtI{q}", [128, 128], BF16) for q in range(16)]
    for q in range(16):
        ql = q % 4
        op("pool", lambda e: e.memset(CtR[q][:], 0.0), writes=[CtR[q]])
        op("pool", lambda e: e.memset(CtI[q][:], 0.0), writes=[CtI[q]])
        for g2 in range(2):
            ps_ = slice(64 * g2, 64 * g2 + 64)
            cs_ = slice((2 * ql + g2) * 16, (2 * ql + g2) * 16 + 16)
            op("dve", lambda e: e.tensor_copy(out=CtR[q][ps_, cs_], in_=cst[ps_, q, 0, :]), reads=[cst], writes=[CtR[q]])
            op("dve", lambda e: e.tensor_scalar(out=CtI[q][ps_, cs_], in0=cst[ps_, q, 1, :], scalar1=-1.0, scalar2=None, op0=ALU.mult), reads=[cst], writes=[CtI[q]])
    uT = k.sb("uT_s", [128, 4, S], BF16)
    if not blend:
        for cc in range(4):
            op("sp" if cc % 2 == 0 else "act", lambda e: e.dma_start(out=uT[:, cc, :], in_=uT_d[cc * 128:(cc + 1) * 128, :]), writes=[uT], dma=True)
    else:
        msk = k.sb("msk_s", [128, 2])
        op("sp", lambda e: e.dma_start(out=msk[:], in_=msk_d), writes=[msk], dma=True)
        CW = TC // 4
        cand = [[k.sb(f"cand{i}_{j}", [128, CW], BF16) for j in range(2)] for i in range(2)]
        n_ = 0
        for i in range(2):
            for cc in range(4):
                for qq in range(4):
                    cd = cand[n_ % 2]; n_ += 1
                    for j in range(2):
                        r0 = i * 1024 + j * 512 + cc * 128
                        op("sp" if j == 0 else "act", lambda e: e.dma_start(out=cd[j][:], in_=uT_d[r0:r0 + 128, qq * CW:(qq + 1) * CW]), writes=[cd[j]], dma=True)
                    op("pool", lambda e: e.tensor_scalar(out=cd[0][:], in0=cd[0][:], scalar1=msk[:, 0:1], scalar2=None, op0=ALU.mult), reads=[cd[0], msk], writes=[cd[0]])
                    op("dve", lambda e: e.scalar_tensor_tensor(out=uT[:, cc, i * TC + qq * CW:i * TC + (qq + 1) * CW], in0=cd[1][:], scalar=msk[:, 1:2], in1=cd[0][:], op0=ALU.mult, op1=ALU.add),
                       reads=[cd[0], cd[1], msk], writes=[uT])
    pBr = [k.ps(f"pBr{i}", [128, 512]) for i in range(2)]
    pBi = [k.ps(f"pBi{i}", [128, 512]) for i in range(2)]
    pY = [k.ps(f"pY{i}", [128, 512]) for i in range(2)]
    bur = [k.sb(f"bur{i}", [128, 512]) for i in range(2)]; bui = [k.sb(f"bui{i}", [128, 512]) for i in range(2)]
    bpr = [k.sb(f"bpr{i}", [128, 512]) for i in range(2)]; bpi = [k.sb(f"bpi{i}", [128, 512]) for i in range(2)]
    vr = [k.sb(f"vr{i}", [128, 512]) for i in range(2)]; vi = [k.sb(f"vi{i}", [128, 512]) for i in range(2)]
    pt1 = k.sb("pt1", [128, 512]); pt2 = k.sb("pt2", [128, 512])
    dt1 = k.sb("dt1", [128, 512]); dt2 = k.sb("dt2", [128, 512])
    xr = [k.sb(f"xr{i}", [128, 512], BF16) for i in range(4)]; xi = [k.sb(f"xi{i}", [128, 512], BF16) for i in range(4)]
    car = [k.sb(f"car{q}", [128, 2]) for q in range(16)]
    cat = [k.sb(f"cat{i}", [128, 2]) for i in range(2)]
    for q in range(16):
        op("pool", lambda e: e.memset(car[q][:], 0.0), writes=[car[q]])
    yt = [k.sb(f"yt{i}", [128, 512]) for i in range(2)]; y2 = [k.sb(f"y2{i}", [128, 512]) for i in range(2)]
    go = [k.sb(f"go{i}", [128, 512], BF16) for i in range(2)]
    toks = []
    it = 0
    for blk in range(NB):
        tsl = slice(blk * 512, (blk + 1) * 512)
        for cc in range(4):
            for ql in range(4):
                q = cc * 4 + ql
                i2 = it % 2; it += 1
                c3 = lambda tab: tab[:, q * 128:(q + 1) * 128][:, None, :].to_broadcast([128, 4, 128]) if False else None
                op("pe", lambda e: e.matmul(pBr[i2][:, :], lhsT=BtR[q][:].rearrange("p a b -> p (a b)"), rhs=uT[:, cc, tsl], start=True, stop=True), reads=[BtR[q], uT], writes=[pBr[i2]])
                op("pe", lambda e: e.matmul(pBi[i2][:, :], lhsT=BtI[q][:].rearrange("p a b -> p (a b)"), rhs=uT[:, cc, tsl], start=True, stop=True), reads=[BtI[q], uT], writes=[pBi[i2]])
                op("act", lambda e: e.copy(out=bur[i2][:], in_=pBr[i2][:, :]), reads=[pBr[i2]], writes=[bur[i2]])
                op("act", lambda e: e.copy(out=bui[i2][:], in_=pBi[i2][:, :]), reads=[pBi[i2]], writes=[bui[i2]])
                ctq = ct[:, q * 128:(q + 1) * 128]; stq = st[:, q * 128:(q + 1) * 128]
                ct3 = ctq.rearrange("p (a b) -> p a b", a=1).to_broadcast([128, 4, 128])
                st3 = stq.rearrange("p (a b) -> p a b", a=1).to_broadcast([128, 4, 128])
                v3 = lambda buf: buf[:].rearrange("p (a b) -> p a b", a=4)
                op("pool", lambda e: e.tensor_tensor(out=v3(pt1), in0=v3(bur[i2]), in1=ct3, op=ALU.mult), reads=[bur[i2], ct], writes=[pt1])
                op("pool", lambda e: e.tensor_tensor(out=v3(pt2), in0=v3(bui[i2]), in1=st3, op=ALU.mult), reads=[bui[i2], st], writes=[pt2])
                op("pool", lambda e: e.tensor_tensor(out=bpr[i2][:], in0=pt1[:], in1=pt2[:], op=ALU.add), reads=[pt1, pt2], writes=[bpr[i2]])
                op("dve", lambda e: e.tensor_tensor(out=v3(dt1), in0=v3(bui[i2]), in1=ct3, op=ALU.mult), reads=[bui[i2], ct], writes=[dt1])
                op("dve", lambda e: e.tensor_tensor(out=v3(dt2), in0=v3(bur[i2]), in1=st3, op=ALU.mult), reads=[bur[i2], st], writes=[dt2])
                op("dve", lambda e: e.tensor_tensor(out=bpi[i2][:], in0=dt1[:], in1=dt2[:], op=ALU.subtract), reads=[dt1, dt2], writes=[bpi[i2]])
                for c4 in range(4):
                    cs = slice(c4 * 128, (c4 + 1) * 128)
                    op("dve", lambda e: e.tensor_tensor_scan(out=vr[i2][:, cs], data0=rtab[:, q, :], data1=bpr[i2][:, cs], initial=car[q][:, 0:1], op0=ALU.mult, op1=ALU.add),
                       reads=[rtab, bpr[i2], car[q]], writes=[vr[i2]])
                    op("dve", lambda e: e.tensor_tensor_scan(out=vi[i2][:, cs], data0=rtab[:, q, :], data1=bpi[i2][:, cs], initial=car[q][:, 1:2], op0=ALU.mult, op1=ALU.add),
                       reads=[rtab, bpi[i2], car[q]], writes=[vi[i2]])
                    l = c4 * 128 + 127
                    c128 = ct[:, q * 128 + 127:q * 128 + 128]; s128 = st[:, q * 128 + 127:q * 128 + 128]
                    ca = cat[c4 % 2]
                    op("dve", lambda e: e.tensor_scalar(out=ca[:, 0:1], in0=vi[i2][:, l:l + 1], scalar1=s128, scalar2=None, op0=ALU.mult), reads=[vi[i2], st], writes=[ca])
                    op("dve", lambda e: e.tensor_scalar(out=ca[:, 1:2], in0=vi[i2][:, l:l + 1], scalar1=c128, scalar2=None, op0=ALU.mult), reads=[vi[i2], ct], writes=[ca])
                    op("dve", lambda e: e.scalar_tensor_tensor(out=car[q][:, 0:1], in0=vr[i2][:, l:l + 1], scalar=c128, in1=ca[:, 0:1], op0=ALU.mult, op1=ALU.subtract),
                       reads=[vr[i2], ct, ca], writes=[car[q]])
                    op("dve", lambda e: e.scalar_tensor_tensor(out=car[q][:, 1:2], in0=vr[i2][:, l:l + 1], scalar=s128, in1=ca[:, 1:2], op0=ALU.mult, op1=ALU.add),
                       reads=[vr[i2], st, ca], writes=[car[q]])
                op("dve", lambda e: e.tensor_tensor(out=v3(dt1), in0=v3(vr[i2]), in1=ct3, op=ALU.mult), reads=[vr[i2], ct], writes=[dt1])
                op("dve", lambda e: e.tensor_tensor(out=v3(dt2), in0=v3(vi[i2]), in1=st3, op=ALU.mult), reads=[vi[i2], st], writes=[dt2])
                op("dve", lambda e: e.tensor_tensor(out=xr[ql][:], in0=dt1[:], in1=dt2[:], op=ALU.subtract), reads=[dt1, dt2], writes=[xr[ql]])
                op("dve", lambda e: e.tensor_tensor(out=v3(dt1), in0=v3(vr[i2]), in1=st3, op=ALU.mult), reads=[vr[i2], st], writes=[dt1])
                op("dve", lambda e: e.tensor_tensor(out=v3(dt2), in0=v3(vi[i2]), in1=ct3, op=ALU.mult), reads=[vi[i2], ct], writes=[dt2])
                op("dve", lambda e: e.tensor_tensor(out=xi[ql][:], in0=dt1[:], in1=dt2[:], op=ALU.add), reads=[dt1, dt2], writes=[xi[ql]])
            j2 = (blk * 4 + cc) % 2
            p = pY[j2]
            for ql in range(4):
                q = cc * 4 + ql
                op("pe", lambda e: e.matmul(p[:, :], lhsT=CtR[q][:], rhs=xr[ql][:], start=(ql == 0), stop=False), reads=[CtR[q], xr[ql]], writes=[p])
                op("pe", lambda e: e.matmul(p[:, :], lhsT=CtI[q][:], rhs=xi[ql][:], start=False, stop=(ql == 3)), reads=[CtI[q], xi[ql]], writes=[p])
            y = yt[j2]; z = y2[j2]
            op("dve", lambda e: e.scalar_tensor_tensor(out=y[:], in0=uT[:, cc, tsl], scalar=dsk[:, cc:cc + 1], in1=p[:, :], op0=ALU.mult, op1=ALU.add),
               reads=[uT, dsk, p], writes=[y])
            op("pool", lambda e: e.tensor_tensor(out=z[:], in0=y[:], in1=y[:], op=ALU.mult), reads=[y], writes=[z])
            op("pool", lambda e: e.tensor_scalar(out=z[:], in0=z[:], scalar1=0.044715, scalar2=1.0, op0=ALU.mult, op1=ALU.add), reads=[z], writes=[z])
            op("pool", lambda e: e.tensor_tensor(out=z[:], in0=z[:], in1=y[:], op=ALU.mult), reads=[z, y], writes=[z])
            op("act", lambda e: e.activation(out=z[:], in_=z[:], func=AF.Sigmoid, scale=2.0 * math.sqrt(2.0 / math.pi)), reads=[z], writes=[z])
            op("pool", lambda e: e.tensor_tensor(out=go[j2][:], in0=z[:], in1=y[:], op=ALU.mult), reads=[z, y], writes=[go[j2]])
            toks.append(op("sp", lambda e: e.dma_start(out=out[cc * 128:(cc + 1) * 128, tsl], in_=go[j2][:]), reads=[go[j2]], dma=True))
    if fz is None:
        k.finish(toks)
    else:
        k.end_stage()
    print("s5 instructions", k.n_ins)
    return nc


def s5_inputs(inp, uT_full_b, half, S):
    f = np.float32
    g0 = half * 32
    lr = inp["s5_lambda_re"][0][g0:g0 + 32]; li = inp["s5_lambda_im"][0][g0:g0 + 32]; ld = inp["s5_log_dt"][0][g0:g0 + 32]
    ldx = np.repeat(ld[:, None], 64, axis=1)
    toS = lambda a: a.reshape(16, 2, 64).transpose(1, 2, 0).reshape(128, 16)
    lamS = np.stack([toS(lr), toS(li), toS(ldx)], axis=1)
    br = inp["s5_b_re"][0][g0:g0 + 32]; bi = inp["s5_b_im"][0][g0:g0 + 32]
    rep = lambda a: np.repeat(a[:, None, :], 16, axis=1)
    toB = lambda a: a.reshape(4, 8 * 16, 64)
    lamB = np.stack([toB(rep(lr)), toB(rep(li)), toB(rep(ldx)), toB(br.transpose(0, 2, 1)), toB(bi.transpose(0, 2, 1))], axis=2)
    cr = inp["s5_c_re"][0][g0:g0 + 32]; ci = inp["s5_c_im"][0][g0:g0 + 32]
    toC = lambda a: a.reshape(16, 2, 16, 64).transpose(0, 1, 3, 2).reshape(16, 128, 16)
    cC = np.stack([toC(cr), toC(ci)], axis=2)
    dT = inp["s5_d"][0][half * 512:(half + 1) * 512].reshape(4, 128).T
    return {"uT": np.ascontiguousarray(uT_full_b[half * 512:(half + 1) * 512, :S]),
            "lamS": np.ascontiguousarray(lamS.astype(f)), "lamB": np.ascontiguousarray(lamB.astype(f)),
            "cC": np.ascontiguousarray(cC.astype(f)), "dT": np.ascontiguousarray(dT.astype(f))}


import ml_dtypes as _mld
import os

_CACHE = {}


def build_fused(S, NB_):
    TC = S // 2
    nc = bass.Bass("TRN2", target_bir_lowering=False)
    k = Ctx(nc)
    fz = Fz(nc, k)
    fz.final = False
    I = lambda n, s, dt: nc.dram_tensor(n, list(s), dt).ap()

    def chunked(name, R_, Cn, W):
        W = min(W, Cn)
        src = [I(f"{name}_s{j}", [R_, W], BF16) for j in range(Cn // W)]
        dst = [I(f"{name}_a{j}", [2 * R_, W], BF16) for j in range(Cn // W)]
        return src, dst, ChunkedAP(src, W), ChunkedAP(dst, W)
    hg_s, hg_d, hg_src, hg_all = chunked("i_hg", 1024, S, 1024)
    u1_s, u1_d, u1_src, u1_all = chunked("i_u1", 1024, TC, 1024)
    g_s, g_d, g_src, g_all = chunked("i_g", 512, S, 2048)
    xmid1 = I("i_xmid1", [TC, 1024], F32); uT1 = I("i_uT1", [1024, TC], BF16)
    x1 = I("i_x1", [TC, 1024], F32)
    xmid2 = I("i_xmid2", [TC, 1024], F32); uT2 = I("i_uT2", [1024, TC], BF16); gates = I("i_gates", [TC, 8], F32)
    groups = [[2 * i, 2 * i + 1] for i in range(NB_)]
    fz.pref = "a_"; fz.ext = {"hgT": hg_src}
    build_m0(S, fz=fz)
    k.collective("AllGather", hg_s, hg_d, groups)
    fz.pref = "b_"; fz.ext = {"inT": hg_all, "xmid": xmid1, "uT": uT1}
    build_proj(TC, 16, False, False, fz=fz, blend=True)
    fz.pref = "c_"; fz.ext = {"uT": uT1, "xmid": xmid1, "xout": x1, "uTn": u1_src}
    build_ffn(TC, 1, min(512, TC), True, fz=fz)
    k.collective("AllGather", u1_s, u1_d, groups)
    fz.pref = "d_"; fz.ext = {"uT": u1_all, "gT": g_src}
    build_s5(S, fz=fz, blend=True)
    k.collective("AllGather", g_s, g_d, groups)
    fz.pref = "e_"; fz.ext = {"inT": g_all, "x": x1, "xmid": xmid2, "uT": uT2, "gates": gates}
    build_proj(TC, 8, True, True, fz=fz, blend=True)
    fz.pref = "f_"; fz.ext = {"uT": uT2, "xmid": xmid2, "gates": gates}; fz.final = True
    build_ffn(TC, 8, min(int(os.environ.get('T2', 1024)), TC), False, fz=fz)
    print("fused instructions", k.n_ins)
    return nc


def kernel_impl(inp, S, NB_=4):
    inp = {k_: np.asarray(v) for k_, v in inp.items()}
    NCO = 2 * NB_
    TC = S // 2
    f32 = np.float32
    cT = lambda b: np.ascontiguousarray(inp["c"][b].reshape(8, 128).T.astype(f32))
    aw = inp["ada_w"]; ab = inp["ada_b"]
    C = lambda a: np.ascontiguousarray(a)
    key = ("fused", S, NB_)
    if key not in _CACHE:
        _CACHE[key] = build_fused(S, NB_)
    nc = _CACHE[key]
    shared = {}
    shared["b_W"] = C(inp["mlstm_w_out"][0])
    shared["b_adaw_f"] = C(aw[0][:, 3072:5120]); shared["b_adab_f"] = fT(ab[0][3072:5120])
    shared["b_adaw_b"] = C(aw[0][:, 2048:3072]); shared["b_adab_b"] = bc(ab[0][2048:3072])
    shared["b_lng"] = bc(inp["ln_mix_g"][0]); shared["b_lnb"] = bc(inp["ln_mix_b"][0])
    shared["c_w13"] = C(inp["ffn_w13"]); shared["c_w2"] = C(inp["ffn_w2"])
    shared["c_adaw_b"] = C(aw[0][:, 5120:6144]); shared["c_adab_b"] = bc(ab[0][5120:6144])
    shared["c_lng"] = bc(inp["ln_ffn_g"][0]); shared["c_lnb"] = bc(inp["ln_ffn_b"][0])
    shared["c_adaw_f"] = C(aw[1][:, 0:2048]); shared["c_adab_f"] = fT(ab[1][0:2048])
    shared["e_W"] = C(inp["s5_w_glu"][0])
    shared["e_adaw_f"] = C(aw[1][:, 3072:5120]); shared["e_adab_f"] = fT(ab[1][3072:5120])
    shared["e_adaw_b"] = C(aw[1][:, 2048:3072]); shared["e_adab_b"] = bc(ab[1][2048:3072])
    shared["e_lng"] = bc(inp["ln_mix_g"][1]); shared["e_lnb"] = bc(inp["ln_mix_b"][1])
    shared["e_bglu"] = bc(inp["s5_b_glu"][0])
    shared["e_wr"] = C(inp["moe_router"][0].reshape(8, 128, 8).transpose(1, 0, 2).astype(f32))
    shared["f_w13"] = C(inp["moe_w13"][0]); shared["f_w2"] = C(inp["moe_w2"][0])
    shared["f_adaw_b"] = C(aw[1][:, 5120:6144]); shared["f_adab_b"] = bc(ab[1][5120:6144])
    shared["f_lng"] = bc(inp["ln_ffn_g"][1]); shared["f_lnb"] = bc(inp["ln_ffn_b"][1])
    dummy_u = np.zeros((1024, S), _mld.bfloat16)
    maps = []
    for c in range(NCO):
        b, h = c // 2, c % 2
        tk = slice(h * TC, (h + 1) * TC)
        m = dict(shared)
        for k_, v in m0_inputs(inp, b, h, S).items():
            m["a_" + k_] = v
        msk = np.zeros((128, 2), f32); msk[:, h] = 1.0
        m["b_msk"] = msk; m["d_msk"] = msk; m["e_msk"] = msk
        m["b_x"] = C(inp["x"][b, :S][tk])
        for p_ in "bcef":
            m[p_ + "_condT"] = cT(b)
        for k_, v in s5_inputs(inp, dummy_u, h, S).items():
            if k_ != "uT":
                m["d_" + k_] = v
        maps.append(m)
    res = run_bass_kernel_spmd(nc, maps, core_ids=list(range(NCO)))
    out = np.zeros((NB_, S, 1024), f32)
    for c in range(NCO):
        b, h = c // 2, c % 2
        out[b, h * TC:(h + 1) * TC] = np.asarray(res.results[c]["f_xout"])
    return out


def kernel(**inputs):
    return kernel_impl(inputs, 8192, 4)
```

```python
import math
import numpy as np
from contextlib import ExitStack
import concourse.bass as bass
import concourse.mybir as mybir
from concourse.bass_utils import run_bass_kernel_spmd

F32 = mybir.dt.float32
BF16 = mybir.dt.bfloat16
AF = mybir.ActivationFunctionType
ALU = mybir.AluOpType
AX = mybir.AxisListType

NPOOL = 12
NOSYNC_SAME = ("pe",)


class Buf:
    __slots__ = ("ap", "last_w", "reads")

    def __init__(self, ap):
        self.ap = ap
        self.last_w = None
        self.reads = {}

    def __getitem__(self, idx):
        return self.ap[idx]


class Ctx:
    def __init__(self, nc):
        self.nc = nc
        self.engs = {"pe": nc.tensor, "act": nc.scalar, "dve": nc.vector,
                     "pool": nc.gpsimd, "sp": nc.sync}
        self.sem = {}
        self.cnt = {}
        for n in ("pe", "act", "dve", "pool"):
            self.sem[n] = nc.alloc_semaphore("s_" + n)
            self.cnt[n] = 0
        self.dma_pool = {}
        self.dma_i = {}
        self.known = {n: {} for n in ("pe", "act", "dve", "pool", "sp")}
        self.out_tokens = []
        self.n_ins = 0
        self.stack = None
        self.pref = ""
        self.dma_last = {}
        self.cc_toks = []

    def begin_stage(self, pref=""):
        self.stack = ExitStack()
        self.pref = pref

    def barrier(self):
        toks = [(n, self.sem[n], self.cnt[n]) for n in ("pe", "act", "dve", "pool") if self.cnt[n] > 0]
        for qn, d in self.dma_last.items():
            for i, v in d.items():
                toks.append(("dma_" + qn, self.dma_pool[qn][i], v))
        toks.extend(self.cc_toks)
        for en in ("pe", "act", "dve", "pool", "sp"):
            for tok in toks:
                self._wait(en, tok)

    def end_stage(self):
        self.barrier()
        self.stack.close()
        self.stack = None

    def collective(self, kind, srcs, dsts, groups):
        self.barrier()
        for src_ap, dst_ap in zip(srcs, dsts):
            sem = self.nc.alloc_semaphore("cc_%d" % len(self.cc_toks))
            ins = self.nc.gpsimd.collective_compute(kind, ALU.bypass, replica_groups=groups, ins=[src_ap.opt()], outs=[dst_ap.opt()])
            ins.then_inc(sem)
            self.cc_toks.append(("cc", sem, 1))
        self.barrier()

    def sb(self, name, shape, dtype=F32):
        if self.stack is not None:
            t = self.stack.enter_context(self.nc.sbuf_tensor(self.pref + name, list(shape), dtype))
        else:
            t = self.nc.alloc_sbuf_tensor(name, list(shape), dtype)
        return Buf(t.ap() if hasattr(t, "ap") else t)

    def ps(self, name, shape, dtype=F32):
        if self.stack is not None:
            t = self.stack.enter_context(self.nc.psum_tensor(self.pref + name, list(shape), dtype))
        else:
            t = self.nc.alloc_psum_tensor(name, list(shape), dtype)
        return Buf(t.ap() if hasattr(t, "ap") else t)

    def _wait(self, en, tok):
        src, sem, val = tok
        k = self.known[en]
        if k.get(sem.num, 0) >= val:
            return
        self.engs[en].wait_ge(sem, val)
        k[sem.num] = val

    def op(self, en, fn, reads=(), writes=(), dma=False, q=None):
        deps = []
        for b in reads:
            if b.last_w is not None:
                deps.append(b.last_w)
        for b in writes:
            if b.last_w is not None:
                deps.append(b.last_w)
            deps.extend(b.reads.values())
        for tok in deps:
            src = tok[0]
            if src == en and en in NOSYNC_SAME and not dma:
                continue
            self._wait(en, tok)
        e = self.engs[en]
        if dma:
            qn = en
            if qn not in self.dma_pool:
                self.dma_pool[qn] = [self.nc.alloc_semaphore(f"d_{qn}_{i}") for i in range(NPOOL)]
                self.dma_i[qn] = 0
            i = self.dma_i[qn]
            self.dma_i[qn] = i + 1
            sem = self.dma_pool[qn][i % NPOOL]
            rnd = i // NPOOL
            if rnd > 0:
                self._wait(en, ("dma_" + qn, sem, 16 * rnd))
            ins = fn(e)
            ins.then_inc(sem, 16)
            tok = ("dma_" + qn, sem, 16 * (rnd + 1))
            self.dma_last.setdefault(qn, {})[i % NPOOL] = 16 * (rnd + 1)
        else:
            ins = fn(e)
            self.cnt[en] += 1
            ins.then_inc(self.sem[en], 1)
            tok = (en, self.sem[en], self.cnt[en])
        self.n_ins += 1
        for b in reads:
            b.reads[tok[1].num] = tok
        for b in writes:
            b.last_w = tok
            b.reads = {}
        return tok

    def finish(self, toks):
        for tok in toks:
            self._wait("sp", tok)


class Fz:
    def __init__(self, nc, k):
        self.nc = nc; self.k = k; self.ext = {}; self.pref = ""


def mkD(nc, fz, pref):
    def D(n, s, dt=F32, kind="ExternalInput"):
        if fz is not None and n in fz.ext:
            return fz.ext[n]
        return nc.dram_tensor(pref + n, list(s), dt, kind=kind).ap()
    return D


class ChunkedAP:
    def __init__(self, aps, W):
        self.aps = aps; self.W = W

    def __getitem__(self, idx):
        rs, cs = idx
        j = cs.start // self.W
        assert (cs.stop - 1) // self.W == j
        return self.aps[j][rs, cs.start - j * self.W: cs.stop - j * self.W]

I32 = mybir.dt.int32
def sincos(k, a, N, outs, outc, pref="sc"):
    op = k.op
    t = k.sb(pref + "_t", [128, N]); ti = k.sb(pref + "_i", [128, N], I32); m = k.sb(pref + "_m", [128, N])
    for dst, sh in ((outs, 0.0), (outc, 0.5 * math.pi)):
        op("dve", lambda e: e.tensor_scalar(out=t[:], in0=a[:], scalar1=sh, scalar2=1.0 / (2 * math.pi), op0=ALU.add, op1=ALU.mult), reads=[a], writes=[t])
        op("dve", lambda e: e.tensor_copy(out=ti[:], in_=t[:]), reads=[t], writes=[ti])
        op("dve", lambda e: e.tensor_copy(out=m[:], in_=ti[:]), reads=[ti], writes=[m])
        op("dve", lambda e: e.tensor_scalar(out=t[:], in0=a[:], scalar1=sh, scalar2=None, op0=ALU.add), reads=[a], writes=[t])
        op("dve", lambda e: e.scalar_tensor_tensor(out=t[:], in0=m[:], scalar=-2 * math.pi, in1=t[:], op0=ALU.mult, op1=ALU.add), reads=[m, t], writes=[t])
        op("dve", lambda e: e.tensor_scalar(out=m[:], in0=t[:], scalar1=math.pi, scalar2=-2 * math.pi, op0=ALU.is_gt, op1=ALU.mult), reads=[t], writes=[m])
        op("dve", lambda e: e.tensor_tensor(out=t[:], in0=t[:], in1=m[:], op=ALU.add), reads=[t, m], writes=[t])
        op("dve", lambda e: e.tensor_scalar(out=m[:], in0=t[:], scalar1=-math.pi, scalar2=2 * math.pi, op0=ALU.is_lt, op1=ALU.mult), reads=[t], writes=[m])
        op("dve", lambda e: e.tensor_tensor(out=t[:], in0=t[:], in1=m[:], op=ALU.add), reads=[t, m], writes=[t])
        op("dve", lambda e: e.tensor_scalar(out=t[:], in0=t[:], scalar1=math.pi, scalar2=-math.pi, op0=ALU.min, op1=ALU.max), reads=[t], writes=[t])
        op("act", lambda e: e.activation(out=dst[:], in_=t[:], func=AF.Sin), reads=[t], writes=[dst])


DH = 512
TB = 256
NT = TB // 128


def build_m0(S, stage=99, HS=(0, 1), TGT=(0, 0), fz=None):
    if fz is None:
        nc = bass.Bass("TRN2", target_bir_lowering=False); k = Ctx(nc); pref = ""
    else:
        nc, k, pref = fz.nc, fz.k, fz.pref
    D = mkD(nc, fz, pref)
    k.begin_stage(pref)
    x = D("x", [S, 1024])
    condT_d = D("condT", [128, 8])
    adaw = D("adaw", [1024, 2048])
    adabT = D("adabT", [128, 16])
    w_in = D("w_in", [1024, 3072])
    convw = D("convw", [128, 16, 4])
    convb = D("convb", [128, 16])
    wqc = D("wqc", [128, 16, 4]); wkc = D("wkc", [128, 16, 4]); wvc = D("wvc", [128, 16, 4])
    wqt = D("wqt", [128, 16, 4]); wkt = D("wkt", [128, 16, 4]); wvt = D("wvt", [128, 16, 4])
    wgq = D("wgq", [128, 16, 4]); wgk = D("wgk", [128, 16, 4]); wgv = D("wgv", [128, 16, 4])
    bg = D("bg", [128, 4])
    nwT = D("nwT", [128, 8]); skT = D("skT", [128, 8])
    out = D("hgT", [1024, S], BF16, kind="ExternalOutput")

    op = k.op
    NB = S // TB

    identf = k.sb("identf", [128, 128]); ident = k.sb("ident", [128, 128], BF16)
    ntri = k.sb("ntri", [128, 128]); negm = k.sb("negm", [128, 128])
    bmask = k.sb("bmask", [128, 32, 4])
    onesb = k.sb("onesb", [128, 2], BF16); onesf = k.sb("onesf", [128, 128])
    op("pool", lambda e: e.memset(identf[:], 0.0), writes=[identf])
    op("pool", lambda e: e.affine_select(out=identf[:], in_=identf[:], pattern=[[-1, 128]], compare_op=ALU.not_equal,
                                         fill=1.0, base=0, channel_multiplier=1), reads=[identf], writes=[identf])
    op("dve", lambda e: e.tensor_copy(out=ident[:], in_=identf[:]), reads=[identf], writes=[ident])
    op("pool", lambda e: e.memset(ntri[:], -1.0), writes=[ntri])
    op("pool", lambda e: e.affine_select(out=ntri[:], in_=ntri[:], pattern=[[1, 128]], compare_op=ALU.is_ge,
                                         fill=0.0, base=0, channel_multiplier=-1), reads=[ntri], writes=[ntri])
    op("pool", lambda e: e.memset(negm[:], 0.0), writes=[negm])
    op("pool", lambda e: e.affine_select(out=negm[:], in_=negm[:], pattern=[[1, 128]], compare_op=ALU.is_ge,
                                         fill=-30000.0, base=0, channel_multiplier=-1), reads=[negm], writes=[negm])
    op("pool", lambda e: e.memset(bmask[:], 1.0), writes=[bmask])
    op("pool", lambda e: e.affine_select(out=bmask[:], in_=bmask[:], pattern=[[-4, 32], [0, 4]], compare_op=ALU.is_ge,
                                         fill=0.0, base=0, channel_multiplier=1), reads=[bmask], writes=[bmask])
    op("pool", lambda e: e.affine_select(out=bmask[:], in_=bmask[:], pattern=[[4, 32], [0, 4]], compare_op=ALU.is_ge,
                                         fill=0.0, base=3, channel_multiplier=-1), reads=[bmask], writes=[bmask])
    op("pool", lambda e: e.memset(onesb[:], 1.0), writes=[onesb])
    op("pool", lambda e: e.memset(onesf[:], 1.0), writes=[onesf])

    def load(name, src, shape, dt=F32, q="sp"):
        b = k.sb(name, shape, dt)
        op(q, lambda e: e.dma_start(out=b[:], in_=src), writes=[b], dma=True)
        return b
    condT = load("condT_s", condT_d, [128, 8]); adab = load("adab_s", adabT, [128, 16])
    cw = load("cw", convw, [128, 16, 4]); cb = load("cb", convb, [128, 16])
    wc = [load("wqc_s", wqc, [128, 16, 4]), load("wkc_s", wkc, [128, 16, 4]), load("wvc_s", wvc, [128, 16, 4])]
    wt = [load("wqt_s", wqt, [128, 16, 4]), load("wkt_s", wkt, [128, 16, 4]), load("wvt_s", wvt, [128, 16, 4])]
    wg = [load("wgq_s", wgq, [128, 16, 4]), load("wgk_s", wgk, [128, 16, 4]), load("wgv_s", wgv, [128, 16, 4])]
    bgs = load("bgs", bg, [128, 4]); nw = load("nw", nwT, [128, 8]); sk = load("sk", skT, [128, 8])

    pA = k.ps("pA", [128, 512]); pB = k.ps("pB", [128, 512])
    import os
    if os.environ.get("M0V", "0") == "1":
        hbA = k.ps("hbA", [128, 512]); hbB = k.ps("hbB", [128, 512])
        pbh = [hbA, hbA]; pSh = [hbB, hbB]
    else:
        hb = [k.ps("hb0", [128, 512]), k.ps("hb1", [128, 512])]
        pbh = [hb[0], hb[1]]
        pSh = [hb[0], hb[1]]
    pnh = [k.ps("pn0", [128, 512]), k.ps("pn1", [128, 512])]
    pS = pnh[0]
    pt = k.ps("pt", [128, 1024], BF16)
    ptF = k.ps("ptF", [128, 1024], BF16)
    ptFh = [ptF, ptF]
    pacc = [pA, pB]
    pacc_i = [0]

    def nextp():
        pacc_i[0] ^= 1
        return pacc[pacc_i[0]]

    cond = k.sb("cond", [128, 8])
    op("act", lambda e: e.activation(out=cond[:], in_=condT[:], func=AF.Silu), reads=[condT], writes=[cond])
    wst = [k.sb("wst0", [128, 8, 256]), k.sb("wst1", [128, 8, 256])]
    for j in range(16):
        st = wst[j % 2]
        op("sp", lambda e: e.dma_start(out=st[:, :, 0:128], in_=adaw[:, j * 128:(j + 1) * 128].rearrange("(c p) f -> p c f", p=128)),
           writes=[st], dma=True)
        for kc in range(8):
            op("pe", lambda e: e.matmul(pS[:, j:j + 1], lhsT=st[:, kc, 0:128], rhs=cond[:, kc:kc + 1], start=(kc == 0), stop=(kc == 7)),
               reads=[st, cond], writes=[pS])
    modT = k.sb("modT", [128, 16])
    op("dve", lambda e: e.tensor_tensor(out=modT[:], in0=pS[:, 0:16], in1=adab[:], op=ALU.add), reads=[pS, adab], writes=[modT])
    op("dve", lambda e: e.tensor_scalar_add(out=modT[:, 8:16], in0=modT[:, 8:16], scalar1=1.0), reads=[modT], writes=[modT])

    winb = k.sb("winb", [128, 8, 3072], BF16)
    for j in range(12):
        st = wst[j % 2]
        op("sp" if j % 2 == 0 else "act", lambda e: e.dma_start(out=st[:], in_=w_in[:, j * 256:(j + 1) * 256].rearrange("(c p) f -> p c f", p=128)),
           writes=[st], dma=True)
        op("pool", lambda e: e.tensor_copy(out=winb[:, :, j * 256:(j + 1) * 256], in_=st[:]), reads=[st], writes=[winb])

    BD = [[k.sb(f"bd{w}_{j}", [128, 128], BF16) for j in range(8)] for w in range(3)]
    for w in range(3):
        for j in range(8):
            op("dve", lambda e: e.tensor_tensor(out=BD[w][j][:].rearrange("p (n o) -> p n o", o=4),
                                                in0=wc[w][:, j:j + 1, :].to_broadcast([128, 32, 4]), in1=bmask[:], op=ALU.mult),
               reads=[wc[w], bmask], writes=[BD[w][j]])
    bdt = [k.sb("bdt0", [128, 128]), k.sb("bdt1", [128, 128])]
    wcg = k.sb("wcg", [128, 16, 4], BF16); wmg = k.sb("wmg", [128, 16, 4], BF16)
    ii = 0
    for j in range(16):
        for w in range(3):
            t = bdt[ii % 2]; ii += 1
            op("dve", lambda e: e.tensor_tensor(out=t[:].rearrange("p (n i) -> p n i", i=4),
                                                in0=wt[w][:, j:j + 1, :].to_broadcast([128, 32, 4]), in1=bmask[:], op=ALU.mult),
               reads=[wt[w], bmask], writes=[t])
            dst = pS[:, 64 + j * 4: 68 + j * 4] if w < 2 else pS[:, 192 + j * 4: 196 + j * 4]
            op("pe", lambda e: e.matmul(dst, lhsT=t[:], rhs=wg[w][:, j, :], start=(w != 1), stop=(w != 0)),
               reads=[t, wg[w]], writes=[pS])
    op("dve", lambda e: e.tensor_copy(out=wcg[:].rearrange("p a b -> p (a b)"), in_=pS[:, 64:128]), reads=[pS], writes=[wcg])
    op("dve", lambda e: e.tensor_copy(out=wmg[:].rearrange("p a b -> p (a b)"), in_=pS[:, 192:256]), reads=[pS], writes=[wmg])

    xs_ = [k.sb("x0", [128, 1024]), k.sb("x1", [128, 1024])]
    xb_ = [k.sb("xb0", [128, 1024], BF16), k.sb("xb1", [128, 1024], BF16)]
    uT = [k.sb("uT0", [128, 8, TB], BF16), k.sb("uT1", [128, 8, TB], BF16)]
    xmt = [k.sb(f"xmt{i}", [128, TB + 3]) for i in range(3)]
    hist = k.sb("hist", [128, 16, 3])
    acc = [k.sb("acc0", [128, TB]), k.sb("acc1", [128, TB])]
    xmb = k.sb("xmb", [128, 8, TB], BF16); xcT = k.sb("xcT", [128, 8, TB], BF16)
    xo = [k.sb(f"xo{i}", [128, 2, TB], BF16) for i in range(2)]
    sz = k.sb("sz", [128, 8, TB], BF16)
    qT = k.sb("qT", [128, 8, TB], BF16); kT = k.sb("kT", [128, 8, TB], BF16)
    ktm = k.sb("ktm", [128, NT, 1024], BF16); vtm = k.sb("vtm", [128, NT, 1024], BF16)
    hg = [k.sb("hg0", [128, 8, TB], BF16), k.sb("hg1", [128, 8, TB], BF16)]
    gT = k.sb("gT", [4, TB]); gts = k.sb("gts", [128, NT, 4]); sp_ = k.sb("sp_", [128, NT, 2]); ex_ = k.sb("ex_", [128, NT, 2])
    Cst = [k.sb(f"C{h}", [128, 4, 512]) for h in range(2)]
    Cb = [k.sb(f"Cb{h}", [128, 4, 512], BF16) for h in range(2)]
    nst = [k.sb(f"n{h}", [128, 4]) for h in range(2)]
    nb_ = [k.sb(f"nb{h}", [128, 4], BF16) for h in range(2)]
    for h in range(2):
        op("pool", lambda e: e.memset(Cst[h][:], 0.0), writes=[Cst[h]])
        op("pool", lambda e: e.memset(Cb[h][:], 0.0), writes=[Cb[h]])
        op("pool", lambda e: e.memset(nst[h][:], 0.0), writes=[nst[h]])
        op("pool", lambda e: e.memset(nb_[h][:], 0.0), writes=[nb_[h]])
    op("pool", lambda e: e.memset(hist[:], 0.0), writes=[hist])
    sprep = [k.sb(f"sprep{h}", [128, 128]) for h in range(2)]
    bias_s = [k.sb(f"bias{h}", [128, 1]) for h in range(2)]
    DT = [k.sb(f"DT{h}", [128, 128]) for h in range(2)]
    EB = [k.sb(f"EB{h}", [128, 128]) for h in range(2)]
    bL = [k.sb(f"bL{h}", [128, 1]) for h in range(2)]
    ws = [k.sb(f"ws{h}", [128, 1]) for h in range(2)]
    PT = [k.sb(f"PT{h}", [128, 128], BF16) for h in range(2)]
    qp = [k.sb(f"qp{h}", [128, 4, 128], BF16) for h in range(2)]
    wk_ = [k.sb(f"wk{h}", [128, 512], BF16) for h in range(2)]
    rden = [k.sb(f"rden{h}", [128, 1]) for h in range(2)]
    hs = [k.sb(f"hs{h}", [128, 512]) for h in range(2)]
    hn = [k.sb(f"hn{h}", [128, 512], BF16) for h in range(2)]
    st6 = [k.sb(f"st6{h}", [128, 6]) for h in range(2)]
    mv = [k.sb(f"mv{h}", [128, 2]) for h in range(2)]
    rstd = [k.sb(f"rstd{h}", [128, 1]) for h in range(2)]
    tmp2 = [k.sb(f"tmp2{h}", [128, 128]) for h in range(2)]
    tmp3 = [k.sb(f"tmp3{h}", [128, 128]) for h in range(2)]
    eps_t = k.sb("eps_t", [128, 1])
    op("pool", lambda e: e.memset(eps_t[:], 1e-5), writes=[eps_t])
    mhalf = k.sb("mhalf", [128, 1])
    op("pool", lambda e: e.memset(mhalf[:], -0.5), writes=[mhalf])
    one_t = k.sb("one_t", [128, 1])
    op("pool", lambda e: e.memset(one_t[:], 1.0), writes=[one_t])

    def bail():
        tk = op("sp", lambda e: e.dma_start(out=out[:, 0:TB].rearrange("(c p) t -> p c t", p=128), in_=hg[0][:]), reads=[hg[0]], dma=True)
        k.finish([tk])
        print("bail instrs", k.n_ins)
        return nc
    out_toks = []
    for blk in range(NB):
        t0 = blk * TB
        u = uT[blk % 2]
        for t4 in range(NT):
            xs = xs_[t4 % 2]; xb = xb_[t4 % 2]
            op("sp", lambda e: e.dma_start(out=xs[:], in_=x[t0 + t4 * 128: t0 + (t4 + 1) * 128, :]), writes=[xs], dma=True)
            op("act", lambda e: e.copy(out=xb[:], in_=xs[:]), reads=[xs], writes=[xb])
            for kc in range(8):
                op("pe", lambda e: e.transpose(out=pt[:, kc * 128:(kc + 1) * 128], in_=xb[:, kc * 128:(kc + 1) * 128], identity=ident[:]),
                   reads=[xb, ident], writes=[pt])
            for kc in range(8):
                op("dve", lambda e: e.tensor_scalar(out=u[:, kc, t4 * 128:(t4 + 1) * 128], in0=pt[:, kc * 128:(kc + 1) * 128],
                                                    scalar1=modT[:, 8 + kc:9 + kc], scalar2=modT[:, kc:kc + 1], op0=ALU.mult, op1=ALU.add),
                   reads=[pt, modT], writes=[u])
        for oc in range(16):
            p = nextp()
            for kc in range(8):
                op("pe", lambda e: e.matmul(p[:, 0:TB], lhsT=winb[:, kc, oc * 128:(oc + 1) * 128], rhs=u[:, kc, :], start=(kc == 0), stop=(kc == 7)),
                   reads=[winb, u], writes=[p])
            xm = xmt[oc % 3]
            op("dve", lambda e: e.tensor_copy(out=xm[:, 0:3], in_=hist[:, oc, :]), reads=[hist], writes=[xm])
            op("act", lambda e: e.copy(out=xm[:, 3:TB + 3], in_=p[:, 0:TB]), reads=[p], writes=[xm])
            op("dve", lambda e: e.tensor_copy(out=hist[:, oc, :], in_=xm[:, TB:TB + 3]), reads=[xm], writes=[hist])
            xmdst = xmb[:, oc, :] if oc < 8 else xo[oc % 2][:, 1, :]
            xmdb = xmb if oc < 8 else xo[oc % 2]
            op("act", lambda e: e.copy(out=xmdst, in_=xm[:, 3:TB + 3]), reads=[xm], writes=[xmdb])
            a = acc[oc % 2]
            op("dve", lambda e: e.tensor_scalar(out=a[:], in0=xm[:, 3:TB + 3], scalar1=cw[:, oc, 3:4], scalar2=None, op0=ALU.mult),
               reads=[xm, cw], writes=[a])
            for jj in (2, 1, 0):
                op("dve", lambda e: e.scalar_tensor_tensor(out=a[:], in0=xm[:, jj:TB + jj], scalar=cw[:, oc, jj:jj + 1], in1=a[:],
                                                           op0=ALU.mult, op1=ALU.add), reads=[xm, cw, a], writes=[a])
            xcdst = xcT[:, oc, :] if oc < 8 else xo[oc % 2][:, 0, :]
            xcdb = xcT if oc < 8 else xo[oc % 2]
            op("act", lambda e: e.activation(out=xcdst, in_=a[:], func=AF.Silu, bias=cb[:, oc:oc + 1]), reads=[a, cb], writes=[xcdb])
            xmsrc = xmdst
            op("pe", lambda e: e.matmul(pS[0:4, 0:TB], lhsT=wcg[:, oc, :], rhs=xcdst, start=(oc == 0), stop=False), reads=[wcg, xcdb], writes=[pS])
            op("pe", lambda e: e.matmul(pS[0:4, 0:TB], lhsT=wmg[:, oc, :], rhs=xmsrc, start=False, stop=(oc == 15)), reads=[wmg, xmdb], writes=[pS])
        op("act", lambda e: e.copy(out=gT[:], in_=pS[0:4, 0:TB]), reads=[pS], writes=[gT])
        for oc in range(8):
            p = nextp()
            for kc in range(8):
                op("pe", lambda e: e.matmul(p[:, 0:TB], lhsT=winb[:, kc, 2048 + oc * 128: 2048 + (oc + 1) * 128], rhs=u[:, kc, :], start=(kc == 0), stop=(kc == 7)),
                   reads=[winb, u], writes=[p])
            op("act", lambda e: e.activation(out=sz[:, oc, :], in_=p[:, 0:TB], func=AF.Silu), reads=[p], writes=[sz])
        for j in range(8):
            p = nextp()
            op("pe", lambda e: e.matmul(p[:, 0:TB], lhsT=BD[0][j][:], rhs=xcT[:, j, :], start=True, stop=True), reads=[BD[0][j], xcT], writes=[p])
            op("act", lambda e: e.mul(out=qT[:, j, :], in_=p[:, 0:TB], mul=DH ** -0.5), reads=[p], writes=[qT])
            p = nextp()
            op("pe", lambda e: e.matmul(p[:, 0:TB], lhsT=BD[1][j][:], rhs=xcT[:, j, :], start=True, stop=True), reads=[BD[1][j], xcT], writes=[p])
            op("dve", lambda e: e.tensor_copy(out=kT[:, j, :], in_=p[:, 0:TB]), reads=[p], writes=[kT])
        for t4 in range(NT):
            for half in range(2):
                p = nextp()
                for jj in range(4):
                    j = half * 4 + jj
                    op("pe", lambda e: e.matmul(p[:, jj * 128:(jj + 1) * 128], lhsT=xcT[:, j, t4 * 128:(t4 + 1) * 128], rhs=BD[1][j][:], start=True, stop=True),
                       reads=[xcT, BD[1][j]], writes=[p])
                op("act", lambda e: e.copy(out=ktm[:, t4, half * 512:(half + 1) * 512], in_=p[:, :]), reads=[p], writes=[ktm])
                p = nextp()
                for jj in range(4):
                    j = half * 4 + jj
                    op("pe", lambda e: e.matmul(p[:, jj * 128:(jj + 1) * 128], lhsT=xmb[:, j, t4 * 128:(t4 + 1) * 128], rhs=BD[2][j][:], start=True, stop=True),
                       reads=[xmb, BD[2][j]], writes=[p])
                op("dve", lambda e: e.tensor_copy(out=vtm[:, t4, half * 512:(half + 1) * 512], in_=p[:, :]), reads=[p], writes=[vtm])
        for t4 in range(NT):
            op("pe", lambda e: e.matmul(pS[:, 256 + t4 * 4: 260 + t4 * 4], lhsT=gT[:, t4 * 128:(t4 + 1) * 128], rhs=identf[0:4, 0:4], start=True, stop=True),
               reads=[gT, identf], writes=[pS])
        op("dve", lambda e: e.tensor_tensor(out=gts[:], in0=pS[:, 256:256 + 4 * NT].rearrange("p (a b) -> p a b", b=4),
                                            in1=bgs[:, None, :].to_broadcast([128, NT, 4]), op=ALU.add), reads=[pS, bgs], writes=[gts])
        op("act", lambda e: e.activation(out=ex_[:], in_=gts[:, :, 2:4], func=AF.Exp, scale=-1.0), reads=[gts], writes=[ex_])
        op("act", lambda e: e.activation(out=sp_[:], in_=ex_[:], func=AF.Ln, bias=one_t[:]), reads=[ex_, one_t], writes=[sp_])
        hgb = hg[blk % 2]

        def chain(t4, h, hgb=hgb):
            tsl = slice(t4 * 128, (t4 + 1) * 128)
            pb = pbh[h]; pS = pSh[h]; pn = pnh[h]; pt = ptFh[h]; pc = [pA, pB]
            if True:
                hc = slice(h * 512, (h + 1) * 512)
                op("dve", lambda e: e.tensor_scalar(out=sprep[h][:], in0=onesf[:], scalar1=sp_[:, t4, h:h + 1], scalar2=None, op0=ALU.mult),
                   reads=[onesf, sp_], writes=[sprep[h]])
                op("pe", lambda e: e.matmul(pb[:, 0:128], lhsT=sprep[h][:], rhs=ntri[:], start=True, stop=False), reads=[sprep[h], ntri], writes=[pb])
                op("pe", lambda e: e.matmul(pb[:, 0:128], lhsT=identf[:], rhs=negm[:], start=False, stop=True), reads=[identf, negm], writes=[pb])
                op("pe", lambda e: e.matmul(pb[:, 128:256], lhsT=sprep[h][:], rhs=ntri[:], start=True, stop=True), reads=[sprep[h], ntri], writes=[pb])
                op("pe", lambda e: e.matmul(pb[:, 256:258], lhsT=ntri[:], rhs=sp_[:, t4, :], start=True, stop=True), reads=[ntri, sp_], writes=[pb])
                yield
                op("dve", lambda e: e.tensor_tensor(out=bias_s[h][:], in0=gts[:, t4, h:h + 1], in1=pb[:, 256 + h:257 + h], op=ALU.subtract),
                   reads=[gts, pb], writes=[bias_s[h]])
                op("dve", lambda e: e.tensor_copy(out=bL[h][:], in_=pb[:, 255:256]), reads=[pb], writes=[bL[h]])
                yield
                op("act", lambda e: e.activation(out=DT[h][:], in_=pb[:, 0:128], func=AF.Exp, bias=bias_s[h][:]), reads=[pb, bias_s[h], bL[h]], writes=[DT[h]])
                op("act", lambda e: e.activation(out=EB[h][:], in_=pb[:, 128:256], func=AF.Exp), reads=[pb, bL[h]], writes=[EB[h]])
                op("act", lambda e: e.activation(out=ws[h][:], in_=bias_s[h][:], func=AF.Exp, bias=bL[h][:]), reads=[bias_s[h], bL[h]], writes=[ws[h]])
                yield
                for dc in range(4):
                    op("pe", lambda e: e.matmul(pS[:, 260:388], lhsT=kT[:, 4 * h + dc, tsl], rhs=qT[:, 4 * h + dc, tsl], start=(dc == 0), stop=(dc == 3)),
                       reads=[kT, qT], writes=[pS])
                yield
                op("dve", lambda e: e.tensor_tensor(out=PT[h][:], in0=pS[:, 260:388], in1=DT[h][:], op=ALU.mult), reads=[pS, DT[h]], writes=[PT[h]])
                yield
                for dc in range(4):
                    op("dve", lambda e: e.tensor_tensor(out=qp[h][:, dc, :], in0=qT[:, 4 * h + dc, tsl], in1=EB[h][:], op=ALU.mult),
                       reads=[qT, EB[h]], writes=[qp[h]])
                op("dve", lambda e: e.tensor_scalar(out=wk_[h][:], in0=ktm[:, t4, hc], scalar1=ws[h][:], scalar2=None, op0=ALU.mult),
                   reads=[ktm, ws[h]], writes=[wk_[h]])
                yield
                op("pe", lambda e: e.matmul(pn[:, :], lhsT=PT[h][:], rhs=vtm[:, t4, hc], start=True, stop=False), reads=[PT[h], vtm], writes=[pn])
                for dc in range(4):
                    op("pe", lambda e: e.matmul(pn[:, :], lhsT=qp[h][:, dc, :], rhs=Cb[h][:, dc, :], start=False, stop=(dc == 3)),
                       reads=[qp[h], Cb[h]], writes=[pn])
                op("pe", lambda e: e.matmul(pS[:, 388:389], lhsT=PT[h][:], rhs=onesb[:, 0:1], start=True, stop=False), reads=[PT[h], onesb], writes=[pS])
                for dc in range(4):
                    op("pe", lambda e: e.matmul(pS[:, 388:389], lhsT=qp[h][:, dc, :], rhs=nb_[h][:, dc:dc + 1], start=False, stop=(dc == 3)),
                       reads=[qp[h], nb_[h]], writes=[pS])
                yield
                for dc in range(4):
                    pcc = pc[dc % 2]
                    op("pe", lambda e: e.matmul(pcc[:, :], lhsT=wk_[h][:, dc * 128:(dc + 1) * 128], rhs=vtm[:, t4, hc], start=True, stop=True),
                       reads=[wk_[h], vtm], writes=[pcc])
                    op("dve", lambda e: e.scalar_tensor_tensor(out=Cst[h][:, dc, :], in0=Cst[h][:, dc, :], scalar=EB[h][:, 127:128], in1=pcc[:, :],
                                                               op0=ALU.mult, op1=ALU.add), reads=[Cst[h], EB[h], pcc], writes=[Cst[h]])
                    yield
                yield
                op("act", lambda e: e.copy(out=Cb[h][:], in_=Cst[h][:]), reads=[Cst[h]], writes=[Cb[h]])
                for dc in range(4):
                    op("pe", lambda e: e.matmul(pS[:, 392 + dc:393 + dc], lhsT=wk_[h][:, dc * 128:(dc + 1) * 128], rhs=onesb[:, 0:1], start=True, stop=True),
                       reads=[wk_[h], onesb], writes=[pS])
                yield
                op("dve", lambda e: e.tensor_scalar(out=rden[h][:], in0=pS[:, 388:389], scalar1=-1.0, scalar2=None, op0=ALU.mult),
                   reads=[pS], writes=[rden[h]])
                op("dve", lambda e: e.tensor_tensor(out=rden[h][:], in0=rden[h][:], in1=pS[:, 388:389], op=ALU.max),
                   reads=[pS, rden[h]], writes=[rden[h]])
                op("dve", lambda e: e.tensor_scalar(out=rden[h][:], in0=rden[h][:], scalar1=1.0, scalar2=None, op0=ALU.max),
                   reads=[rden[h]], writes=[rden[h]])
                op("dve", lambda e: e.scalar_tensor_tensor(out=nst[h][:], in0=nst[h][:], scalar=EB[h][:, 127:128], in1=pS[:, 392:396],
                                                           op0=ALU.mult, op1=ALU.add), reads=[nst[h], EB[h], pS], writes=[nst[h]])
                op("dve", lambda e: e.tensor_copy(out=nb_[h][:], in_=nst[h][:]), reads=[nst[h]], writes=[nb_[h]])
                op("dve", lambda e: e.reciprocal(out=rden[h][:], in_=rden[h][:]), reads=[rden[h]], writes=[rden[h]])
                op("dve", lambda e: e.tensor_scalar(out=hs[h][:], in0=pn[:, :], scalar1=rden[h][:], scalar2=None, op0=ALU.mult), reads=[pn, rden[h]], writes=[hs[h]])
                yield
                op("dve", lambda e: e.bn_stats(out=st6[h][:], in_=hs[h][:]), reads=[hs[h]], writes=[st6[h]])
                op("dve", lambda e: e.bn_aggr(out=mv[h][:], in_=st6[h][:]), reads=[st6[h]], writes=[mv[h]])
                yield
                op("pool", lambda e: e.tensor_scalar(out=rstd[h][:], in0=mv[h][:, 1:2], scalar1=1e-5, scalar2=None, op0=ALU.add), reads=[mv[h]], writes=[rstd[h]])
                op("pool", lambda e: e.tensor_tensor(out=rstd[h][:], in0=rstd[h][:], in1=mhalf[:], op=ALU.pow), reads=[rstd[h], mhalf], writes=[rstd[h]])
                yield
                op("dve", lambda e: e.tensor_scalar(out=hn[h][:], in0=hs[h][:], scalar1=mv[h][:, 0:1], scalar2=rstd[h][:], op0=ALU.subtract, op1=ALU.mult),
                   reads=[hs[h], mv[h], rstd[h]], writes=[hn[h]])
                yield
                for dc in range(4):
                    op("pe", lambda e: e.transpose(out=pt[:, h * 512 + dc * 128:h * 512 + (dc + 1) * 128], in_=hn[h][:, dc * 128:(dc + 1) * 128], identity=ident[:]),
                       reads=[hn[h], ident], writes=[pt])
                yield
                for dc in range(4):
                    j = 4 * h + dc
                    op("dve", lambda e: e.tensor_scalar(out=tmp2[h][:], in0=xcT[:, j, tsl], scalar1=sk[:, j:j + 1], scalar2=None, op0=ALU.mult),
                       reads=[xcT, sk], writes=[tmp2[h]])
                    op("dve", lambda e: e.scalar_tensor_tensor(out=tmp3[h][:], in0=pt[:, h * 512 + dc * 128:h * 512 + (dc + 1) * 128], scalar=nw[:, j:j + 1], in1=tmp2[h][:],
                                                               op0=ALU.mult, op1=ALU.add), reads=[pt, nw, tmp2[h]], writes=[tmp3[h]])
                    op("dve", lambda e: e.tensor_tensor(out=hgb[:, j, tsl], in0=tmp3[h][:], in1=sz[:, j, tsl], op=ALU.mult),
                       reads=[tmp3[h], sz], writes=[hgb])
                    yield
        for t4 in range(NT):
            gens = [chain(t4, h) for h in HS]
            while gens:
                for g_ in list(gens):
                    try:
                        next(g_)
                    except StopIteration:
                        gens.remove(g_)
        tk = op("pool", lambda e: e.dma_start(out=out[:, t0:t0 + TB].rearrange("(c p) t -> p c t", p=128), in_=hgb[:]), reads=[hgb], dma=True)
        out_toks.append(tk)
    if fz is None:
        k.finish(out_toks)
    else:
        k.end_stage()
    print("m0 instructions:", k.n_ins)
    return nc


def m0_inputs(inp, b, hp, S):
    f = np.float32
    own = np.arange(hp * 1024, (hp + 1) * 1024); oth = np.arange((1 - hp) * 1024, (2 - hp) * 1024)
    ch = np.concatenate([own, oth])
    w_in = inp["mlstm_w_in"][0]
    d = {}
    d["x"] = np.ascontiguousarray(inp["x"][b, :S])
    d["condT"] = np.ascontiguousarray(inp["c"][b].reshape(8, 128).T)
    d["adaw"] = np.ascontiguousarray(inp["ada_w"][0][:, 0:2048])
    d["adabT"] = np.ascontiguousarray(inp["ada_b"][0][0:2048].reshape(16, 128).T)
    d["w_in"] = np.ascontiguousarray(np.concatenate([w_in[:, ch], w_in[:, 2048 + own]], axis=1))
    d["convw"] = np.ascontiguousarray(inp["mlstm_conv_w"][0].T[ch].reshape(16, 128, 4).transpose(1, 0, 2))
    d["convb"] = np.ascontiguousarray(inp["mlstm_conv_b"][0][ch].reshape(16, 128).T)
    blk = ch.reshape(-1, 4)[:, 0] // 4
    for nm, key in (("q", "mlstm_wq"), ("k", "mlstm_wk"), ("v", "mlstm_wv")):
        w = inp[key][0][blk]
        d["w%sc" % nm] = np.ascontiguousarray(w.reshape(16, 128, 4).transpose(1, 0, 2))
        d["w%st" % nm] = np.ascontiguousarray(w.transpose(0, 2, 1).reshape(16, 128, 4).transpose(1, 0, 2))
    gcols = [2 * hp, 2 * hp + 1, 4 + 2 * hp, 5 + 2 * hp]
    wgates = inp["mlstm_w_gates"][0]
    for i, nm in enumerate("qkv"):
        wg = wgates[i * 2048:(i + 1) * 2048][ch][:, gcols]
        d["wg" + nm] = np.ascontiguousarray(wg.reshape(16, 128, 4).transpose(1, 0, 2))
    d["bg"] = np.ascontiguousarray(np.tile(inp["mlstm_b_gates"][0][gcols][None, :], (128, 1)))
    d["nwT"] = np.ascontiguousarray(inp["mlstm_norm_w"][0][own].reshape(8, 128).T)
    d["skT"] = np.ascontiguousarray(inp["mlstm_skip"][0][own].reshape(8, 128).T)
    return {k_: v.astype(f) for k_, v in d.items()}


ALPHA = 4 ** 0.25
EPS = 1e-5


def consts(k):
    op = k.op
    c = {}
    c["identf"] = k.sb("identf", [128, 128])
    c["onesf"] = k.sb("onesf", [128, 128])
    c["mhalf"] = k.sb("mhalf", [128, 1])
    op("pool", lambda e: e.memset(c["identf"][:], 0.0), writes=[c["identf"]])
    op("pool", lambda e: e.affine_select(out=c["identf"][:], in_=c["identf"][:], pattern=[[-1, 128]], compare_op=ALU.not_equal,
                                         fill=1.0, base=0, channel_multiplier=1), reads=[c["identf"]], writes=[c["identf"]])
    op("pool", lambda e: e.memset(c["onesf"][:], 1.0), writes=[c["onesf"]])
    op("pool", lambda e: e.memset(c["mhalf"][:], -0.5), writes=[c["mhalf"]])
    return c


def adaln(k, c, condT_d, adaw_f, adab_f, nf, adaw_b, adab_b, nb, pbank, stg):
    op = k.op
    condT = k.sb("condT_s", [128, 8]); cond = k.sb("cond", [128, 8])
    op("sp", lambda e: e.dma_start(out=condT[:], in_=condT_d), writes=[condT], dma=True)
    op("act", lambda e: e.activation(out=cond[:], in_=condT[:], func=AF.Silu), reads=[condT], writes=[cond])
    modT = None
    if nf:
        modT = k.sb("modT", [128, nf * 8]); adab = k.sb("adabf", [128, nf * 8])
        op("sp", lambda e: e.dma_start(out=adab[:], in_=adab_f), writes=[adab], dma=True)
        for j in range(nf * 8):
            st = stg[j % 2]
            op("sp", lambda e: e.dma_start(out=st[:, :, 0:128], in_=adaw_f[:, j * 128:(j + 1) * 128].rearrange("(c p) f -> p c f", p=128)), writes=[st], dma=True)
            for kc in range(8):
                op("pe", lambda e: e.matmul(pbank[:, j:j + 1], lhsT=st[:, kc, 0:128], rhs=cond[:, kc:kc + 1], start=(kc == 0), stop=(kc == 7)),
                   reads=[st, cond], writes=[pbank])
        op("dve", lambda e: e.tensor_tensor(out=modT[:], in0=pbank[:, 0:nf * 8], in1=adab[:], op=ALU.add), reads=[pbank, adab], writes=[modT])
    bts = []
    if nb:
        crep = k.sb("crep", [128, 8, 128])
        for kc in range(8):
            op("dve", lambda e: e.tensor_scalar(out=crep[:, kc, :], in0=c["onesf"][:], scalar1=cond[:, kc:kc + 1], scalar2=None, op0=ALU.mult),
               reads=[c["onesf"], cond], writes=[crep])
        for v in range(nb):
            bt = k.sb(f"bt{v}", [128, 1024])
            op("sp", lambda e: e.dma_start(out=bt[:], in_=adab_b[:, v * 1024:(v + 1) * 1024]), writes=[bt], dma=True)
            for q in range(4):
                st = stg[q % 2]
                op("sp", lambda e: e.dma_start(out=st[:], in_=adaw_b[:, v * 1024 + q * 256: v * 1024 + (q + 1) * 256].rearrange("(c p) f -> p c f", p=128)),
                   writes=[st], dma=True)
                for kc in range(8):
                    op("pe", lambda e: e.matmul(pbank[:, 0:256], lhsT=crep[:, kc, :], rhs=st[:, kc, :], start=(kc == 0), stop=(kc == 7)),
                       reads=[crep, st], writes=[pbank])
                op("dve", lambda e: e.tensor_tensor(out=bt[:, q * 256:(q + 1) * 256], in0=bt[:, q * 256:(q + 1) * 256], in1=pbank[:, 0:256], op=ALU.add),
                   reads=[bt, pbank], writes=[bt])
            bts.append(bt)
    return modT, bts


def res_ln(k, c, ysrc, ybuf, xs, G, lng, lnb, tmp, st12, mv, rstd, xo):
    op = k.op
    op("dve", lambda e: e.tensor_tensor(out=tmp[:], in0=ysrc, in1=G[:], op=ALU.mult), reads=ybuf + [G], writes=[tmp])
    op("dve", lambda e: e.scalar_tensor_tensor(out=tmp[:], in0=xs[:], scalar=ALPHA, in1=tmp[:], op0=ALU.mult, op1=ALU.add), reads=[xs, tmp], writes=[tmp])
    for hh in range(2):
        op("dve", lambda e: e.bn_stats(out=st12[:, hh * 6:(hh + 1) * 6], in_=tmp[:, hh * 512:(hh + 1) * 512]), reads=[tmp], writes=[st12])
    op("dve", lambda e: e.bn_aggr(out=mv[:], in_=st12[:]), reads=[st12], writes=[mv])
    op("pool", lambda e: e.tensor_scalar(out=rstd[:], in0=mv[:, 1:2], scalar1=EPS, scalar2=None, op0=ALU.add), reads=[mv], writes=[rstd])
    op("pool", lambda e: e.tensor_tensor(out=rstd[:], in0=rstd[:], in1=c["mhalf"][:], op=ALU.pow), reads=[rstd, c["mhalf"]], writes=[rstd])
    op("dve", lambda e: e.tensor_scalar(out=tmp[:], in0=tmp[:], scalar1=mv[:, 0:1], scalar2=rstd[:], op0=ALU.subtract, op1=ALU.mult),
       reads=[tmp, mv, rstd], writes=[tmp])
    op("dve", lambda e: e.tensor_tensor(out=tmp[:], in0=tmp[:], in1=lng[:], op=ALU.mult), reads=[tmp, lng], writes=[tmp])
    op("dve", lambda e: e.tensor_tensor(out=xo[:], in0=tmp[:], in1=lnb[:], op=ALU.add), reads=[tmp, lnb], writes=[xo])


def mod_transpose(k, c, xo, modT, m0, pT, uTf, uTb):
    op = k.op
    for kc in range(8):
        op("pe", lambda e: e.transpose(out=pT[kc // 4][:, (kc % 4) * 128:(kc % 4 + 1) * 128], in_=xo[:, kc * 128:(kc + 1) * 128], identity=c["identf"][:]),
           reads=[xo, c["identf"]], writes=[pT[kc // 4]])
    for kc in range(8):
        dst = uTf if uTf is not None else uTb
        op("dve", lambda e: e.tensor_scalar(out=dst[:, kc, :], in0=pT[kc // 4][:, (kc % 4) * 128:(kc % 4 + 1) * 128],
                                            scalar1=modT[:, m0 + 8 + kc:m0 + 9 + kc], scalar2=modT[:, m0 + kc:m0 + kc + 1], op0=ALU.mult, op1=ALU.add),
           reads=[pT[kc // 4], modT], writes=[dst])
    if uTf is not None:
        op("act", lambda e: e.copy(out=uTb[:], in_=uTf[:]), reads=[uTf], writes=[uTb])


def build_proj(TC, KC, glu, router, fz=None, blend=False):
    if fz is None:
        nc = bass.Bass("TRN2", target_bir_lowering=False); k = Ctx(nc); pref = ""
    else:
        nc, k, pref = fz.nc, fz.k, fz.pref
    D = mkD(nc, fz, pref)
    k.begin_stage(pref)
    NOUT = 2048 if glu else 1024
    inT = D("inT", [KC * 128, TC * (2 if blend else 1)], BF16)
    if blend:
        msk_d = D("msk", [128, 2])
    W = D("W", [KC * 128, NOUT])
    x = D("x", [TC, 1024])
    condT_d = D("condT", [128, 8])
    adaw_f = D("adaw_f", [1024, 2048]); adab_f = D("adab_f", [128, 16])
    adaw_b = D("adaw_b", [1024, 1024]); adab_b = D("adab_b", [128, 1024])
    lng_d = D("lng", [128, 1024]); lnb_d = D("lnb", [128, 1024])
    if glu:
        bglu_d = D("bglu", [128, 2048])
    if router:
        wr_d = D("wr", [128, 8, 8])
        gates_o = D("gates", [TC, 8], kind="ExternalOutput")
    xmid_o = D("xmid", [TC, 1024], kind="ExternalOutput")
    uT_o = D("uT", [1024, TC], BF16, kind="ExternalOutput")
    op = k.op
    c = consts(k)
    if blend:
        msk = k.sb("msk_s", [128, 2])
        op("sp", lambda e: e.dma_start(out=msk[:], in_=msk_d), writes=[msk], dma=True)
        itc = [[k.sb(f"itc{i}_{j}", [128, KC, 128], BF16) for j in range(2)] for i in range(2)]
    pY = [k.ps(f"pY{i}", [128, 512]) for i in range(4)]
    pT = [k.ps("pT0", [128, 512]), k.ps("pT1", [128, 512])]
    pM = k.ps("pM", [128, 512])
    stg = [k.sb("stg0", [128, 8, 256]), k.sb("stg1", [128, 8, 256])]
    modT, bts = adaln(k, c, condT_d, adaw_f, adab_f, 2, adaw_b, adab_b, 1, pM, stg)
    op("dve", lambda e: e.tensor_scalar_add(out=modT[:, 8:16], in0=modT[:, 8:16], scalar1=1.0), reads=[modT], writes=[modT])
    G = bts[0]
    op("dve", lambda e: e.tensor_scalar_add(out=G[:], in0=G[:], scalar1=1.0), reads=[G], writes=[G])
    lng = k.sb("lng_s", [128, 1024]); lnb = k.sb("lnb_s", [128, 1024])
    op("sp", lambda e: e.dma_start(out=lng[:], in_=lng_d), writes=[lng], dma=True)
    op("sp", lambda e: e.dma_start(out=lnb[:], in_=lnb_d), writes=[lnb], dma=True)
    if glu:
        bglu = k.sb("bglu_s", [128, 2048])
        op("sp", lambda e: e.dma_start(out=bglu[:], in_=bglu_d), writes=[bglu], dma=True)
    if router:
        wr = k.sb("wr_s", [128, 8, 8])
        op("sp", lambda e: e.dma_start(out=wr[:], in_=wr_d), writes=[wr], dma=True)
    Wb = k.sb("Wb", [128, KC, NOUT], BF16)
    ws2 = [k.sb("ws2_0", [128, NOUT]), k.sb("ws2_1", [128, NOUT])]
    for kc in range(KC):
        st = ws2[kc % 2]
        op("sp" if kc % 2 == 0 else "act", lambda e: e.dma_start(out=st[:], in_=W[kc * 128:(kc + 1) * 128, :]), writes=[st], dma=True)
        op("pool", lambda e: e.tensor_copy(out=Wb[:, kc, :], in_=st[:]), reads=[st], writes=[Wb])
    NTL = TC // 128
    it = [k.sb(f"it{i}", [128, KC, 128], BF16) for i in range(2)]
    xs_ = [k.sb(f"xs{i}", [128, 1024]) for i in range(2)]
    tmp = [k.sb(f"tmp{i}", [128, 1024]) for i in range(2)]
    xo = [k.sb(f"xo{i}", [128, 1024]) for i in range(2)]
    yv = [k.sb(f"yv{i}", [128, 1024]) for i in range(2)]
    sg = [k.sb(f"sg{i}", [128, 1024]) for i in range(2)]
    st12 = [k.sb(f"st12{i}", [128, 12]) for i in range(2)]
    mv = [k.sb(f"mv{i}", [128, 2]) for i in range(2)]
    rstd = [k.sb(f"rstd{i}", [128, 1]) for i in range(2)]
    uTf = [k.sb(f"uTf{i}", [128, 8, 128]) for i in range(2)]
    uTb = [k.sb(f"uTb{i}", [128, 8, 128], BF16) for i in range(2)]
    if router:
        m8 = [k.sb(f"m8{i}", [128, 8]) for i in range(2)]
        lg = [k.sb(f"lg{i}", [128, 8]) for i in range(2)]
        gw = [k.sb(f"gw{i}", [128, 2]) for i in range(2)]
        gt = [k.sb(f"gt{i}", [128, 8]) for i in range(2)]
        eq = [k.sb(f"eq{i}", [128, 8]) for i in range(2)]
    toks = []
    for t in range(NTL):
        i2 = t % 2
        tsl = slice(t * 128, (t + 1) * 128)
        if not blend:
            op("sp", lambda e: e.dma_start(out=it[i2][:], in_=inT[:, tsl].rearrange("(c p) t -> p c t", p=128)), writes=[it[i2]], dma=True)
        else:
            for j in range(2):
                cs_ = slice(j * TC + t * 128, j * TC + (t + 1) * 128)
                op("sp", lambda e: e.dma_start(out=itc[i2][j][:], in_=inT[:, cs_].rearrange("(c p) t -> p c t", p=128)), writes=[itc[i2][j]], dma=True)
            op("dve", lambda e: e.tensor_scalar(out=itc[i2][0][:], in0=itc[i2][0][:], scalar1=msk[:, 0:1], scalar2=None, op0=ALU.mult), reads=[itc[i2][0], msk], writes=[itc[i2][0]])
            op("dve", lambda e: e.scalar_tensor_tensor(out=it[i2][:], in0=itc[i2][1][:], scalar=msk[:, 1:2], in1=itc[i2][0][:], op0=ALU.mult, op1=ALU.add),
               reads=[itc[i2][0], itc[i2][1], msk], writes=[it[i2]])
        op("act", lambda e: e.dma_start(out=xs_[i2][:], in_=x[tsl, :]), writes=[xs_[i2]], dma=True)
        for nb in range(NOUT // 512):
            p = pY[nb]
            for kc in range(KC):
                op("pe", lambda e: e.matmul(p[:, :], lhsT=it[i2][:, kc, :], rhs=Wb[:, kc, nb * 512:(nb + 1) * 512], start=(kc == 0), stop=(kc == KC - 1)),
                   reads=[it[i2], Wb], writes=[p])
        if glu:
            for nb in range(2):
                op("dve", lambda e: e.tensor_tensor(out=sg[i2][:, nb * 512:(nb + 1) * 512], in0=pY[2 + nb][:, :], in1=bglu[:, 1024 + nb * 512:1024 + (nb + 1) * 512], op=ALU.add),
                   reads=[pY[2 + nb], bglu], writes=[sg[i2]])
                op("dve", lambda e: e.tensor_tensor(out=yv[i2][:, nb * 512:(nb + 1) * 512], in0=pY[nb][:, :], in1=bglu[:, nb * 512:(nb + 1) * 512], op=ALU.add),
                   reads=[pY[nb], bglu], writes=[yv[i2]])
            op("act", lambda e: e.activation(out=sg[i2][:], in_=sg[i2][:], func=AF.Sigmoid), reads=[sg[i2]], writes=[sg[i2]])
            op("dve", lambda e: e.tensor_tensor(out=yv[i2][:], in0=yv[i2][:], in1=sg[i2][:], op=ALU.mult), reads=[yv[i2], sg[i2]], writes=[yv[i2]])
        else:
            for nb in range(2):
                op("act", lambda e: e.copy(out=yv[i2][:, nb * 512:(nb + 1) * 512], in_=pY[nb][:, :]), reads=[pY[nb]], writes=[yv[i2]])
        res_ln(k, c, yv[i2][:], [yv[i2]], xs_[i2], G, lng, lnb, tmp[i2], st12[i2], mv[i2], rstd[i2], xo[i2])
        toks.append(op("pool", lambda e: e.dma_start(out=xmid_o[tsl, :], in_=xo[i2][:]), reads=[xo[i2]], dma=True))
        mod_transpose(k, c, xo[i2], modT, 0, pT, uTf[i2], uTb[i2])
        toks.append(op("pool", lambda e: e.dma_start(out=uT_o[:, tsl].rearrange("(c p) t -> p c t", p=128), in_=uTb[i2][:]), reads=[uTb[i2]], dma=True))
        if router:
            for kc in range(8):
                op("pe", lambda e: e.matmul(pM[:, 0:8], lhsT=uTf[i2][:, kc, :], rhs=wr[:, kc, :], start=(kc == 0), stop=(kc == 7)),
                   reads=[uTf[i2], wr], writes=[pM])
            op("dve", lambda e: e.tensor_copy(out=lg[i2][:], in_=pM[:, 0:8]), reads=[pM], writes=[lg[i2]])
            op("dve", lambda e: e.max(out=m8[i2][:], in_=lg[i2][:]), reads=[lg[i2]], writes=[m8[i2]])
            op("dve", lambda e: e.tensor_tensor(out=gw[i2][:, 0:1], in0=m8[i2][:, 1:2], in1=m8[i2][:, 0:1], op=ALU.subtract), reads=[m8[i2]], writes=[gw[i2]])
            op("act", lambda e: e.activation(out=gw[i2][:, 1:2], in_=gw[i2][:, 0:1], func=AF.Sigmoid), reads=[gw[i2]], writes=[gw[i2]])
            op("dve", lambda e: e.tensor_scalar(out=gw[i2][:, 0:1], in0=gw[i2][:, 1:2], scalar1=-1.0, scalar2=1.0, op0=ALU.mult, op1=ALU.add),
               reads=[gw[i2]], writes=[gw[i2]])
            op("dve", lambda e: e.tensor_scalar(out=gt[i2][:], in0=lg[i2][:], scalar1=m8[i2][:, 0:1], scalar2=gw[i2][:, 0:1], op0=ALU.is_equal, op1=ALU.mult),
               reads=[lg[i2], m8[i2], gw[i2]], writes=[gt[i2]])
            op("dve", lambda e: e.tensor_scalar(out=eq[i2][:], in0=lg[i2][:], scalar1=m8[i2][:, 1:2], scalar2=gw[i2][:, 1:2], op0=ALU.is_equal, op1=ALU.mult),
               reads=[lg[i2], m8[i2], gw[i2]], writes=[eq[i2]])
            op("dve", lambda e: e.tensor_tensor(out=gt[i2][:], in0=gt[i2][:], in1=eq[i2][:], op=ALU.add), reads=[gt[i2], eq[i2]], writes=[gt[i2]])
            toks.append(op("pool", lambda e: e.dma_start(out=gates_o[tsl, :], in_=gt[i2][:]), reads=[gt[i2]], dma=True))
    if fz is None:
        k.finish(toks)
    else:
        k.end_stage()
    print("proj instructions", k.n_ins)
    return nc


def build_ffn(TC, NE, T, emit_u, fz=None):
    if fz is None:
        nc = bass.Bass("TRN2", target_bir_lowering=False); k = Ctx(nc); pref = ""
    else:
        nc, k, pref = fz.nc, fz.k, fz.pref
    D = mkD(nc, fz, pref)
    k.begin_stage(pref)
    uT_d = D("uT", [1024, TC], BF16)
    xmid = D("xmid", [TC, 1024])
    w13 = D("w13", [NE, 1024, 5632]); w2 = D("w2", [NE, 2816, 1024])
    if NE > 1:
        gates_d = D("gates", [TC, NE])
    condT_d = D("condT", [128, 8])
    adaw_b = D("adaw_b", [1024, 1024]); adab_b = D("adab_b", [128, 1024])
    lng_d = D("lng", [128, 1024]); lnb_d = D("lnb", [128, 1024])
    if emit_u:
        adaw_f = D("adaw_f", [1024, 2048]); adab_f = D("adab_f", [128, 16])
        uT_o = D("uTn", [1024, TC], BF16, kind="ExternalOutput")
    xout = D("xout", [TC, 1024], kind="ExternalOutput")
    op = k.op
    c = consts(k)
    pH = [k.ps(f"pH{i}", [128, 512]) for i in range(4)]
    pY = [k.ps(f"pY{i}", [128, 512]) for i in range(2)]
    pT = [k.ps("pT0", [128, 512]), k.ps("pT1", [128, 512])]
    stg = [k.sb("stg0", [128, 8, 256]), k.sb("stg1", [128, 8, 256])]
    modT, bts = adaln(k, c, condT_d, adaw_f if emit_u else None, adab_f if emit_u else None, 2 if emit_u else 0, adaw_b, adab_b, 1, pT[0], stg)
    if emit_u:
        op("dve", lambda e: e.tensor_scalar_add(out=modT[:, 8:16], in0=modT[:, 8:16], scalar1=1.0), reads=[modT], writes=[modT])
    G = bts[0]
    op("dve", lambda e: e.tensor_scalar_add(out=G[:], in0=G[:], scalar1=1.0), reads=[G], writes=[G])
    lng = k.sb("lng_s", [128, 1024]); lnb = k.sb("lnb_s", [128, 1024])
    op("sp", lambda e: e.dma_start(out=lng[:], in_=lng_d), writes=[lng], dma=True)
    op("sp", lambda e: e.dma_start(out=lnb[:], in_=lnb_d), writes=[lnb], dma=True)
    NTB = T // 128
    NBLK = TC // T
    if NBLK > 1:
        scr13 = nc.dram_tensor(pref + "scr13", [NE * 22 * 128, 2048], BF16).ap()
        scr2 = nc.dram_tensor(pref + "scr2", [NE * 4 * 128, 11 * 512], BF16).ap()
        S13 = [[Buf(scr13[(ex * 22 + j) * 128:(ex * 22 + j + 1) * 128, :]) for j in range(22)] for ex in range(NE)]
        S2 = [[Buf(scr2[(ex * 4 + q4) * 128:(ex * 4 + q4 + 1) * 128, :]) for q4 in range(4)] for ex in range(NE)]
    uT = k.sb("uT_s", [128, 8, T], BF16)
    aT = k.sb("aT", [128, 22, T], BF16)
    yacc = [k.sb(f"yacc{i}", [128, 1024]) for i in range(NTB)]
    w13b = [k.sb(f"w13b{i}", [128, 8, 256], BF16) for i in range(2)]
    w2s = [k.sb(f"w2s{i}", [128, 512]) for i in range(2)]
    w2b = [k.sb(f"w2b{i}", [128, 11, 512], BF16) for i in range(2)]
    qi = [0]
    sgt = [k.sb(f"sgt{i}", [128, 512], BF16) for i in range(2)]
    if NE > 1:
        gts = k.sb("gts", [128, NTB, NE])
    xs_ = [k.sb(f"xs{i}", [128, 1024]) for i in range(2)]
    tmp = [k.sb(f"tmp{i}", [128, 1024]) for i in range(2)]
    xo = [k.sb(f"xo{i}", [128, 1024]) for i in range(2)]
    st12 = [k.sb(f"st12{i}", [128, 12]) for i in range(2)]
    mv = [k.sb(f"mv{i}", [128, 2]) for i in range(2)]
    rstd = [k.sb(f"rstd{i}", [128, 1]) for i in range(2)]
    uTb = [k.sb(f"uTb{i}", [128, 8, 128], BF16) for i in range(2)]
    toks = []
    ph_i = 0
    dq = 0
    for blk in range(TC // T):
        t0 = blk * T
        op("sp", lambda e: e.dma_start(out=uT[:], in_=uT_d[:, t0:t0 + T].rearrange("(c p) t -> p c t", p=128)), writes=[uT], dma=True)
        if NE > 1:
            op("sp", lambda e: e.dma_start(out=gts[:], in_=gates_d[t0:t0 + T, :].rearrange("(n p) g -> p n g", p=128)), writes=[gts], dma=True)
        for ex in range(NE):
            for j in range(22):
                st = stg[j % 2]; wb = w13b[j % 2]
                if blk == 0:
                    op("sp", lambda e: e.dma_start(out=st[:, :, 0:128], in_=w13[ex, :, j * 128:(j + 1) * 128].rearrange("(c p) f -> p c f", p=128)), writes=[st], dma=True)
                    op("act", lambda e: e.dma_start(out=st[:, :, 128:256], in_=w13[ex, :, 2816 + j * 128:2816 + (j + 1) * 128].rearrange("(c p) f -> p c f", p=128)), writes=[st], dma=True)
                    op("act", lambda e: e.copy(out=wb[:], in_=st[:]), reads=[st], writes=[wb])
                    if NBLK > 1:
                        op("pool", lambda e: e.dma_start(out=S13[ex][j][:, :], in_=wb[:].rearrange("p a b -> p (a b)")), reads=[wb], writes=[S13[ex][j]], dma=True)
                else:
                    op("sp" if j % 2 == 0 else "act", lambda e: e.dma_start(out=wb[:].rearrange("p a b -> p (a b)"), in_=S13[ex][j][:, :]), reads=[S13[ex][j]], writes=[wb], dma=True)
                for tb in range(T // 512):
                    pg = pH[ph_i % 4]; pu = pH[(ph_i + 1) % 4]; ph_i += 2
                    for kc in range(8):
                        op("pe", lambda e: e.matmul(pg[:, :], lhsT=wb[:, kc, 0:128], rhs=uT[:, kc, tb * 512:(tb + 1) * 512], start=(kc == 0), stop=(kc == 7)),
                           reads=[wb, uT], writes=[pg])
                    for kc in range(8):
                        op("pe", lambda e: e.matmul(pu[:, :], lhsT=wb[:, kc, 128:256], rhs=uT[:, kc, tb * 512:(tb + 1) * 512], start=(kc == 0), stop=(kc == 7)),
                           reads=[wb, uT], writes=[pu])
                    s_ = sgt[tb % 2]
                    op("act", lambda e: e.activation(out=s_[:], in_=pg[:, :], func=AF.Silu), reads=[pg], writes=[s_])
                    op("dve", lambda e: e.tensor_tensor(out=aT[:, j, tb * 512:(tb + 1) * 512], in0=s_[:], in1=pu[:, :], op=ALU.mult), reads=[s_, pu], writes=[aT])
            for half in range(2):
                for kh in range(2):
                    wb2 = w2b[qi[0] % 2]; qi[0] += 1
                    q4 = half * 2 + kh
                    if blk == 0:
                        for jj in range(11):
                            j = kh * 11 + jj
                            st = w2s[jj % 2]
                            op("sp" if jj % 2 == 0 else "act", lambda e: e.dma_start(out=st[:], in_=w2[ex, j * 128:(j + 1) * 128, half * 512:(half + 1) * 512]), writes=[st], dma=True)
                            op("act", lambda e: e.copy(out=wb2[:, jj, :], in_=st[:]), reads=[st], writes=[wb2])
                        if NBLK > 1:
                            op("pool", lambda e: e.dma_start(out=S2[ex][q4][:, :], in_=wb2[:].rearrange("p a b -> p (a b)")), reads=[wb2], writes=[S2[ex][q4]], dma=True)
                    else:
                        op("sp" if q4 % 2 == 0 else "act", lambda e: e.dma_start(out=wb2[:].rearrange("p a b -> p (a b)"), in_=S2[ex][q4][:, :]), reads=[S2[ex][q4]], writes=[wb2], dma=True)
                    for tl in range(NTB):
                        p = pY[tl % 2]
                        for jj in range(11):
                            j = kh * 11 + jj
                            op("pe", lambda e: e.matmul(p[:, :], lhsT=aT[:, j, tl * 128:(tl + 1) * 128], rhs=wb2[:, jj, :], start=(jj == 0), stop=(jj == 10)),
                               reads=[aT, wb2], writes=[p])
                        ydst = yacc[tl][:, half * 512:(half + 1) * 512]
                        first = (ex == 0 and kh == 0)
                        if NE == 1:
                            if first:
                                op("act", lambda e: e.copy(out=ydst, in_=p[:, :]), reads=[p], writes=[yacc[tl]])
                            else:
                                op("dve", lambda e: e.tensor_tensor(out=ydst, in0=ydst, in1=p[:, :], op=ALU.add), reads=[p, yacc[tl]], writes=[yacc[tl]])
                        elif first:
                            op("dve", lambda e: e.tensor_scalar(out=ydst, in0=p[:, :], scalar1=gts[:, tl, ex:ex + 1], scalar2=None, op0=ALU.mult),
                               reads=[p, gts], writes=[yacc[tl]])
                        else:
                            op("dve", lambda e: e.scalar_tensor_tensor(out=ydst, in0=p[:, :], scalar=gts[:, tl, ex:ex + 1], in1=ydst, op0=ALU.mult, op1=ALU.add),
                               reads=[p, gts, yacc[tl]], writes=[yacc[tl]])
        for tl in range(NTB):
            i2 = tl % 2
            tsl = slice(t0 + tl * 128, t0 + (tl + 1) * 128)
            op("act", lambda e: e.dma_start(out=xs_[i2][:], in_=xmid[tsl, :]), writes=[xs_[i2]], dma=True)
            res_ln(k, c, yacc[tl][:], [yacc[tl]], xs_[i2], G, lng, lnb, tmp[i2], st12[i2], mv[i2], rstd[i2], xo[i2])
            toks.append(op("pool", lambda e: e.dma_start(out=xout[tsl, :], in_=xo[i2][:]), reads=[xo[i2]], dma=True))
            if emit_u:
                mod_transpose(k, c, xo[i2], modT, 0, pT, None, uTb[i2])
                toks.append(op("pool", lambda e: e.dma_start(out=uT_o[:, tsl].rearrange("(c p) t -> p c t", p=128), in_=uTb[i2][:]), reads=[uTb[i2]], dma=True))
    if fz is None or fz.final:
        k.finish(toks)
    if fz is not None:
        k.end_stage()
    print("ffn instructions", k.n_ins)
    return nc


def bc(v, n=128):
    return np.ascontiguousarray(np.tile(np.asarray(v, np.float32)[None, :], (n, 1)))


def fT(v):
    v = np.asarray(v, np.float32)
    return np.ascontiguousarray(v.reshape(-1, 128).T)


I32 = mybir.dt.int32


def build_s5(S, fz=None, blend=False):
    if fz is None:
        nc = bass.Bass("TRN2", target_bir_lowering=False); k = Ctx(nc); pref = ""
    else:
        nc, k, pref = fz.nc, fz.k, fz.pref
    D = mkD(nc, fz, pref)
    k.begin_stage(pref)
    TC = S // 2
    uT_d = D("uT", [2 * 1024, TC] if blend else [512, S], BF16)
    if blend:
        msk_d = D("msk", [128, 2])
    lamS = D("lamS", [128, 3, 16])
    lamB = D("lamB", [4, 128, 5, 64])
    cC = D("cC", [16, 128, 2, 16])
    dT = D("dT", [128, 4])
    out = D("gT", [512, S], BF16, kind="ExternalOutput")
    op = k.op
    NB = S // 512

    onesf = k.sb("onesf", [128, 128])
    op("pool", lambda e: e.memset(onesf[:], 1.0), writes=[onesf])
    ls = k.sb("ls", [128, 3, 16]); dsk = k.sb("dsk", [128, 4])
    op("sp", lambda e: e.dma_start(out=ls[:], in_=lamS), writes=[ls], dma=True)
    op("sp", lambda e: e.dma_start(out=dsk[:], in_=dT), writes=[dsk], dma=True)
    dtS = k.sb("dtS", [128, 16]); thS = k.sb("thS", [128, 16]); rS = k.sb("rS", [128, 16])
    op("act", lambda e: e.activation(out=dtS[:], in_=ls[:, 2, :], func=AF.Exp), reads=[ls], writes=[dtS])
    op("dve", lambda e: e.tensor_tensor(out=thS[:], in0=ls[:, 1, :], in1=dtS[:], op=ALU.mult), reads=[ls, dtS], writes=[thS])
    op("dve", lambda e: e.tensor_tensor(out=rS[:], in0=ls[:, 0, :], in1=dtS[:], op=ALU.mult), reads=[ls, dtS], writes=[rS])
    op("act", lambda e: e.activation(out=rS[:], in_=rS[:], func=AF.Exp), reads=[rS], writes=[rS])
    ioi = k.sb("ioi", [128, 128], I32); io = k.sb("io", [128, 128])
    op("pool", lambda e: e.iota(ioi[:], pattern=[[1, 128]], base=1, channel_multiplier=0), writes=[ioi])
    op("dve", lambda e: e.tensor_copy(out=io[:], in_=ioi[:]), reads=[ioi], writes=[io])
    ang = k.sb("ang", [128, 16 * 128]); st = k.sb("st", [128, 16 * 128]); ct = k.sb("ct", [128, 16 * 128])
    rtab = k.sb("rtab", [128, 16, 128])
    for q in range(16):
        op("dve", lambda e: e.tensor_scalar(out=ang[:, q * 128:(q + 1) * 128], in0=io[:], scalar1=thS[:, q:q + 1], scalar2=None, op0=ALU.mult),
           reads=[io, thS], writes=[ang])
        op("pool", lambda e: e.tensor_scalar(out=rtab[:, q, :], in0=onesf[:], scalar1=rS[:, q:q + 1], scalar2=None, op0=ALU.mult),
           reads=[onesf, rS], writes=[rtab])
    sincos(k, ang, 16 * 128, st, ct, "scS")
    rm = k.sb("rm", [128, 8])
    op("pool", lambda e: e.memset(rm[:], 1.0), writes=[rm])
    op("pool", lambda e: e.affine_select(out=rm[:], in_=rm[:], pattern=[[-16, 8]], compare_op=ALU.is_ge, fill=0.0, base=0, channel_multiplier=1),
       reads=[rm], writes=[rm])
    op("pool", lambda e: e.affine_select(out=rm[:], in_=rm[:], pattern=[[16, 8]], compare_op=ALU.is_ge, fill=0.0, base=15, channel_multiplier=-1),
       reads=[rm], writes=[rm])
    BtR = [k.sb(f"BtR{q}", [128, 2, 64], BF16) for q in range(16)]
    BtI = [k.sb(f"BtI{q}", [128, 2, 64], BF16) for q in range(16)]
    lb = k.sb("lb", [128, 5, 64])
    W = lambda n: k.sb(n, [128, 64])
    dtB, lrdt, lidt, mag, sB, cB, ar, ai, den, t1, t2, kr, ki, bbr, bbi = [W(n) for n in
        ("dtB", "lrdt", "lidt", "mag", "sB", "cB", "ar", "ai", "den", "t1", "t2", "kr", "ki", "bbr", "bbi")]
    TT = lambda o, a, b, o_: op("dve", lambda e: e.tensor_tensor(out=o[:], in0=a, in1=b, op=o_), reads=[lb, dtB, lrdt, lidt, mag, sB, cB, ar, ai, den, t1, t2, kr, ki], writes=[o])
    for cc in range(4):
        op("sp", lambda e: e.dma_start(out=lb[:], in_=lamB[cc]), writes=[lb], dma=True)
        op("act", lambda e: e.activation(out=dtB[:], in_=lb[:, 2, :], func=AF.Exp), reads=[lb], writes=[dtB])
        TT(lrdt, lb[:, 0, :], dtB[:], ALU.mult)
        TT(lidt, lb[:, 1, :], dtB[:], ALU.mult)
        op("act", lambda e: e.activation(out=mag[:], in_=lrdt[:], func=AF.Exp), reads=[lrdt], writes=[mag])
        sincos(k, lidt, 64, sB, cB, f"scB{cc}")
        TT(ar, mag[:], cB[:], ALU.mult)
        TT(ai, mag[:], sB[:], ALU.mult)
        op("dve", lambda e: e.tensor_scalar_add(out=ar[:], in0=ar[:], scalar1=-1.0), reads=[ar], writes=[ar])
        TT(den, lb[:, 0, :], lb[:, 0, :], ALU.mult)
        TT(t1, lb[:, 1, :], lb[:, 1, :], ALU.mult)
        TT(den, den[:], t1[:], ALU.add)
        op("dve", lambda e: e.reciprocal(out=den[:], in_=den[:]), reads=[den], writes=[den])
        TT(t1, ar[:], lb[:, 0, :], ALU.mult)
        TT(t2, ai[:], lb[:, 1, :], ALU.mult)
        TT(kr, t1[:], t2[:], ALU.add)
        TT(kr, kr[:], den[:], ALU.mult)
        TT(t1, ai[:], lb[:, 0, :], ALU.mult)
        TT(t2, ar[:], lb[:, 1, :], ALU.mult)
        TT(ki, t1[:], t2[:], ALU.subtract)
        TT(ki, ki[:], den[:], ALU.mult)
        TT(t1, kr[:], lb[:, 3, :], ALU.mult)
        TT(t2, ki[:], lb[:, 4, :], ALU.mult)
        op("dve", lambda e: e.tensor_tensor(out=bbr[:], in0=t1[:], in1=t2[:], op=ALU.subtract), reads=[t1, t2], writes=[bbr])
        TT(t1, kr[:], lb[:, 4, :], ALU.mult)
        TT(t2, ki[:], lb[:, 3, :], ALU.mult)
        op("dve", lambda e: e.tensor_tensor(out=bbi[:], in0=t1[:], in1=t2[:], op=ALU.add), reads=[t1, t2], writes=[bbi])
        for ql in range(4):
            q = cc * 4 + ql
            for g2 in range(2):
                op("dve", lambda e: e.tensor_scalar(out=BtR[q][:, g2, :], in0=bbr[:], scalar1=rm[:, 2 * ql + g2:2 * ql + g2 + 1], scalar2=None, op0=ALU.mult),
                   reads=[bbr, rm], writes=[BtR[q]])
                op("dve", lambda e: e.tensor_scalar(out=BtI[q][:, g2, :], in0=bbi[:], scalar1=rm[:, 2 * ql + g2:2 * ql + g2 + 1], scalar2=None, op0=ALU.mult),
                   reads=[bbi, rm], writes=[BtI[q]])
    cst = k.sb("cst", [128, 16, 2, 16])
    op("sp", lambda e: e.dma_start(out=cst[:], in_=cC.rearrange("q s r h -> s q r h")), writes=[cst], dma=True)
    CtR = [k.sb(f"CtR{q}", [128, 128], BF16) for q in range(16)]
    CtI = [k.sb(f"CtI{q}", [128, 128], BF16) for q in range(16)]
    for q in range(16):
        ql = q % 4
        op("pool", lambda e: e.memset(CtR[q][:], 0.0), writes=[CtR[q]])
        op("pool", lambda e: e.memset(CtI[q][:], 0.0), writes=[CtI[q]])
        for g2 in range(2):
            ps_ = slice(64 * g2, 64 * g2 + 64)
            cs_ = slice((2 * ql + g2) * 16, (2 * ql + g2) * 16 + 16)
            op("dve", lambda e: e.tensor_copy(out=CtR[q][ps_, cs_], in_=cst[ps_, q, 0, :]), reads=[cst], writes=[CtR[q]])
            op("dve", lambda e: e.tensor_scalar(out=CtI[q][ps_, cs_], in0=cst[ps_, q, 1, :], scalar1=-1.0, scalar2=None, op0=ALU.mult), reads=[cst], writes=[CtI[q]])
    uT = k.sb("uT_s", [128, 4, S], BF16)
    if not blend:
        for cc in range(4):
            op("sp" if cc % 2 == 0 else "act", lambda e: e.dma_start(out=uT[:, cc, :], in_=uT_d[cc * 128:(cc + 1) * 128, :]), writes=[uT], dma=True)
    else:
        msk = k.sb("msk_s", [128, 2])
        op("sp", lambda e: e.dma_start(out=msk[:], in_=msk_d), writes=[msk], dma=True)
        CW = TC // 4
        cand = [[k.sb(f"cand{i}_{j}", [128, CW], BF16) for j in range(2)] for i in range(2)]
        n_ = 0
        for i in range(2):
            for cc in range(4):
                for qq in range(4):
                    cd = cand[n_ % 2]; n_ += 1
                    for j in range(2):
                        r0 = i * 1024 + j * 512 + cc * 128
                        op("sp" if j == 0 else "act", lambda e: e.dma_start(out=cd[j][:], in_=uT_d[r0:r0 + 128, qq * CW:(qq + 1) * CW]), writes=[cd[j]], dma=True)
                    op("pool", lambda e: e.tensor_scalar(out=cd[0][:], in0=cd[0][:], scalar1=msk[:, 0:1], scalar2=None, op0=ALU.mult), reads=[cd[0], msk], writes=[cd[0]])
                    op("dve", lambda e: e.scalar_tensor_tensor(out=uT[:, cc, i * TC + qq * CW:i * TC + (qq + 1) * CW], in0=cd[1][:], scalar=msk[:, 1:2], in1=cd[0][:], op0=ALU.mult, op1=ALU.add),
                       reads=[cd[0], cd[1], msk], writes=[uT])
    pBr = [k.ps(f"pBr{i}", [128, 512]) for i in range(2)]
    pBi = [k.ps(f"pBi{i}", [128, 512]) for i in range(2)]
    pY = [k.ps(f"pY{i}", [128, 512]) for i in range(2)]
    bur = [k.sb(f"bur{i}", [128, 512]) for i in range(2)]; bui = [k.sb(f"bui{i}", [128, 512]) for i in range(2)]
    bpr = [k.sb(f"bpr{i}", [128, 512]) for i in range(2)]; bpi = [k.sb(f"bpi{i}", [128, 512]) for i in range(2)]
    vr = [k.sb(f"vr{i}", [128, 512]) for i in range(2)]; vi = [k.sb(f"vi{i}", [128, 512]) for i in range(2)]
    pt1 = k.sb("pt1", [128, 512]); pt2 = k.sb("pt2", [128, 512])
    dt1 = k.sb("dt1", [128, 512]); dt2 = k.sb("dt2", [128, 512])
    xr = [k.sb(f"xr{i}", [128, 512], BF16) for i in range(4)]; xi = [k.sb(f"xi{i}", [128, 512], BF16) for i in range(4)]
    car = [k.sb(f"car{q}", [128, 2]) for q in range(16)]
    cat = [k.sb(f"cat{i}", [128, 2]) for i in range(2)]
    for q in range(16):
        op("pool", lambda e: e.memset(car[q][:], 0.0), writes=[car[q]])
    yt = [k.sb(f"yt{i}", [128, 512]) for i in range(2)]; y2 = [k.sb(f"y2{i}", [128, 512]) for i in range(2)]
    go = [k.sb(f"go{i}", [128, 512], BF16) for i in range(2)]
    toks = []
    it = 0
    for blk in range(NB):
        tsl = slice(blk * 512, (blk + 1) * 512)
        for cc in range(4):
            for ql in range(4):
                q = cc * 4 + ql
                i2 = it % 2; it += 1
                c3 = lambda tab: tab[:, q * 128:(q + 1) * 128][:, None, :].to_broadcast([128, 4, 128]) if False else None
                op("pe", lambda e: e.matmul(pBr[i2][:, :], lhsT=BtR[q][:].rearrange("p a b -> p (a b)"), rhs=uT[:, cc, tsl], start=True, stop=True), reads=[BtR[q], uT], writes=[pBr[i2]])
                op("pe", lambda e: e.matmul(pBi[i2][:, :], lhsT=BtI[q][:].rearrange("p a b -> p (a b)"), rhs=uT[:, cc, tsl], start=True, stop=True), reads=[BtI[q], uT], writes=[pBi[i2]])
                op("act", lambda e: e.copy(out=bur[i2][:], in_=pBr[i2][:, :]), reads=[pBr[i2]], writes=[bur[i2]])
                op("act", lambda e: e.copy(out=bui[i2][:], in_=pBi[i2][:, :]), reads=[pBi[i2]], writes=[bui[i2]])
                ctq = ct[:, q * 128:(q + 1) * 128]; stq = st[:, q * 128:(q + 1) * 128]
                ct3 = ctq.rearrange("p (a b) -> p a b", a=1).to_broadcast([128, 4, 128])
                st3 = stq.rearrange("p (a b) -> p a b", a=1).to_broadcast([128, 4, 128])
                v3 = lambda buf: buf[:].rearrange("p (a b) -> p a b", a=4)
                op("pool", lambda e: e.tensor_tensor(out=v3(pt1), in0=v3(bur[i2]), in1=ct3, op=ALU.mult), reads=[bur[i2], ct], writes=[pt1])
                op("pool", lambda e: e.tensor_tensor(out=v3(pt2), in0=v3(bui[i2]), in1=st3, op=ALU.mult), reads=[bui[i2], st], writes=[pt2])
                op("pool", lambda e: e.tensor_tensor(out=bpr[i2][:], in0=pt1[:], in1=pt2[:], op=ALU.add), reads=[pt1, pt2], writes=[bpr[i2]])
                op("dve", lambda e: e.tensor_tensor(out=v3(dt1), in0=v3(bui[i2]), in1=ct3, op=ALU.mult), reads=[bui[i2], ct], writes=[dt1])
                op("dve", lambda e: e.tensor_tensor(out=v3(dt2), in0=v3(bur[i2]), in1=st3, op=ALU.mult), reads=[bur[i2], st], writes=[dt2])
                op("dve", lambda e: e.tensor_tensor(out=bpi[i2][:], in0=dt1[:], in1=dt2[:], op=ALU.subtract), reads=[dt1, dt2], writes=[bpi[i2]])
                for c4 in range(4):
                    cs = slice(c4 * 128, (c4 + 1) * 128)
                    op("dve", lambda e: e.tensor_tensor_scan(out=vr[i2][:, cs], data0=rtab[:, q, :], data1=bpr[i2][:, cs], initial=car[q][:, 0:1], op0=ALU.mult, op1=ALU.add),
                       reads=[rtab, bpr[i2], car[q]], writes=[vr[i2]])
                    op("dve", lambda e: e.tensor_tensor_scan(out=vi[i2][:, cs], data0=rtab[:, q, :], data1=bpi[i2][:, cs], initial=car[q][:, 1:2], op0=ALU.mult, op1=ALU.add),
                       reads=[rtab, bpi[i2], car[q]], writes=[vi[i2]])
                    l = c4 * 128 + 127
                    c128 = ct[:, q * 128 + 127:q * 128 + 128]; s128 = st[:, q * 128 + 127:q * 128 + 128]
                    ca = cat[c4 % 2]
                    op("dve", lambda e: e.tensor_scalar(out=ca[:, 0:1], in0=vi[i2][:, l:l + 1], scalar1=s128, scalar2=None, op0=ALU.mult), reads=[vi[i2], st], writes=[ca])
                    op("dve", lambda e: e.tensor_scalar(out=ca[:, 1:2], in0=vi[i2][:, l:l + 1], scalar1=c128, scalar2=None, op0=ALU.mult), reads=[vi[i2], ct], writes=[ca])
                    op("dve", lambda e: e.scalar_tensor_tensor(out=car[q][:, 0:1], in0=vr[i2][:, l:l + 1], scalar=c128, in1=ca[:, 0:1], op0=ALU.mult, op1=ALU.subtract),
                       reads=[vr[i2], ct, ca], writes=[car[q]])
                    op("dve", lambda e: e.scalar_tensor_tensor(out=car[q][:, 1:2], in0=vr[i2][:, l:l + 1], scalar=s128, in1=ca[:, 1:2], op0=ALU.mult, op1=ALU.add),
                       reads=[vr[i2], st, ca], writes=[car[q]])
                op("dve", lambda e: e.tensor_tensor(out=v3(dt1), in0=v3(vr[i2]), in1=ct3, op=ALU.mult), reads=[vr[i2], ct], writes=[dt1])
                op("dve", lambda e: e.tensor_tensor(out=v3(dt2), in0=v3(vi[i2]), in1=st3, op=ALU.mult), reads=[vi[i2], st], writes=[dt2])
                op("dve", lambda e: e.tensor_tensor(out=xr[ql][:], in0=dt1[:], in1=dt2[:], op=ALU.subtract), reads=[dt1, dt2], writes=[xr[ql]])
                op("dve", lambda e: e.tensor_tensor(out=v3(dt1), in0=v3(vr[i2]), in1=st3, op=ALU.mult), reads=[vr[i2], st], writes=[dt1])
                op("dve", lambda e: e.tensor_tensor(out=v3(dt2), in0=v3(vi[i2]), in1=ct3, op=ALU.mult), reads=[vi[i2], ct], writes=[dt2])
                op("dve", lambda e: e.tensor_tensor(out=xi[ql][:], in0=dt1[:], in1=dt2[:], op=ALU.add), reads=[dt1, dt2], writes=[xi[ql]])
            j2 = (blk * 4 + cc) % 2
            p = pY[j2]
            for ql in range(4):
                q = cc * 4 + ql
                op("pe", lambda e: e.matmul(p[:, :], lhsT=CtR[q][:], rhs=xr[ql][:], start=(ql == 0), stop=False), reads=[CtR[q], xr[ql]], writes=[p])
                op("pe", lambda e: e.matmul(p[:, :], lhsT=CtI[q][:], rhs=xi[ql][:], start=False, stop=(ql == 3)), reads=[CtI[q], xi[ql]], writes=[p])
            y = yt[j2]; z = y2[j2]
            op("dve", lambda e: e.scalar_tensor_tensor(out=y[:], in0=uT[:, cc, tsl], scalar=dsk[:, cc:cc + 1], in1=p[:, :], op0=ALU.mult, op1=ALU.add),
               reads=[uT, dsk, p], writes=[y])
            op("pool", lambda e: e.tensor_tensor(out=z[:], in0=y[:], in1=y[:], op=ALU.mult), reads=[y], writes=[z])
            op("dve", lambda e: e.tensor_scalar(out=z[:], in0=z[:], scalar1=0.044715, scalar2=1.0, op0=ALU.mult, op1=ALU.add), reads=[z], writes=[z])
            op("pool", lambda e: e.tensor_tensor(out=z[:], in0=z[:], in1=y[:], op=ALU.mult), reads=[z, y], writes=[z])
            op("act", lambda e: e.activation(out=z[:], in_=z[:], func=AF.Sigmoid, scale=2.0 * math.sqrt(2.0 / math.pi)), reads=[z], writes=[z])
            op("pool", lambda e: e.tensor_tensor(out=go[j2][:], in0=z[:], in1=y[:], op=ALU.mult), reads=[z, y], writes=[go[j2]])
            toks.append(op("sp", lambda e: e.dma_start(out=out[cc * 128:(cc + 1) * 128, tsl], in_=go[j2][:]), reads=[go[j2]], dma=True))
    if fz is None:
        k.finish(toks)
    else:
        k.end_stage()
    print("s5 instructions", k.n_ins)
    return nc


def s5_inputs(inp, uT_full_b, half, S):
    f = np.float32
    g0 = half * 32
    lr = inp["s5_lambda_re"][0][g0:g0 + 32]; li = inp["s5_lambda_im"][0][g0:g0 + 32]; ld = inp["s5_log_dt"][0][g0:g0 + 32]
    ldx = np.repeat(ld[:, None], 64, axis=1)
    toS = lambda a: a.reshape(16, 2, 64).transpose(1, 2, 0).reshape(128, 16)
    lamS = np.stack([toS(lr), toS(li), toS(ldx)], axis=1)
    br = inp["s5_b_re"][0][g0:g0 + 32]; bi = inp["s5_b_im"][0][g0:g0 + 32]
    rep = lambda a: np.repeat(a[:, None, :], 16, axis=1)
    toB = lambda a: a.reshape(4, 8 * 16, 64)
    lamB = np.stack([toB(rep(lr)), toB(rep(li)), toB(rep(ldx)), toB(br.transpose(0, 2, 1)), toB(bi.transpose(0, 2, 1))], axis=2)
    cr = inp["s5_c_re"][0][g0:g0 + 32]; ci = inp["s5_c_im"][0][g0:g0 + 32]
    toC = lambda a: a.reshape(16, 2, 16, 64).transpose(0, 1, 3, 2).reshape(16, 128, 16)
    cC = np.stack([toC(cr), toC(ci)], axis=2)
    dT = inp["s5_d"][0][half * 512:(half + 1) * 512].reshape(4, 128).T
    return {"uT": np.ascontiguousarray(uT_full_b[half * 512:(half + 1) * 512, :S]),
            "lamS": np.ascontiguousarray(lamS.astype(f)), "lamB": np.ascontiguousarray(lamB.astype(f)),
            "cC": np.ascontiguousarray(cC.astype(f)), "dT": np.ascontiguousarray(dT.astype(f))}


import ml_dtypes as _mld
import os

_CACHE = {}


def build_fused(S, NB_):
    TC = S // 2
    nc = bass.Bass("TRN2", target_bir_lowering=False)
    k = Ctx(nc)
    fz = Fz(nc, k)
    fz.final = False
    I = lambda n, s, dt: nc.dram_tensor(n, list(s), dt).ap()

    def chunked(name, R_, Cn, W):
        W = min(W, Cn)
        src = [I(f"{name}_s{j}", [R_, W], BF16) for j in range(Cn // W)]
        dst = [I(f"{name}_a{j}", [2 * R_, W], BF16) for j in range(Cn // W)]
        return src, dst, ChunkedAP(src, W), ChunkedAP(dst, W)
    hg_s, hg_d, hg_src, hg_all = chunked("i_hg", 1024, S, 1024)
    u1_s, u1_d, u1_src, u1_all = chunked("i_u1", 1024, TC, 1024)
    g_s, g_d, g_src, g_all = chunked("i_g", 512, S, 2048)
    xmid1 = I("i_xmid1", [TC, 1024], F32); uT1 = I("i_uT1", [1024, TC], BF16)
    x1 = I("i_x1", [TC, 1024], F32)
    xmid2 = I("i_xmid2", [TC, 1024], F32); uT2 = I("i_uT2", [1024, TC], BF16); gates = I("i_gates", [TC, 8], F32)
    groups = [[2 * i, 2 * i + 1] for i in range(NB_)]
    fz.pref = "a_"; fz.ext = {"hgT": hg_src}
    build_m0(S, fz=fz)
    k.collective("AllGather", hg_s, hg_d, groups)
    fz.pref = "b_"; fz.ext = {"inT": hg_all, "xmid": xmid1, "uT": uT1}
    build_proj(TC, 16, False, False, fz=fz, blend=True)
    fz.pref = "c_"; fz.ext = {"uT": uT1, "xmid": xmid1, "xout": x1, "uTn": u1_src}
    build_ffn(TC, 1, min(512, TC), True, fz=fz)
    k.collective("AllGather", u1_s, u1_d, groups)
    fz.pref = "d_"; fz.ext = {"uT": u1_all, "gT": g_src}
    build_s5(S, fz=fz, blend=True)
    k.collective("AllGather", g_s, g_d, groups)
    fz.pref = "e_"; fz.ext = {"inT": g_all, "x": x1, "xmid": xmid2, "uT": uT2, "gates": gates}
    build_proj(TC, 8, True, True, fz=fz, blend=True)
    fz.pref = "f_"; fz.ext = {"uT": uT2, "xmid": xmid2, "gates": gates}; fz.final = True
    build_ffn(TC, 8, min(int(os.environ.get('T2', 1024)), TC), False, fz=fz)
    print("fused instructions", k.n_ins)
    return nc


def kernel_impl(inp, S, NB_=4):
    inp = {k_: np.asarray(v) for k_, v in inp.items()}
    NCO = 2 * NB_
    TC = S // 2
    f32 = np.float32
    cT = lambda b: np.ascontiguousarray(inp["c"][b].reshape(8, 128).T.astype(f32))
    aw = inp["ada_w"]; ab = inp["ada_b"]
    C = lambda a: np.ascontiguousarray(a)
    key = ("fused", S, NB_)
    if key not in _CACHE:
        _CACHE[key] = build_fused(S, NB_)
    nc = _CACHE[key]
    shared = {}
    shared["b_W"] = C(inp["mlstm_w_out"][0])
    shared["b_adaw_f"] = C(aw[0][:, 3072:5120]); shared["b_adab_f"] = fT(ab[0][3072:5120])
    shared["b_adaw_b"] = C(aw[0][:, 2048:3072]); shared["b_adab_b"] = bc(ab[0][2048:3072])
    shared["b_lng"] = bc(inp["ln_mix_g"][0]); shared["b_lnb"] = bc(inp["ln_mix_b"][0])
    shared["c_w13"] = C(inp["ffn_w13"]); shared["c_w2"] = C(inp["ffn_w2"])
    shared["c_adaw_b"] = C(aw[0][:, 5120:6144]); shared["c_adab_b"] = bc(ab[0][5120:6144])
    shared["c_lng"] = bc(inp["ln_ffn_g"][0]); shared["c_lnb"] = bc(inp["ln_ffn_b"][0])
    shared["c_adaw_f"] = C(aw[1][:, 0:2048]); shared["c_adab_f"] = fT(ab[1][0:2048])
    shared["e_W"] = C(inp["s5_w_glu"][0])
    shared["e_adaw_f"] = C(aw[1][:, 3072:5120]); shared["e_adab_f"] = fT(ab[1][3072:5120])
    shared["e_adaw_b"] = C(aw[1][:, 2048:3072]); shared["e_adab_b"] = bc(ab[1][2048:3072])
    shared["e_lng"] = bc(inp["ln_mix_g"][1]); shared["e_lnb"] = bc(inp["ln_mix_b"][1])
    shared["e_bglu"] = bc(inp["s5_b_glu"][0])
    shared["e_wr"] = C(inp["moe_router"][0].reshape(8, 128, 8).transpose(1, 0, 2).astype(f32))
    shared["f_w13"] = C(inp["moe_w13"][0]); shared["f_w2"] = C(inp["moe_w2"][0])
    shared["f_adaw_b"] = C(aw[1][:, 5120:6144]); shared["f_adab_b"] = bc(ab[1][5120:6144])
    shared["f_lng"] = bc(inp["ln_ffn_g"][1]); shared["f_lnb"] = bc(inp["ln_ffn_b"][1])
    dummy_u = np.zeros((1024, S), _mld.bfloat16)
    maps = []
    for c in range(NCO):
        b, h = c // 2, c % 2
        tk = slice(h * TC, (h + 1) * TC)
        m = dict(shared)
        for k_, v in m0_inputs(inp, b, h, S).items():
            m["a_" + k_] = v
        msk = np.zeros((128, 2), f32); msk[:, h] = 1.0
        m["b_msk"] = msk; m["d_msk"] = msk; m["e_msk"] = msk
        m["b_x"] = C(inp["x"][b, :S][tk])
        for p_ in "bcef":
            m[p_ + "_condT"] = cT(b)
        for k_, v in s5_inputs(inp, dummy_u, h, S).items():
            if k_ != "uT":
                m["d_" + k_] = v
        maps.append(m)
    res = run_bass_kernel_spmd(nc, maps, core_ids=list(range(NCO)))
    out = np.zeros((NB_, S, 1024), f32)
    for c in range(NCO):
        b, h = c // 2, c % 2
        out[b, h * TC:(h + 1) * TC] = np.asarray(res.results[c]["f_xout"])
    return out


def kernel(**inputs):
    return kernel_impl(inputs, 8192, 4)
```

```python
import math
import numpy as np
from contextlib import ExitStack
import concourse.bass as bass
import concourse.mybir as mybir
from concourse.bass_utils import run_bass_kernel_spmd

F32 = mybir.dt.float32
BF16 = mybir.dt.bfloat16
AF = mybir.ActivationFunctionType
ALU = mybir.AluOpType
AX = mybir.AxisListType

NPOOL = 12
NOSYNC_SAME = ("pe",)


class Buf:
    __slots__ = ("ap", "last_w", "reads")

    def __init__(self, ap):
        self.ap = ap
        self.last_w = None
        self.reads = {}

    def __getitem__(self, idx):
        return self.ap[idx]


class Ctx:
    def __init__(self, nc):
        self.nc = nc
        self.engs = {"pe": nc.tensor, "act": nc.scalar, "dve": nc.vector,
                     "pool": nc.gpsimd, "sp": nc.sync}
        self.sem = {}
        self.cnt = {}
        for n in ("pe", "act", "dve", "pool"):
            self.sem[n] = nc.alloc_semaphore("s_" + n)
            self.cnt[n] = 0
        self.dma_pool = {}
        self.dma_i = {}
        self.known = {n: {} for n in ("pe", "act", "dve", "pool", "sp")}
        self.out_tokens = []
        self.n_ins = 0
        self.stack = None
        self.pref = ""
        self.dma_last = {}
        self.cc_toks = []

    def begin_stage(self, pref=""):
        self.stack = ExitStack()
        self.pref = pref

    def barrier(self):
        toks = [(n, self.sem[n], self.cnt[n]) for n in ("pe", "act", "dve", "pool") if self.cnt[n] > 0]
        for qn, d in self.dma_last.items():
            for i, v in d.items():
                toks.append(("dma_" + qn, self.dma_pool[qn][i], v))
        toks.extend(self.cc_toks)
        for en in ("pe", "act", "dve", "pool", "sp"):
            for tok in toks:
                self._wait(en, tok)

    def end_stage(self):
        self.barrier()
        self.stack.close()
        self.stack = None

    def collective(self, kind, srcs, dsts, groups):
        self.barrier()
        for src_ap, dst_ap in zip(srcs, dsts):
            sem = self.nc.alloc_semaphore("cc_%d" % len(self.cc_toks))
            ins = self.nc.gpsimd.collective_compute(kind, ALU.bypass, replica_groups=groups, ins=[src_ap.opt()], outs=[dst_ap.opt()])
            ins.then_inc(sem)
            self.cc_toks.append(("cc", sem, 1))
        self.barrier()

    def sb(self, name, shape, dtype=F32):
        if self.stack is not None:
            t = self.stack.enter_context(self.nc.sbuf_tensor(self.pref + name, list(shape), dtype))
        else:
            t = self.nc.alloc_sbuf_tensor(name, list(shape), dtype)
        return Buf(t.ap() if hasattr(t, "ap") else t)

    def ps(self, name, shape, dtype=F32):
        if self.stack is not None:
            t = self.stack.enter_context(self.nc.psum_tensor(self.pref + name, list(shape), dtype))
        else:
            t = self.nc.alloc_psum_tensor(name, list(shape), dtype)
        return Buf(t.ap() if hasattr(t, "ap") else t)

    def _wait(self, en, tok):
        src, sem, val = tok
        k = self.known[en]
        if k.get(sem.num, 0) >= val:
            return
        self.engs[en].wait_ge(sem, val)
        k[sem.num] = val

    def op(self, en, fn, reads=(), writes=(), dma=False, q=None):
        deps = []
        for b in reads:
            if b.last_w is not None:
                deps.append(b.last_w)
        for b in writes:
            if b.last_w is not None:
                deps.append(b.last_w)
            deps.extend(b.reads.values())
        for tok in deps:
            src = tok[0]
            if src == en and en in NOSYNC_SAME and not dma:
                continue
            self._wait(en, tok)
        e = self.engs[en]
        if dma:
            qn = en
            if qn not in self.dma_pool:
                self.dma_pool[qn] = [self.nc.alloc_semaphore(f"d_{qn}_{i}") for i in range(NPOOL)]
                self.dma_i[qn] = 0
            i = self.dma_i[qn]
            self.dma_i[qn] = i + 1
            sem = self.dma_pool[qn][i % NPOOL]
            rnd = i // NPOOL
            if rnd > 0:
                self._wait(en, ("dma_" + qn, sem, 16 * rnd))
            ins = fn(e)
            ins.then_inc(sem, 16)
            tok = ("dma_" + qn, sem, 16 * (rnd + 1))
            self.dma_last.setdefault(qn, {})[i % NPOOL] = 16 * (rnd + 1)
        else:
            ins = fn(e)
            self.cnt[en] += 1
            ins.then_inc(self.sem[en], 1)
            tok = (en, self.sem[en], self.cnt[en])
        self.n_ins += 1
        for b in reads:
            b.reads[tok[1].num] = tok
        for b in writes:
            b.last_w = tok
            b.reads = {}
        return tok

    def finish(self, toks):
        for tok in toks:
            self._wait("sp", tok)


class Fz:
    def __init__(self, nc, k):
        self.nc = nc; self.k = k; self.ext = {}; self.pref = ""


def mkD(nc, fz, pref):
    def D(n, s, dt=F32, kind="ExternalInput"):
        if fz is not None and n in fz.ext:
            return fz.ext[n]
        return nc.dram_tensor(pref + n, list(s), dt, kind=kind).ap()
    return D


class ChunkedAP:
    def __init__(self, aps, W):
        self.aps = aps; self.W = W

    def __getitem__(self, idx):
        rs, cs = idx
        j = cs.start // self.W
        assert (cs.stop - 1) // self.W == j
        return self.aps[j][rs, cs.start - j * self.W: cs.stop - j * self.W]

I32 = mybir.dt.int32
def sincos(k, a, N, outs, outc, pref="sc", NCH=1):
    op = k.op
    W_ = N // NCH
    t = k.sb(pref + "_t", [128, W_]); ti = k.sb(pref + "_i", [128, W_], I32); m = k.sb(pref + "_m", [128, W_])
    for ch in range(NCH):
      sl = slice(ch * W_, (ch + 1) * W_)
      for dst, sh in ((outs, 0.0), (outc, 0.5 * math.pi)):
        op("dve", lambda e: e.tensor_scalar(out=t[:], in0=a[:, sl], scalar1=sh, scalar2=1.0 / (2 * math.pi), op0=ALU.add, op1=ALU.mult), reads=[a], writes=[t])
        op("dve", lambda e: e.tensor_copy(out=ti[:], in_=t[:]), reads=[t], writes=[ti])
        op("dve", lambda e: e.tensor_copy(out=m[:], in_=ti[:]), reads=[ti], writes=[m])
        op("dve", lambda e: e.tensor_scalar(out=t[:], in0=a[:, sl], scalar1=sh, scalar2=None, op0=ALU.add), reads=[a], writes=[t])
        op("dve", lambda e: e.scalar_tensor_tensor(out=t[:], in0=m[:], scalar=-2 * math.pi, in1=t[:], op0=ALU.mult, op1=ALU.add), reads=[m, t], writes=[t])
        op("dve", lambda e: e.tensor_scalar(out=m[:], in0=t[:], scalar1=math.pi, scalar2=-2 * math.pi, op0=ALU.is_gt, op1=ALU.mult), reads=[t], writes=[m])
        op("dve", lambda e: e.tensor_tensor(out=t[:], in0=t[:], in1=m[:], op=ALU.add), reads=[t, m], writes=[t])
        op("dve", lambda e: e.tensor_scalar(out=m[:], in0=t[:], scalar1=-math.pi, scalar2=2 * math.pi, op0=ALU.is_lt, op1=ALU.mult), reads=[t], writes=[m])
        op("dve", lambda e: e.tensor_tensor(out=t[:], in0=t[:], in1=m[:], op=ALU.add), reads=[t, m], writes=[t])
        op("dve", lambda e: e.tensor_scalar(out=t[:], in0=t[:], scalar1=math.pi, scalar2=-math.pi, op0=ALU.min, op1=ALU.max), reads=[t], writes=[t])
        op("act", lambda e: e.activation(out=dst[:, sl], in_=t[:], func=AF.Sin), reads=[t], writes=[dst])


DH = 512
TB = 256
NT = TB // 128


def build_m0(S, stage=99, HS=(0, 1), TGT=(0, 0), fz=None):
    if fz is None:
        nc = bass.Bass("TRN2", target_bir_lowering=False); k = Ctx(nc); pref = ""
    else:
        nc, k, pref = fz.nc, fz.k, fz.pref
    D = mkD(nc, fz, pref)
    k.begin_stage(pref)
    x = D("x", [S, 1024])
    condT_d = D("condT", [128, 8])
    adaw = D("adaw", [1024, 2048])
    adabT = D("adabT", [128, 16])
    w_in = D("w_in", [1024, 3072])
    convw = D("convw", [128, 16, 4])
    convb = D("convb", [128, 16])
    wqc = D("wqc", [128, 16, 4]); wkc = D("wkc", [128, 16, 4]); wvc = D("wvc", [128, 16, 4])
    wqt = D("wqt", [128, 16, 4]); wkt = D("wkt", [128, 16, 4]); wvt = D("wvt", [128, 16, 4])
    wgq = D("wgq", [128, 16, 4]); wgk = D("wgk", [128, 16, 4]); wgv = D("wgv", [128, 16, 4])
    bg = D("bg", [128, 4])
    nwT = D("nwT", [128, 8]); skT = D("skT", [128, 8])
    out = D("hgT", [1024, S], BF16, kind="ExternalOutput")

    op = k.op
    NB = S // TB

    identf = k.sb("identf", [128, 128]); ident = k.sb("ident", [128, 128], BF16)
    ntri = k.sb("ntri", [128, 128]); negm = k.sb("negm", [128, 128])
    bmask = k.sb("bmask", [128, 32, 4])
    onesb = k.sb("onesb", [128, 2], BF16); onesf = k.sb("onesf", [128, 128])
    op("pool", lambda e: e.memset(identf[:], 0.0), writes=[identf])
    op("pool", lambda e: e.affine_select(out=identf[:], in_=identf[:], pattern=[[-1, 128]], compare_op=ALU.not_equal,
                                         fill=1.0, base=0, channel_multiplier=1), reads=[identf], writes=[identf])
    op("dve", lambda e: e.tensor_copy(out=ident[:], in_=identf[:]), reads=[identf], writes=[ident])
    op("pool", lambda e: e.memset(ntri[:], -1.0), writes=[ntri])
    op("pool", lambda e: e.affine_select(out=ntri[:], in_=ntri[:], pattern=[[1, 128]], compare_op=ALU.is_ge,
                                         fill=0.0, base=0, channel_multiplier=-1), reads=[ntri], writes=[ntri])
    op("pool", lambda e: e.memset(negm[:], 0.0), writes=[negm])
    op("pool", lambda e: e.affine_select(out=negm[:], in_=negm[:], pattern=[[1, 128]], compare_op=ALU.is_ge,
                                         fill=-30000.0, base=0, channel_multiplier=-1), reads=[negm], writes=[negm])
    op("pool", lambda e: e.memset(bmask[:], 1.0), writes=[bmask])
    op("pool", lambda e: e.affine_select(out=bmask[:], in_=bmask[:], pattern=[[-4, 32], [0, 4]], compare_op=ALU.is_ge,
                                         fill=0.0, base=0, channel_multiplier=1), reads=[bmask], writes=[bmask])
    op("pool", lambda e: e.affine_select(out=bmask[:], in_=bmask[:], pattern=[[4, 32], [0, 4]], compare_op=ALU.is_ge,
                                         fill=0.0, base=3, channel_multiplier=-1), reads=[bmask], writes=[bmask])
    op("pool", lambda e: e.memset(onesb[:], 1.0), writes=[onesb])
    op("pool", lambda e: e.memset(onesf[:], 1.0), writes=[onesf])

    def load(name, src, shape, dt=F32, q="sp"):
        b = k.sb(name, shape, dt)
        op(q, lambda e: e.dma_start(out=b[:], in_=src), writes=[b], dma=True)
        return b
    condT = load("condT_s", condT_d, [128, 8]); adab = load("adab_s", adabT, [128, 16])
    cw = load("cw", convw, [128, 16, 4]); cb = load("cb", convb, [128, 16])
    wc = [load("wqc_s", wqc, [128, 16, 4]), load("wkc_s", wkc, [128, 16, 4]), load("wvc_s", wvc, [128, 16, 4])]
    wt = [load("wqt_s", wqt, [128, 16, 4]), load("wkt_s", wkt, [128, 16, 4]), load("wvt_s", wvt, [128, 16, 4])]
    wg = [load("wgq_s", wgq, [128, 16, 4]), load("wgk_s", wgk, [128, 16, 4]), load("wgv_s", wgv, [128, 16, 4])]
    bgs = load("bgs", bg, [128, 4]); nw = load("nw", nwT, [128, 8]); sk = load("sk", skT, [128, 8])

    pA = k.ps("pA", [128, 512]); pB = k.ps("pB", [128, 512])
    import os
    if os.environ.get("M0V", "0") == "1":
        hbA = k.ps("hbA", [128, 512]); hbB = k.ps("hbB", [128, 512])
        pbh = [hbA, hbA]; pSh = [hbB, hbB]
    else:
        hb = [k.ps("hb0", [128, 512]), k.ps("hb1", [128, 512])]
        pbh = [hb[0], hb[1]]
        pSh = [hb[0], hb[1]]
    pnh = [k.ps("pn0", [128, 512]), k.ps("pn1", [128, 512])]
    pS = pnh[0]
    pt = k.ps("pt", [128, 1024], BF16)
    ptF = k.ps("ptF", [128, 1024], BF16)
    ptFh = [ptF, ptF]
    pacc = [pA, pB]
    pacc_i = [0]

    def nextp():
        pacc_i[0] ^= 1
        return pacc[pacc_i[0]]

    cond = k.sb("cond", [128, 8])
    op("act", lambda e: e.activation(out=cond[:], in_=condT[:], func=AF.Silu), reads=[condT], writes=[cond])
    wst = [k.sb("wst0", [128, 8, 256]), k.sb("wst1", [128, 8, 256])]
    for j in range(16):
        st = wst[j % 2]
        op("sp", lambda e: e.dma_start(out=st[:, :, 0:128], in_=adaw[:, j * 128:(j + 1) * 128].rearrange("(c p) f -> p c f", p=128)),
           writes=[st], dma=True)
        for kc in range(8):
            op("pe", lambda e: e.matmul(pS[:, j:j + 1], lhsT=st[:, kc, 0:128], rhs=cond[:, kc:kc + 1], start=(kc == 0), stop=(kc == 7)),
               reads=[st, cond], writes=[pS])
    modT = k.sb("modT", [128, 16])
    op("dve", lambda e: e.tensor_tensor(out=modT[:], in0=pS[:, 0:16], in1=adab[:], op=ALU.add), reads=[pS, adab], writes=[modT])
    op("dve", lambda e: e.tensor_scalar_add(out=modT[:, 8:16], in0=modT[:, 8:16], scalar1=1.0), reads=[modT], writes=[modT])

    winb = k.sb("winb", [128, 8, 3072], BF16)
    for j in range(12):
        st = wst[j % 2]
        op("sp" if j % 2 == 0 else "act", lambda e: e.dma_start(out=st[:], in_=w_in[:, j * 256:(j + 1) * 256].rearrange("(c p) f -> p c f", p=128)),
           writes=[st], dma=True)
        op("pool", lambda e: e.tensor_copy(out=winb[:, :, j * 256:(j + 1) * 256], in_=st[:]), reads=[st], writes=[winb])

    BD = [[k.sb(f"bd{w}_{j}", [128, 128], BF16) for j in range(8)] for w in range(3)]
    for w in range(3):
        for j in range(8):
            op("dve", lambda e: e.tensor_tensor(out=BD[w][j][:].rearrange("p (n o) -> p n o", o=4),
                                                in0=wc[w][:, j:j + 1, :].to_broadcast([128, 32, 4]), in1=bmask[:], op=ALU.mult),
               reads=[wc[w], bmask], writes=[BD[w][j]])
    bdt = [k.sb("bdt0", [128, 128]), k.sb("bdt1", [128, 128])]
    wcg = k.sb("wcg", [128, 16, 4], BF16); wmg = k.sb("wmg", [128, 16, 4], BF16)
    ii = 0
    for j in range(16):
        for w in range(3):
            t = bdt[ii % 2]; ii += 1
            op("dve", lambda e: e.tensor_tensor(out=t[:].rearrange("p (n i) -> p n i", i=4),
                                                in0=wt[w][:, j:j + 1, :].to_broadcast([128, 32, 4]), in1=bmask[:], op=ALU.mult),
               reads=[wt[w], bmask], writes=[t])
            dst = pS[:, 64 + j * 4: 68 + j * 4] if w < 2 else pS[:, 192 + j * 4: 196 + j * 4]
            op("pe", lambda e: e.matmul(dst, lhsT=t[:], rhs=wg[w][:, j, :], start=(w != 1), stop=(w != 0)),
               reads=[t, wg[w]], writes=[pS])
    op("dve", lambda e: e.tensor_copy(out=wcg[:].rearrange("p a b -> p (a b)"), in_=pS[:, 64:128]), reads=[pS], writes=[wcg])
    op("dve", lambda e: e.tensor_copy(out=wmg[:].rearrange("p a b -> p (a b)"), in_=pS[:, 192:256]), reads=[pS], writes=[wmg])

    xs_ = [k.sb("x0", [128, 1024]), k.sb("x1", [128, 1024])]
    xb_ = [k.sb("xb0", [128, 1024], BF16), k.sb("xb1", [128, 1024], BF16)]
    uT = [k.sb("uT0", [128, 8, TB], BF16), k.sb("uT1", [128, 8, TB], BF16)]
    xmt = [k.sb(f"xmt{i}", [128, TB + 3]) for i in range(3)]
    hist = k.sb("hist", [128, 16, 3])
    acc = [k.sb("acc0", [128, TB]), k.sb("acc1", [128, TB])]
    xmb = k.sb("xmb", [128, 8, TB], BF16); xcT = k.sb("xcT", [128, 8, TB], BF16)
    xo = [k.sb(f"xo{i}", [128, 2, TB], BF16) for i in range(2)]
    sz = k.sb("sz", [128, 8, TB], BF16)
    qT = k.sb("qT", [128, 8, TB], BF16); kT = k.sb("kT", [128, 8, TB], BF16)
    ktm = k.sb("ktm", [128, NT, 1024], BF16); vtm = k.sb("vtm", [128, NT, 1024], BF16)
    hg = [k.sb("hg0", [128, 8, TB], BF16), k.sb("hg1", [128, 8, TB], BF16)]
    gT = k.sb("gT", [4, TB]); gts = k.sb("gts", [128, NT, 4]); sp_ = k.sb("sp_", [128, NT, 2]); ex_ = k.sb("ex_", [128, NT, 2])
    Cst = [k.sb(f"C{h}", [128, 4, 512]) for h in range(2)]
    Cb = [k.sb(f"Cb{h}", [128, 4, 512], BF16) for h in range(2)]
    nst = [k.sb(f"n{h}", [128, 4]) for h in range(2)]
    nb_ = [k.sb(f"nb{h}", [128, 4], BF16) for h in range(2)]
    for h in range(2):
        op("pool", lambda e: e.memset(Cst[h][:], 0.0), writes=[Cst[h]])
        op("pool", lambda e: e.memset(Cb[h][:], 0.0), writes=[Cb[h]])
        op("pool", lambda e: e.memset(nst[h][:], 0.0), writes=[nst[h]])
        op("pool", lambda e: e.memset(nb_[h][:], 0.0), writes=[nb_[h]])
    op("pool", lambda e: e.memset(hist[:], 0.0), writes=[hist])
    sprep = [k.sb(f"sprep{h}", [128, 128]) for h in range(2)]
    bias_s = [k.sb(f"bias{h}", [128, 1]) for h in range(2)]
    DT = [k.sb(f"DT{h}", [128, 128]) for h in range(2)]
    EB = [k.sb(f"EB{h}", [128, 128]) for h in range(2)]
    bL = [k.sb(f"bL{h}", [128, 1]) for h in range(2)]
    ws = [k.sb(f"ws{h}", [128, 1]) for h in range(2)]
    PT = [k.sb(f"PT{h}", [128, 128], BF16) for h in range(2)]
    qp = [k.sb(f"qp{h}", [128, 4, 128], BF16) for h in range(2)]
    wk_ = [k.sb(f"wk{h}", [128, 512], BF16) for h in range(2)]
    rden = [k.sb(f"rden{h}", [128, 1]) for h in range(2)]
    hs = [k.sb(f"hs{h}", [128, 512]) for h in range(2)]
    hn = [k.sb(f"hn{h}", [128, 512], BF16) for h in range(2)]
    st6 = [k.sb(f"st6{h}", [128, 6]) for h in range(2)]
    mv = [k.sb(f"mv{h}", [128, 2]) for h in range(2)]
    rstd = [k.sb(f"rstd{h}", [128, 1]) for h in range(2)]
    tmp2 = [k.sb(f"tmp2{h}", [128, 128]) for h in range(2)]
    tmp3 = [k.sb(f"tmp3{h}", [128, 128]) for h in range(2)]
    eps_t = k.sb("eps_t", [128, 1])
    op("pool", lambda e: e.memset(eps_t[:], 1e-5), writes=[eps_t])
    mhalf = k.sb("mhalf", [128, 1])
    op("pool", lambda e: e.memset(mhalf[:], -0.5), writes=[mhalf])
    one_t = k.sb("one_t", [128, 1])
    op("pool", lambda e: e.memset(one_t[:], 1.0), writes=[one_t])

    def bail():
        tk = op("sp", lambda e: e.dma_start(out=out[:, 0:TB].rearrange("(c p) t -> p c t", p=128), in_=hg[0][:]), reads=[hg[0]], dma=True)
        k.finish([tk])
        print("bail instrs", k.n_ins)
        return nc
    out_toks = []
    for blk in range(NB):
        t0 = blk * TB
        u = uT[blk % 2]
        for t4 in range(NT):
            xs = xs_[t4 % 2]; xb = xb_[t4 % 2]
            op("sp", lambda e: e.dma_start(out=xs[:], in_=x[t0 + t4 * 128: t0 + (t4 + 1) * 128, :]), writes=[xs], dma=True)
            op("act", lambda e: e.copy(out=xb[:], in_=xs[:]), reads=[xs], writes=[xb])
            for kc in range(8):
                op("pe", lambda e: e.transpose(out=pt[:, kc * 128:(kc + 1) * 128], in_=xb[:, kc * 128:(kc + 1) * 128], identity=ident[:]),
                   reads=[xb, ident], writes=[pt])
            for kc in range(8):
                op("dve", lambda e: e.tensor_scalar(out=u[:, kc, t4 * 128:(t4 + 1) * 128], in0=pt[:, kc * 128:(kc + 1) * 128],
                                                    scalar1=modT[:, 8 + kc:9 + kc], scalar2=modT[:, kc:kc + 1], op0=ALU.mult, op1=ALU.add),
                   reads=[pt, modT], writes=[u])
        for oc in range(16):
            p = nextp()
            for kc in range(8):
                op("pe", lambda e: e.matmul(p[:, 0:TB], lhsT=winb[:, kc, oc * 128:(oc + 1) * 128], rhs=u[:, kc, :], start=(kc == 0), stop=(kc == 7)),
                   reads=[winb, u], writes=[p])
            xm = xmt[oc % 3]
            op("dve", lambda e: e.tensor_copy(out=xm[:, 0:3], in_=hist[:, oc, :]), reads=[hist], writes=[xm])
            op("act", lambda e: e.copy(out=xm[:, 3:TB + 3], in_=p[:, 0:TB]), reads=[p], writes=[xm])
            op("dve", lambda e: e.tensor_copy(out=hist[:, oc, :], in_=xm[:, TB:TB + 3]), reads=[xm], writes=[hist])
            xmdst = xmb[:, oc, :] if oc < 8 else xo[oc % 2][:, 1, :]
            xmdb = xmb if oc < 8 else xo[oc % 2]
            op("act", lambda e: e.copy(out=xmdst, in_=xm[:, 3:TB + 3]), reads=[xm], writes=[xmdb])
            a = acc[oc % 2]
            op("dve", lambda e: e.tensor_scalar(out=a[:], in0=xm[:, 3:TB + 3], scalar1=cw[:, oc, 3:4], scalar2=None, op0=ALU.mult),
               reads=[xm, cw], writes=[a])
            for jj in (2, 1, 0):
                op("dve", lambda e: e.scalar_tensor_tensor(out=a[:], in0=xm[:, jj:TB + jj], scalar=cw[:, oc, jj:jj + 1], in1=a[:],
                                                           op0=ALU.mult, op1=ALU.add), reads=[xm, cw, a], writes=[a])
            xcdst = xcT[:, oc, :] if oc < 8 else xo[oc % 2][:, 0, :]
            xcdb = xcT if oc < 8 else xo[oc % 2]
            op("act", lambda e: e.activation(out=xcdst, in_=a[:], func=AF.Silu, bias=cb[:, oc:oc + 1]), reads=[a, cb], writes=[xcdb])
            xmsrc = xmdst
            op("pe", lambda e: e.matmul(pS[0:4, 0:TB], lhsT=wcg[:, oc, :], rhs=xcdst, start=(oc == 0), stop=False), reads=[wcg, xcdb], writes=[pS])
            op("pe", lambda e: e.matmul(pS[0:4, 0:TB], lhsT=wmg[:, oc, :], rhs=xmsrc, start=False, stop=(oc == 15)), reads=[wmg, xmdb], writes=[pS])
        op("act", lambda e: e.copy(out=gT[:], in_=pS[0:4, 0:TB]), reads=[pS], writes=[gT])
        for oc in range(8):
            p = nextp()
            for kc in range(8):
                op("pe", lambda e: e.matmul(p[:, 0:TB], lhsT=winb[:, kc, 2048 + oc * 128: 2048 + (oc + 1) * 128], rhs=u[:, kc, :], start=(kc == 0), stop=(kc == 7)),
                   reads=[winb, u], writes=[p])
            op("act", lambda e: e.activation(out=sz[:, oc, :], in_=p[:, 0:TB], func=AF.Silu), reads=[p], writes=[sz])
        for j in range(8):
            p = nextp()
            op("pe", lambda e: e.matmul(p[:, 0:TB], lhsT=BD[0][j][:], rhs=xcT[:, j, :], start=True, stop=True), reads=[BD[0][j], xcT], writes=[p])
            op("act", lambda e: e.mul(out=qT[:, j, :], in_=p[:, 0:TB], mul=DH ** -0.5), reads=[p], writes=[qT])
            p = nextp()
            op("pe", lambda e: e.matmul(p[:, 0:TB], lhsT=BD[1][j][:], rhs=xcT[:, j, :], start=True, stop=True), reads=[BD[1][j], xcT], writes=[p])
            op("dve", lambda e: e.tensor_copy(out=kT[:, j, :], in_=p[:, 0:TB]), reads=[p], writes=[kT])
        for t4 in range(NT):
            for half in range(2):
                p = nextp()
                for jj in range(4):
                    j = half * 4 + jj
                    op("pe", lambda e: e.matmul(p[:, jj * 128:(jj + 1) * 128], lhsT=xcT[:, j, t4 * 128:(t4 + 1) * 128], rhs=BD[1][j][:], start=True, stop=True),
                       reads=[xcT, BD[1][j]], writes=[p])
                op("act", lambda e: e.copy(out=ktm[:, t4, half * 512:(half + 1) * 512], in_=p[:, :]), reads=[p], writes=[ktm])
                p = nextp()
                for jj in range(4):
                    j = half * 4 + jj
                    op("pe", lambda e: e.matmul(p[:, jj * 128:(jj + 1) * 128], lhsT=xmb[:, j, t4 * 128:(t4 + 1) * 128], rhs=BD[2][j][:], start=True, stop=True),
                       reads=[xmb, BD[2][j]], writes=[p])
                op("dve", lambda e: e.tensor_copy(out=vtm[:, t4, half * 512:(half + 1) * 512], in_=p[:, :]), reads=[p], writes=[vtm])
        for t4 in range(NT):
            op("pe", lambda e: e.matmul(pS[:, 256 + t4 * 4: 260 + t4 * 4], lhsT=gT[:, t4 * 128:(t4 + 1) * 128], rhs=identf[0:4, 0:4], start=True, stop=True),
               reads=[gT, identf], writes=[pS])
        op("dve", lambda e: e.tensor_tensor(out=gts[:], in0=pS[:, 256:256 + 4 * NT].rearrange("p (a b) -> p a b", b=4),
                                            in1=bgs[:, None, :].to_broadcast([128, NT, 4]), op=ALU.add), reads=[pS, bgs], writes=[gts])
        op("act", lambda e: e.activation(out=ex_[:], in_=gts[:, :, 2:4], func=AF.Exp, scale=-1.0), reads=[gts], writes=[ex_])
        op("act", lambda e: e.activation(out=sp_[:], in_=ex_[:], func=AF.Ln, bias=one_t[:]), reads=[ex_, one_t], writes=[sp_])
        hgb = hg[blk % 2]

        def chain(t4, h, hgb=hgb):
            tsl = slice(t4 * 128, (t4 + 1) * 128)
            pb = pbh[h]; pS = pSh[h]; pn = pnh[h]; pt = ptFh[h]; pc = [pA, pB]
            if True:
                hc = slice(h * 512, (h + 1) * 512)
                op("dve", lambda e: e.tensor_scalar(out=sprep[h][:], in0=onesf[:], scalar1=sp_[:, t4, h:h + 1], scalar2=None, op0=ALU.mult),
                   reads=[onesf, sp_], writes=[sprep[h]])
                op("pe", lambda e: e.matmul(pb[:, 0:128], lhsT=sprep[h][:], rhs=ntri[:], start=True, stop=False), reads=[sprep[h], ntri], writes=[pb])
                op("pe", lambda e: e.matmul(pb[:, 0:128], lhsT=identf[:], rhs=negm[:], start=False, stop=True), reads=[identf, negm], writes=[pb])
                op("pe", lambda e: e.matmul(pb[:, 128:256], lhsT=sprep[h][:], rhs=ntri[:], start=True, stop=True), reads=[sprep[h], ntri], writes=[pb])
                op("pe", lambda e: e.matmul(pb[:, 256:258], lhsT=ntri[:], rhs=sp_[:, t4, :], start=True, stop=True), reads=[ntri, sp_], writes=[pb])
                yield
                op("dve", lambda e: e.tensor_tensor(out=bias_s[h][:], in0=gts[:, t4, h:h + 1], in1=pb[:, 256 + h:257 + h], op=ALU.subtract),
                   reads=[gts, pb], writes=[bias_s[h]])
                op("dve", lambda e: e.tensor_copy(out=bL[h][:], in_=pb[:, 255:256]), reads=[pb], writes=[bL[h]])
                yield
                op("act", lambda e: e.activation(out=DT[h][:], in_=pb[:, 0:128], func=AF.Exp, bias=bias_s[h][:]), reads=[pb, bias_s[h], bL[h]], writes=[DT[h]])
                op("act", lambda e: e.activation(out=EB[h][:], in_=pb[:, 128:256], func=AF.Exp), reads=[pb, bL[h]], writes=[EB[h]])
                op("act", lambda e: e.activation(out=ws[h][:], in_=bias_s[h][:], func=AF.Exp, bias=bL[h][:]), reads=[bias_s[h], bL[h]], writes=[ws[h]])
                yield
                for dc in range(4):
                    op("pe", lambda e: e.matmul(pS[:, 260:388], lhsT=kT[:, 4 * h + dc, tsl], rhs=qT[:, 4 * h + dc, tsl], start=(dc == 0), stop=(dc == 3)),
                       reads=[kT, qT], writes=[pS])
                yield
                op("dve", lambda e: e.tensor_tensor(out=PT[h][:], in0=pS[:, 260:388], in1=DT[h][:], op=ALU.mult), reads=[pS, DT[h]], writes=[PT[h]])
                yield
                for dc in range(4):
                    op("dve", lambda e: e.tensor_tensor(out=qp[h][:, dc, :], in0=qT[:, 4 * h + dc, tsl], in1=EB[h][:], op=ALU.mult),
                       reads=[qT, EB[h]], writes=[qp[h]])
                op("dve", lambda e: e.tensor_scalar(out=wk_[h][:], in0=ktm[:, t4, hc], scalar1=ws[h][:], scalar2=None, op0=ALU.mult),
                   reads=[ktm, ws[h]], writes=[wk_[h]])
                yield
                op("pe", lambda e: e.matmul(pn[:, :], lhsT=PT[h][:], rhs=vtm[:, t4, hc], start=True, stop=False), reads=[PT[h], vtm], writes=[pn])
                for dc in range(4):
                    op("pe", lambda e: e.matmul(pn[:, :], lhsT=qp[h][:, dc, :], rhs=Cb[h][:, dc, :], start=False, stop=(dc == 3)),
                       reads=[qp[h], Cb[h]], writes=[pn])
                op("pe", lambda e: e.matmul(pS[:, 388:389], lhsT=PT[h][:], rhs=onesb[:, 0:1], start=True, stop=False), reads=[PT[h], onesb], writes=[pS])
                for dc in range(4):
                    op("pe", lambda e: e.matmul(pS[:, 388:389], lhsT=qp[h][:, dc, :], rhs=nb_[h][:, dc:dc + 1], start=False, stop=(dc == 3)),
                       reads=[qp[h], nb_[h]], writes=[pS])
                yield
                for dc in range(4):
                    pcc = pc[dc % 2]
                    op("pe", lambda e: e.matmul(pcc[:, :], lhsT=wk_[h][:, dc * 128:(dc + 1) * 128], rhs=vtm[:, t4, hc], start=True, stop=True),
                       reads=[wk_[h], vtm], writes=[pcc])
                    op("dve", lambda e: e.scalar_tensor_tensor(out=Cst[h][:, dc, :], in0=Cst[h][:, dc, :], scalar=EB[h][:, 127:128], in1=pcc[:, :],
                                                               op0=ALU.mult, op1=ALU.add), reads=[Cst[h], EB[h], pcc], writes=[Cst[h]])
                    yield
                yield
                op("act", lambda e: e.copy(out=Cb[h][:], in_=Cst[h][:]), reads=[Cst[h]], writes=[Cb[h]])
                for dc in range(4):
                    op("pe", lambda e: e.matmul(pS[:, 392 + dc:393 + dc], lhsT=wk_[h][:, dc * 128:(dc + 1) * 128], rhs=onesb[:, 0:1], start=True, stop=True),
                       reads=[wk_[h], onesb], writes=[pS])
                yield
                op("dve", lambda e: e.tensor_scalar(out=rden[h][:], in0=pS[:, 388:389], scalar1=-1.0, scalar2=None, op0=ALU.mult),
                   reads=[pS], writes=[rden[h]])
                op("dve", lambda e: e.tensor_tensor(out=rden[h][:], in0=rden[h][:], in1=pS[:, 388:389], op=ALU.max),
                   reads=[pS, rden[h]], writes=[rden[h]])
                op("dve", lambda e: e.tensor_scalar(out=rden[h][:], in0=rden[h][:], scalar1=1.0, scalar2=None, op0=ALU.max),
                   reads=[rden[h]], writes=[rden[h]])
                op("dve", lambda e: e.scalar_tensor_tensor(out=nst[h][:], in0=nst[h][:], scalar=EB[h][:, 127:128], in1=pS[:, 392:396],
                                                           op0=ALU.mult, op1=ALU.add), reads=[nst[h], EB[h], pS], writes=[nst[h]])
                op("dve", lambda e: e.tensor_copy(out=nb_[h][:], in_=nst[h][:]), reads=[nst[h]], writes=[nb_[h]])
                op("dve", lambda e: e.reciprocal(out=rden[h][:], in_=rden[h][:]), reads=[rden[h]], writes=[rden[h]])
                op("dve", lambda e: e.tensor_scalar(out=hs[h][:], in0=pn[:, :], scalar1=rden[h][:], scalar2=None, op0=ALU.mult), reads=[pn, rden[h]], writes=[hs[h]])
                yield
                op("dve", lambda e: e.bn_stats(out=st6[h][:], in_=hs[h][:]), reads=[hs[h]], writes=[st6[h]])
                op("dve", lambda e: e.bn_aggr(out=mv[h][:], in_=st6[h][:]), reads=[st6[h]], writes=[mv[h]])
                yield
                op("pool", lambda e: e.tensor_scalar(out=rstd[h][:], in0=mv[h][:, 1:2], scalar1=1e-5, scalar2=None, op0=ALU.add), reads=[mv[h]], writes=[rstd[h]])
                op("pool", lambda e: e.tensor_tensor(out=rstd[h][:], in0=rstd[h][:], in1=mhalf[:], op=ALU.pow), reads=[rstd[h], mhalf], writes=[rstd[h]])
                yield
                op("dve", lambda e: e.tensor_scalar(out=hn[h][:], in0=hs[h][:], scalar1=mv[h][:, 0:1], scalar2=rstd[h][:], op0=ALU.subtract, op1=ALU.mult),
                   reads=[hs[h], mv[h], rstd[h]], writes=[hn[h]])
                yield
                for dc in range(4):
                    op("pe", lambda e: e.transpose(out=pt[:, h * 512 + dc * 128:h * 512 + (dc + 1) * 128], in_=hn[h][:, dc * 128:(dc + 1) * 128], identity=ident[:]),
                       reads=[hn[h], ident], writes=[pt])
                yield
                for dc in range(4):
                    j = 4 * h + dc
                    op("dve", lambda e: e.tensor_scalar(out=tmp2[h][:], in0=xcT[:, j, tsl], scalar1=sk[:, j:j + 1], scalar2=None, op0=ALU.mult),
                       reads=[xcT, sk], writes=[tmp2[h]])
                    op("dve", lambda e: e.scalar_tensor_tensor(out=tmp3[h][:], in0=pt[:, h * 512 + dc * 128:h * 512 + (dc + 1) * 128], scalar=nw[:, j:j + 1], in1=tmp2[h][:],
                                                               op0=ALU.mult, op1=ALU.add), reads=[pt, nw, tmp2[h]], writes=[tmp3[h]])
                    op("dve", lambda e: e.tensor_tensor(out=hgb[:, j, tsl], in0=tmp3[h][:], in1=sz[:, j, tsl], op=ALU.mult),
                       reads=[tmp3[h], sz], writes=[hgb])
                    yield
        for t4 in range(NT):
            gens = [chain(t4, h) for h in HS]
            while gens:
                for g_ in list(gens):
                    try:
                        next(g_)
                    except StopIteration:
                        gens.remove(g_)
        tk = op("pool", lambda e: e.dma_start(out=out[:, t0:t0 + TB].rearrange("(c p) t -> p c t", p=128), in_=hgb[:]), reads=[hgb], dma=True)
        out_toks.append(tk)
    if fz is None:
        k.finish(out_toks)
    else:
        k.end_stage()
    print("m0 instructions:", k.n_ins)
    return nc


def m0_inputs(inp, b, hp, S):
    f = np.float32
    own = np.arange(hp * 1024, (hp + 1) * 1024); oth = np.arange((1 - hp) * 1024, (2 - hp) * 1024)
    ch = np.concatenate([own, oth])
    w_in = inp["mlstm_w_in"][0]
    d = {}
    d["x"] = np.ascontiguousarray(inp["x"][b, :S])
    d["condT"] = np.ascontiguousarray(inp["c"][b].reshape(8, 128).T)
    d["adaw"] = np.ascontiguousarray(inp["ada_w"][0][:, 0:2048])
    d["adabT"] = np.ascontiguousarray(inp["ada_b"][0][0:2048].reshape(16, 128).T)
    d["w_in"] = np.ascontiguousarray(np.concatenate([w_in[:, ch], w_in[:, 2048 + own]], axis=1))
    d["convw"] = np.ascontiguousarray(inp["mlstm_conv_w"][0].T[ch].reshape(16, 128, 4).transpose(1, 0, 2))
    d["convb"] = np.ascontiguousarray(inp["mlstm_conv_b"][0][ch].reshape(16, 128).T)
    blk = ch.reshape(-1, 4)[:, 0] // 4
    for nm, key in (("q", "mlstm_wq"), ("k", "mlstm_wk"), ("v", "mlstm_wv")):
        w = inp[key][0][blk]
        d["w%sc" % nm] = np.ascontiguousarray(w.reshape(16, 128, 4).transpose(1, 0, 2))
        d["w%st" % nm] = np.ascontiguousarray(w.transpose(0, 2, 1).reshape(16, 128, 4).transpose(1, 0, 2))
    gcols = [2 * hp, 2 * hp + 1, 4 + 2 * hp, 5 + 2 * hp]
    wgates = inp["mlstm_w_gates"][0]
    for i, nm in enumerate("qkv"):
        wg = wgates[i * 2048:(i + 1) * 2048][ch][:, gcols]
        d["wg" + nm] = np.ascontiguousarray(wg.reshape(16, 128, 4).transpose(1, 0, 2))
    d["bg"] = np.ascontiguousarray(np.tile(inp["mlstm_b_gates"][0][gcols][None, :], (128, 1)))
    d["nwT"] = np.ascontiguousarray(inp["mlstm_norm_w"][0][own].reshape(8, 128).T)
    d["skT"] = np.ascontiguousarray(inp["mlstm_skip"][0][own].reshape(8, 128).T)
    return {k_: v.astype(f) for k_, v in d.items()}


ALPHA = 4 ** 0.25
EPS = 1e-5


def consts(k):
    op = k.op
    c = {}
    c["identf"] = k.sb("identf", [128, 128])
    c["onesf"] = k.sb("onesf", [128, 128])
    c["mhalf"] = k.sb("mhalf", [128, 1])
    op("pool", lambda e: e.memset(c["identf"][:], 0.0), writes=[c["identf"]])
    op("pool", lambda e: e.affine_select(out=c["identf"][:], in_=c["identf"][:], pattern=[[-1, 128]], compare_op=ALU.not_equal,
                                         fill=1.0, base=0, channel_multiplier=1), reads=[c["identf"]], writes=[c["identf"]])
    op("pool", lambda e: e.memset(c["onesf"][:], 1.0), writes=[c["onesf"]])
    op("pool", lambda e: e.memset(c["mhalf"][:], -0.5), writes=[c["mhalf"]])
    return c


def adaln(k, c, condT_d, adaw_f, adab_f, nf, adaw_b, adab_b, nb, pbank, stg):
    op = k.op
    condT = k.sb("condT_s", [128, 8]); cond = k.sb("cond", [128, 8])
    op("sp", lambda e: e.dma_start(out=condT[:], in_=condT_d), writes=[condT], dma=True)
    op("act", lambda e: e.activation(out=cond[:], in_=condT[:], func=AF.Silu), reads=[condT], writes=[cond])
    modT = None
    if nf:
        modT = k.sb("modT", [128, nf * 8]); adab = k.sb("adabf", [128, nf * 8])
        op("sp", lambda e: e.dma_start(out=adab[:], in_=adab_f), writes=[adab], dma=True)
        for j in range(nf * 8):
            st = stg[j % 2]
            op("sp", lambda e: e.dma_start(out=st[:, :, 0:128], in_=adaw_f[:, j * 128:(j + 1) * 128].rearrange("(c p) f -> p c f", p=128)), writes=[st], dma=True)
            for kc in range(8):
                op("pe", lambda e: e.matmul(pbank[:, j:j + 1], lhsT=st[:, kc, 0:128], rhs=cond[:, kc:kc + 1], start=(kc == 0), stop=(kc == 7)),
                   reads=[st, cond], writes=[pbank])
        op("dve", lambda e: e.tensor_tensor(out=modT[:], in0=pbank[:, 0:nf * 8], in1=adab[:], op=ALU.add), reads=[pbank, adab], writes=[modT])
    bts = []
    if nb:
        crep = k.sb("crep", [128, 8, 128])
        for kc in range(8):
            op("dve", lambda e: e.tensor_scalar(out=crep[:, kc, :], in0=c["onesf"][:], scalar1=cond[:, kc:kc + 1], scalar2=None, op0=ALU.mult),
               reads=[c["onesf"], cond], writes=[crep])
        for v in range(nb):
            bt = k.sb(f"bt{v}", [128, 1024])
            op("sp", lambda e: e.dma_start(out=bt[:], in_=adab_b[:, v * 1024:(v + 1) * 1024]), writes=[bt], dma=True)
            for q in range(4):
                st = stg[q % 2]
                op("sp", lambda e: e.dma_start(out=st[:], in_=adaw_b[:, v * 1024 + q * 256: v * 1024 + (q + 1) * 256].rearrange("(c p) f -> p c f", p=128)),
                   writes=[st], dma=True)
                for kc in range(8):
                    op("pe", lambda e: e.matmul(pbank[:, 0:256], lhsT=crep[:, kc, :], rhs=st[:, kc, :], start=(kc == 0), stop=(kc == 7)),
                       reads=[crep, st], writes=[pbank])
                op("dve", lambda e: e.tensor_tensor(out=bt[:, q * 256:(q + 1) * 256], in0=bt[:, q * 256:(q + 1) * 256], in1=pbank[:, 0:256], op=ALU.add),
                   reads=[bt, pbank], writes=[bt])
            bts.append(bt)
    return modT, bts


def res_ln(k, c, ysrc, ybuf, xs, G, lng, lnb, tmp, st12, mv, rstd, xo):
    op = k.op
    op("dve", lambda e: e.tensor_tensor(out=tmp[:], in0=ysrc, in1=G[:], op=ALU.mult), reads=ybuf + [G], writes=[tmp])
    op("dve", lambda e: e.scalar_tensor_tensor(out=tmp[:], in0=xs[:], scalar=ALPHA, in1=tmp[:], op0=ALU.mult, op1=ALU.add), reads=[xs, tmp], writes=[tmp])
    for hh in range(2):
        op("dve", lambda e: e.bn_stats(out=st12[:, hh * 6:(hh + 1) * 6], in_=tmp[:, hh * 512:(hh + 1) * 512]), reads=[tmp], writes=[st12])
    op("dve", lambda e: e.bn_aggr(out=mv[:], in_=st12[:]), reads=[st12], writes=[mv])
    op("pool", lambda e: e.tensor_scalar(out=rstd[:], in0=mv[:, 1:2], scalar1=EPS, scalar2=None, op0=ALU.add), reads=[mv], writes=[rstd])
    op("pool", lambda e: e.tensor_tensor(out=rstd[:], in0=rstd[:], in1=c["mhalf"][:], op=ALU.pow), reads=[rstd, c["mhalf"]], writes=[rstd])
    op("dve", lambda e: e.tensor_scalar(out=tmp[:], in0=tmp[:], scalar1=mv[:, 0:1], scalar2=rstd[:], op0=ALU.subtract, op1=ALU.mult),
       reads=[tmp, mv, rstd], writes=[tmp])
    op("dve", lambda e: e.tensor_tensor(out=tmp[:], in0=tmp[:], in1=lng[:], op=ALU.mult), reads=[tmp, lng], writes=[tmp])
    op("dve", lambda e: e.tensor_tensor(out=xo[:], in0=tmp[:], in1=lnb[:], op=ALU.add), reads=[tmp, lnb], writes=[xo])


def mod_transpose(k, c, xo, modT, m0, pT, uTf, uTb):
    op = k.op
    for kc in range(8):
        op("pe", lambda e: e.transpose(out=pT[kc // 4][:, (kc % 4) * 128:(kc % 4 + 1) * 128], in_=xo[:, kc * 128:(kc + 1) * 128], identity=c["identf"][:]),
           reads=[xo, c["identf"]], writes=[pT[kc // 4]])
    for kc in range(8):
        dst = uTf if uTf is not None else uTb
        op("dve", lambda e: e.tensor_scalar(out=dst[:, kc, :], in0=pT[kc // 4][:, (kc % 4) * 128:(kc % 4 + 1) * 128],
                                            scalar1=modT[:, m0 + 8 + kc:m0 + 9 + kc], scalar2=modT[:, m0 + kc:m0 + kc + 1], op0=ALU.mult, op1=ALU.add),
           reads=[pT[kc // 4], modT], writes=[dst])
    if uTf is not None:
        op("act", lambda e: e.copy(out=uTb[:], in_=uTf[:]), reads=[uTf], writes=[uTb])


def build_proj(TC, KC, glu, router, fz=None, blend=False):
    if fz is None:
        nc = bass.Bass("TRN2", target_bir_lowering=False); k = Ctx(nc); pref = ""
    else:
        nc, k, pref = fz.nc, fz.k, fz.pref
    D = mkD(nc, fz, pref)
    k.begin_stage(pref)
    NOUT = 2048 if glu else 1024
    inT = D("inT", [KC * 128, TC * (2 if blend else 1)], BF16)
    if blend:
        msk_d = D("msk", [128, 2])
    W = D("W", [KC * 128, NOUT])
    x = D("x", [TC, 1024])
    condT_d = D("condT", [128, 8])
    adaw_f = D("adaw_f", [1024, 2048]); adab_f = D("adab_f", [128, 16])
    adaw_b = D("adaw_b", [1024, 1024]); adab_b = D("adab_b", [128, 1024])
    lng_d = D("lng", [128, 1024]); lnb_d = D("lnb", [128, 1024])
    if glu:
        bglu_d = D("bglu", [128, 2048])
    if router:
        wr_d = D("wr", [128, 8, 8])
        gates_o = D("gates", [TC, 8], kind="ExternalOutput")
    xmid_o = D("xmid", [TC, 1024], kind="ExternalOutput")
    uT_o = D("uT", [1024, TC], BF16, kind="ExternalOutput")
    op = k.op
    c = consts(k)
    if blend:
        msk = k.sb("msk_s", [128, 2])
        op("sp", lambda e: e.dma_start(out=msk[:], in_=msk_d), writes=[msk], dma=True)
        itc = [[k.sb(f"itc{i}_{j}", [128, KC, 128], BF16) for j in range(2)] for i in range(2)]
    pY = [k.ps(f"pY{i}", [128, 512]) for i in range(4)]
    pT = [k.ps("pT0", [128, 512]), k.ps("pT1", [128, 512])]
    pM = k.ps("pM", [128, 512])
    stg = [k.sb("stg0", [128, 8, 256]), k.sb("stg1", [128, 8, 256])]
    modT, bts = adaln(k, c, condT_d, adaw_f, adab_f, 2, adaw_b, adab_b, 1, pM, stg)
    op("dve", lambda e: e.tensor_scalar_add(out=modT[:, 8:16], in0=modT[:, 8:16], scalar1=1.0), reads=[modT], writes=[modT])
    G = bts[0]
    op("dve", lambda e: e.tensor_scalar_add(out=G[:], in0=G[:], scalar1=1.0), reads=[G], writes=[G])
    lng = k.sb("lng_s", [128, 1024]); lnb = k.sb("lnb_s", [128, 1024])
    op("sp", lambda e: e.dma_start(out=lng[:], in_=lng_d), writes=[lng], dma=True)
    op("sp", lambda e: e.dma_start(out=lnb[:], in_=lnb_d), writes=[lnb], dma=True)
    if glu:
        bglu = k.sb("bglu_s", [128, 2048])
        op("sp", lambda e: e.dma_start(out=bglu[:], in_=bglu_d), writes=[bglu], dma=True)
    if router:
        wr = k.sb("wr_s", [128, 8, 8])
        op("sp", lambda e: e.dma_start(out=wr[:], in_=wr_d), writes=[wr], dma=True)
    Wb = k.sb("Wb", [128, KC, NOUT], BF16)
    ws2 = [k.sb("ws2_0", [128, NOUT]), k.sb("ws2_1", [128, NOUT])]
    for kc in range(KC):
        st = ws2[kc % 2]
        op("sp" if kc % 2 == 0 else "act", lambda e: e.dma_start(out=st[:], in_=W[kc * 128:(kc + 1) * 128, :]), writes=[st], dma=True)
        op("pool", lambda e: e.tensor_copy(out=Wb[:, kc, :], in_=st[:]), reads=[st], writes=[Wb])
    NTL = TC // 128
    it = [k.sb(f"it{i}", [128, KC, 128], BF16) for i in range(2)]
    xs_ = [k.sb(f"xs{i}", [128, 1024]) for i in range(2)]
    tmp = [k.sb(f"tmp{i}", [128, 1024]) for i in range(2)]
    xo = [k.sb(f"xo{i}", [128, 1024]) for i in range(2)]
    yv = [k.sb(f"yv{i}", [128, 1024]) for i in range(2)]
    sg = [k.sb(f"sg{i}", [128, 1024]) for i in range(2)]
    st12 = [k.sb(f"st12{i}", [128, 12]) for i in range(2)]
    mv = [k.sb(f"mv{i}", [128, 2]) for i in range(2)]
    rstd = [k.sb(f"rstd{i}", [128, 1]) for i in range(2)]
    uTf = [k.sb(f"uTf{i}", [128, 8, 128]) for i in range(2)]
    uTb = [k.sb(f"uTb{i}", [128, 8, 128], BF16) for i in range(2)]
    if router:
        m8 = [k.sb(f"m8{i}", [128, 8]) for i in range(2)]
        lg = [k.sb(f"lg{i}", [128, 8]) for i in range(2)]
        gw = [k.sb(f"gw{i}", [128, 2]) for i in range(2)]
        gt = [k.sb(f"gt{i}", [128, 8]) for i in range(2)]
        eq = [k.sb(f"eq{i}", [128, 8]) for i in range(2)]
    toks = []
    for t in range(NTL):
        i2 = t % 2
        tsl = slice(t * 128, (t + 1) * 128)
        if not blend:
            op("sp", lambda e: e.dma_start(out=it[i2][:], in_=inT[:, tsl].rearrange("(c p) t -> p c t", p=128)), writes=[it[i2]], dma=True)
        else:
            for j in range(2):
                cs_ = slice(j * TC + t * 128, j * TC + (t + 1) * 128)
                op("sp", lambda e: e.dma_start(out=itc[i2][j][:], in_=inT[:, cs_].rearrange("(c p) t -> p c t", p=128)), writes=[itc[i2][j]], dma=True)
            op("dve", lambda e: e.tensor_scalar(out=itc[i2][0][:], in0=itc[i2][0][:], scalar1=msk[:, 0:1], scalar2=None, op0=ALU.mult), reads=[itc[i2][0], msk], writes=[itc[i2][0]])
            op("dve", lambda e: e.scalar_tensor_tensor(out=it[i2][:], in0=itc[i2][1][:], scalar=msk[:, 1:2], in1=itc[i2][0][:], op0=ALU.mult, op1=ALU.add),
               reads=[itc[i2][0], itc[i2][1], msk], writes=[it[i2]])
        op("act", lambda e: e.dma_start(out=xs_[i2][:], in_=x[tsl, :]), writes=[xs_[i2]], dma=True)
        for nb in range(NOUT // 512):
            p = pY[nb]
            for kc in range(KC):
                op("pe", lambda e: e.matmul(p[:, :], lhsT=it[i2][:, kc, :], rhs=Wb[:, kc, nb * 512:(nb + 1) * 512], start=(kc == 0), stop=(kc == KC - 1)),
                   reads=[it[i2], Wb], writes=[p])
        if glu:
            for nb in range(2):
                op("dve", lambda e: e.tensor_tensor(out=sg[i2][:, nb * 512:(nb + 1) * 512], in0=pY[2 + nb][:, :], in1=bglu[:, 1024 + nb * 512:1024 + (nb + 1) * 512], op=ALU.add),
                   reads=[pY[2 + nb], bglu], writes=[sg[i2]])
                op("dve", lambda e: e.tensor_tensor(out=yv[i2][:, nb * 512:(nb + 1) * 512], in0=pY[nb][:, :], in1=bglu[:, nb * 512:(nb + 1) * 512], op=ALU.add),
                   reads=[pY[nb], bglu], writes=[yv[i2]])
            op("act", lambda e: e.activation(out=sg[i2][:], in_=sg[i2][:], func=AF.Sigmoid), reads=[sg[i2]], writes=[sg[i2]])
            op("dve", lambda e: e.tensor_tensor(out=yv[i2][:], in0=yv[i2][:], in1=sg[i2][:], op=ALU.mult), reads=[yv[i2], sg[i2]], writes=[yv[i2]])
        else:
            for nb in range(2):
                op("act", lambda e: e.copy(out=yv[i2][:, nb * 512:(nb + 1) * 512], in_=pY[nb][:, :]), reads=[pY[nb]], writes=[yv[i2]])
        res_ln(k, c, yv[i2][:], [yv[i2]], xs_[i2], G, lng, lnb, tmp[i2], st12[i2], mv[i2], rstd[i2], xo[i2])
        toks.append(op("pool", lambda e: e.dma_start(out=xmid_o[tsl, :], in_=xo[i2][:]), reads=[xo[i2]], dma=True))
        mod_transpose(k, c, xo[i2], modT, 0, pT, uTf[i2], uTb[i2])
        toks.append(op("pool", lambda e: e.dma_start(out=uT_o[:, tsl].rearrange("(c p) t -> p c t", p=128), in_=uTb[i2][:]), reads=[uTb[i2]], dma=True))
        if router:
            for kc in range(8):
                op("pe", lambda e: e.matmul(pM[:, 0:8], lhsT=uTf[i2][:, kc, :], rhs=wr[:, kc, :], start=(kc == 0), stop=(kc == 7)),
                   reads=[uTf[i2], wr], writes=[pM])
            op("dve", lambda e: e.tensor_copy(out=lg[i2][:], in_=pM[:, 0:8]), reads=[pM], writes=[lg[i2]])
            op("dve", lambda e: e.max(out=m8[i2][:], in_=lg[i2][:]), reads=[lg[i2]], writes=[m8[i2]])
            op("dve", lambda e: e.tensor_tensor(out=gw[i2][:, 0:1], in0=m8[i2][:, 1:2], in1=m8[i2][:, 0:1], op=ALU.subtract), reads=[m8[i2]], writes=[gw[i2]])
            op("act", lambda e: e.activation(out=gw[i2][:, 1:2], in_=gw[i2][:, 0:1], func=AF.Sigmoid), reads=[gw[i2]], writes=[gw[i2]])
            op("dve", lambda e: e.tensor_scalar(out=gw[i2][:, 0:1], in0=gw[i2][:, 1:2], scalar1=-1.0, scalar2=1.0, op0=ALU.mult, op1=ALU.add),
               reads=[gw[i2]], writes=[gw[i2]])
            op("dve", lambda e: e.tensor_scalar(out=gt[i2][:], in0=lg[i2][:], scalar1=m8[i2][:, 0:1], scalar2=gw[i2][:, 0:1], op0=ALU.is_equal, op1=ALU.mult),
               reads=[lg[i2], m8[i2], gw[i2]], writes=[gt[i2]])
            op("dve", lambda e: e.tensor_scalar(out=eq[i2][:], in0=lg[i2][:], scalar1=m8[i2][:, 1:2], scalar2=gw[i2][:, 1:2], op0=ALU.is_equal, op1=ALU.mult),
               reads=[lg[i2], m8[i2], gw[i2]], writes=[eq[i2]])
            op("dve", lambda e: e.tensor_tensor(out=gt[i2][:], in0=gt[i2][:], in1=eq[i2][:], op=ALU.add), reads=[gt[i2], eq[i2]], writes=[gt[i2]])
            toks.append(op("pool", lambda e: e.dma_start(out=gates_o[tsl, :], in_=gt[i2][:]), reads=[gt[i2]], dma=True))
    if fz is None:
        k.finish(toks)
    else:
        k.end_stage()
    print("proj instructions", k.n_ins)
    return nc


def build_ffn(TC, NE, T, emit_u, fz=None):
    if fz is None:
        nc = bass.Bass("TRN2", target_bir_lowering=False); k = Ctx(nc); pref = ""
    else:
        nc, k, pref = fz.nc, fz.k, fz.pref
    D = mkD(nc, fz, pref)
    k.begin_stage(pref)
    uT_d = D("uT", [1024, TC], BF16)
    xmid = D("xmid", [TC, 1024])
    w13 = D("w13", [NE, 1024, 5632]); w2 = D("w2", [NE, 2816, 1024])
    if NE > 1:
        gates_d = D("gates", [TC, NE])
    condT_d = D("condT", [128, 8])
    adaw_b = D("adaw_b", [1024, 1024]); adab_b = D("adab_b", [128, 1024])
    lng_d = D("lng", [128, 1024]); lnb_d = D("lnb", [128, 1024])
    if emit_u:
        adaw_f = D("adaw_f", [1024, 2048]); adab_f = D("adab_f", [128, 16])
        uT_o = D("uTn", [1024, TC], BF16, kind="ExternalOutput")
    xout = D("xout", [TC, 1024], kind="ExternalOutput")
    op = k.op
    c = consts(k)
    pH = [k.ps(f"pH{i}", [128, 512]) for i in range(4)]
    pY = [k.ps(f"pY{i}", [128, 512]) for i in range(2)]
    pT = [k.ps("pT0", [128, 512]), k.ps("pT1", [128, 512])]
    stg = [k.sb("stg0", [128, 8, 256]), k.sb("stg1", [128, 8, 256])]
    modT, bts = adaln(k, c, condT_d, adaw_f if emit_u else None, adab_f if emit_u else None, 2 if emit_u else 0, adaw_b, adab_b, 1, pT[0], stg)
    if emit_u:
        op("dve", lambda e: e.tensor_scalar_add(out=modT[:, 8:16], in0=modT[:, 8:16], scalar1=1.0), reads=[modT], writes=[modT])
    G = bts[0]
    op("dve", lambda e: e.tensor_scalar_add(out=G[:], in0=G[:], scalar1=1.0), reads=[G], writes=[G])
    lng = k.sb("lng_s", [128, 1024]); lnb = k.sb("lnb_s", [128, 1024])
    op("sp", lambda e: e.dma_start(out=lng[:], in_=lng_d), writes=[lng], dma=True)
    op("sp", lambda e: e.dma_start(out=lnb[:], in_=lnb_d), writes=[lnb], dma=True)
    NTB = T // 128
    NBLK = TC // T
    if NBLK > 1:
        scr13 = nc.dram_tensor(pref + "scr13", [NE * 22 * 128, 2048], BF16).ap()
        scr2 = nc.dram_tensor(pref + "scr2", [NE * 4 * 128, 11 * 512], BF16).ap()
        S13 = [[Buf(scr13[(ex * 22 + j) * 128:(ex * 22 + j + 1) * 128, :]) for j in range(22)] for ex in range(NE)]
        S2 = [[Buf(scr2[(ex * 4 + q4) * 128:(ex * 4 + q4 + 1) * 128, :]) for q4 in range(4)] for ex in range(NE)]
    uT = k.sb("uT_s", [128, 8, T], BF16)
    aT = k.sb("aT", [128, 22, T], BF16)
    yacc = [k.sb(f"yacc{i}", [128, 1024]) for i in range(NTB)]
    w13b = [k.sb(f"w13b{i}", [128, 8, 256], BF16) for i in range(2)]
    w2s = [k.sb(f"w2s{i}", [128, 512]) for i in range(2)]
    w2b = [k.sb(f"w2b{i}", [128, 11, 512], BF16) for i in range(2)]
    qi = [0]
    sgt = [k.sb(f"sgt{i}", [128, 512], BF16) for i in range(2)]
    if NE > 1:
        gts = k.sb("gts", [128, NTB, NE])
    xs_ = [k.sb(f"xs{i}", [128, 1024]) for i in range(2)]
    tmp = [k.sb(f"tmp{i}", [128, 1024]) for i in range(2)]
    xo = [k.sb(f"xo{i}", [128, 1024]) for i in range(2)]
    st12 = [k.sb(f"st12{i}", [128, 12]) for i in range(2)]
    mv = [k.sb(f"mv{i}", [128, 2]) for i in range(2)]
    rstd = [k.sb(f"rstd{i}", [128, 1]) for i in range(2)]
    uTb = [k.sb(f"uTb{i}", [128, 8, 128], BF16) for i in range(2)]
    toks = []
    ph_i = 0
    dq = 0
    for blk in range(TC // T):
        t0 = blk * T
        op("sp", lambda e: e.dma_start(out=uT[:], in_=uT_d[:, t0:t0 + T].rearrange("(c p) t -> p c t", p=128)), writes=[uT], dma=True)
        if NE > 1:
            op("sp", lambda e: e.dma_start(out=gts[:], in_=gates_d[t0:t0 + T, :].rearrange("(n p) g -> p n g", p=128)), writes=[gts], dma=True)
        for ex in range(NE):
            for j in range(22):
                st = stg[j % 2]; wb = w13b[j % 2]
                if blk == 0:
                    op("sp", lambda e: e.dma_start(out=st[:, :, 0:128], in_=w13[ex, :, j * 128:(j + 1) * 128].rearrange("(c p) f -> p c f", p=128)), writes=[st], dma=True)
                    op("act", lambda e: e.dma_start(out=st[:, :, 128:256], in_=w13[ex, :, 2816 + j * 128:2816 + (j + 1) * 128].rearrange("(c p) f -> p c f", p=128)), writes=[st], dma=True)
                    op("act", lambda e: e.copy(out=wb[:], in_=st[:]), reads=[st], writes=[wb])
                    if NBLK > 1:
                        op("pool", lambda e: e.dma_start(out=S13[ex][j][:, :], in_=wb[:].rearrange("p a b -> p (a b)")), reads=[wb], writes=[S13[ex][j]], dma=True)
                else:
                    op("sp" if j % 2 == 0 else "act", lambda e: e.dma_start(out=wb[:].rearrange("p a b -> p (a b)"), in_=S13[ex][j][:, :]), reads=[S13[ex][j]], writes=[wb], dma=True)
                for tb in range(T // 512):
                    pg = pH[ph_i % 4]; pu = pH[(ph_i + 1) % 4]; ph_i += 2
                    for kc in range(8):
                        op("pe", lambda e: e.matmul(pg[:, :], lhsT=wb[:, kc, 0:128], rhs=uT[:, kc, tb * 512:(tb + 1) * 512], start=(kc == 0), stop=(kc == 7)),
                           reads=[wb, uT], writes=[pg])
                    for kc in range(8):
                        op("pe", lambda e: e.matmul(pu[:, :], lhsT=wb[:, kc, 128:256], rhs=uT[:, kc, tb * 512:(tb + 1) * 512], start=(kc == 0), stop=(kc == 7)),
                           reads=[wb, uT], writes=[pu])
                    s_ = sgt[tb % 2]
                    op("act", lambda e: e.activation(out=s_[:], in_=pg[:, :], func=AF.Silu), reads=[pg], writes=[s_])
                    op("dve", lambda e: e.tensor_tensor(out=aT[:, j, tb * 512:(tb + 1) * 512], in0=s_[:], in1=pu[:, :], op=ALU.mult), reads=[s_, pu], writes=[aT])
            for half in range(2):
                for kh in range(2):
                    wb2 = w2b[qi[0] % 2]; qi[0] += 1
                    q4 = half * 2 + kh
                    if blk == 0:
                        for jj in range(11):
                            j = kh * 11 + jj
                            st = w2s[jj % 2]
                            op("sp" if jj % 2 == 0 else "act", lambda e: e.dma_start(out=st[:], in_=w2[ex, j * 128:(j + 1) * 128, half * 512:(half + 1) * 512]), writes=[st], dma=True)
                            op("act", lambda e: e.copy(out=wb2[:, jj, :], in_=st[:]), reads=[st], writes=[wb2])
                        if NBLK > 1:
                            op("pool", lambda e: e.dma_start(out=S2[ex][q4][:, :], in_=wb2[:].rearrange("p a b -> p (a b)")), reads=[wb2], writes=[S2[ex][q4]], dma=True)
                    else:
                        op("sp" if q4 % 2 == 0 else "act", lambda e: e.dma_start(out=wb2[:].rearrange("p a b -> p (a b)"), in_=S2[ex][q4][:, :]), reads=[S2[ex][q4]], writes=[wb2], dma=True)
                    for tl in range(NTB):
                        p = pY[tl % 2]
                        for jj in range(11):
                            j = kh * 11 + jj
                            op("pe", lambda e: e.matmul(p[:, :], lhsT=aT[:, j, tl * 128:(tl + 1) * 128], rhs=wb2[:, jj, :], start=(jj == 0), stop=(jj == 10)),
                               reads=[aT, wb2], writes=[p])
                        ydst = yacc[tl][:, half * 512:(half + 1) * 512]
                        first = (ex == 0 and kh == 0)
                        if NE == 1:
                            if first:
                                op("act", lambda e: e.copy(out=ydst, in_=p[:, :]), reads=[p], writes=[yacc[tl]])
                            else:
                                op("dve", lambda e: e.tensor_tensor(out=ydst, in0=ydst, in1=p[:, :], op=ALU.add), reads=[p, yacc[tl]], writes=[yacc[tl]])
                        elif first:
                            op("dve", lambda e: e.tensor_scalar(out=ydst, in0=p[:, :], scalar1=gts[:, tl, ex:ex + 1], scalar2=None, op0=ALU.mult),
                               reads=[p, gts], writes=[yacc[tl]])
                        else:
                            op("dve", lambda e: e.scalar_tensor_tensor(out=ydst, in0=p[:, :], scalar=gts[:, tl, ex:ex + 1], in1=ydst, op0=ALU.mult, op1=ALU.add),
                               reads=[p, gts, yacc[tl]], writes=[yacc[tl]])
        for tl in range(NTB):
            i2 = tl % 2
            tsl = slice(t0 + tl * 128, t0 + (tl + 1) * 128)
            op("act", lambda e: e.dma_start(out=xs_[i2][:], in_=xmid[tsl, :]), writes=[xs_[i2]], dma=True)
            res_ln(k, c, yacc[tl][:], [yacc[tl]], xs_[i2], G, lng, lnb, tmp[i2], st12[i2], mv[i2], rstd[i2], xo[i2])
            toks.append(op("pool", lambda e: e.dma_start(out=xout[tsl, :], in_=xo[i2][:]), reads=[xo[i2]], dma=True))
            if emit_u:
                mod_transpose(k, c, xo[i2], modT, 0, pT, None, uTb[i2])
                toks.append(op("pool", lambda e: e.dma_start(out=uT_o[:, tsl].rearrange("(c p) t -> p c t", p=128), in_=uTb[i2][:]), reads=[uTb[i2]], dma=True))
    if fz is None or fz.final:
        k.finish(toks)
    if fz is not None:
        k.end_stage()
    print("ffn instructions", k.n_ins)
    return nc


def bc(v, n=128):
    return np.ascontiguousarray(np.tile(np.asarray(v, np.float32)[None, :], (n, 1)))


def fT(v):
    v = np.asarray(v, np.float32)
    return np.ascontiguousarray(v.reshape(-1, 128).T)


I32 = mybir.dt.int32


def build_s5(S, fz=None, blend=False):
    if fz is None:
        nc = bass.Bass("TRN2", target_bir_lowering=False); k = Ctx(nc); pref = ""
    else:
        nc, k, pref = fz.nc, fz.k, fz.pref
    D = mkD(nc, fz, pref)
    k.begin_stage(pref)
    TC = S // 2
    uT_d = D("uT", [2 * 1024, TC] if blend else [512, S], BF16)
    if blend:
        msk_d = D("msk", [128, 2])
    lamS = D("lamS", [128, 3, 16])
    lamB = D("lamB", [4, 128, 5, 64])
    cC = D("cC", [16, 128, 2, 16])
    dT = D("dT", [128, 4])
    out = D("gT", [512, S], BF16, kind="ExternalOutput")
    op = k.op
    NB = S // 512

    onesf = k.sb("onesf", [128, 128])
    op("pool", lambda e: e.memset(onesf[:], 1.0), writes=[onesf])
    ls = k.sb("ls", [128, 3, 16]); dsk = k.sb("dsk", [128, 4])
    op("sp", lambda e: e.dma_start(out=ls[:], in_=lamS), writes=[ls], dma=True)
    op("sp", lambda e: e.dma_start(out=dsk[:], in_=dT), writes=[dsk], dma=True)
    dtS = k.sb("dtS", [128, 16]); thS = k.sb("thS", [128, 16]); rS = k.sb("rS", [128, 16])
    op("act", lambda e: e.activation(out=dtS[:], in_=ls[:, 2, :], func=AF.Exp), reads=[ls], writes=[dtS])
    op("dve", lambda e: e.tensor_tensor(out=thS[:], in0=ls[:, 1, :], in1=dtS[:], op=ALU.mult), reads=[ls, dtS], writes=[thS])
    op("dve", lambda e: e.tensor_tensor(out=rS[:], in0=ls[:, 0, :], in1=dtS[:], op=ALU.mult), reads=[ls, dtS], writes=[rS])
    op("act", lambda e: e.activation(out=rS[:], in_=rS[:], func=AF.Exp), reads=[rS], writes=[rS])
    ioi = k.sb("ioi", [128, 128], I32); io = k.sb("io", [128, 128])
    op("pool", lambda e: e.iota(ioi[:], pattern=[[1, 128]], base=1, channel_multiplier=0), writes=[ioi])
    op("dve", lambda e: e.tensor_copy(out=io[:], in_=ioi[:]), reads=[ioi], writes=[io])
    ang = k.sb("ang", [128, 16 * 128]); st = k.sb("st", [128, 16 * 128]); ct = k.sb("ct", [128, 16 * 128])
    rtab = k.sb("rtab", [128, 16, 128])
    for q in range(16):
        op("dve", lambda e: e.tensor_scalar(out=ang[:, q * 128:(q + 1) * 128], in0=io[:], scalar1=thS[:, q:q + 1], scalar2=None, op0=ALU.mult),
           reads=[io, thS], writes=[ang])
        op("pool", lambda e: e.tensor_scalar(out=rtab[:, q, :], in0=onesf[:], scalar1=rS[:, q:q + 1], scalar2=None, op0=ALU.mult),
           reads=[onesf, rS], writes=[rtab])
    sincos(k, ang, 16 * 128, st, ct, "scS", NCH=4)
    rm = k.sb("rm", [128, 8])
    op("pool", lambda e: e.memset(rm[:], 1.0), writes=[rm])
    op("pool", lambda e: e.affine_select(out=rm[:], in_=rm[:], pattern=[[-16, 8]], compare_op=ALU.is_ge, fill=0.0, base=0, channel_multiplier=1),
       reads=[rm], writes=[rm])
    op("pool", lambda e: e.affine_select(out=rm[:], in_=rm[:], pattern=[[16, 8]], compare_op=ALU.is_ge, fill=0.0, base=15, channel_multiplier=-1),
       reads=[rm], writes=[rm])
    BtR = [k.sb(f"BtR{q}", [128, 2, 64], BF16) for q in range(16)]
    BtI = [k.sb(f"BtI{q}", [128, 2, 64], BF16) for q in range(16)]
    lb = k.sb("lb", [128, 5, 64])
    W = lambda n: k.sb(n, [128, 64])
    dtB, lrdt, lidt, mag, sB, cB, ar, ai, den, t1, t2, kr, ki, bbr, bbi = [W(n) for n in
        ("dtB", "lrdt", "lidt", "mag", "sB", "cB", "ar", "ai", "den", "t1", "t2", "kr", "ki", "bbr", "bbi")]
    TT = lambda o, a, b, o_: op("dve", lambda e: e.tensor_tensor(out=o[:], in0=a, in1=b, op=o_), reads=[lb, dtB, lrdt, lidt, mag, sB, cB, ar, ai, den, t1, t2, kr, ki], writes=[o])
    for cc in range(4):
        op("sp", lambda e: e.dma_start(out=lb[:], in_=lamB[cc]), writes=[lb], dma=True)
        op("act", lambda e: e.activation(out=dtB[:], in_=lb[:, 2, :], func=AF.Exp), reads=[lb], writes=[dtB])
        TT(lrdt, lb[:, 0, :], dtB[:], ALU.mult)
        TT(lidt, lb[:, 1, :], dtB[:], ALU.mult)
        op("act", lambda e: e.activation(out=mag[:], in_=lrdt[:], func=AF.Exp), reads=[lrdt], writes=[mag])
        sincos(k, lidt, 64, sB, cB, f"scB{cc}")
        TT(ar, mag[:], cB[:], ALU.mult)
        TT(ai, mag[:], sB[:], ALU.mult)
        op("dve", lambda e: e.tensor_scalar_add(out=ar[:], in0=ar[:], scalar1=-1.0), reads=[ar], writes=[ar])
        TT(den, lb[:, 0, :], lb[:, 0, :], ALU.mult)
        TT(t1, lb[:, 1, :], lb[:, 1, :], ALU.mult)
        TT(den, den[:], t1[:], ALU.add)
        op("dve", lambda e: e.reciprocal(out=den[:], in_=den[:]), reads=[den], writes=[den])
        TT(t1, ar[:], lb[:, 0, :], ALU.mult)
        TT(t2, ai[:], lb[:, 1, :], ALU.mult)
        TT(kr, t1[:], t2[:], ALU.add)
        TT(kr, kr[:], den[:], ALU.mult)
        TT(t1, ai[:], lb[:, 0, :], ALU.mult)
        TT(t2, ar[:], lb[:, 1, :], ALU.mult)
        TT(ki, t1[:], t2[:], ALU.subtract)
        TT(ki, ki[:], den[:], ALU.mult)
        TT(t1, kr[:], lb[:, 3, :], ALU.mult)
        TT(t2, ki[:], lb[:, 4, :], ALU.mult)
        op("dve", lambda e: e.tensor_tensor(out=bbr[:], in0=t1[:], in1=t2[:], op=ALU.subtract), reads=[t1, t2], writes=[bbr])
        TT(t1, kr[:], lb[:, 4, :], ALU.mult)
        TT(t2, ki[:], lb[:, 3, :], ALU.mult)
        op("dve", lambda e: e.tensor_tensor(out=bbi[:], in0=t1[:], in1=t2[:], op=ALU.add), reads=[t1, t2], writes=[bbi])
        for ql in range(4):
            q = cc * 4 + ql
            for g2 in range(2):
                op("dve", lambda e: e.tensor_scalar(out=BtR[q][:, g2, :], in0=bbr[:], scalar1=rm[:, 2 * ql + g2:2 * ql + g2 + 1], scalar2=None, op0=ALU.mult),
                   reads=[bbr, rm], writes=[BtR[q]])
                op("dve", lambda e: e.tensor_scalar(out=BtI[q][:, g2, :], in0=bbi[:], scalar1=rm[:, 2 * ql + g2:2 * ql + g2 + 1], scalar2=None, op0=ALU.mult),
                   reads=[bbi, rm], writes=[BtI[q]])
    cst = k.sb("cst", [128, 16, 2, 16])
    op("sp", lambda e: e.dma_start(out=cst[:], in_=cC.rearrange("q s r h -> s q r h")), writes=[cst], dma=True)
    CtR = [k.sb(f"CtR{q}", [128, 128], BF16) for q in range(16)]
    CtI = [k.sb(f"CtI{q}", [128, 128], BF16) for q in range(16)]
    for q in range(16):
        ql = q % 4
        op("pool", lambda e: e.memset(CtR[q][:], 0.0), writes=[CtR[q]])
        op("pool", lambda e: e.memset(CtI[q][:], 0.0), writes=[CtI[q]])
        for g2 in range(2):
            ps_ = slice(64 * g2, 64 * g2 + 64)
            cs_ = slice((2 * ql + g2) * 16, (2 * ql + g2) * 16 + 16)
            op("dve", lambda e: e.tensor_copy(out=CtR[q][ps_, cs_], in_=cst[ps_, q, 0, :]), reads=[cst], writes=[CtR[q]])
            op("dve", lambda e: e.tensor_scalar(out=CtI[q][ps_, cs_], in0=cst[ps_, q, 1, :], scalar1=-1.0, scalar2=None, op0=ALU.mult), reads=[cst], writes=[CtI[q]])
    uT = k.sb("uT_s", [128, 4, S], BF16)
    if not blend:
        for cc in range(4):
            op("sp" if cc % 2 == 0 else "act", lambda e: e.dma_start(out=uT[:, cc, :], in_=uT_d[cc * 128:(cc + 1) * 128, :]), writes=[uT], dma=True)
    else:
        msk = k.sb("msk_s", [128, 2])
        op("sp", lambda e: e.dma_start(out=msk[:], in_=msk_d), writes=[msk], dma=True)
        CW = TC // 8
        cand = [[k.sb(f"cand{i}_{j}", [128, CW], BF16) for j in range(2)] for i in range(2)]
        n_ = 0
        for i in range(2):
            for cc in range(4):
                for qq in range(8):
                    cd = cand[n_ % 2]; n_ += 1
                    for j in range(2):
                        r0 = i * 1024 + j * 512 + cc * 128
                        op("sp" if j == 0 else "act", lambda e: e.dma_start(out=cd[j][:], in_=uT_d[r0:r0 + 128, qq * CW:(qq + 1) * CW]), writes=[cd[j]], dma=True)
                    op("pool", lambda e: e.tensor_scalar(out=cd[0][:], in0=cd[0][:], scalar1=msk[:, 0:1], scalar2=None, op0=ALU.mult), reads=[cd[0], msk], writes=[cd[0]])
                    op("dve", lambda e: e.scalar_tensor_tensor(out=uT[:, cc, i * TC + qq * CW:i * TC + (qq + 1) * CW], in0=cd[1][:], scalar=msk[:, 1:2], in1=cd[0][:], op0=ALU.mult, op1=ALU.add),
                       reads=[cd[0], cd[1], msk], writes=[uT])
    pBr = [k.ps(f"pBr{i}", [128, 512]) for i in range(2)]
    pBi = [k.ps(f"pBi{i}", [128, 512]) for i in range(2)]
    pY = [k.ps(f"pY{i}", [128, 512]) for i in range(2)]
    bur = [k.sb(f"bur{i}", [128, 512]) for i in range(2)]; bui = [k.sb(f"bui{i}", [128, 512]) for i in range(2)]
    bpr = [k.sb(f"bpr{i}", [128, 512]) for i in range(2)]; bpi = [k.sb(f"bpi{i}", [128, 512]) for i in range(2)]
    vr = [k.sb(f"vr{i}", [128, 512]) for i in range(2)]; vi = [k.sb(f"vi{i}", [128, 512]) for i in range(2)]
    pt1_ = [k.sb(f"pt1_{i}", [128, 512]) for i in range(2)]; pt2_ = [k.sb(f"pt2_{i}", [128, 512]) for i in range(2)]
    dt1_ = [k.sb(f"dt1_{i}", [128, 512]) for i in range(2)]; dt2_ = [k.sb(f"dt2_{i}", [128, 512]) for i in range(2)]
    xr = [k.sb(f"xr{i}", [128, 512], BF16) for i in range(4)]; xi = [k.sb(f"xi{i}", [128, 512], BF16) for i in range(4)]
    car = [k.sb(f"car{q}", [128, 2]) for q in range(16)]
    cat = [k.sb(f"cat{i}", [128, 2]) for i in range(2)]
    for q in range(16):
        op("pool", lambda e: e.memset(car[q][:], 0.0), writes=[car[q]])
    yt = [k.sb(f"yt{i}", [128, 512]) for i in range(2)]; y2 = [k.sb(f"y2{i}", [128, 512]) for i in range(2)]
    go = [k.sb(f"go{i}", [128, 512], BF16) for i in range(2)]
    toks = []
    for blk in range(NB):
        tsl = slice(blk * 512, (blk + 1) * 512)
        for cc in range(4):
            def pair_chain(ql, i2, cc=cc, tsl=tsl):
                q = cc * 4 + ql
                pt1, pt2, dt1, dt2 = pt1_[i2], pt2_[i2], dt1_[i2], dt2_[i2]
                op("pe", lambda e: e.matmul(pBr[i2][:, :], lhsT=BtR[q][:].rearrange("p a b -> p (a b)"), rhs=uT[:, cc, tsl], start=True, stop=True), reads=[BtR[q], uT], writes=[pBr[i2]])
                op("pe", lambda e: e.matmul(pBi[i2][:, :], lhsT=BtI[q][:].rearrange("p a b -> p (a b)"), rhs=uT[:, cc, tsl], start=True, stop=True), reads=[BtI[q], uT], writes=[pBi[i2]])
                op("act", lambda e: e.copy(out=bur[i2][:], in_=pBr[i2][:, :]), reads=[pBr[i2]], writes=[bur[i2]])
                op("act", lambda e: e.copy(out=bui[i2][:], in_=pBi[i2][:, :]), reads=[pBi[i2]], writes=[bui[i2]])
                yield
                ctq = ct[:, q * 128:(q + 1) * 128]; stq = st[:, q * 128:(q + 1) * 128]
                ct3 = ctq.rearrange("p (a b) -> p a b", a=1).to_broadcast([128, 4, 128])
                st3 = stq.rearrange("p (a b) -> p a b", a=1).to_broadcast([128, 4, 128])
                v3 = lambda buf: buf[:].rearrange("p (a b) -> p a b", a=4)
                op("pool", lambda e: e.tensor_tensor(out=v3(pt1), in0=v3(bur[i2]), in1=ct3, op=ALU.mult), reads=[bur[i2], ct], writes=[pt1])
                op("pool", lambda e: e.tensor_tensor(out=v3(pt2), in0=v3(bui[i2]), in1=st3, op=ALU.mult), reads=[bui[i2], st], writes=[pt2])
                op("pool", lambda e: e.tensor_tensor(out=bpr[i2][:], in0=pt1[:], in1=pt2[:], op=ALU.add), reads=[pt1, pt2], writes=[bpr[i2]])
                op("dve", lambda e: e.tensor_tensor(out=v3(dt1), in0=v3(bui[i2]), in1=ct3, op=ALU.mult), reads=[bui[i2], ct], writes=[dt1])
                op("dve", lambda e: e.tensor_tensor(out=v3(dt2), in0=v3(bur[i2]), in1=st3, op=ALU.mult), reads=[bur[i2], st], writes=[dt2])
                yield
                op("dve", lambda e: e.tensor_tensor(out=bpi[i2][:], in0=dt1[:], in1=dt2[:], op=ALU.subtract), reads=[dt1, dt2], writes=[bpi[i2]])
                yield
                for c4 in range(4):
                    cs = slice(c4 * 128, (c4 + 1) * 128)
                    op("dve", lambda e: e.tensor_tensor_scan(out=vr[i2][:, cs], data0=rtab[:, q, :], data1=bpr[i2][:, cs], initial=car[q][:, 0:1], op0=ALU.mult, op1=ALU.add),
                       reads=[rtab, bpr[i2], car[q]], writes=[vr[i2]])
                    op("dve", lambda e: e.tensor_tensor_scan(out=vi[i2][:, cs], data0=rtab[:, q, :], data1=bpi[i2][:, cs], initial=car[q][:, 1:2], op0=ALU.mult, op1=ALU.add),
                       reads=[rtab, bpi[i2], car[q]], writes=[vi[i2]])
                    yield
                    l = c4 * 128 + 127
                    c128 = ct[:, q * 128 + 127:q * 128 + 128]; s128 = st[:, q * 128 + 127:q * 128 + 128]
                    ca = cat[i2]
                    op("dve", lambda e: e.tensor_scalar(out=ca[:, 0:1], in0=vi[i2][:, l:l + 1], scalar1=s128, scalar2=None, op0=ALU.mult), reads=[vi[i2], st], writes=[ca])
                    op("dve", lambda e: e.tensor_scalar(out=ca[:, 1:2], in0=vi[i2][:, l:l + 1], scalar1=c128, scalar2=None, op0=ALU.mult), reads=[vi[i2], ct], writes=[ca])
                    yield
                    op("dve", lambda e: e.scalar_tensor_tensor(out=car[q][:, 0:1], in0=vr[i2][:, l:l + 1], scalar=c128, in1=ca[:, 0:1], op0=ALU.mult, op1=ALU.subtract),
                       reads=[vr[i2], ct, ca], writes=[car[q]])
                    op("dve", lambda e: e.scalar_tensor_tensor(out=car[q][:, 1:2], in0=vr[i2][:, l:l + 1], scalar=s128, in1=ca[:, 1:2], op0=ALU.mult, op1=ALU.add),
                       reads=[vr[i2], st, ca], writes=[car[q]])
                    yield
                op("dve", lambda e: e.tensor_tensor(out=v3(dt1), in0=v3(vr[i2]), in1=ct3, op=ALU.mult), reads=[vr[i2], ct], writes=[dt1])
                op("dve", lambda e: e.tensor_tensor(out=v3(dt2), in0=v3(vi[i2]), in1=st3, op=ALU.mult), reads=[vi[i2], st], writes=[dt2])
                yield
                op("dve", lambda e: e.tensor_tensor(out=xr[ql][:], in0=dt1[:], in1=dt2[:], op=ALU.subtract), reads=[dt1, dt2], writes=[xr[ql]])
                yield
                op("dve", lambda e: e.tensor_tensor(out=v3(dt1), in0=v3(vr[i2]), in1=st3, op=ALU.mult), reads=[vr[i2], st], writes=[dt1])
                op("dve", lambda e: e.tensor_tensor(out=v3(dt2), in0=v3(vi[i2]), in1=ct3, op=ALU.mult), reads=[vi[i2], ct], writes=[dt2])
                yield
                op("dve", lambda e: e.tensor_tensor(out=xi[ql][:], in0=dt1[:], in1=dt2[:], op=ALU.add), reads=[dt1, dt2], writes=[xi[ql]])
            for qp_ in range(2):
                gens = [pair_chain(2 * qp_, 0), pair_chain(2 * qp_ + 1, 1)]
                while gens:
                    for g_ in list(gens):
                        try:
                            next(g_)
                        except StopIteration:
                            gens.remove(g_)
            j2 = (blk * 4 + cc) % 2
            p = pY[j2]
            for ql in range(4):
                q = cc * 4 + ql
                op("pe", lambda e: e.matmul(p[:, :], lhsT=CtR[q][:], rhs=xr[ql][:], start=(ql == 0), stop=False), reads=[CtR[q], xr[ql]], writes=[p])
                op("pe", lambda e: e.matmul(p[:, :], lhsT=CtI[q][:], rhs=xi[ql][:], start=False, stop=(ql == 3)), reads=[CtI[q], xi[ql]], writes=[p])
            y = yt[j2]; z = y2[j2]
            op("dve", lambda e: e.scalar_tensor_tensor(out=y[:], in0=uT[:, cc, tsl], scalar=dsk[:, cc:cc + 1], in1=p[:, :], op0=ALU.mult, op1=ALU.add),
               reads=[uT, dsk, p], writes=[y])
            op("pool", lambda e: e.tensor_tensor(out=z[:], in0=y[:], in1=y[:], op=ALU.mult), reads=[y], writes=[z])
            op("dve", lambda e: e.tensor_scalar(out=z[:], in0=z[:], scalar1=0.044715, scalar2=1.0, op0=ALU.mult, op1=ALU.add), reads=[z], writes=[z])
            op("pool", lambda e: e.tensor_tensor(out=z[:], in0=z[:], in1=y[:], op=ALU.mult), reads=[z, y], writes=[z])
            op("act", lambda e: e.activation(out=z[:], in_=z[:], func=AF.Sigmoid, scale=2.0 * math.sqrt(2.0 / math.pi)), reads=[z], writes=[z])
            op("pool", lambda e: e.tensor_tensor(out=go[j2][:], in0=z[:], in1=y[:], op=ALU.mult), reads=[z, y], writes=[go[j2]])
            toks.append(op("sp", lambda e: e.dma_start(out=out[cc * 128:(cc + 1) * 128, tsl], in_=go[j2][:]), reads=[go[j2]], dma=True))
    if fz is None:
        k.finish(toks)
    else:
        k.end_stage()
    print("s5 instructions", k.n_ins)
    return nc


def s5_inputs(inp, uT_full_b, half, S):
    f = np.float32
    g0 = half * 32
    lr = inp["s5_lambda_re"][0][g0:g0 + 32]; li = inp["s5_lambda_im"][0][g0:g0 + 32]; ld = inp["s5_log_dt"][0][g0:g0 + 32]
    ldx = np.repeat(ld[:, None], 64, axis=1)
    toS = lambda a: a.reshape(16, 2, 64).transpose(1, 2, 0).reshape(128, 16)
    lamS = np.stack([toS(lr), toS(li), toS(ldx)], axis=1)
    br = inp["s5_b_re"][0][g0:g0 + 32]; bi = inp["s5_b_im"][0][g0:g0 + 32]
    rep = lambda a: np.repeat(a[:, None, :], 16, axis=1)
    toB = lambda a: a.reshape(4, 8 * 16, 64)
    lamB = np.stack([toB(rep(lr)), toB(rep(li)), toB(rep(ldx)), toB(br.transpose(0, 2, 1)), toB(bi.transpose(0, 2, 1))], axis=2)
    cr = inp["s5_c_re"][0][g0:g0 + 32]; ci = inp["s5_c_im"][0][g0:g0 + 32]
    toC = lambda a: a.reshape(16, 2, 16, 64).transpose(0, 1, 3, 2).reshape(16, 128, 16)
    cC = np.stack([toC(cr), toC(ci)], axis=2)
    dT = inp["s5_d"][0][half * 512:(half + 1) * 512].reshape(4, 128).T
    return {"uT": np.ascontiguousarray(uT_full_b[half * 512:(half + 1) * 512, :S]),
            "lamS": np.ascontiguousarray(lamS.astype(f)), "lamB": np.ascontiguousarray(lamB.astype(f)),
            "cC": np.ascontiguousarray(cC.astype(f)), "dT": np.ascontiguousarray(dT.astype(f))}


import ml_dtypes as _mld
import os

_CACHE = {}


def build_fused(S, NB_):
    TC = S // 2
    nc = bass.Bass("TRN2", target_bir_lowering=False)
    k = Ctx(nc)
    fz = Fz(nc, k)
    fz.final = False
    I = lambda n, s, dt: nc.dram_tensor(n, list(s), dt).ap()

    def chunked(name, R_, Cn, W):
        W = min(W, Cn)
        src = [I(f"{name}_s{j}", [R_, W], BF16) for j in range(Cn // W)]
        dst = [I(f"{name}_a{j}", [2 * R_, W], BF16) for j in range(Cn // W)]
        return src, dst, ChunkedAP(src, W), ChunkedAP(dst, W)
    hg_s, hg_d, hg_src, hg_all = chunked("i_hg", 1024, S, 1024)
    u1_s, u1_d, u1_src, u1_all = chunked("i_u1", 1024, TC, 1024)
    g_s, g_d, g_src, g_all = chunked("i_g", 512, S, 2048)
    xmid1 = I("i_xmid1", [TC, 1024], F32); uT1 = I("i_uT1", [1024, TC], BF16)
    x1 = I("i_x1", [TC, 1024], F32)
    xmid2 = I("i_xmid2", [TC, 1024], F32); uT2 = I("i_uT2", [1024, TC], BF16); gates = I("i_gates", [TC, 8], F32)
    groups = [[2 * i, 2 * i + 1] for i in range(NB_)]
    fz.pref = "a_"; fz.ext = {"hgT": hg_src}
    build_m0(S, fz=fz)
    k.collective("AllGather", hg_s, hg_d, groups)
    fz.pref = "b_"; fz.ext = {"inT": hg_all, "xmid": xmid1, "uT": uT1}
    build_proj(TC, 16, False, False, fz=fz, blend=True)
    fz.pref = "c_"; fz.ext = {"uT": uT1, "xmid": xmid1, "xout": x1, "uTn": u1_src}
    build_ffn(TC, 1, min(1024, TC), True, fz=fz)
    k.collective("AllGather", u1_s, u1_d, groups)
    fz.pref = "d_"; fz.ext = {"uT": u1_all, "gT": g_src}
    build_s5(S, fz=fz, blend=True)
    k.collective("AllGather", g_s, g_d, groups)
    fz.pref = "e_"; fz.ext = {"inT": g_all, "x": x1, "xmid": xmid2, "uT": uT2, "gates": gates}
    build_proj(TC, 8, True, True, fz=fz, blend=True)
    fz.pref = "f_"; fz.ext = {"uT": uT2, "xmid": xmid2, "gates": gates}; fz.final = True
    build_ffn(TC, 8, min(int(os.environ.get('T2', 1024)), TC), False, fz=fz)
    print("fused instructions", k.n_ins)
    return nc


def kernel_impl(inp, S, NB_=4):
    inp = {k_: np.asarray(v) for k_, v in inp.items()}
    NCO = 2 * NB_
    TC = S // 2
    f32 = np.float32
    cT = lambda b: np.ascontiguousarray(inp["c"][b].reshape(8, 128).T.astype(f32))
    aw = inp["ada_w"]; ab = inp["ada_b"]
    C = lambda a: np.ascontiguousarray(a)
    key = ("fused", S, NB_)
    if key not in _CACHE:
        _CACHE[key] = build_fused(S, NB_)
    nc = _CACHE[key]
    shared = {}
    shared["b_W"] = C(inp["mlstm_w_out"][0])
    shared["b_adaw_f"] = C(aw[0][:, 3072:5120]); shared["b_adab_f"] = fT(ab[0][3072:5120])
    shared["b_adaw_b"] = C(aw[0][:, 2048:3072]); shared["b_adab_b"] = bc(ab[0][2048:3072])
    shared["b_lng"] = bc(inp["ln_mix_g"][0]); shared["b_lnb"] = bc(inp["ln_mix_b"][0])
    shared["c_w13"] = C(inp["ffn_w13"]); shared["c_w2"] = C(inp["ffn_w2"])
    shared["c_adaw_b"] = C(aw[0][:, 5120:6144]); shared["c_adab_b"] = bc(ab[0][5120:6144])
    shared["c_lng"] = bc(inp["ln_ffn_g"][0]); shared["c_lnb"] = bc(inp["ln_ffn_b"][0])
    shared["c_adaw_f"] = C(aw[1][:, 0:2048]); shared["c_adab_f"] = fT(ab[1][0:2048])
    shared["e_W"] = C(inp["s5_w_glu"][0])
    shared["e_adaw_f"] = C(aw[1][:, 3072:5120]); shared["e_adab_f"] = fT(ab[1][3072:5120])
    shared["e_adaw_b"] = C(aw[1][:, 2048:3072]); shared["e_adab_b"] = bc(ab[1][2048:3072])
    shared["e_lng"] = bc(inp["ln_mix_g"][1]); shared["e_lnb"] = bc(inp["ln_mix_b"][1])
    shared["e_bglu"] = bc(inp["s5_b_glu"][0])
    shared["e_wr"] = C(inp["moe_router"][0].reshape(8, 128, 8).transpose(1, 0, 2).astype(f32))
    shared["f_w13"] = C(inp["moe_w13"][0]); shared["f_w2"] = C(inp["moe_w2"][0])
    shared["f_adaw_b"] = C(aw[1][:, 5120:6144]); shared["f_adab_b"] = bc(ab[1][5120:6144])
    shared["f_lng"] = bc(inp["ln_ffn_g"][1]); shared["f_lnb"] = bc(inp["ln_ffn_b"][1])
    dummy_u = np.zeros((1024, S), _mld.bfloat16)
    maps = []
    for c in range(NCO):
        b, h = c // 2, c % 2
        tk = slice(h * TC, (h + 1) * TC)
        m = dict(shared)
        for k_, v in m0_inputs(inp, b, h, S).items():
            m["a_" + k_] = v
        msk = np.zeros((128, 2), f32); msk[:, h] = 1.0
        m["b_msk"] = msk; m["d_msk"] = msk; m["e_msk"] = msk
        m["b_x"] = C(inp["x"][b, :S][tk])
        for p_ in "bcef":
            m[p_ + "_condT"] = cT(b)
        for k_, v in s5_inputs(inp, dummy_u, h, S).items():
            if k_ != "uT":
                m["d_" + k_] = v
        maps.append(m)
    res = run_bass_kernel_spmd(nc, maps, core_ids=list(range(NCO)))
    out = np.zeros((NB_, S, 1024), f32)
    for c in range(NCO):
        b, h = c // 2, c % 2
        out[b, h * TC:(h + 1) * TC] = np.asarray(res.results[c]["f_xout"])
    return out


def kernel(**inputs):
    return kernel_impl(inputs, 8192, 4)
```
